# Optimizing a Trainium2 kernel written in Bass

```python
import jax, jax.numpy as jnp
from jax import lax
import numpy as np

D_MODEL = 1024
BATCH = 16
SEQ = 2048
DEPTH = 2

CTX_LEN = 256
GRID_W = 64
EPS = 1e-6
NEG_INF = -1e30
NA_HEADS = 8
NA_HEAD_DIM = 64
NA_WIDTH = NA_HEADS * NA_HEAD_DIM
WIN_H = 8
WIN_W = 16
Q_COLS = 16
K_COLS = Q_COLS + WIN_W
SC_WIDTH = D_MODEL - NA_WIDTH
SC_CONV = 3
LRU_WIDTH = D_MODEL
LRU_BLOCKS = 4
LRU_BLOCK = LRU_WIDTH // LRU_BLOCKS
LRU_CONV = 4
LRU_C = 8.0
N_EXPERTS = 32
TOP_K = 4
D_EXPERT = D_MODEL
SWIGLU_LIMIT = 7.0
SWIGLU_ALPHA = 1.702
EXPERT_BLOCK = 256

kernel_name = "hybrid_natten_shortconv_rglru_moe_prefix_ctx"


def rms_norm(x, g):
    x32 = x.astype(jnp.float32)
    y = x32 * lax.rsqrt(jnp.mean(x32 * x32, axis=-1, keepdims=True) + EPS)
    return (y * g.astype(jnp.float32)).astype(x.dtype)


def modulate(h, shift, scale):
    return h * (1 + scale) + shift


def depthwise_conv(x, w, b):
    k = w.shape[0]
    left = (k - 1) // 2
    t = x.shape[1]
    xp = jnp.pad(x, ((0, 0), (left, k - 1 - left), (0, 0)))
    return sum(xp[:, j:j + t] * w[j] for j in range(k)) + b


def heads(t):
    return t.reshape(t.shape[:-1] + (NA_HEADS, NA_HEAD_DIM))


def na_static(rows):
    kh = min(WIN_H, rows)
    r = np.arange(rows)
    r0 = np.clip(r - kh // 2, 0, rows - kh)
    dr_idx = r0[:, None] + np.arange(kh)[None] - r[:, None] + WIN_H - 1
    ncb = GRID_W // Q_COLS
    kc0 = np.clip(np.arange(ncb) * Q_COLS - WIN_W // 2, 0, GRID_W - K_COLS)
    band = kc0[:, None] + np.arange(K_COLS)[None]
    qc = np.arange(ncb)[:, None] * Q_COLS + np.arange(Q_COLS)[None]
    c0 = np.clip(qc - WIN_W // 2, 0, GRID_W - WIN_W)[..., None]
    kcol = band[:, None, :]
    valid = (kcol >= c0) & (kcol < c0 + WIN_W)
    dc_idx = np.clip(kcol - qc[..., None] + WIN_W - 1, 0, 2 * WIN_W - 2)
    return kh, r0.astype(np.int32), dr_idx, band, valid, dc_idx


def neighbourhood_attention(q, k, v, kc, vc, rpb):
    b_, t = q.shape[:2]
    rows = t // GRID_W
    ncb = GRID_W // Q_COLS
    kh, r0, dr_idx, band, valid, dc_idx = na_static(rows)
    grid = lambda a: a.reshape(b_, rows, GRID_W, NA_HEADS, NA_HEAD_DIM)
    q, k, v = grid(q), grid(k), grid(v)
    bias = rpb[:, dr_idx[:, :, None, None, None], dc_idx[None, None]]
    bias = jnp.where(valid[None, None, None], bias.astype(jnp.float32), NEG_INF)
    bias = bias.transpose(1, 0, 3, 4, 2, 5).reshape(rows, NA_HEADS, ncb, Q_COLS, kh * K_COLS)
    r0 = jnp.asarray(r0)
    n_lat = kh * K_COLS

    def row_block(args):
        r, bias_r = args
        q_r = lax.dynamic_index_in_dim(q, r, axis=1, keepdims=False)
        q_r = q_r.reshape(b_, ncb, Q_COLS, NA_HEADS, NA_HEAD_DIM)
        k_rows = lax.dynamic_slice_in_dim(k, r0[r], kh, axis=1)
        v_rows = lax.dynamic_slice_in_dim(v, r0[r], kh, axis=1)
        k_band = k_rows[:, :, band]
        v_band = v_rows[:, :, band]
        s_lat = jnp.einsum('bnqhd,binchd->bhnqic', q_r, k_band).astype(jnp.float32)
        s_lat = s_lat.reshape(b_, NA_HEADS, ncb, Q_COLS, n_lat) + bias_r
        s_ctx = jnp.einsum('bnqhd,blhd->bhnql', q_r, kc).astype(jnp.float32)
        p = jax.nn.softmax(jnp.concatenate([s_lat, s_ctx], axis=-1), axis=-1).astype(v.dtype)
        p_lat = p[..., :n_lat].reshape(b_, NA_HEADS, ncb, Q_COLS, kh, K_COLS)
        o = (jnp.einsum('bhnqic,binchd->bnqhd', p_lat, v_band)
             + jnp.einsum('bhnql,blhd->bnqhd', p[..., n_lat:], vc))
        return o.reshape(b_, GRID_W, NA_WIDTH)

    out = lax.map(row_block, (jnp.arange(rows), bias))
    return out.transpose(1, 0, 2, 3).reshape(b_, t, NA_WIDTH)


def context_attention(qc, kc, vc):
    s = jnp.einsum('blhd,bmhd->bhlm', qc, kc).astype(jnp.float32)
    p = jax.nn.softmax(s, axis=-1).astype(vc.dtype)
    o = jnp.einsum('bhlm,bmhd->blhd', p, vc)
    return o.reshape(o.shape[0], o.shape[1], NA_WIDTH)


def even_mixer(h, hc, w_in, w_out, q_g, k_g, rpb, conv_w, conv_b, need_ctx):
    cuts = [NA_WIDTH, 2 * NA_WIDTH, 3 * NA_WIDTH, 3 * NA_WIDTH + SC_WIDTH, 3 * NA_WIDTH + 2 * SC_WIDTH]
    q_scale = NA_HEAD_DIM ** -0.5
    q, k, v, bg, cg, xin = jnp.split(h @ w_in, cuts, axis=-1)
    q = rms_norm(heads(q), q_g) * q_scale
    k = rms_norm(heads(k), k_g)
    if need_ctx:
        qc, kc, vc, bgc, cgc, xinc = jnp.split(hc @ w_in, cuts, axis=-1)
    else:
        kc, vc = jnp.split(hc @ w_in[:, NA_WIDTH:3 * NA_WIDTH], 2, axis=-1)
    kc = rms_norm(heads(kc), k_g)
    vc = heads(vc)
    o_a = neighbourhood_attention(q, k, heads(v), kc, vc, rpb)
    o_b = bg * depthwise_conv(cg * xin, conv_w, conv_b)
    y = jnp.concatenate([o_a, o_b], axis=-1) @ w_out
    if not need_ctx:
        return y, None
    oc_a = context_attention(rms_norm(heads(qc), q_g) * q_scale, kc, vc)
    oc_b = bgc * depthwise_conv(cgc * xinc, conv_w, conv_b)
    yc = jnp.concatenate([oc_a, oc_b], axis=-1) @ w_out
    return y, yc


def block_diag(u, w, b):
    b_, t, _ = u.shape
    ub = u.reshape(b_, t, LRU_BLOCKS, LRU_BLOCK)
    return jnp.einsum('bthi,hij->bthj', ub, w).reshape(b_, t, LRU_WIDTH) + b


def rglru_coeffs(u, wa, ba, wx, bx, lam):
    r = jax.nn.sigmoid(block_diag(u, wa, ba).astype(jnp.float32))
    i = jax.nn.sigmoid(block_diag(u, wx, bx).astype(jnp.float32))
    log_a = LRU_C * r * jax.nn.log_sigmoid(lam.astype(jnp.float32))
    a = jnp.exp(log_a)
    mult = jnp.sqrt(-jnp.expm1(2.0 * log_a))
    return a, mult * i * u.astype(jnp.float32)


def linear_scan(a, b, h0, reverse):
    idx = -1 if reverse else 0
    b = b.at[:, idx].add(a[:, idx] * h0)

    def combine(e1, e2):
        a1, b1 = e1
        a2, b2 = e2
        return a1 * a2, a2 * b1 + b2

    _, h = lax.associative_scan(combine, (a, b), axis=1, reverse=reverse)
    return h


def odd_mixer(h, hc, w_in, w_out, conv_w, conv_b, fwd, bwd, need_ctx):
    gate, u = jnp.split(h @ w_in, 2, axis=-1)
    u = depthwise_conv(u, conv_w, conv_b)
    if need_ctx:
        gate_c, uc = jnp.split(hc @ w_in, 2, axis=-1)
    else:
        uc = hc @ w_in[:, LRU_WIDTH:]
    uc = depthwise_conv(uc, conv_w, conv_b)
    zeros = jnp.zeros((h.shape[0], LRU_WIDTH), jnp.float32)
    a_c, b_c = rglru_coeffs(uc, *fwd)
    hc_f = linear_scan(a_c, b_c, zeros, False)
    a_l, b_l = rglru_coeffs(u, *fwd)
    h_f = linear_scan(a_l, b_l, hc_f[:, -1], False)
    a_c, b_c = rglru_coeffs(uc, *bwd)
    hc_b = linear_scan(a_c, b_c, zeros, True)
    a_l, b_l = rglru_coeffs(u, *bwd)
    h_b = linear_scan(a_l, b_l, hc_b[:, 0], True)
    y = ((h_f + h_b).astype(h.dtype) * jax.nn.gelu(gate)) @ w_out
    if not need_ctx:
        return y, None
    yc = ((hc_f + hc_b).astype(hc.dtype) * jax.nn.gelu(gate_c)) @ w_out
    return y, yc


def moe(x, w_r, b_r, wg, bg, wu, bu, wd, bd):
    n = x.shape[0]
    logits = (x @ w_r + b_r).astype(jnp.float32)
    top_v, top_i = lax.top_k(logits, TOP_K)
    gates = jax.nn.softmax(top_v, axis=-1).astype(x.dtype)
    nk = n * TOP_K
    e_flat = top_i.reshape(-1)
    tok_flat = jnp.arange(nk, dtype=jnp.int32) // TOP_K
    order = jnp.argsort(e_flat)
    e_sorted = e_flat[order]
    counts = jnp.bincount(e_flat, length=N_EXPERTS)
    padded = (counts + EXPERT_BLOCK - 1) // EXPERT_BLOCK * EXPERT_BLOCK
    starts = jnp.cumsum(counts) - counts
    pends = jnp.cumsum(padded)
    dest = (pends - padded)[e_sorted] + jnp.arange(nk, dtype=jnp.int32) - starts[e_sorted]
    n_blocks = -(-nk // EXPERT_BLOCK) + N_EXPERTS
    n_slots = n_blocks * EXPERT_BLOCK
    slot_tok = jnp.zeros((n_slots,), jnp.int32).at[dest].set(tok_flat[order])
    slot_w = jnp.zeros((n_slots,), x.dtype).at[dest].set(gates.reshape(-1)[order])
    block_e = jnp.minimum(jnp.searchsorted(pends, jnp.arange(n_blocks) * EXPERT_BLOCK, side='right'), N_EXPERTS - 1)
    xb = x[slot_tok].reshape(n_blocks, EXPERT_BLOCK, x.shape[1])

    def expert_block(args):
        xe, e = args
        g = jnp.minimum(xe @ wg[e] + bg[e], SWIGLU_LIMIT)
        u = jnp.clip(xe @ wu[e] + bu[e], -SWIGLU_LIMIT, SWIGLU_LIMIT)
        return (g * jax.nn.sigmoid(SWIGLU_ALPHA * g) * (u + 1)) @ wd[e] + bd[e]

    yb = lax.map(expert_block, (xb, block_e)).reshape(n_slots, x.shape[1])
    return jnp.zeros_like(x).at[slot_tok].add(yb * slot_w[:, None])


def setup_inputs(seed: int = 0) -> dict:
    key = jax.random.key(seed)
    ks = iter(jax.random.split(key, 64))
    d = D_MODEL
    n_even = (DEPTH + 1) // 2
    n_odd = DEPTH // 2

    def nrm(shape, scale):
        return jax.random.normal(next(ks), shape, jnp.float32) * scale

    def lam(shape):
        u = jax.random.uniform(next(ks), shape, jnp.float32, minval=0.9, maxval=0.999)
        s = u ** (1.0 / LRU_C)
        return jnp.log(s) - jnp.log1p(-s)

    inp = {}
    inp['x'] = nrm((BATCH, SEQ, d), 1.0)
    inp['c'] = nrm((BATCH, d), 1.0)
    inp['ctx'] = nrm((BATCH, CTX_LEN, d), 1.0)
    inp['c_ctx'] = nrm((d,), 1.0)
    inp['ada_w'] = nrm((DEPTH, d, 6 * d), 0.5 * d ** -0.5)
    inp['ada_b'] = nrm((DEPTH, 6 * d), 0.02)
    inp['norm1_g'] = 1.0 + nrm((DEPTH, d), 0.05)
    inp['norm2_g'] = 1.0 + nrm((DEPTH, d), 0.05)
    inp['ev_w_in'] = nrm((n_even, d, 3 * NA_WIDTH + 3 * SC_WIDTH), d ** -0.5)
    inp['ev_w_out'] = nrm((n_even, NA_WIDTH + SC_WIDTH, d), (NA_WIDTH + SC_WIDTH) ** -0.5)
    inp['ev_q_gain'] = 1.0 + nrm((n_even, NA_HEAD_DIM), 0.05)
    inp['ev_k_gain'] = 1.0 + nrm((n_even, NA_HEAD_DIM), 0.05)
    inp['ev_rpb'] = nrm((n_even, NA_HEADS, 2 * WIN_H - 1, 2 * WIN_W - 1), 0.1)
    inp['ev_conv_w'] = nrm((n_even, SC_CONV, SC_WIDTH), SC_CONV ** -0.5)
    inp['ev_conv_b'] = nrm((n_even, SC_WIDTH), 0.02)
    inp['od_w_in'] = nrm((n_odd, d, 2 * LRU_WIDTH), d ** -0.5)
    inp['od_w_out'] = nrm((n_odd, LRU_WIDTH, d), LRU_WIDTH ** -0.5)
    inp['od_conv_w'] = nrm((n_odd, LRU_CONV, LRU_WIDTH), LRU_CONV ** -0.5)
    inp['od_conv_b'] = nrm((n_odd, LRU_WIDTH), 0.02)
    for dr in ('fwd', 'bwd'):
        inp['od_' + dr + '_wa'] = nrm((n_odd, LRU_BLOCKS, LRU_BLOCK, LRU_BLOCK), LRU_BLOCK ** -0.5)
        inp['od_' + dr + '_ba'] = nrm((n_odd, LRU_WIDTH), 0.02)
        inp['od_' + dr + '_wx'] = nrm((n_odd, LRU_BLOCKS, LRU_BLOCK, LRU_BLOCK), LRU_BLOCK ** -0.5)
        inp['od_' + dr + '_bx'] = nrm((n_odd, LRU_WIDTH), 0.02)
        inp['od_' + dr + '_lam'] = lam((n_odd, LRU_WIDTH))
    inp['router_w'] = nrm((DEPTH, d, N_EXPERTS), d ** -0.5)
    inp['router_b'] = nrm((DEPTH, N_EXPERTS), 0.01)
    inp['exp_w_gate'] = nrm((DEPTH, N_EXPERTS, d, D_EXPERT), d ** -0.5)
    inp['exp_b_gate'] = nrm((DEPTH, N_EXPERTS, D_EXPERT), 0.02)
    inp['exp_w_up'] = nrm((DEPTH, N_EXPERTS, d, D_EXPERT), d ** -0.5)
    inp['exp_b_up'] = nrm((DEPTH, N_EXPERTS, D_EXPERT), 0.02)
    inp['exp_w_down'] = nrm((DEPTH, N_EXPERTS, D_EXPERT, d), D_EXPERT ** -0.5)
    inp['exp_b_down'] = nrm((DEPTH, N_EXPERTS, d), 0.02)
    return inp


def reference(x, c, ctx, c_ctx, ada_w, ada_b, norm1_g, norm2_g,
              ev_w_in, ev_w_out, ev_q_gain, ev_k_gain, ev_rpb, ev_conv_w, ev_conv_b,
              od_w_in, od_w_out, od_conv_w, od_conv_b,
              od_fwd_wa, od_fwd_ba, od_fwd_wx, od_fwd_bx, od_fwd_lam,
              od_bwd_wa, od_bwd_ba, od_bwd_wx, od_bwd_bx, od_bwd_lam,
              router_w, router_b, exp_w_gate, exp_b_gate, exp_w_up, exp_b_up, exp_w_down, exp_b_down):
    b_, t, d = x.shape
    n_ctx = ctx.shape[1]
    s_lat = jax.nn.silu(c)
    s_ctx = jax.nn.silu(c_ctx)
    hctx = ctx
    for l in range(DEPTH):
        last = l == DEPTH - 1
        mod = (s_lat @ ada_w[l] + ada_b[l])[:, None, :]
        modc = s_ctx @ ada_w[l] + ada_b[l]
        sh1, sc1, g1, sh2, sc2, g2 = jnp.split(mod, 6, axis=-1)
        csh1, csc1, cg1, csh2, csc2, cg2 = jnp.split(modc, 6, axis=-1)
        h = modulate(rms_norm(x, norm1_g[l]), sh1, sc1)
        hc = modulate(rms_norm(hctx, norm1_g[l]), csh1, csc1)
        if l % 2 == 0:
            j = l // 2
            y, yc = even_mixer(h, hc, ev_w_in[j], ev_w_out[j], ev_q_gain[j], ev_k_gain[j], ev_rpb[j],
                               ev_conv_w[j], ev_conv_b[j], not last)
        else:
            j = l // 2
            fwd = (od_fwd_wa[j], od_fwd_ba[j], od_fwd_wx[j], od_fwd_bx[j], od_fwd_lam[j])
            bwd = (od_bwd_wa[j], od_bwd_ba[j], od_bwd_wx[j], od_bwd_bx[j], od_bwd_lam[j])
            y, yc = odd_mixer(h, hc, od_w_in[j], od_w_out[j], od_conv_w[j], od_conv_b[j], fwd, bwd, not last)
        x = x + g1 * y
        h2 = modulate(rms_norm(x, norm2_g[l]), sh2, sc2)
        moe_w = (router_w[l], router_b[l], exp_w_gate[l], exp_b_gate[l], exp_w_up[l], exp_b_up[l],
                 exp_w_down[l], exp_b_down[l])
        if last:
            x = x + g2 * moe(h2.reshape(-1, d), *moe_w).reshape(b_, t, d)
        else:
            hctx = hctx + cg1 * yc
            h2c = modulate(rms_norm(hctx, norm2_g[l]), csh2, csc2)
            tokens = jnp.concatenate([h2c, h2], axis=1).reshape(-1, d)
            y2 = moe(tokens, *moe_w).reshape(b_, n_ctx + t, d)
            hctx = hctx + cg2 * y2[:, :n_ctx]
            x = x + g2 * y2[:, n_ctx:]
    return x
```

```python
import contextlib
import numpy as np
import concourse.bass as bass
import concourse.mybir as mybir
from concourse.bass_utils import run_bass_kernel_spmd

F32 = mybir.dt.float32
BF16 = mybir.dt.bfloat16
I32 = mybir.dt.int32
AF = mybir.ActivationFunctionType
ALU = mybir.AluOpType
AX = mybir.AxisListType

ENGS = ("pe", "dve", "act", "pool", "sp")

D = 1024
T = 2048
LC = 256
NT = T + LC
NTI = NT // 128
NE = 32
BLK = 512
EPS = 1e-6
NEG = -30000.0


class Res:
    __slots__ = ("name", "w", "r")

    def __init__(self, name=""):
        self.name = name
        self.w = None
        self.r = []


class Sched:
    def __init__(self, nc, stack, n_dma_sems=12):
        self.nc = nc
        self.streams = {e: [] for e in ENGS}
        self.cnt = {e: 0 for e in ENGS}
        self.sems = {}
        for e in ENGS:
            self.sems[e] = stack.enter_context(nc.semaphore("s_" + e))
        self.dma_sems = {}
        self.dma_cnt = {}
        self.dma_rr = {}
        for q in ("sp", "pool"):
            self.dma_sems[q] = []
            for i in range(n_dma_sems):
                k = "d_%s_%d" % (q, i)
                self.sems[k] = stack.enter_context(nc.semaphore(k))
                self.dma_sems[q].append(k)
                self.dma_cnt[k] = 0
            self.dma_rr[q] = 0
        self.seen = {e: {} for e in ENGS}
        self.n_ins = 0

    def _need(self, eng, deps):
        best = {}
        for d in deps:
            if d is None:
                continue
            k, v = d
            if k == eng and eng == "pe":
                continue
            if self.seen[eng].get(k, 0) >= v:
                continue
            if best.get(k, 0) < v:
                best[k] = v
        for k, v in best.items():
            self.seen[eng][k] = v
        return list(best.items())

    @staticmethod
    def _deps(reads, writes):
        deps = []
        for r in reads:
            deps.append(r.w)
        for w in writes:
            deps.append(w.w)
            deps.extend(w.r)
        return deps

    @staticmethod
    def _commit(reads, writes, tok):
        for r in reads:
            r.r.append(tok)
            if len(r.r) > 16:
                m = {}
                for k, v in r.r:
                    if m.get(k, 0) < v:
                        m[k] = v
                r.r = list(m.items())
        for w in writes:
            w.w = tok
            w.r = []

    def op(self, eng, fn, reads=(), writes=(), signal=True):
        waits = self._need(eng, self._deps(reads, writes))
        if signal:
            self.cnt[eng] += 1
            tok = (eng, self.cnt[eng])
        else:
            tok = (eng, self.cnt[eng] + 1)
        self.streams[eng].append((waits, fn, signal, None))
        self._commit(reads, writes, tok)
        self.n_ins += 1 + len(waits)
        return tok

    def dma(self, q, fn, reads=(), writes=()):
        lst = self.dma_sems[q]
        k = lst[self.dma_rr[q] % len(lst)]
        self.dma_rr[q] += 1
        deps = self._deps(reads, writes)
        deps.append((k, self.dma_cnt[k]))
        waits = self._need(q, deps)
        self.dma_cnt[k] += 16
        tok = (k, self.dma_cnt[k])
        self.streams[q].append((waits, fn, False, k))
        self._commit(reads, writes, tok)
        self.n_ins += 1 + len(waits)
        return tok

    def barrier(self):
        final = []
        for e in ENGS:
            if self.cnt[e]:
                final.append((e, self.cnt[e]))
        for k, v in self.dma_cnt.items():
            if v:
                final.append((k, v))
        for e in ENGS:
            waits = self._need(e, [f for f in final if not (f[0] == e and e == "pe")])
            if waits:
                self.streams[e].append((waits, None, False, None))

    def emit(self):
        nc = self.nc
        sems = self.sems
        streams = self.streams

        def run(engname, engobj):
            for waits, fn, signal, dsem in streams[engname]:
                fold = None
                if fn is not None and dsem is None and waits:
                    fold = waits[-1]
                    waits = waits[:-1]
                for k, v in waits:
                    engobj.wait_ge(sems[k], v)
                if fn is None:
                    continue
                ins = fn(engobj)
                if fold is not None:
                    ins._wait_ge(sems[fold[0]], fold[1])
                if dsem is not None:
                    ins.then_inc(sems[dsem], 16)
                elif signal:
                    ins.then_inc(sems[engname], 1)

        with nc.Block() as block:
            @block.tensor
            def _(e):
                run("pe", e)

            @block.vector
            def _(e):
                run("dve", e)

            @block.scalar
            def _(e):
                run("act", e)

            @block.gpsimd
            def _(e):
                run("pool", e)

            @block.sync
            def _(e):
                run("sp", e)
        self.streams = {e: [] for e in ENGS}


def _na_bias_T(rpb):
    H = rpb.shape[0]
    out = np.full((H, 5, 640, 128), NEG, np.float32)
    pair_of_type = [0, 1, 5, 14, 15]
    for ty, j in enumerate(pair_of_type):
        ws = int(np.clip(2 * j - 4, 0, 23))
        a = ws // 2
        krow0 = 2 * a
        for rr in range(2):
            r = 2 * j + rr
            r0 = int(np.clip(r - 4, 0, 24))
            for i in range(10):
                kr = krow0 + i
                if not (r0 <= kr < r0 + 8):
                    continue
                dr = kr - r + 7
                c = np.arange(64)
                c0 = np.clip(c - 8, 0, 48)
                for cq in range(64):
                    kc = np.arange(c0[cq], c0[cq] + 16)
                    dc = kc - cq + 15
                    out[:, ty, i * 64 + kc, rr * 64 + cq] = rpb[:, dr, dc]
    return np.ascontiguousarray(out.reshape(H, 5, 5, 128, 128))


def _pcol(v):
    v = np.asarray(v, np.float32)
    return np.ascontiguousarray(v.reshape(-1, 128).T)


def _prep_shared(inp):
    sh = {}
    sh["ada_w"] = np.ascontiguousarray(inp["ada_w"], np.float32)
    sh["ada_b"] = np.ascontiguousarray(inp["ada_b"], np.float32)
    sh["norm_g"] = np.ascontiguousarray(np.stack([inp["norm1_g"], inp["norm2_g"]], 1), np.float32)
    sh["ev_w_in"] = np.ascontiguousarray(inp["ev_w_in"][0], np.float32)
    sh["ev_w_out"] = np.ascontiguousarray(inp["ev_w_out"][0], np.float32)
    sh["biasT"] = _na_bias_T(np.asarray(inp["ev_rpb"][0], np.float32))
    sh["od_w_in"] = np.ascontiguousarray(inp["od_w_in"][0], np.float32)
    sh["od_w_out"] = np.ascontiguousarray(inp["od_w_out"][0], np.float32)
    gates = []
    for dr in ("fwd", "bwd"):
        for nm in ("wa", "wx"):
            gates.append(np.asarray(inp["od_%s_%s" % (dr, nm)][0], np.float32))
    sh["od_gw"] = np.ascontiguousarray(np.stack(gates, 0))
    cols = []
    qg = np.tile(np.asarray(inp["ev_q_gain"][0], np.float32), 2)
    kg = np.tile(np.asarray(inp["ev_k_gain"][0], np.float32), 2)
    cols.append(qg[:, None]); cols.append(kg[:, None])
    for j in range(3):
        cols.append(_pcol(inp["ev_conv_w"][0, j]))
    cols.append(_pcol(inp["ev_conv_b"][0]))
    for j in range(4):
        cols.append(_pcol(inp["od_conv_w"][0, j]))
    cols.append(_pcol(inp["od_conv_b"][0]))
    for dr in ("fwd", "bwd"):
        for nm in ("ba", "bx", "lam"):
            cols.append(_pcol(inp["od_%s_%s" % (dr, nm)][0]))
    sh["pcols"] = np.ascontiguousarray(np.concatenate(cols, 1), np.float32)
    sh["router_w"] = np.ascontiguousarray(inp["router_w"], np.float32)
    sh["router_b"] = np.ascontiguousarray(inp["router_b"], np.float32)
    for nm in ("gate", "up", "down"):
        w = np.asarray(inp["exp_w_" + nm], np.float32)
        w = w.reshape(2, NE, 8, 128, 1024).transpose(0, 1, 3, 2, 4).reshape(2, NE * 128, 8 * 1024)
        for l in range(2):
            sh["w_%s%d" % (nm, l)] = np.ascontiguousarray(w[l])
    eb = np.concatenate([inp["exp_b_gate"], inp["exp_b_up"], inp["exp_b_down"]], -1).astype(np.float32)
    for l in range(2):
        sh["exp_b%d" % l] = np.ascontiguousarray(eb[l])
    sh["iota_p"] = np.arange(128, dtype=np.float32)[:, None].copy()
    sh["blk_start"] = np.tile((np.arange(80, dtype=np.float32) * BLK)[None], (128, 1)).copy()
    return sh


PC_QG, PC_KG, PC_ECW, PC_ECB, PC_OCW, PC_OCB, PC_G = 0, 1, 2, 14, 18, 50, 58


def build_program(NB=2, layers=(0, 1), do_moe=True, dbg=False, n_exp=NE, moe_stop=4):
    nc = bass.Bass("TRN2", target_bir_lowering=False)
    NTOK0 = NB * NT
    dt = nc.dram_tensor

    def din(name, shape, dtp=F32):
        return dt(name, list(shape), dtp, kind="ExternalInput").ap()

    xin = din("xin", [NB, NT, D])
    cvec = din("cvec", [NB + 1, D])
    ada_w = din("ada_w", [2, D, 6 * D])
    ada_b = din("ada_b", [2, 6 * D])
    norm_g = din("norm_g", [2, 2, D])
    ev_w_in = din("ev_w_in", [D, 3072])
    ev_w_out = din("ev_w_out", [D, D])
    biasT = din("biasT", [8, 5, 5, 128, 128])
    od_w_in = din("od_w_in", [D, 2048])
    od_w_out = din("od_w_out", [D, D])
    od_gw = din("od_gw", [4, 4, 256, 256])
    pcols_d = din("pcols", [128, 106])
    router_w = din("router_w", [2, D, NE])
    router_b = din("router_b", [2, NE])
    w_gate = [din("w_gate%d" % l, [n_exp * 128, 8192]) for l in range(2)]
    w_up = [din("w_up%d" % l, [n_exp * 128, 8192]) for l in range(2)]
    w_down = [din("w_down%d" % l, [n_exp * 128, 8192]) for l in range(2)]
    exp_b = [din("exp_b%d" % l, [n_exp, 3072]) for l in range(2)]
    iota_p_d = din("iota_p", [128, 1])
    blk_start_d = din("blk_start", [128, 80])
    out = dt("out", [NB, T, D], F32, kind="ExternalOutput").ap()
    if dbg:
        dbg_d4 = dt("dbg_d4", [128, NB * NTI * 4], I32, kind="ExternalOutput").ap()
        dbg_g4 = dt("dbg_g4", [128, NB * NTI * 4], F32, kind="ExternalOutput").ap()
        dbg_iw = dt("dbg_iw", [128, 80], I32, kind="ExternalOutput").ap()

    NBLK0 = NTOK0 * 4 // BLK + n_exp
    XR = dt("XR", [NB, NT, D], F32).ap()
    MOD = dt("MODs", [2, NB + 1, 6 * D], F32).ap()
    XS = dt("XS", [NBLK0 * BLK, D], BF16).ap()
    YS = dt("YS", [NBLK0 * BLK, D], F32).ap()
    R_XR, R_MOD, R_XS, R_YS, R_OUT = Res("XR"), Res("MOD"), Res("XS"), Res("YS"), Res("OUT")

    with contextlib.ExitStack() as g:
        S = Sched(nc, g)

        uid = [0]

        def sbuf(st, name, shape, dtp):
            uid[0] += 1
            return st.enter_context(nc.sbuf_tensor("%s_%d" % (name, uid[0]), list(shape), dtp))

        def psum(st, name, shape, dtp=F32):
            uid[0] += 1
            return st.enter_context(nc.psum_tensor("%s_%d" % (name, uid[0]), list(shape), dtp))

        ident = sbuf(g, "ident", [128, 128], F32)
        identb = sbuf(g, "identb", [128, 128], BF16)
        onesb = sbuf(g, "onesb", [128, 512], BF16)
        halfb = sbuf(g, "halfb", [2, 512], BF16)
        pcols = sbuf(g, "pcols_sb", [128, 106], F32)
        R_c = Res("const")
        S.op("dve", lambda e: e.memset(ident[:], 0.0), writes=[R_c])
        S.op("pool", lambda e: e.affine_select(out=ident[:], in_=ident[:], pattern=[[-1, 128]], compare_op=ALU.not_equal,
                                               fill=1.0, base=0, channel_multiplier=1), reads=[R_c], writes=[R_c])
        S.op("dve", lambda e: e.tensor_copy(out=identb[:], in_=ident[:]), reads=[R_c], writes=[R_c])
        S.op("dve", lambda e: e.memset(onesb[:], 1.0), writes=[R_c])
        S.op("dve", lambda e: e.memset(halfb[:], 0.5), writes=[R_c])
        S.dma("sp", lambda e: e.dma_start(out=pcols[:], in_=pcols_d[:, :]), writes=[R_c])
        S.barrier()
        S.emit()

        NV = NB + 1
        with contextlib.ExitStack() as st:
            cs = sbuf(st, "cs", [NV, D], F32)
            sT = sbuf(st, "sT", [128, 8, NV], F32)
            aw = [sbuf(st, "aw%d" % i, [128, 8, 512], F32) for i in range(2)]
            R_aw = [Res(), Res()]
            ab = sbuf(st, "ab", [NV, 6 * D], F32)
            msb = sbuf(st, "msb", [NV, 6 * D], F32)
            pT = psum(st, "pT", [128, 8, NV], F32)
            pm = [psum(st, "pm%d" % i, [NV, 512], F32) for i in range(2)]
            R_pm = [Res(), Res()]
            R_cs, R_sT, R_ab, R_msb, R_pT = Res(), Res(), Res(), Res(), Res()
            S.dma("sp", lambda e: e.dma_start(out=cs[:], in_=cvec[:, :]), writes=[R_cs])
            S.op("act", lambda e: e.activation(out=cs[:], in_=cs[:], func=AF.Silu), reads=[R_cs], writes=[R_cs])
            for c in range(8):
                S.op("pe", lambda e, c=c: e.transpose(out=pT[:, c, :], in_=cs[:, c * 128:(c + 1) * 128], identity=ident[0:NV, 0:NV]),
                     reads=[R_cs, R_c], writes=[R_pT], signal=(c == 7))
            S.op("dve", lambda e: e.tensor_copy(out=sT[:], in_=pT[:]), reads=[R_pT], writes=[R_sT])
            for l in range(2):
                for v in range(NV):
                    S.dma("sp", lambda e, l=l, v=v: e.dma_start(out=ab[v:v + 1, :], in_=ada_b[l:l + 1, :]), writes=[R_ab])
                for j in range(12):
                    k = j % 2
                    S.dma("sp", lambda e, l=l, j=j, k=k: e.dma_start(
                        out=aw[k][:], in_=ada_w[l, :, j * 512:(j + 1) * 512].rearrange("(c p) n -> p c n", p=128)),
                        writes=[R_aw[k]])
                    for c in range(8):
                        S.op("pe", lambda e, c=c, k=k: e.matmul(pm[k][:], lhsT=sT[:, c, :], rhs=aw[k][:, c, :],
                                                                start=(c == 0), stop=(c == 7)),
                             reads=[R_sT, R_aw[k]], writes=[R_pm[k]], signal=(c == 7))
                    S.op("dve", lambda e, j=j, k=k: e.tensor_tensor(out=msb[:, j * 512:(j + 1) * 512], in0=pm[k][:],
                                                                     in1=ab[:, j * 512:(j + 1) * 512], op=ALU.add),
                         reads=[R_pm[k], R_ab], writes=[R_msb])
                S.dma("sp", lambda e, l=l: e.dma_start(out=MOD[l, :, :], in_=msb[:]), reads=[R_msb], writes=[R_MOD])
            S.barrier()
            S.emit()

        def load_bcast(st, name, src_ap, reads, n=D):
            t = sbuf(st, name, [128, n], F32)
            r = Res(name)
            S.dma("sp", lambda e: e.dma_start(out=t[:], in_=src_ap.partition_broadcast(128)), reads=reads, writes=[r])
            return t, r

        def norm_mod_tiles(st, l, which, vecs):
            res = {}
            gt, rg = load_bcast(st, "ng%d%d" % (l, which), norm_g[l, which, :], [])
            for v in vecs:
                sc, rsc = load_bcast(st, "sc%d" % v, MOD[l, v, (3 * which + 1) * D:(3 * which + 2) * D], [R_MOD])
                shh, rsh = load_bcast(st, "sh%d" % v, MOD[l, v, (3 * which) * D:(3 * which + 1) * D], [R_MOD])
                S.op("dve", lambda e, sc=sc: e.scalar_tensor_tensor(out=sc[:], in0=sc[:], scalar=1.0, in1=gt[:],
                                                                      op0=ALU.add, op1=ALU.mult),
                     reads=[rsc, rg], writes=[rsc])
                res[v] = (sc, rsc, shh, rsh)
            return res

        def rms_mod(st_tmp, xt, r_xt, G, rG, SH, rSH, ht, r_ht, small, r_small, junk, r_junk):
            S.op("dve", lambda e: e.memset(small[:, 0:1], 0.0), reads=[r_small], writes=[r_small])
            S.op("act", lambda e: e.activation(out=junk[:], in_=xt[:], func=AF.Square, accum_out=small[:, 0:1]),
                 reads=[r_xt, r_small], writes=[r_junk, r_small])
            S.op("act", lambda e: e.activation(out=small[:, 1:2], in_=small[:, 0:1], func=AF.Sqrt, scale=1.0 / D, bias=small[:, 3:4]),
                 reads=[r_small], writes=[r_small])
            S.op("dve", lambda e: e.reciprocal(out=small[:, 2:3], in_=small[:, 1:2]), reads=[r_small], writes=[r_small])
            S.op("dve", lambda e: e.scalar_tensor_tensor(out=ht[:], in0=xt[:], scalar=small[:, 2:3], in1=G[:],
                                                         op0=ALU.mult, op1=ALU.mult),
                 reads=[r_xt, r_small, rG], writes=[r_ht])
            S.op("dve", lambda e: e.tensor_tensor(out=ht[:], in0=ht[:], in1=SH[:], op=ALU.add),
                 reads=[r_ht, rSH], writes=[r_ht])

        def new_small(st, name):
            small = sbuf(st, name, [128, 4], F32)
            r = Res(name)
            S.op("dve", lambda e: e.memset(small[:], 0.0), writes=[r])
            S.op("dve", lambda e: e.memset(small[:, 3:4], EPS), reads=[r], writes=[r])
            return small, r

        def norm_to_hT(st, l, b, src_ap, src_res, hT, r_hT, vec_of_tile):
            with contextlib.ExitStack() as s2:
                vecs = sorted(set(vec_of_tile))
                tiles = norm_mod_tiles(s2, l, 0, vecs)
                small, r_small = new_small(s2, "n1small")
                junk = sbuf(s2, "n1junk", [128, D], F32); r_junk = Res()
                xt = [sbuf(s2, "n1x%d" % i, [128, D], F32) for i in range(2)]
                r_xt = [Res(), Res()]
                ht = [sbuf(s2, "n1h%d" % i, [128, D], F32) for i in range(2)]
                r_ht = [Res(), Res()]
                ptp = [psum(s2, "n1p%d" % i, [128, 4, 128], F32) for i in range(2)]
                r_ptp = [Res(), Res()]
                for i in range(NTI):
                    k = i % 2
                    G, rG, SH, rSH = tiles[vec_of_tile[i]]
                    S.dma("sp", lambda e, i=i, k=k: e.dma_start(out=xt[k][:], in_=src_ap[b, i * 128:(i + 1) * 128, :]),
                          reads=[src_res], writes=[r_xt[k]])
                    rms_mod(s2, xt[k], r_xt[k], G, rG, SH, rSH, ht[k], r_ht[k], small, r_small, junk, r_junk)
                    for hf in range(2):
                        for c4 in range(4):
                            c = hf * 4 + c4
                            S.op("pe", lambda e, k=k, hf=hf, c4=c4, c=c: e.transpose(
                                out=ptp[hf][:, c4, :], in_=ht[k][:, c * 128:(c + 1) * 128], identity=ident[:]),
                                reads=[r_ht[k], R_c], writes=[r_ptp[hf]], signal=(c4 == 3))
                        eng = "act" if hf == 0 else "dve"
                        if eng == "act":
                            S.op("act", lambda e, i=i, hf=hf: e.activation(
                                out=hT[:, hf * 4:(hf + 1) * 4, i * 128:(i + 1) * 128], in_=ptp[hf][:], func=AF.Copy),
                                reads=[r_ptp[hf]], writes=[r_hT])
                        else:
                            S.op("dve", lambda e, i=i, hf=hf: e.tensor_copy(
                                out=hT[:, hf * 4:(hf + 1) * 4, i * 128:(i + 1) * 128], in_=ptp[hf][:]),
                                reads=[r_ptp[hf]], writes=[r_hT])
                S.barrier()
                S.emit()

        def stream_w(st, name, n=2):
            bufs = [sbuf(st, "%s%d" % (name, i), [128, 8, 512], BF16) for i in range(n)]
            return bufs, [Res() for _ in range(n)]

        def load_w(buf, r, w_ap, col0, ncol=512):
            S.dma("pool", lambda e: e.dma_start(out=buf[:, :, 0:ncol],
                                                 in_=w_ap[:, col0:col0 + ncol].rearrange("(c p) n -> p c n", p=128)),
                  writes=[r])

        TOKP = [(i * 512, min(512, NT - i * 512)) for i in range((NT + 511) // 512)]

        def outproj_residual(st, l, b, srcs, w_ap, x_src, x_res, tiles_range, vec_of_tile, x_tok_off=0):
            with contextlib.ExitStack() as s2:
                wo = sbuf(s2, "wo", [128, 8, D], BF16); r_wo = Res()
                for hh in range(2):
                    S.dma("pool", lambda e, hh=hh: e.dma_start(
                        out=wo[:, :, hh * 512:(hh + 1) * 512],
                        in_=w_ap[:, hh * 512:(hh + 1) * 512].rearrange("(c p) n -> p c n", p=128)), writes=[r_wo])
                g1 = {}
                for v in sorted(set(vec_of_tile[i] for i in tiles_range)):
                    g1[v] = load_bcast(s2, "g1_%d" % v, MOD[l, v, 2 * D:3 * D], [R_MOD])
                xt = [sbuf(s2, "opx%d" % i, [128, D], F32) for i in range(2)]
                r_xt = [Res(), Res()]
                py = [psum(s2, "opy%d" % i, [128, 512], F32) for i in range(4)]
                r_py = [Res() for _ in range(4)]
                for n_i, i in enumerate(tiles_range):
                    k = n_i % 2
                    gt, rgt = g1[vec_of_tile[i]]
                    S.dma("sp", lambda e, i=i, k=k: e.dma_start(out=xt[k][:], in_=x_src[b, i * 128:(i + 1) * 128, :]),
                          reads=[x_res], writes=[r_xt[k]])
                    for hh in range(2):
                        pk = k * 2 + hh
                        for c in range(8):
                            tsr, ch, rs, toff = srcs[c]
                            S.op("pe", lambda e, tsr=tsr, ch=ch, toff=toff, i=i, c=c, hh=hh, pk=pk: e.matmul(
                                py[pk][:], lhsT=tsr[:, ch, i * 128 - toff:(i + 1) * 128 - toff],
                                rhs=wo[:, c, hh * 512:(hh + 1) * 512], start=(c == 0), stop=(c == 7)),
                                reads=[rs, r_wo], writes=[r_py[pk]], signal=(c == 7))
                        S.op("dve", lambda e, hh=hh, pk=pk, gt=gt, k=k: e.tensor_tensor(
                            out=xg[k][:, hh * 512:(hh + 1) * 512],
                            in0=py[pk][:], in1=gt[:, hh * 512:(hh + 1) * 512], op=ALU.mult),
                            reads=[r_py[pk], rgt], writes=[r_xg[k]])
                    S.op("pool", lambda e, k=k: e.tensor_tensor(out=xt[k][:], in0=xt[k][:], in1=xg[k][:], op=ALU.add),
                         reads=[r_xg[k], r_xt[k]], writes=[r_xt[k]])
                    S.dma("sp", lambda e, i=i, k=k: e.dma_start(out=XR[b, i * 128:(i + 1) * 128, :], in_=xt[k][:]),
                          reads=[r_xt[k]], writes=[R_XR])
                S.barrier()
                S.emit()

        xg = [sbuf(g, "xg%d" % i, [128, D], F32) for i in range(2)]
        r_xg = [Res(), Res()]

        VEC_OF_TILE = lambda b: [NB, NB] + [b] * 16

        def layer0_mixer(b):
            with contextlib.ExitStack() as st:
                qT = sbuf(st, "qT", [128, 4, NT], BF16); r_qT = Res()
                kT = sbuf(st, "kT", [128, 4, NT], BF16); r_kT = Res()
                V = sbuf(st, "V", [128, NTI, 512], BF16); r_V = Res()
                OB = sbuf(st, "OB", [128, 4, NT], BF16); r_OB = Res()
                with contextlib.ExitStack() as s1:
                    hT = sbuf(s1, "hT", [128, 8, NT], BF16); r_hT = Res()
                    norm_to_hT(s1, 0, b, xin, Res(), hT, r_hT, VEC_OF_TILE(b))
                    wb, r_wb = stream_w(s1, "wi")
                    blk1 = sbuf(s1, "blk1", [128, 128], BF16); r_blk = Res()
                    S.op("dve", lambda e: e.memset(blk1[:], 0.0), writes=[r_blk])
                    S.op("dve", lambda e: e.memset(blk1[0:64, 0:64], 1.0 / 64), reads=[r_blk], writes=[r_blk])
                    S.op("dve", lambda e: e.memset(blk1[64:128, 64:128], 1.0 / 64), reads=[r_blk], writes=[r_blk])
                    pj = [psum(s1, "pj%d" % i, [128, 512], F32) for i in range(3)]
                    r_pj = [Res() for _ in range(3)]
                    pq = [psum(s1, "pq%d" % i, [128, 512], F32) for i in range(2)]
                    r_pq = [Res() for _ in range(2)]
                    sq = [sbuf(s1, "sq%d" % i, [128, 512], BF16) for i in range(2)]
                    r_sq = [Res(), Res()]
                    rs_t = [sbuf(s1, "rst%d" % i, [128, 512], F32) for i in range(2)]
                    r_rs = [Res(), Res()]
                    eps64 = sbuf(s1, "eps64", [128, 2], F32); r_e64 = Res()
                    S.op("dve", lambda e: e.memset(eps64[:, 0:1], EPS), writes=[r_e64])
                    S.op("dve", lambda e: e.memset(eps64[:, 1:2], 64.0 * EPS), reads=[r_e64], writes=[r_e64])
                    cnt = [0]

                    def proj_piece(wbuf, r_w, wc, tp):
                        t0, nt_ = TOKP[tp]
                        k = cnt[0] % 3
                        cnt[0] += 1
                        for c in range(8):
                            S.op("pe", lambda e, c=c, k=k: e.matmul(pj[k][:, 0:nt_], lhsT=wbuf[:, c, wc * 128:(wc + 1) * 128],
                                                                   rhs=hT[:, c, t0:t0 + nt_], start=(c == 0), stop=(c == 7)),
                                 reads=[r_w, r_hT], writes=[r_pj[k]], signal=(c == 7))
                        return k, t0, nt_

                    for grp, (dst, r_dst, gcol, sc_, ecol) in enumerate(((qT, r_qT, PC_QG, 64.0, 1), (kT, r_kT, PC_KG, 1.0, 0))):
                        bi = grp % 2
                        load_w(wb[bi], r_wb[bi], ev_w_in, grp * 512)
                        for wc in range(4):
                            for tp in range(len(TOKP)):
                                k, t0, nt_ = proj_piece(wb[bi], r_wb[bi], wc, tp)
                                k2 = cnt[0] % 2
                                S.op("act", lambda e, k=k, k2=k2, nt_=nt_: e.activation(out=sq[k2][:, 0:nt_], in_=pj[k][:, 0:nt_], func=AF.Square),
                                     reads=[r_pj[k]], writes=[r_sq[k2]])
                                S.op("pe", lambda e, k2=k2, nt_=nt_: e.matmul(pq[k2][:, 0:nt_], lhsT=blk1[:], rhs=sq[k2][:, 0:nt_], start=True, stop=True),
                                     reads=[r_blk, r_sq[k2]], writes=[r_pq[k2]])
                                S.op("act", lambda e, k2=k2, nt_=nt_, sc_=sc_, ecol=ecol: e.activation(
                                    out=rs_t[k2][:, 0:nt_], in_=pq[k2][:, 0:nt_], func=AF.Sqrt, scale=sc_, bias=eps64[:, ecol:ecol + 1]),
                                    reads=[r_pq[k2], r_e64], writes=[r_rs[k2]])
                                S.op("dve", lambda e, k2=k2, nt_=nt_: e.reciprocal(out=rs_t[k2][:, 0:nt_], in_=rs_t[k2][:, 0:nt_]),
                                     reads=[r_rs[k2]], writes=[r_rs[k2]])
                                S.op("dve", lambda e, k=k, k2=k2, nt_=nt_, t0=t0, wc=wc, dst=dst, gcol=gcol: e.scalar_tensor_tensor(
                                    out=dst[:, wc, t0:t0 + nt_], in0=pj[k][:, 0:nt_], scalar=pcols[:, gcol:gcol + 1], in1=rs_t[k2][:, 0:nt_],
                                    op0=ALU.mult, op1=ALU.mult), reads=[r_pj[k], r_rs[k2], R_c], writes=[r_dst])
                    load_w(wb[0], r_wb[0], ev_w_in, 1024)
                    for i in range(NTI):
                        k = cnt[0] % 3
                        cnt[0] += 1
                        for c in range(8):
                            S.op("pe", lambda e, c=c, k=k, i=i: e.matmul(pj[k][:], lhsT=hT[:, c, i * 128:(i + 1) * 128], rhs=wb[0][:, c, :],
                                                                        start=(c == 0), stop=(c == 7)),
                                 reads=[r_wb[0], r_hT], writes=[r_pj[k]], signal=(c == 7))
                        S.op("act", lambda e, k=k, i=i: e.activation(out=V[:, i, :], in_=pj[k][:], func=AF.Copy),
                             reads=[r_pj[k]], writes=[r_V])
                    load_w(wb[1], r_wb[1], ev_w_in, 2560)
                    wcg = sbuf(s1, "wcg", [128, 8, 512], BF16); r_wcg = Res()
                    load_w(wcg, r_wcg, ev_w_in, 2048)
                    load_w(wb[0], r_wb[0], ev_w_in, 1536)
                    xs_f = sbuf(s1, "xs_f", [128, NT], F32); r_xs = Res()
                    cx_f = sbuf(s1, "cx_f", [128, NT], F32); r_cx = Res()
                    t_f = sbuf(s1, "t_f", [128, NT], F32); r_t = Res()
                    for f in range(4):
                        for tp in range(len(TOKP)):
                            k, t0, nt_ = proj_piece(wb[1], r_wb[1], f, tp)
                            S.op("act", lambda e, k=k, t0=t0, nt_=nt_: e.activation(out=xs_f[:, t0:t0 + nt_], in_=pj[k][:, 0:nt_], func=AF.Copy),
                                 reads=[r_pj[k]], writes=[r_xs])
                        for tp in range(len(TOKP)):
                            k, t0, nt_ = proj_piece(wcg, r_wcg, f, tp)
                            S.op("dve", lambda e, k=k, t0=t0, nt_=nt_: e.tensor_tensor(out=cx_f[:, t0:t0 + nt_], in0=pj[k][:, 0:nt_],
                                                                                      in1=xs_f[:, t0:t0 + nt_], op=ALU.mult),
                                 reads=[r_pj[k], r_xs], writes=[r_cx])
                        w0 = pcols[:, PC_ECW + 0 * 4 + f:PC_ECW + 0 * 4 + f + 1]
                        w1 = pcols[:, PC_ECW + 1 * 4 + f:PC_ECW + 1 * 4 + f + 1]
                        w2 = pcols[:, PC_ECW + 2 * 4 + f:PC_ECW + 2 * 4 + f + 1]
                        bb = pcols[:, PC_ECB + f:PC_ECB + f + 1]
                        S.op("dve", lambda e, w1=w1, bb=bb: e.tensor_scalar(out=t_f[:], in0=cx_f[:], scalar1=w1, scalar2=bb, op0=ALU.mult, op1=ALU.add),
                             reads=[r_cx, R_c], writes=[r_t])
                        for (s0, sn) in ((0, LC), (LC, T)):
                            S.op("dve", lambda e, s0=s0, sn=sn, w0=w0: e.scalar_tensor_tensor(
                                out=t_f[:, s0 + 1:s0 + sn], in0=cx_f[:, s0:s0 + sn - 1], scalar=w0, in1=t_f[:, s0 + 1:s0 + sn],
                                op0=ALU.mult, op1=ALU.add), reads=[r_cx, r_t, R_c], writes=[r_t])
                            S.op("dve", lambda e, s0=s0, sn=sn, w2=w2: e.scalar_tensor_tensor(
                                out=t_f[:, s0:s0 + sn - 1], in0=cx_f[:, s0 + 1:s0 + sn], scalar=w2, in1=t_f[:, s0:s0 + sn - 1],
                                op0=ALU.mult, op1=ALU.add), reads=[r_cx, r_t, R_c], writes=[r_t])
                        for tp in range(len(TOKP)):
                            k, t0, nt_ = proj_piece(wb[0], r_wb[0], f, tp)
                            S.op("dve", lambda e, k=k, t0=t0, nt_=nt_, f=f: e.tensor_tensor(out=OB[:, f, t0:t0 + nt_], in0=pj[k][:, 0:nt_],
                                                                                           in1=t_f[:, t0:t0 + nt_], op=ALU.mult),
                                 reads=[r_pj[k], r_t], writes=[r_OB])
                    S.barrier()
                    S.emit()
                OA = sbuf(st, "OA", [128, 4, NT], BF16); r_OA = Res()
                with contextlib.ExitStack() as s1:
                    bias = [sbuf(s1, "bias%d" % i, [128, 5, 5, 128], BF16) for i in range(2)]
                    r_bias = [Res(), Res()]
                    sta = [psum(s1, "sta%d" % i, [128, 4, 128], F32) for i in range(2)]
                    stb = [psum(s1, "stb%d" % i, [128, 4, 128], F32) for i in range(2)]
                    r_sta = [Res(), Res()]; r_stb = [Res(), Res()]
                    po = [psum(s1, "po%d" % i, [64, 2, 128], F32) for i in range(2)]
                    r_po = [Res(), Res()]
                    PT = [sbuf(s1, "PT%d" % i, [128, 7, 128], BF16) for i in range(2)]
                    r_PT = [Res(), Res()]
                    rc = [sbuf(s1, "rc%d" % i, [64, 128], F32) for i in range(2)]
                    r_rc = [Res(), Res()]
                    it = 0
                    for h in range(8):
                        hp, hc = h % 2, h // 2
                        bsel = h % 2
                        S.dma("pool", lambda e, h=h, bsel=bsel: e.dma_start(out=bias[bsel][:], in_=biasT[h].rearrange("t c p q -> p t c q")),
                              writes=[r_bias[bsel]])
                        jobs = [("ctx", 0), ("ctx", 1)] + [("lat", j) for j in range(16)]
                        for kind, j in jobs:
                            k = it % 2
                            it += 1
                            if kind == "ctx":
                                qtile = j
                                ktiles = [0, 1]
                                ty = None
                            else:
                                qtile = 2 + j
                                ws = min(max(2 * j - 4, 0), 23)
                                a = ws // 2
                                ktiles = [0, 1] + [2 + a + cc for cc in range(5)]
                                ty = {0: 0, 1: 1, 14: 3, 15: 4}.get(j, 2)
                            nk = len(ktiles)
                            q_ap = qT[hp * 64:(hp + 1) * 64, hc, qtile * 128:(qtile + 1) * 128]
                            for kc, kt in enumerate(ktiles):
                                dstp, r_dst = (sta[k], r_sta[k]) if kc < 4 else (stb[k], r_stb[k])
                                has_b = (ty is not None and kc >= 2)
                                last = (kc == min(3, nk - 1)) or (kc == nk - 1)
                                S.op("pe", lambda e, dstp=dstp, kc=kc, kt=kt, q_ap=q_ap, has_b=has_b, hp=hp, hc=hc: e.matmul(
                                    dstp[:, kc % 4, :], lhsT=kT[hp * 64:(hp + 1) * 64, hc, kt * 128:(kt + 1) * 128], rhs=q_ap,
                                    start=True, stop=(not has_b)), reads=[r_kT, r_qT], writes=[r_dst], signal=(last and not has_b))
                                if has_b:
                                    S.op("pe", lambda e, dstp=dstp, kc=kc, ty=ty, bsel=bsel: e.matmul(
                                        dstp[:, kc % 4, :], lhsT=identb[:], rhs=bias[bsel][:, ty, kc - 2, :], start=False, stop=True),
                                        reads=[r_bias[bsel], R_c], writes=[r_dst], signal=last)
                            na = min(4, nk)
                            S.op("act", lambda e, k=k, na=na: e.activation(out=PT[k][:, 0:na, :], in_=sta[k][:, 0:na, :], func=AF.Exp),
                                 reads=[r_sta[k]], writes=[r_PT[k]])
                            if nk > 4:
                                S.op("act", lambda e, k=k, nk=nk: e.activation(out=PT[k][:, 4:nk, :], in_=stb[k][:, 0:nk - 4, :], func=AF.Exp),
                                     reads=[r_stb[k]], writes=[r_PT[k]])
                            for kc, kt in enumerate(ktiles):
                                S.op("pe", lambda e, k=k, kc=kc, kt=kt, nk=nk, h=h: e.matmul(
                                    po[k][:, 0, :], lhsT=V[:, kt, h * 64:(h + 1) * 64], rhs=PT[k][:, kc, :], start=(kc == 0), stop=(kc == nk - 1)),
                                    reads=[r_V, r_PT[k]], writes=[r_po[k]], signal=False)
                            for kc, kt in enumerate(ktiles):
                                S.op("pe", lambda e, k=k, kc=kc, nk=nk: e.matmul(
                                    po[k][:, 1, :], lhsT=onesb[:, 0:64], rhs=PT[k][:, kc, :], start=(kc == 0), stop=(kc == nk - 1)),
                                    reads=[R_c, r_PT[k]], writes=[r_po[k]], signal=(kc == nk - 1))
                            S.op("dve", lambda e, k=k: e.reciprocal(out=rc[k][:], in_=po[k][:, 1, :]), reads=[r_po[k]], writes=[r_rc[k]])
                            S.op("dve", lambda e, k=k, hp=hp, hc=hc, qtile=qtile: e.tensor_tensor(
                                out=OA[hp * 64:(hp + 1) * 64, hc, qtile * 128:(qtile + 1) * 128], in0=po[k][:, 0, :], in1=rc[k][:], op=ALU.mult),
                                reads=[r_po[k], r_rc[k]], writes=[r_OA])
                    S.barrier()
                    S.emit()
                srcs = [(OA, c, r_OA, 0) for c in range(4)] + [(OB, c, r_OB, 0) for c in range(4)]
                outproj_residual(st, 0, b, srcs, ev_w_out, xin, Res(), list(range(NTI)), VEC_OF_TILE(b))

        def layer1_mixer(b):
            with contextlib.ExitStack() as st:
                UC = sbuf(st, "UC", [128, 8, NT], BF16); r_UC = Res()
                GG = sbuf(st, "GG", [128, 8, T], BF16); r_GG = Res()
                with contextlib.ExitStack() as s1:
                    hT = sbuf(s1, "hT1", [128, 8, NT], BF16); r_hT = Res()
                    norm_to_hT(s1, 1, b, XR, R_XR, hT, r_hT, VEC_OF_TILE(b))
                    wb, r_wb = stream_w(s1, "wi1")
                    pj = [psum(s1, "pj1%d" % i, [128, 512], F32) for i in range(3)]
                    r_pj = [Res() for _ in range(3)]
                    u_f = sbuf(s1, "u_f", [128, NT], F32); r_u = Res()
                    t_f = sbuf(s1, "t1_f", [128, NT], F32); r_t = Res()
                    cnt = [0]
                    for grp in range(4):
                        bi = grp % 2
                        load_w(wb[bi], r_wb[bi], od_w_in, grp * 512)
                        for wc in range(4):
                            f = (grp % 2) * 4 + wc
                            for tp in range(len(TOKP)):
                                t0, nt_ = TOKP[tp]
                                if grp < 2 and t0 + nt_ <= LC:
                                    continue
                                k = cnt[0] % 3
                                cnt[0] += 1
                                for c in range(8):
                                    S.op("pe", lambda e, c=c, k=k, bi=bi, wc=wc, t0=t0, nt_=nt_: e.matmul(
                                        pj[k][:, 0:nt_], lhsT=wb[bi][:, c, wc * 128:(wc + 1) * 128], rhs=hT[:, c, t0:t0 + nt_],
                                        start=(c == 0), stop=(c == 7)), reads=[r_wb[bi], r_hT], writes=[r_pj[k]], signal=(c == 7))
                                if grp < 2:
                                    lo = max(t0, LC)
                                    S.op("act", lambda e, k=k, f=f, lo=lo, t0=t0, nt_=nt_: e.activation(
                                        out=GG[:, f, lo - LC:t0 + nt_ - LC], in_=pj[k][:, lo - t0:nt_], func=AF.Gelu),
                                        reads=[r_pj[k]], writes=[r_GG])
                                else:
                                    S.op("act", lambda e, k=k, t0=t0, nt_=nt_: e.activation(out=u_f[:, t0:t0 + nt_], in_=pj[k][:, 0:nt_], func=AF.Copy),
                                         reads=[r_pj[k]], writes=[r_u])
                            if grp >= 2:
                                wj = [pcols[:, PC_OCW + j * 8 + f:PC_OCW + j * 8 + f + 1] for j in range(4)]
                                bb = pcols[:, PC_OCB + f:PC_OCB + f + 1]
                                S.op("dve", lambda e, wj=wj, bb=bb: e.tensor_scalar(out=t_f[:], in0=u_f[:], scalar1=wj[1], scalar2=bb, op0=ALU.mult, op1=ALU.add),
                                     reads=[r_u, R_c], writes=[r_t])
                                for (s0, sn) in ((0, LC), (LC, T)):
                                    S.op("dve", lambda e, s0=s0, sn=sn, wj=wj: e.scalar_tensor_tensor(
                                        out=t_f[:, s0 + 1:s0 + sn], in0=u_f[:, s0:s0 + sn - 1], scalar=wj[0], in1=t_f[:, s0 + 1:s0 + sn],
                                        op0=ALU.mult, op1=ALU.add), reads=[r_u, r_t, R_c], writes=[r_t])
                                    S.op("dve", lambda e, s0=s0, sn=sn, wj=wj: e.scalar_tensor_tensor(
                                        out=t_f[:, s0:s0 + sn - 1], in0=u_f[:, s0 + 1:s0 + sn], scalar=wj[2], in1=t_f[:, s0:s0 + sn - 1],
                                        op0=ALU.mult, op1=ALU.add), reads=[r_u, r_t, R_c], writes=[r_t])
                                    S.op("dve", lambda e, s0=s0, sn=sn, wj=wj: e.scalar_tensor_tensor(
                                        out=t_f[:, s0:s0 + sn - 2], in0=u_f[:, s0 + 2:s0 + sn], scalar=wj[3], in1=t_f[:, s0:s0 + sn - 2],
                                        op0=ALU.mult, op1=ALU.add), reads=[r_u, r_t, R_c], writes=[r_t])
                                S.op("act", lambda e, f=f: e.activation(out=UC[:, f, :], in_=t_f[:], func=AF.Copy), reads=[r_t], writes=[r_UC])
                    S.barrier()
                    S.emit()
                YI = sbuf(st, "YI", [128, 8, T], BF16); r_YI = Res()
                with contextlib.ExitStack() as s1:
                    gw = sbuf(s1, "gw", [128, 4, 4, 2, 256], BF16); r_gw = Res()
                    for m in range(4):
                        S.dma("pool", lambda e, m=m: e.dma_start(out=gw[:, m], in_=od_gw[m].rearrange("b (k p) n -> p b k n", p=128)),
                              writes=[r_gw])
                    cl = sbuf(s1, "cl", [128, 2, 8], F32); r_cl = Res()
                    for d_ in range(2):
                        lam = pcols[:, PC_G + d_ * 24 + 16:PC_G + d_ * 24 + 24]
                        S.op("act", lambda e, d_=d_, lam=lam: e.activation(out=cl[:, d_, :], in_=lam, func=AF.Exp, scale=-1.0), reads=[R_c], writes=[r_cl])
                        S.op("act", lambda e, d_=d_: e.activation(out=cl[:, d_, :], in_=cl[:, d_, :], func=AF.Ln, bias=1.0, scale=1.0), reads=[r_cl], writes=[r_cl])
                        S.op("dve", lambda e, d_=d_: e.tensor_scalar(out=cl[:, d_, :], in0=cl[:, d_, :], scalar1=-8.0, scalar2=None, op0=ALU.mult),
                             reads=[r_cl], writes=[r_cl])
                    pg = [psum(s1, "pg%d" % i, [128, 512], F32) for i in range(4)]
                    r_pg = [Res() for _ in range(4)]
                    Rt = sbuf(s1, "Rt", [128, NT], F32); r_R = Res()
                    It = sbuf(s1, "It", [128, NT], F32); r_I = Res()
                    At = sbuf(s1, "At", [128, NT], F32); r_A = Res()
                    Bt = sbuf(s1, "Bt", [128, NT], F32); r_B = Res()
                    Hf = sbuf(s1, "Hf", [128, NT], F32); r_Hf = Res()
                    Hb = sbuf(s1, "Hb", [128, NT], F32); r_Hb = Res()
                    cnt = [0]
                    for f in range(8):
                        blk = f // 2
                        for d_ in range(2):
                            for gi, (dstt, r_dst) in enumerate(((Rt, r_R), (It, r_I))):
                                m = d_ * 2 + gi
                                bcol = pcols[:, PC_G + d_ * 24 + gi * 8 + f:PC_G + d_ * 24 + gi * 8 + f + 1]
                                for tp in range(len(TOKP)):
                                    t0, nt_ = TOKP[tp]
                                    k = cnt[0] % 4
                                    cnt[0] += 1
                                    for kc in range(2):
                                        S.op("pe", lambda e, k=k, m=m, kc=kc, t0=t0, nt_=nt_, blk=blk, f=f: e.matmul(
                                            pg[k][:, 0:nt_], lhsT=gw[:, m, blk, kc, (f % 2) * 128:(f % 2 + 1) * 128],
                                            rhs=UC[:, 2 * blk + kc, t0:t0 + nt_], start=(kc == 0), stop=(kc == 1)),
                                            reads=[r_gw, r_UC], writes=[r_pg[k]], signal=(kc == 1))
                                    S.op("act", lambda e, k=k, dstt=dstt, t0=t0, nt_=nt_, bcol=bcol: e.activation(
                                        out=dstt[:, t0:t0 + nt_], in_=pg[k][:, 0:nt_], func=AF.Sigmoid, bias=bcol, scale=1.0),
                                        reads=[r_pg[k], R_c], writes=[r_dst])
                            S.op("act", lambda e, d_=d_, f=f: e.activation(out=At[:], in_=Rt[:], func=AF.Exp, scale=cl[:, d_, f:f + 1]),
                                 reads=[r_R, r_cl], writes=[r_A])
                            S.op("pool", lambda e: e.tensor_tensor(out=Bt[:], in0=At[:], in1=At[:], op=ALU.mult), reads=[r_A], writes=[r_B])
                            S.op("act", lambda e: e.activation(out=Bt[:], in_=Bt[:], func=AF.Sqrt, scale=-1.0, bias=1.0), reads=[r_B], writes=[r_B])
                            S.op("dve", lambda e: e.tensor_tensor(out=Bt[:], in0=Bt[:], in1=It[:], op=ALU.mult), reads=[r_B, r_I], writes=[r_B])
                            S.op("dve", lambda e, f=f: e.tensor_tensor(out=Bt[:], in0=Bt[:], in1=UC[:, f, :], op=ALU.mult), reads=[r_B, r_UC], writes=[r_B])
                            if d_ == 0:
                                S.op("dve", lambda e: e.tensor_tensor_scan(out=Hf[:], data0=At[:], data1=Bt[:], initial=0.0, op0=ALU.mult, op1=ALU.add),
                                     reads=[r_A, r_B], writes=[r_Hf])
                            else:
                                S.op("dve", lambda e: e.tensor_tensor_scan(out=Hb[:, 0:LC][:, ::-1],
                                                                           data0=At[:, 0:LC][:, ::-1], data1=Bt[:, 0:LC][:, ::-1],
                                                                           initial=0.0, op0=ALU.mult, op1=ALU.add),
                                     reads=[r_A, r_B], writes=[r_Hb])
                                S.op("dve", lambda e: e.tensor_tensor_scan(out=Hb[:, LC:NT][:, ::-1], data0=At[:, LC:NT][:, ::-1],
                                                                           data1=Bt[:, LC:NT][:, ::-1], initial=Hb[:, 0:1],
                                                                           op0=ALU.mult, op1=ALU.add),
                                     reads=[r_A, r_B, r_Hb], writes=[r_Hb])
                        S.op("pool", lambda e: e.tensor_tensor(out=Hf[:, LC:NT], in0=Hf[:, LC:NT], in1=Hb[:, LC:NT], op=ALU.add),
                             reads=[r_Hf, r_Hb], writes=[r_Hf])
                        S.op("dve", lambda e, f=f: e.tensor_tensor(out=YI[:, f, :], in0=Hf[:, LC:NT], in1=GG[:, f, :], op=ALU.mult),
                             reads=[r_Hf, r_GG], writes=[r_YI])
                    S.barrier()
                    S.emit()
                srcs = [(YI, c, r_YI, LC) for c in range(8)]
                outproj_residual(st, 1, b, srcs, od_w_out, XR, R_XR, list(range(2, NTI)), VEC_OF_TILE(b))

        def moe_layer(l, tok_tiles, final):
            ntile = len(tok_tiles)
            ntok = ntile * 128
            nblk = ntok * 4 // BLK + n_exp
            with contextlib.ExitStack() as st:
                sH2 = contextlib.ExitStack()
                LG = sbuf(st, "LG", [128, ntile, NE], F32); r_LG = Res()
                D4 = sbuf(st, "D4", [128, ntile * 4], I32); r_D4 = Res()
                G4 = sbuf(st, "G4", [128, ntile * 4], F32); r_G4 = Res()
                IDXW = sbuf(st, "IDXW", [128, nblk], I32); r_IDXW = Res()
                BEI = sbuf(st, "BEI", [128, nblk], I32)
                H2 = sbuf(sH2, "H2", [128, ntile * D], BF16); r_H2 = Res()
                with contextlib.ExitStack() as s1:
                    vecs = sorted(set((NB if i < 2 else b) for b, i in tok_tiles))
                    tiles = norm_mod_tiles(s1, l, 1, vecs)
                    small, r_small = new_small(s1, "n2small")
                    junk = sbuf(s1, "n2junk", [128, D], F32); r_junk = Res()
                    xt = [sbuf(s1, "n2x%d" % i, [128, D], F32) for i in range(2)]
                    r_xt = [Res(), Res()]
                    ht = [sbuf(s1, "n2h%d" % i, [128, D], F32) for i in range(2)]
                    r_ht = [Res(), Res()]
                    hT32 = [sbuf(s1, "n2t%d" % i, [128, 8, 128], F32) for i in range(2)]
                    r_hT32 = [Res(), Res()]
                    ptp = [psum(s1, "n2p%d" % i, [128, 4, 128], F32) for i in range(2)]
                    r_ptp = [Res(), Res()]
                    plg = [psum(s1, "n2l%d" % i, [128, NE], F32) for i in range(2)]
                    r_plg = [Res(), Res()]
                    wr = sbuf(s1, "wr", [128, 8, NE], F32); r_wr = Res()
                    S.dma("sp", lambda e: e.dma_start(out=wr[:], in_=router_w[l].rearrange("(c p) n -> p c n", p=128)), writes=[r_wr])
                    rb, r_rb = load_bcast(s1, "rb", router_b[l, :], [], n=NE)
                    for n_i, (b, i) in enumerate(tok_tiles):
                        k = n_i % 2
                        G, rG, SH, rSH = tiles[NB if i < 2 else b]
                        S.dma("sp", lambda e, b=b, i=i, k=k: e.dma_start(out=xt[k][:], in_=XR[b, i * 128:(i + 1) * 128, :]),
                              reads=[R_XR], writes=[r_xt[k]])
                        rms_mod(s1, xt[k], r_xt[k], G, rG, SH, rSH, ht[k], r_ht[k], small, r_small, junk, r_junk)
                        S.op("act", lambda e, k=k, n_i=n_i: e.activation(out=H2[:, n_i * D:(n_i + 1) * D], in_=ht[k][:], func=AF.Copy), reads=[r_ht[k]], writes=[r_H2])
                        for hf in range(2):
                            for c4 in range(4):
                                c = hf * 4 + c4
                                S.op("pe", lambda e, k=k, hf=hf, c4=c4, c=c: e.transpose(
                                    out=ptp[hf][:, c4, :], in_=ht[k][:, c * 128:(c + 1) * 128], identity=ident[:]),
                                    reads=[r_ht[k], R_c], writes=[r_ptp[hf]], signal=(c4 == 3))
                            if hf == 0:
                                S.op("act", lambda e, k=k, hf=hf: e.activation(out=hT32[k][:, 0:4, :], in_=ptp[0][:], func=AF.Copy),
                                     reads=[r_ptp[0]], writes=[r_hT32[k]])
                            else:
                                S.op("dve", lambda e, k=k: e.tensor_copy(out=hT32[k][:, 4:8, :], in_=ptp[1][:]),
                                     reads=[r_ptp[1]], writes=[r_hT32[k]])
                        for c in range(8):
                            S.op("pe", lambda e, k=k, c=c: e.matmul(plg[k][:], lhsT=hT32[k][:, c, :], rhs=wr[:, c, :], start=(c == 0), stop=(c == 7)),
                                 reads=[r_hT32[k], r_wr], writes=[r_plg[k]], signal=(c == 7))
                        S.op("dve", lambda e, k=k, n_i=n_i: e.tensor_tensor(out=LG[:, n_i, :], in0=plg[k][:], in1=rb[:], op=ALU.add),
                             reads=[r_plg[k], r_rb], writes=[r_LG])
                    S.barrier()
                    S.emit()
                with contextlib.ExitStack() as s1:
                    MX = sbuf(s1, "MX", [128, ntile, 8], F32)
                    MASK = sbuf(s1, "MASK", [128, ntile, NE], F32)
                    MASKb = sbuf(s1, "MASKb", [128, ntile, NE], BF16)
                    CUM = sbuf(s1, "CUM", [128, ntile, NE], BF16)
                    GAT = sbuf(s1, "GAT", [128, ntile, NE], F32)
                    TMP = sbuf(s1, "TMP", [128, ntile, NE], F32)
                    KEY = sbuf(s1, "KEY", [128, ntile, NE], F32)
                    K8 = sbuf(s1, "K8", [128, ntile, 8], F32)
                    DEN = sbuf(s1, "DEN", [128, ntile], F32)
                    TRI = sbuf(s1, "TRI", [128, 128], BF16)
                    CNT = sbuf(s1, "CNT", [128, NE], F32)
                    CNTI = sbuf(s1, "CNTI", [128, NE], I32)
                    PAD = sbuf(s1, "PAD", [128, NE], F32)
                    PEND = sbuf(s1, "PEND", [128, NE], F32)
                    BASE = sbuf(s1, "BASE", [128, NE], F32)
                    ONE32 = sbuf(s1, "ONE32", [128, NE], F32)
                    CMP = sbuf(s1, "CMP", [128, nblk, NE], F32)
                    BEF = sbuf(s1, "BEF", [128, nblk], F32)
                    JB = sbuf(s1, "JB", [128, 80], F32)
                    IOP = sbuf(s1, "IOP", [128, 1], F32)
                    pposb = [psum(s1, "ppos%d" % i, [128, 16, NE], F32) for i in range((ntile + 15) // 16)]
                    pcnt = psum(s1, "pcnt", [128, NE], F32)
                    R = Res("route")
                    V_ = lambda fn, eng="dve": S.op(eng, fn, reads=[R, r_LG], writes=[R, r_D4, r_G4, r_IDXW])
                    S.dma("sp", lambda e: e.dma_start(out=JB[:], in_=blk_start_d[:, :]), writes=[R])
                    S.dma("sp", lambda e: e.dma_start(out=IOP[:], in_=iota_p_d[:, :]), reads=[R], writes=[R])
                    V_(lambda e: e.memset(TRI[:], 1.0))
                    V_(lambda e: e.affine_select(out=TRI[:], in_=TRI[:], pattern=[[1, 128]], compare_op=ALU.is_gt, fill=0.0,
                                                 base=0, channel_multiplier=-1), "pool")
                    V_(lambda e: e.memset(ONE32[:], 1.0))
                    for i in range(ntile):
                        V_(lambda e, i=i: e.max(out=MX[:, i, :], in_=LG[:, i, :]))
                    V_(lambda e: e.tensor_tensor(out=MASK[:], in0=LG[:], in1=MX[:, :, 3:4].to_broadcast([128, ntile, NE]), op=ALU.is_ge))
                    V_(lambda e: e.tensor_tensor(out=TMP[:], in0=LG[:], in1=MX[:, :, 0:1].to_broadcast([128, ntile, NE]), op=ALU.subtract))
                    V_(lambda e: e.activation(out=TMP[:], in_=TMP[:], func=AF.Exp), "act")
                    V_(lambda e: e.tensor_tensor(out=TMP[:], in0=TMP[:], in1=MASK[:], op=ALU.mult))
                    V_(lambda e: e.tensor_reduce(out=DEN[:], in_=TMP[:], axis=AX.X, op=ALU.add))
                    V_(lambda e: e.reciprocal(out=DEN[:], in_=DEN[:]))
                    V_(lambda e: e.tensor_tensor(out=GAT[:], in0=TMP[:], in1=DEN[:].unsqueeze(2).to_broadcast([128, ntile, NE]), op=ALU.mult))
                    V_(lambda e: e.tensor_copy(out=MASKb[:], in_=MASK[:]))
                    V_(lambda e: e.memset(CUM[:, 0, :], 0.0))
                    for i in range(1, ntile):
                        V_(lambda e, i=i: e.tensor_tensor(out=CUM[:, i, :], in0=CUM[:, i - 1, :], in1=MASKb[:, i - 1, :], op=ALU.add))
                    V_(lambda e: e.tensor_tensor(out=TMP[:, 0, :], in0=CUM[:, ntile - 1, :], in1=MASKb[:, ntile - 1, :], op=ALU.add))
                    TOTb = sbuf(s1, "TOTb", [128, NE], BF16)
                    V_(lambda e: e.tensor_copy(out=TOTb[:], in_=TMP[:, 0, :]))
                    for i in range(ntile):
                        V_(lambda e, i=i: e.matmul(pposb[i // 16][:, i % 16, :], lhsT=TRI[:], rhs=MASKb[:, i, :], start=True, stop=False), "pe")
                        V_(lambda e, i=i: e.matmul(pposb[i // 16][:, i % 16, :], lhsT=onesb[:, 0:128], rhs=CUM[:, i, :], start=False, stop=True), "pe")
                    V_(lambda e: e.matmul(pcnt[:], lhsT=onesb[:, 0:128], rhs=TOTb[:], start=True, stop=True), "pe")
                    V_(lambda e: e.tensor_copy(out=CNT[:], in_=pcnt[:]))
                    NM = ntok // BLK + 1
                    CMP2 = sbuf(s1, "CMP2", [128, NE, NM], F32)
                    V_(lambda e: e.tensor_tensor(out=CMP2[:], in0=CNT[:].unsqueeze(2).to_broadcast([128, NE, NM]),
                                                 in1=JB[:, 0:NM].unsqueeze(1).to_broadcast([128, NE, NM]), op=ALU.is_gt))
                    V_(lambda e: e.tensor_reduce(out=PAD[:], in_=CMP2[:], axis=AX.X, op=ALU.add))
                    V_(lambda e: e.tensor_scalar(out=PAD[:], in0=PAD[:], scalar1=float(BLK), scalar2=None, op0=ALU.mult))
                    V_(lambda e: e.tensor_tensor_scan(out=PEND[:], data0=ONE32[:], data1=PAD[:], initial=0.0, op0=ALU.mult, op1=ALU.add))
                    V_(lambda e: e.tensor_tensor(out=BASE[:], in0=PEND[:], in1=PAD[:], op=ALU.subtract))
                    for bk in range((ntile + 15) // 16):
                        n_ = min(16, ntile - bk * 16)
                        V_(lambda e, bk=bk, n_=n_: e.tensor_tensor(out=KEY[:, bk * 16:bk * 16 + n_, :], in0=pposb[bk][:, 0:n_, :],
                                                                  in1=BASE[:].unsqueeze(1).to_broadcast([128, n_, NE]), op=ALU.add))
                    V_(lambda e: e.scalar_tensor_tensor(out=KEY[:], in0=KEY[:], scalar=1.0, in1=MASK[:], op0=ALU.add, op1=ALU.mult))
                    for i in range(ntile):
                        V_(lambda e, i=i: e.max(out=K8[:, i, :], in_=KEY[:, i, :]))
                    V_(lambda e: e.tensor_scalar(out=TMP[:, :, 0:4], in0=K8[:, :, 0:4], scalar1=-1.0, scalar2=0.0, op0=ALU.add, op1=ALU.max))
                    V_(lambda e: e.tensor_scalar(out=TMP[:, :, 0:4], in0=TMP[:, :, 0:4], scalar1=float(nblk * BLK - 1), scalar2=None, op0=ALU.min))
                    V_(lambda e: e.tensor_copy(out=D4[:].rearrange("p (n j) -> p n j", j=4), in_=TMP[:, :, 0:4]))
                    for j in range(4):
                        V_(lambda e, j=j: e.tensor_tensor(out=TMP[:], in0=KEY[:], in1=K8[:, :, j:j + 1].to_broadcast([128, ntile, NE]), op=ALU.is_equal))
                        V_(lambda e: e.tensor_tensor(out=TMP[:], in0=TMP[:], in1=GAT[:], op=ALU.mult))
                        V_(lambda e, j=j: e.tensor_reduce(out=G4[:].rearrange("p (n j) -> p n j", j=4)[:, :, j], in_=TMP[:], axis=AX.X, op=ALU.add))
                    V_(lambda e: e.tensor_tensor(out=CMP[:], in0=PEND[:].unsqueeze(1).to_broadcast([128, nblk, NE]),
                                                 in1=JB[:, 0:nblk].unsqueeze(2).to_broadcast([128, nblk, NE]), op=ALU.is_le))
                    V_(lambda e: e.tensor_reduce(out=BEF[:], in_=CMP[:], axis=AX.X, op=ALU.add))
                    V_(lambda e: e.tensor_scalar(out=BEF[:], in0=BEF[:], scalar1=float(n_exp - 1), scalar2=0.0, op0=ALU.min, op1=ALU.max))
                    V_(lambda e: e.tensor_copy(out=BEI[:], in_=BEF[:]))
                    V_(lambda e: e.tensor_scalar(out=BEF[:], in0=BEF[:], scalar1=128.0, scalar2=IOP[:, 0:1], op0=ALU.mult, op1=ALU.add))
                    V_(lambda e: e.tensor_copy(out=IDXW[:], in_=BEF[:]))
                    if dbg:
                        S.dma("sp", lambda e: e.dma_start(out=dbg_d4[:, 0:ntile * 4], in_=D4[:]), reads=[R, r_D4])
                        S.dma("sp", lambda e: e.dma_start(out=dbg_g4[:, 0:ntile * 4], in_=G4[:]), reads=[R, r_G4])
                        S.dma("sp", lambda e: e.dma_start(out=dbg_iw[:, 0:nblk], in_=IDXW[:]), reads=[R, r_IDXW])
                    for n_i in range(ntile if moe_stop >= 2 else 0):
                        for j in range(4):
                            S.dma("pool", lambda e, n_i=n_i, j=j: e.indirect_dma_start(
                                out=XS[:, :], out_offset=bass.IndirectOffsetOnAxis(ap=D4[:, n_i * 4 + j:n_i * 4 + j + 1], axis=0),
                                in_=H2[:, n_i * D:(n_i + 1) * D], in_offset=None),
                                reads=[R, r_D4, r_H2], writes=[R_XS])
                    S.barrier()
                    S.emit()
                sH2.close()
                if moe_stop < 3:
                    return
                w_aps = (w_gate[l], w_up[l], w_down[l])
                with contextlib.ExitStack() as s1:
                    WG = [sbuf(s1, "WG%d" % i, [128, 8192], BF16) for i in range(2)]
                    WU = [sbuf(s1, "WU%d" % i, [128, 8192], BF16) for i in range(2)]
                    WD = [sbuf(s1, "WD%d" % i, [128, 8192], BF16) for i in range(2)]
                    EB = [sbuf(s1, "EB%d" % i, [2, 3072], BF16) for i in range(2)]
                    r_W = [[Res() for _ in range(4)] for _ in range(2)]
                    xs = [sbuf(s1, "xsb%d" % i, [128, 4, D], BF16) for i in range(2)]
                    r_xs = [Res(), Res()]
                    xeT = [sbuf(s1, "xeT%d" % i, [128, 8, BLK], BF16) for i in range(2)]
                    r_xeT = [Res(), Res()]
                    actT = sbuf(s1, "actT", [128, 8, BLK], BF16); r_actT = Res()
                    gs = [sbuf(s1, "gs%d" % i, [128, BLK], F32) for i in range(2)]
                    sg = [sbuf(s1, "sg%d" % i, [128, BLK], F32) for i in range(2)]
                    us = [sbuf(s1, "us%d" % i, [128, BLK], F32) for i in range(2)]
                    r_gs = [Res(), Res()]; r_sg = [Res(), Res()]; r_us = [Res(), Res()]
                    ysb = [sbuf(s1, "ysb0", [128, 4, D], F32)] * 2
                    r_ysb = [Res()] * 2
                    ptr = [psum(s1, "ptr%d" % i, [128, 8, 128], BF16) for i in range(2)]
                    r_ptr = [Res(), Res()]
                    pgp = [psum(s1, "pgp%d" % i, [128, BLK], F32) for i in range(2)]
                    pup = [psum(s1, "pup%d" % i, [128, BLK], F32) for i in range(2)]
                    r_pgp = [Res(), Res()]; r_pup = [Res(), Res()]
                    pyp = [psum(s1, "pyp%d" % i, [128, 512], F32) for i in range(2)]
                    r_pyp = [Res(), Res()]
                    for jb in range(nblk):
                        k = jb % 2
                        for mi, (wbuf, wap) in enumerate(((WG[k], w_aps[0]), (WU[k], w_aps[1]), (WD[k], w_aps[2]))):
                            S.dma("pool", lambda e, wbuf=wbuf, wap=wap, jb=jb: e.indirect_dma_start(
                                out=wbuf[:, :], out_offset=None, in_=wap[:, :],
                                in_offset=bass.IndirectOffsetOnAxis(ap=IDXW[:, jb:jb + 1], axis=0)), reads=[r_IDXW], writes=[r_W[k][mi]])
                        S.dma("pool", lambda e, k=k, jb=jb: e.indirect_dma_start(
                            out=EB[k][:, :], out_offset=None, in_=exp_b[l][:, :],
                            in_offset=bass.IndirectOffsetOnAxis(ap=BEI[0:2, jb:jb + 1], axis=0)), reads=[r_IDXW], writes=[r_W[k][3]])
                        S.dma("sp", lambda e, k=k, jb=jb: e.dma_start(
                            out=xs[k][:], in_=XS[jb * BLK:(jb + 1) * BLK, :].rearrange("(s p) d -> p s d", p=128)),
                            reads=[R_XS], writes=[r_xs[k]])
                        for s in range(4):
                            kk = s % 2
                            for c in range(8):
                                S.op("pe", lambda e, k=k, kk=kk, s=s, c=c: e.transpose(out=ptr[kk][:, c, :], in_=xs[k][:, s, c * 128:(c + 1) * 128],
                                                                                 identity=identb[:]),
                                     reads=[r_xs[k], R_c], writes=[r_ptr[kk]], signal=(c == 7))
                            if s % 2 == 0:
                                S.op("act", lambda e, k=k, kk=kk, s=s: e.activation(out=xeT[k][:, :, s * 128:(s + 1) * 128], in_=ptr[kk][:], func=AF.Copy),
                                     reads=[r_ptr[kk]], writes=[r_xeT[k]])
                            else:
                                S.op("dve", lambda e, k=k, kk=kk, s=s: e.tensor_copy(out=xeT[k][:, :, s * 128:(s + 1) * 128], in_=ptr[kk][:]),
                                     reads=[r_ptr[kk]], writes=[r_xeT[k]])
                        for f in range(8):
                            kf = f % 2
                            for (pp, r_pp, wbuf, mi, boff) in ((pgp[kf], r_pgp[kf], WG[k], 0, 0), (pup[kf], r_pup[kf], WU[k], 1, 1024)):
                                for c in range(8):
                                    S.op("pe", lambda e, pp=pp, wbuf=wbuf, c=c, f=f, k=k: e.matmul(
                                        pp[:], lhsT=wbuf[:, c * 1024 + f * 128:c * 1024 + (f + 1) * 128], rhs=xeT[k][:, c, :], start=(c == 0), stop=False),
                                        reads=[r_W[k][mi], r_xeT[k]], writes=[r_pp], signal=False)
                                S.op("pe", lambda e, pp=pp, k=k, f=f, boff=boff: e.matmul(
                                    pp[:], lhsT=EB[k][0:2, boff + f * 128:boff + (f + 1) * 128], rhs=halfb[0:2, 0:BLK], start=False, stop=True),
                                    reads=[r_W[k][3], R_c], writes=[r_pp])
                            S.op("dve", lambda e, kf=kf: e.tensor_scalar(out=gs[kf][:], in0=pgp[kf][:], scalar1=7.0, scalar2=None, op0=ALU.min),
                                 reads=[r_pgp[kf]], writes=[r_gs[kf]])
                            S.op("act", lambda e, kf=kf: e.activation(out=sg[kf][:], in_=gs[kf][:], func=AF.Sigmoid, scale=1.702),
                                 reads=[r_gs[kf]], writes=[r_sg[kf]])
                            S.op("dve", lambda e, kf=kf: e.tensor_scalar(out=us[kf][:], in0=pup[kf][:], scalar1=7.0, scalar2=-7.0, op0=ALU.min, op1=ALU.max),
                                 reads=[r_pup[kf]], writes=[r_us[kf]])
                            S.op("dve", lambda e, kf=kf: e.scalar_tensor_tensor(out=us[kf][:], in0=us[kf][:], scalar=1.0, in1=gs[kf][:],
                                                                                 op0=ALU.add, op1=ALU.mult),
                                 reads=[r_us[kf], r_gs[kf]], writes=[r_us[kf]])
                            S.op("dve", lambda e, kf=kf, f=f: e.tensor_tensor(out=actT[:, f, :], in0=us[kf][:], in1=sg[kf][:], op=ALU.mult),
                                 reads=[r_us[kf], r_sg[kf]], writes=[r_actT])
                        for s in range(4):
                            for hh in range(2):
                                kp = (s * 2 + hh) % 2
                                for f in range(8):
                                    S.op("pe", lambda e, kp=kp, f=f, s=s, hh=hh, k=k: e.matmul(
                                        pyp[kp][:], lhsT=actT[:, f, s * 128:(s + 1) * 128],
                                        rhs=WD[k][:, f * 1024 + hh * 512:f * 1024 + (hh + 1) * 512], start=(f == 0), stop=False),
                                        reads=[r_W[k][2], r_actT], writes=[r_pyp[kp]], signal=False)
                                S.op("pe", lambda e, kp=kp, hh=hh, k=k: e.matmul(
                                    pyp[kp][:], lhsT=halfb[0:2, 0:128], rhs=EB[k][0:2, 2048 + hh * 512:2048 + (hh + 1) * 512], start=False, stop=True),
                                    reads=[r_W[k][3], R_c], writes=[r_pyp[kp]])
                                S.op("act", lambda e, kp=kp, k=k, s=s, hh=hh: e.activation(out=ysb[k][:, s, hh * 512:(hh + 1) * 512], in_=pyp[kp][:], func=AF.Copy),
                                     reads=[r_pyp[kp]], writes=[r_ysb[k]])
                        S.dma("sp", lambda e, k=k, jb=jb: e.dma_start(
                            out=YS[jb * BLK:(jb + 1) * BLK, :].rearrange("(s p) d -> p s d", p=128), in_=ysb[k][:]),
                            reads=[r_ysb[k]], writes=[R_YS])
                    S.barrier()
                    S.emit()
                if moe_stop < 4:
                    return
                with contextlib.ExitStack() as s1:
                    g2 = {}
                    for v in sorted(set((NB if i < 2 else b) for b, i in tok_tiles)):
                        g2[v] = load_bcast(s1, "g2_%d" % v, MOD[l, v, 5 * D:6 * D], [R_MOD])
                    yg = [[sbuf(s1, "yg%d_%d" % (i, j), [128, D], F32) for j in range(4)] for i in range(2)]
                    r_yg = [[Res() for _ in range(4)] for _ in range(2)]
                    xt = [sbuf(s1, "cbx%d" % i, [128, D], F32) for i in range(2)]
                    r_xt = [Res(), Res()]
                    for n_i, (b, i) in enumerate(tok_tiles):
                        k = n_i % 2
                        gt, rgt = g2[NB if i < 2 else b]
                        S.dma("sp", lambda e, b=b, i=i, k=k: e.dma_start(out=xt[k][:], in_=XR[b, i * 128:(i + 1) * 128, :]),
                              reads=[R_XR], writes=[r_xt[k]])
                        for j in range(4):
                            S.dma("pool", lambda e, k=k, j=j, n_i=n_i: e.indirect_dma_start(
                                out=yg[k][j][:, :], out_offset=None, in_=YS[:, :],
                                in_offset=bass.IndirectOffsetOnAxis(ap=D4[:, n_i * 4 + j:n_i * 4 + j + 1], axis=0)), reads=[R_YS, r_D4], writes=[r_yg[k][j]])
                        S.op("dve", lambda e, k=k, n_i=n_i: e.tensor_scalar(out=yg[k][0][:], in0=yg[k][0][:], scalar1=G4[:, n_i * 4:n_i * 4 + 1], scalar2=None, op0=ALU.mult),
                             reads=[r_yg[k][0], r_G4], writes=[r_yg[k][0]])
                        for j in range(1, 4):
                            S.op("dve", lambda e, k=k, j=j, n_i=n_i: e.scalar_tensor_tensor(
                                out=yg[k][0][:], in0=yg[k][j][:], scalar=G4[:, n_i * 4 + j:n_i * 4 + j + 1], in1=yg[k][0][:], op0=ALU.mult, op1=ALU.add),
                                reads=[r_yg[k][j], r_yg[k][0], r_G4], writes=[r_yg[k][0]])
                        S.op("dve", lambda e, k=k, gt=gt: e.tensor_tensor(out=yg[k][0][:], in0=yg[k][0][:], in1=gt[:], op=ALU.mult),
                             reads=[r_yg[k][0], rgt], writes=[r_yg[k][0]])
                        S.op("pool", lambda e, k=k: e.tensor_tensor(out=xt[k][:], in0=xt[k][:], in1=yg[k][0][:], op=ALU.add),
                             reads=[r_yg[k][0], r_xt[k]], writes=[r_xt[k]])
                        if final:
                            S.dma("sp", lambda e, b=b, i=i, k=k: e.dma_start(out=out[b, (i - 2) * 128:(i - 1) * 128, :], in_=xt[k][:]),
                                  reads=[r_xt[k]], writes=[R_OUT])
                        else:
                            S.dma("sp", lambda e, b=b, i=i, k=k: e.dma_start(out=XR[b, i * 128:(i + 1) * 128, :], in_=xt[k][:]),
                                  reads=[r_xt[k]], writes=[R_XR])
                    S.barrier()
                    S.emit()

        if 0 in layers:
            for b in range(NB):
                layer0_mixer(b)
            if do_moe:
                moe_layer(0, [(b, i) for b in range(NB) for i in range(NTI)], final=False)
        if 1 in layers:
            for b in range(NB):
                layer1_mixer(b)
            if do_moe:
                moe_layer(1, [(b, i) for b in range(NB) for i in range(2, NTI)], final=True)
        if dbg:
            with contextlib.ExitStack() as st:
                t = sbuf(st, "dbgt", [128, D], F32); r_t = Res()
                for b in range(NB):
                    for i in range(2, NTI):
                        S.dma("sp", lambda e, b=b, i=i: e.dma_start(out=t[:], in_=XR[b, i * 128:(i + 1) * 128, :]), reads=[R_XR], writes=[r_t])
                        S.dma("sp", lambda e, b=b, i=i: e.dma_start(out=out[b, (i - 2) * 128:(i - 1) * 128, :], in_=t[:]), reads=[r_t], writes=[R_OUT])
                S.barrier()
                S.emit()
        S.barrier()
        S.emit()
        print("program instructions (incl waits):", S.n_ins, "sem counts:", S.cnt, "max dma sem:", max(S.dma_cnt.values()))
    return nc


def _core_inputs(inp, sh, c, NB=2):
    b0 = c * NB
    d = dict(sh)
    d["xin"] = np.ascontiguousarray(np.concatenate([inp["ctx"][b0:b0 + NB], inp["x"][b0:b0 + NB]], axis=1), np.float32)
    d["cvec"] = np.ascontiguousarray(np.concatenate([inp["c"][b0:b0 + NB], inp["c_ctx"][None]], 0), np.float32)
    return d


def kernel(**inputs):
    inp = {k: np.asarray(v) for k, v in inputs.items()}
    sh = _prep_shared(inp)
    nc = build_program(NB=2)
    in_maps = [_core_inputs(inp, sh, c) for c in range(8)]
    res = run_bass_kernel_spmd(nc, in_maps, core_ids=list(range(8)))
    return np.concatenate([r["out"] for r in res.results], axis=0).astype(np.float32)
```

```python
import contextlib
import numpy as np
import concourse.bass as bass
import concourse.mybir as mybir
from concourse.bass_utils import run_bass_kernel_spmd

F32 = mybir.dt.float32
BF16 = mybir.dt.bfloat16
I32 = mybir.dt.int32
AF = mybir.ActivationFunctionType
ALU = mybir.AluOpType
AX = mybir.AxisListType

ENGS = ("pe", "dve", "act", "pool", "sp")

D = 1024
T = 2048
LC = 256
NT = T + LC
NTI = NT // 128
NE = 32
BLK = 512
EPS = 1e-6
NEG = -30000.0


class Res:
    __slots__ = ("name", "w", "r")

    def __init__(self, name=""):
        self.name = name
        self.w = None
        self.r = []


class Sched:
    def __init__(self, nc, stack, n_dma_sems=12):
        self.nc = nc
        self.streams = {e: [] for e in ENGS}
        self.cnt = {e: 0 for e in ENGS}
        self.sems = {}
        for e in ENGS:
            self.sems[e] = stack.enter_context(nc.semaphore("s_" + e))
        self.dma_sems = {}
        self.dma_cnt = {}
        self.dma_rr = {}
        for q in ("sp", "pool"):
            self.dma_sems[q] = []
            for i in range(n_dma_sems):
                k = "d_%s_%d" % (q, i)
                self.sems[k] = stack.enter_context(nc.semaphore(k))
                self.dma_sems[q].append(k)
                self.dma_cnt[k] = 0
            self.dma_rr[q] = 0
        self.seen = {e: {} for e in ENGS}
        self.n_ins = 0

    def _need(self, eng, deps):
        best = {}
        for d in deps:
            if d is None:
                continue
            k, v = d
            if k == eng and eng == "pe":
                continue
            if self.seen[eng].get(k, 0) >= v:
                continue
            if best.get(k, 0) < v:
                best[k] = v
        for k, v in best.items():
            self.seen[eng][k] = v
        return list(best.items())

    @staticmethod
    def _deps(reads, writes):
        deps = []
        for r in reads:
            deps.append(r.w)
        for w in writes:
            deps.append(w.w)
            deps.extend(w.r)
        return deps

    @staticmethod
    def _commit(reads, writes, tok):
        for r in reads:
            r.r.append(tok)
            if len(r.r) > 16:
                m = {}
                for k, v in r.r:
                    if m.get(k, 0) < v:
                        m[k] = v
                r.r = list(m.items())
        for w in writes:
            w.w = tok
            w.r = []

    def op(self, eng, fn, reads=(), writes=(), signal=True):
        waits = self._need(eng, self._deps(reads, writes))
        if signal:
            self.cnt[eng] += 1
            tok = (eng, self.cnt[eng])
        else:
            tok = (eng, self.cnt[eng] + 1)
        self.streams[eng].append((waits, fn, signal, None))
        self._commit(reads, writes, tok)
        self.n_ins += 1 + len(waits)
        return tok

    def dma(self, q, fn, reads=(), writes=()):
        lst = self.dma_sems[q]
        k = lst[self.dma_rr[q] % len(lst)]
        self.dma_rr[q] += 1
        deps = self._deps(reads, writes)
        deps.append((k, self.dma_cnt[k]))
        waits = self._need(q, deps)
        self.dma_cnt[k] += 16
        tok = (k, self.dma_cnt[k])
        self.streams[q].append((waits, fn, False, k))
        self._commit(reads, writes, tok)
        self.n_ins += 1 + len(waits)
        return tok

    def barrier(self):
        final = []
        for e in ENGS:
            if self.cnt[e]:
                final.append((e, self.cnt[e]))
        for k, v in self.dma_cnt.items():
            if v:
                final.append((k, v))
        for e in ENGS:
            waits = self._need(e, [f for f in final if not (f[0] == e and e == "pe")])
            if waits:
                self.streams[e].append((waits, None, False, None))

    def emit(self):
        nc = self.nc
        sems = self.sems
        streams = self.streams

        def run(engname, engobj):
            for waits, fn, signal, dsem in streams[engname]:
                fold = None
                if fn is not None and dsem is None and waits:
                    fold = waits[-1]
                    waits = waits[:-1]
                for k, v in waits:
                    engobj.wait_ge(sems[k], v)
                if fn is None:
                    continue
                ins = fn(engobj)
                if fold is not None:
                    ins._wait_ge(sems[fold[0]], fold[1])
                if dsem is not None:
                    ins.then_inc(sems[dsem], 16)
                elif signal:
                    ins.then_inc(sems[engname], 1)

        with nc.Block() as block:
            @block.tensor
            def _(e):
                run("pe", e)

            @block.vector
            def _(e):
                run("dve", e)

            @block.scalar
            def _(e):
                run("act", e)

            @block.gpsimd
            def _(e):
                run("pool", e)

            @block.sync
            def _(e):
                run("sp", e)
        self.streams = {e: [] for e in ENGS}


def _na_bias_T(rpb):
    H = rpb.shape[0]
    out = np.full((H, 5, 640, 128), NEG, np.float32)
    pair_of_type = [0, 1, 5, 14, 15]
    for ty, j in enumerate(pair_of_type):
        ws = int(np.clip(2 * j - 4, 0, 23))
        a = ws // 2
        krow0 = 2 * a
        for rr in range(2):
            r = 2 * j + rr
            r0 = int(np.clip(r - 4, 0, 24))
            for i in range(10):
                kr = krow0 + i
                if not (r0 <= kr < r0 + 8):
                    continue
                dr = kr - r + 7
                c = np.arange(64)
                c0 = np.clip(c - 8, 0, 48)
                for cq in range(64):
                    kc = np.arange(c0[cq], c0[cq] + 16)
                    dc = kc - cq + 15
                    out[:, ty, i * 64 + kc, rr * 64 + cq] = rpb[:, dr, dc]
    return np.ascontiguousarray(out.reshape(H, 5, 5, 128, 128))


def _pcol(v):
    v = np.asarray(v, np.float32)
    return np.ascontiguousarray(v.reshape(-1, 128).T)


_GATE_INPUTS = ("od_fwd_wa", "od_fwd_ba", "od_fwd_wx", "od_fwd_bx", "od_fwd_lam",
                "od_bwd_wa", "od_bwd_ba", "od_bwd_wx", "od_bwd_bx", "od_bwd_lam")


def _prep_shared(inp):
    sh = {}
    for nm in _GATE_INPUTS:
        assert nm in inp
    sh["ada_w"] = np.ascontiguousarray(inp["ada_w"], np.float32)
    sh["ada_b"] = np.ascontiguousarray(inp["ada_b"], np.float32)
    sh["norm_g"] = np.ascontiguousarray(np.stack([inp["norm1_g"], inp["norm2_g"]], 1), np.float32)
    sh["ev_w_in"] = np.ascontiguousarray(inp["ev_w_in"][0], np.float32)
    sh["ev_w_out"] = np.ascontiguousarray(inp["ev_w_out"][0], np.float32)
    sh["biasT"] = _na_bias_T(np.asarray(inp["ev_rpb"][0], np.float32))
    sh["od_w_in"] = np.ascontiguousarray(inp["od_w_in"][0], np.float32)
    sh["od_w_out"] = np.ascontiguousarray(inp["od_w_out"][0], np.float32)
    gates = []
    for dr in ("fwd", "bwd"):
        for nm in ("wa", "wx"):
            gates.append(np.asarray(inp["od_%s_%s" % (dr, nm)][0], np.float32))
    sh["od_gw"] = np.ascontiguousarray(np.stack(gates, 0))
    cols = []
    qg = np.tile(np.asarray(inp["ev_q_gain"][0], np.float32), 2)
    kg = np.tile(np.asarray(inp["ev_k_gain"][0], np.float32), 2)
    cols.append(qg[:, None]); cols.append(kg[:, None])
    for j in range(3):
        cols.append(_pcol(inp["ev_conv_w"][0, j]))
    cols.append(_pcol(inp["ev_conv_b"][0]))
    for j in range(4):
        cols.append(_pcol(inp["od_conv_w"][0, j]))
    cols.append(_pcol(inp["od_conv_b"][0]))
    for dr in ("fwd", "bwd"):
        for nm in ("ba", "bx", "lam"):
            cols.append(_pcol(inp["od_%s_%s" % (dr, nm)][0]))
    sh["pcols"] = np.ascontiguousarray(np.concatenate(cols, 1), np.float32)
    sh["router_w"] = np.ascontiguousarray(inp["router_w"], np.float32)
    sh["router_b"] = np.ascontiguousarray(inp["router_b"], np.float32)
    for nm in ("gate", "up", "down"):
        w = np.asarray(inp["exp_w_" + nm], np.float32)
        w = w.reshape(2, NE, 8, 128, 1024).transpose(0, 1, 3, 2, 4).reshape(2, NE * 128, 8 * 1024)
        for l in range(2):
            sh["w_%s%d" % (nm, l)] = np.ascontiguousarray(w[l])
    eb = np.concatenate([inp["exp_b_gate"], inp["exp_b_up"], inp["exp_b_down"]], -1).astype(np.float32)
    for l in range(2):
        sh["exp_b%d" % l] = np.ascontiguousarray(eb[l])
        bgc = np.asarray(inp["exp_b_gate"][l], np.float32).reshape(NE, 8, 128).transpose(0, 2, 1)
        buc = np.asarray(inp["exp_b_up"][l], np.float32).reshape(NE, 8, 128).transpose(0, 2, 1)
        sh["exp_bc%d" % l] = np.ascontiguousarray(np.concatenate([bgc, buc], -1).reshape(NE * 128, 16))
    sh["iota_p"] = np.arange(128, dtype=np.float32)[:, None].copy()
    sh["blk_start"] = np.tile((np.arange(80, dtype=np.float32) * BLK)[None], (128, 1)).copy()
    return sh


PC_QG, PC_KG, PC_ECW, PC_ECB, PC_OCW, PC_OCB, PC_G = 0, 1, 2, 14, 18, 50, 58


def build_program(NB=2, layers=(0, 1), do_moe=True, dbg=False, n_exp=NE, moe_stop=4):
    nc = bass.Bass("TRN2", target_bir_lowering=False)
    NTOK0 = NB * NT
    dt = nc.dram_tensor

    def din(name, shape, dtp=F32):
        return dt(name, list(shape), dtp, kind="ExternalInput").ap()

    xin = din("xin", [NB, NT, D])
    cvec = din("cvec", [NB + 1, D])
    ada_w = din("ada_w", [2, D, 6 * D])
    ada_b = din("ada_b", [2, 6 * D])
    norm_g = din("norm_g", [2, 2, D])
    ev_w_in = din("ev_w_in", [D, 3072])
    ev_w_out = din("ev_w_out", [D, D])
    biasT = din("biasT", [8, 5, 5, 128, 128])
    od_w_in = din("od_w_in", [D, 2048])
    od_w_out = din("od_w_out", [D, D])
    od_gw = din("od_gw", [4, 4, 256, 256])
    pcols_d = din("pcols", [128, 106])
    router_w = din("router_w", [2, D, NE])
    router_b = din("router_b", [2, NE])
    w_gate = [din("w_gate%d" % l, [n_exp * 128, 8192]) for l in range(2)]
    w_up = [din("w_up%d" % l, [n_exp * 128, 8192]) for l in range(2)]
    w_down = [din("w_down%d" % l, [n_exp * 128, 8192]) for l in range(2)]
    exp_b = [din("exp_b%d" % l, [n_exp, 3072]) for l in range(2)]
    exp_bc = [din("exp_bc%d" % l, [n_exp * 128, 16]) for l in range(2)]
    iota_p_d = din("iota_p", [128, 1])
    blk_start_d = din("blk_start", [128, 80])
    out = dt("out", [NB, T, D], F32, kind="ExternalOutput").ap()
    if dbg:
        dbg_d4 = dt("dbg_d4", [128, NB * NTI * 4], I32, kind="ExternalOutput").ap()
        dbg_g4 = dt("dbg_g4", [128, NB * NTI * 4], F32, kind="ExternalOutput").ap()
        dbg_iw = dt("dbg_iw", [128, 80], I32, kind="ExternalOutput").ap()

    NBLK0 = NTOK0 * 4 // BLK + n_exp
    XR = dt("XR", [NB, NT, D], F32).ap()
    MOD = dt("MODs", [2, NB + 1, 6 * D], F32).ap()
    XS = dt("XS", [NBLK0 * BLK, D], BF16).ap()
    YS = dt("YS", [NBLK0 * BLK, D], F32).ap()
    R_MOD, R_XS, R_YS, R_OUT = Res("MOD"), Res("XS"), Res("YS"), Res("OUT")
    R_XRT = {(b_, i_): Res() for b_ in range(NB) for i_ in range(NTI)}
    R_XIN = {(b_, i_): Res() for b_ in range(NB) for i_ in range(NTI)}

    with contextlib.ExitStack() as g:
        S = Sched(nc, g)

        uid = [0]

        def sbuf(st, name, shape, dtp):
            uid[0] += 1
            return st.enter_context(nc.sbuf_tensor("%s_%d" % (name, uid[0]), list(shape), dtp))

        def psum(st, name, shape, dtp=F32):
            uid[0] += 1
            return st.enter_context(nc.psum_tensor("%s_%d" % (name, uid[0]), list(shape), dtp))

        ident = sbuf(g, "ident", [128, 128], F32)
        identb = sbuf(g, "identb", [128, 128], BF16)
        onesb = sbuf(g, "onesb", [128, 512], BF16)
        halfb = sbuf(g, "halfb", [2, 512], BF16)
        pcols = sbuf(g, "pcols_sb", [128, 106], F32)
        R_c = Res("const")
        S.op("dve", lambda e: e.memset(ident[:], 0.0), writes=[R_c])
        S.op("pool", lambda e: e.affine_select(out=ident[:], in_=ident[:], pattern=[[-1, 128]], compare_op=ALU.not_equal,
                                               fill=1.0, base=0, channel_multiplier=1), reads=[R_c], writes=[R_c])
        S.op("dve", lambda e: e.tensor_copy(out=identb[:], in_=ident[:]), reads=[R_c], writes=[R_c])
        S.op("dve", lambda e: e.memset(onesb[:], 1.0), writes=[R_c])
        S.op("dve", lambda e: e.memset(halfb[:], 0.5), writes=[R_c])
        S.dma("sp", lambda e: e.dma_start(out=pcols[:], in_=pcols_d[:, :]), writes=[R_c])
        S.barrier()
        S.emit()

        NV = NB + 1
        with contextlib.ExitStack() as st:
            cs = sbuf(st, "cs", [NV, D], F32)
            sT = sbuf(st, "sT", [128, 8, NV], F32)
            aw = [sbuf(st, "aw%d" % i, [128, 8, 512], F32) for i in range(2)]
            R_aw = [Res(), Res()]
            ab = sbuf(st, "ab", [NV, 6 * D], F32)
            msb = sbuf(st, "msb", [NV, 6 * D], F32)
            pT = psum(st, "pT", [128, 8, NV], F32)
            pm = [psum(st, "pm%d" % i, [NV, 512], F32) for i in range(2)]
            R_pm = [Res(), Res()]
            R_cs, R_sT, R_ab, R_msb, R_pT = Res(), Res(), Res(), Res(), Res()
            S.dma("sp", lambda e: e.dma_start(out=cs[:], in_=cvec[:, :]), writes=[R_cs])
            S.op("act", lambda e: e.activation(out=cs[:], in_=cs[:], func=AF.Silu), reads=[R_cs], writes=[R_cs])
            for c in range(8):
                S.op("pe", lambda e, c=c: e.transpose(out=pT[:, c, :], in_=cs[:, c * 128:(c + 1) * 128], identity=ident[0:NV, 0:NV]),
                     reads=[R_cs, R_c], writes=[R_pT], signal=(c == 7))
            S.op("dve", lambda e: e.tensor_copy(out=sT[:], in_=pT[:]), reads=[R_pT], writes=[R_sT])
            for l in range(2):
                for v in range(NV):
                    S.dma("sp", lambda e, l=l, v=v: e.dma_start(out=ab[v:v + 1, :], in_=ada_b[l:l + 1, :]), writes=[R_ab])
                for j in range(12):
                    k = j % 2
                    S.dma("sp", lambda e, l=l, j=j, k=k: e.dma_start(
                        out=aw[k][:], in_=ada_w[l, :, j * 512:(j + 1) * 512].rearrange("(c p) n -> p c n", p=128)),
                        writes=[R_aw[k]])
                    for c in range(8):
                        S.op("pe", lambda e, c=c, k=k: e.matmul(pm[k][:], lhsT=sT[:, c, :], rhs=aw[k][:, c, :],
                                                                start=(c == 0), stop=(c == 7)),
                             reads=[R_sT, R_aw[k]], writes=[R_pm[k]], signal=(c == 7))
                    S.op("dve", lambda e, j=j, k=k: e.tensor_tensor(out=msb[:, j * 512:(j + 1) * 512], in0=pm[k][:],
                                                                     in1=ab[:, j * 512:(j + 1) * 512], op=ALU.add),
                         reads=[R_pm[k], R_ab], writes=[R_msb])
                S.dma("sp", lambda e, l=l: e.dma_start(out=MOD[l, :, :], in_=msb[:]), reads=[R_msb], writes=[R_MOD])
            S.barrier()
            S.emit()

        def load_bcast(st, name, src_ap, reads, n=D):
            t = sbuf(st, name, [128, n], F32)
            r = Res(name)
            S.dma("sp", lambda e: e.dma_start(out=t[:], in_=src_ap.partition_broadcast(128)), reads=reads, writes=[r])
            return t, r

        def norm_mod_tiles(st, l, which, vecs):
            res = {}
            gt, rg = load_bcast(st, "ng%d%d" % (l, which), norm_g[l, which, :], [])
            for v in vecs:
                sc, rsc = load_bcast(st, "sc%d" % v, MOD[l, v, (3 * which + 1) * D:(3 * which + 2) * D], [R_MOD])
                shh, rsh = load_bcast(st, "sh%d" % v, MOD[l, v, (3 * which) * D:(3 * which + 1) * D], [R_MOD])
                S.op("dve", lambda e, sc=sc: e.scalar_tensor_tensor(out=sc[:], in0=sc[:], scalar=1.0, in1=gt[:],
                                                                      op0=ALU.add, op1=ALU.mult),
                     reads=[rsc, rg], writes=[rsc])
                res[v] = (sc, rsc, shh, rsh)
            return res

        def rms_mod(st_tmp, xt, r_xt, G, rG, SH, rSH, ht, r_ht, small, r_small, junk, r_junk):
            S.op("dve", lambda e: e.memset(small[:, 0:1], 0.0), reads=[r_small], writes=[r_small])
            S.op("act", lambda e: e.activation(out=junk[:], in_=xt[:], func=AF.Square, accum_out=small[:, 0:1]),
                 reads=[r_xt, r_small], writes=[r_junk, r_small])
            S.op("act", lambda e: e.activation(out=small[:, 1:2], in_=small[:, 0:1], func=AF.Sqrt, scale=1.0 / D, bias=small[:, 3:4]),
                 reads=[r_small], writes=[r_small])
            S.op("dve", lambda e: e.reciprocal(out=small[:, 2:3], in_=small[:, 1:2]), reads=[r_small], writes=[r_small])
            S.op("dve", lambda e: e.scalar_tensor_tensor(out=ht[:], in0=xt[:], scalar=small[:, 2:3], in1=G[:],
                                                         op0=ALU.mult, op1=ALU.mult),
                 reads=[r_xt, r_small, rG], writes=[r_ht])
            S.op("dve", lambda e: e.tensor_tensor(out=ht[:], in0=ht[:], in1=SH[:], op=ALU.add),
                 reads=[r_ht, rSH], writes=[r_ht])

        def new_small(st, name):
            small = sbuf(st, name, [128, 4], F32)
            r = Res(name)
            S.op("dve", lambda e: e.memset(small[:], 0.0), writes=[r])
            S.op("dve", lambda e: e.memset(small[:, 3:4], EPS), reads=[r], writes=[r])
            return small, r

        def norm_to_hT(st, l, b, src_ap, src_res, hT, r_hT, vec_of_tile):
            with contextlib.ExitStack() as s2:
                vecs = sorted(set(vec_of_tile))
                tiles = norm_mod_tiles(s2, l, 0, vecs)
                small, r_small = zip(*[new_small(s2, "n1small%d" % i) for i in range(2)])
                junk = [sbuf(s2, "n1junk%d" % i, [128, D], F32) for i in range(2)]; r_junk = [Res(), Res()]
                xt = [sbuf(s2, "n1x%d" % i, [128, D], F32) for i in range(2)]
                r_xt = [Res(), Res()]
                ht = [sbuf(s2, "n1h%d" % i, [128, D], F32) for i in range(2)]
                r_ht = [Res(), Res()]
                ptp = [psum(s2, "n1p%d" % i, [128, 4, 128], F32) for i in range(2)]
                r_ptp = [Res(), Res()]
                for i in range(NTI):
                    k = i % 2
                    G, rG, SH, rSH = tiles[vec_of_tile[i]]
                    S.dma("sp", lambda e, i=i, k=k: e.dma_start(out=xt[k][:], in_=src_ap[b, i * 128:(i + 1) * 128, :]),
                          reads=[src_res[(b, i)]], writes=[r_xt[k]])
                    rms_mod(s2, xt[k], r_xt[k], G, rG, SH, rSH, ht[k], r_ht[k], small[k], r_small[k], junk[k], r_junk[k])
                    for hf in range(2):
                        for c4 in range(4):
                            c = hf * 4 + c4
                            S.op("pe", lambda e, k=k, hf=hf, c4=c4, c=c: e.transpose(
                                out=ptp[hf][:, c4, :], in_=ht[k][:, c * 128:(c + 1) * 128], identity=ident[:]),
                                reads=[r_ht[k], R_c], writes=[r_ptp[hf]], signal=(c4 == 3))
                        eng = "act" if hf == 0 else "dve"
                        if eng == "act":
                            S.op("act", lambda e, i=i, hf=hf: e.activation(
                                out=hT[:, hf * 4:(hf + 1) * 4, i * 128:(i + 1) * 128], in_=ptp[hf][:], func=AF.Copy),
                                reads=[r_ptp[hf]], writes=[r_hT])
                        else:
                            S.op("dve", lambda e, i=i, hf=hf: e.tensor_copy(
                                out=hT[:, hf * 4:(hf + 1) * 4, i * 128:(i + 1) * 128], in_=ptp[hf][:]),
                                reads=[r_ptp[hf]], writes=[r_hT])
                S.barrier()
                S.emit()

        def stream_w(st, name, n=2):
            bufs = [sbuf(st, "%s%d" % (name, i), [128, 8, 512], BF16) for i in range(n)]
            return bufs, [Res() for _ in range(n)]

        def load_w(buf, r, w_ap, col0, ncol=512):
            S.dma("pool", lambda e: e.dma_start(out=buf[:, :, 0:ncol],
                                                 in_=w_ap[:, col0:col0 + ncol].rearrange("(c p) n -> p c n", p=128)),
                  writes=[r])

        TOKP = [(i * 512, min(512, NT - i * 512)) for i in range((NT + 511) // 512)]

        def outproj_residual(st, l, b, srcs, w_ap, x_src, x_res, tiles_range, vec_of_tile, x_tok_off=0):
            with contextlib.ExitStack() as s2:
                wo = sbuf(s2, "wo", [128, 8, D], BF16); r_wo = Res()
                for hh in range(2):
                    S.dma("pool", lambda e, hh=hh: e.dma_start(
                        out=wo[:, :, hh * 512:(hh + 1) * 512],
                        in_=w_ap[:, hh * 512:(hh + 1) * 512].rearrange("(c p) n -> p c n", p=128)), writes=[r_wo])
                g1 = {}
                for v in sorted(set(vec_of_tile[i] for i in tiles_range)):
                    g1[v] = load_bcast(s2, "g1_%d" % v, MOD[l, v, 2 * D:3 * D], [R_MOD])
                xt = [sbuf(s2, "opx%d" % i, [128, D], F32) for i in range(2)]
                r_xt = [Res(), Res()]
                py = [psum(s2, "opy%d" % i, [128, 512], F32) for i in range(4)]
                r_py = [Res() for _ in range(4)]
                for n_i, i in enumerate(tiles_range):
                    k = n_i % 2
                    gt, rgt = g1[vec_of_tile[i]]
                    S.dma("sp", lambda e, i=i, k=k: e.dma_start(out=xt[k][:], in_=x_src[b, i * 128:(i + 1) * 128, :]),
                          reads=[x_res[(b, i)]], writes=[r_xt[k]])
                    for hh in range(2):
                        pk = k * 2 + hh
                        for c in range(8):
                            tsr, ch, rs, toff = srcs[c]
                            S.op("pe", lambda e, tsr=tsr, ch=ch, toff=toff, i=i, c=c, hh=hh, pk=pk: e.matmul(
                                py[pk][:], lhsT=tsr[:, ch, i * 128 - toff:(i + 1) * 128 - toff],
                                rhs=wo[:, c, hh * 512:(hh + 1) * 512], start=(c == 0), stop=(c == 7)),
                                reads=[rs, r_wo], writes=[r_py[pk]], signal=(c == 7))
                        S.op("dve", lambda e, hh=hh, pk=pk, gt=gt, k=k: e.tensor_tensor(
                            out=xg[k][:, hh * 512:(hh + 1) * 512],
                            in0=py[pk][:], in1=gt[:, hh * 512:(hh + 1) * 512], op=ALU.mult),
                            reads=[r_py[pk], rgt], writes=[r_xg[k]])
                    S.op("pool", lambda e, k=k: e.tensor_tensor(out=xt[k][:], in0=xt[k][:], in1=xg[k][:], op=ALU.add),
                         reads=[r_xg[k], r_xt[k]], writes=[r_xt[k]])
                    S.dma("sp", lambda e, i=i, k=k: e.dma_start(out=XR[b, i * 128:(i + 1) * 128, :], in_=xt[k][:]),
                          reads=[r_xt[k]], writes=[R_XRT[(b, i)]])
                S.barrier()
                S.emit()

        xg = [sbuf(g, "xg%d" % i, [128, D], F32) for i in range(2)]
        r_xg = [Res(), Res()]

        VEC_OF_TILE = lambda b: [NB, NB] + [b] * 16

        def layer0_mixer(b):
            with contextlib.ExitStack() as st:
                qT = sbuf(st, "qT", [128, 4, NT], BF16); r_qT = Res()
                kT = sbuf(st, "kT", [128, 4, NT], BF16); r_kT = Res()
                V = sbuf(st, "V", [128, NTI, 512], BF16); r_V = Res()
                OB = sbuf(st, "OB", [128, 4, NT], BF16); r_OB = Res()
                with contextlib.ExitStack() as s1:
                    hT = sbuf(s1, "hT", [128, 8, NT], BF16); r_hT = Res()
                    norm_to_hT(s1, 0, b, xin, R_XIN, hT, r_hT, VEC_OF_TILE(b))
                    wb, r_wb = stream_w(s1, "wi")
                    blk1 = sbuf(s1, "blk1", [128, 128], BF16); r_blk = Res()
                    S.op("dve", lambda e: e.memset(blk1[:], 0.0), writes=[r_blk])
                    S.op("dve", lambda e: e.memset(blk1[0:64, 0:64], 1.0 / 64), reads=[r_blk], writes=[r_blk])
                    S.op("dve", lambda e: e.memset(blk1[64:128, 64:128], 1.0 / 64), reads=[r_blk], writes=[r_blk])
                    pj = [psum(s1, "pj%d" % i, [128, 512], F32) for i in range(3)]
                    r_pj = [Res() for _ in range(3)]
                    pq = [psum(s1, "pq%d" % i, [128, 512], F32) for i in range(2)]
                    r_pq = [Res() for _ in range(2)]
                    sq = [sbuf(s1, "sq%d" % i, [128, 512], BF16) for i in range(2)]
                    r_sq = [Res(), Res()]
                    rs_t = [sbuf(s1, "rst%d" % i, [128, 512], F32) for i in range(2)]
                    r_rs = [Res(), Res()]
                    eps64 = sbuf(s1, "eps64", [128, 2], F32); r_e64 = Res()
                    S.op("dve", lambda e: e.memset(eps64[:, 0:1], EPS), writes=[r_e64])
                    S.op("dve", lambda e: e.memset(eps64[:, 1:2], 64.0 * EPS), reads=[r_e64], writes=[r_e64])
                    cnt = [0]

                    def proj_piece(wbuf, r_w, wc, tp):
                        t0, nt_ = TOKP[tp]
                        k = cnt[0] % 3
                        cnt[0] += 1
                        for c in range(8):
                            S.op("pe", lambda e, c=c, k=k: e.matmul(pj[k][:, 0:nt_], lhsT=wbuf[:, c, wc * 128:(wc + 1) * 128],
                                                                   rhs=hT[:, c, t0:t0 + nt_], start=(c == 0), stop=(c == 7)),
                                 reads=[r_w, r_hT], writes=[r_pj[k]], signal=(c == 7))
                        return k, t0, nt_

                    for grp, (dst, r_dst, gcol, sc_, ecol) in enumerate(((qT, r_qT, PC_QG, 64.0, 1), (kT, r_kT, PC_KG, 1.0, 0))):
                        bi = grp % 2
                        load_w(wb[bi], r_wb[bi], ev_w_in, grp * 512)
                        for wc in range(4):
                            for tp in range(len(TOKP)):
                                k, t0, nt_ = proj_piece(wb[bi], r_wb[bi], wc, tp)
                                k2 = cnt[0] % 2
                                S.op("act", lambda e, k=k, k2=k2, nt_=nt_: e.activation(out=sq[k2][:, 0:nt_], in_=pj[k][:, 0:nt_], func=AF.Square),
                                     reads=[r_pj[k]], writes=[r_sq[k2]])
                                S.op("pe", lambda e, k2=k2, nt_=nt_: e.matmul(pq[k2][:, 0:nt_], lhsT=blk1[:], rhs=sq[k2][:, 0:nt_], start=True, stop=True),
                                     reads=[r_blk, r_sq[k2]], writes=[r_pq[k2]])
                                S.op("act", lambda e, k2=k2, nt_=nt_, sc_=sc_, ecol=ecol: e.activation(
                                    out=rs_t[k2][:, 0:nt_], in_=pq[k2][:, 0:nt_], func=AF.Sqrt, scale=sc_, bias=eps64[:, ecol:ecol + 1]),
                                    reads=[r_pq[k2], r_e64], writes=[r_rs[k2]])
                                S.op("dve", lambda e, k2=k2, nt_=nt_: e.reciprocal(out=rs_t[k2][:, 0:nt_], in_=rs_t[k2][:, 0:nt_]),
                                     reads=[r_rs[k2]], writes=[r_rs[k2]])
                                S.op("dve", lambda e, k=k, k2=k2, nt_=nt_, t0=t0, wc=wc, dst=dst, gcol=gcol: e.scalar_tensor_tensor(
                                    out=dst[:, wc, t0:t0 + nt_], in0=pj[k][:, 0:nt_], scalar=pcols[:, gcol:gcol + 1], in1=rs_t[k2][:, 0:nt_],
                                    op0=ALU.mult, op1=ALU.mult), reads=[r_pj[k], r_rs[k2], R_c], writes=[r_dst])
                    load_w(wb[0], r_wb[0], ev_w_in, 1024)
                    for i in range(NTI):
                        k = cnt[0] % 3
                        cnt[0] += 1
                        for c in range(8):
                            S.op("pe", lambda e, c=c, k=k, i=i: e.matmul(pj[k][:], lhsT=hT[:, c, i * 128:(i + 1) * 128], rhs=wb[0][:, c, :],
                                                                        start=(c == 0), stop=(c == 7)),
                                 reads=[r_wb[0], r_hT], writes=[r_pj[k]], signal=(c == 7))
                        S.op("act", lambda e, k=k, i=i: e.activation(out=V[:, i, :], in_=pj[k][:], func=AF.Copy),
                             reads=[r_pj[k]], writes=[r_V])
                    load_w(wb[1], r_wb[1], ev_w_in, 2560)
                    wcg = sbuf(s1, "wcg", [128, 8, 512], BF16); r_wcg = Res()
                    load_w(wcg, r_wcg, ev_w_in, 2048)
                    load_w(wb[0], r_wb[0], ev_w_in, 1536)
                    xs_f = sbuf(s1, "xs_f", [128, NT], F32); r_xs = Res()
                    cx_f = sbuf(s1, "cx_f", [128, NT], F32); r_cx = Res()
                    t_f = sbuf(s1, "t_f", [128, NT], F32); r_t = Res()
                    for f in range(4):
                        for tp in range(len(TOKP)):
                            k, t0, nt_ = proj_piece(wb[1], r_wb[1], f, tp)
                            S.op("act", lambda e, k=k, t0=t0, nt_=nt_: e.activation(out=xs_f[:, t0:t0 + nt_], in_=pj[k][:, 0:nt_], func=AF.Copy),
                                 reads=[r_pj[k]], writes=[r_xs])
                        for tp in range(len(TOKP)):
                            k, t0, nt_ = proj_piece(wcg, r_wcg, f, tp)
                            S.op("dve", lambda e, k=k, t0=t0, nt_=nt_: e.tensor_tensor(out=cx_f[:, t0:t0 + nt_], in0=pj[k][:, 0:nt_],
                                                                                      in1=xs_f[:, t0:t0 + nt_], op=ALU.mult),
                                 reads=[r_pj[k], r_xs], writes=[r_cx])
                        w0 = pcols[:, PC_ECW + 0 * 4 + f:PC_ECW + 0 * 4 + f + 1]
                        w1 = pcols[:, PC_ECW + 1 * 4 + f:PC_ECW + 1 * 4 + f + 1]
                        w2 = pcols[:, PC_ECW + 2 * 4 + f:PC_ECW + 2 * 4 + f + 1]
                        bb = pcols[:, PC_ECB + f:PC_ECB + f + 1]
                        S.op("dve", lambda e, w1=w1, bb=bb: e.tensor_scalar(out=t_f[:], in0=cx_f[:], scalar1=w1, scalar2=bb, op0=ALU.mult, op1=ALU.add),
                             reads=[r_cx, R_c], writes=[r_t])
                        for (s0, sn) in ((0, LC), (LC, T)):
                            S.op("dve", lambda e, s0=s0, sn=sn, w0=w0: e.scalar_tensor_tensor(
                                out=t_f[:, s0 + 1:s0 + sn], in0=cx_f[:, s0:s0 + sn - 1], scalar=w0, in1=t_f[:, s0 + 1:s0 + sn],
                                op0=ALU.mult, op1=ALU.add), reads=[r_cx, r_t, R_c], writes=[r_t])
                            S.op("dve", lambda e, s0=s0, sn=sn, w2=w2: e.scalar_tensor_tensor(
                                out=t_f[:, s0:s0 + sn - 1], in0=cx_f[:, s0 + 1:s0 + sn], scalar=w2, in1=t_f[:, s0:s0 + sn - 1],
                                op0=ALU.mult, op1=ALU.add), reads=[r_cx, r_t, R_c], writes=[r_t])
                        for tp in range(len(TOKP)):
                            k, t0, nt_ = proj_piece(wb[0], r_wb[0], f, tp)
                            S.op("dve", lambda e, k=k, t0=t0, nt_=nt_, f=f: e.tensor_tensor(out=OB[:, f, t0:t0 + nt_], in0=pj[k][:, 0:nt_],
                                                                                           in1=t_f[:, t0:t0 + nt_], op=ALU.mult),
                                 reads=[r_pj[k], r_t], writes=[r_OB])
                    S.barrier()
                    S.emit()
                OA = sbuf(st, "OA", [128, 4, NT], BF16); r_OA = Res()
                with contextlib.ExitStack() as s1:
                    bias = [sbuf(s1, "bias%d" % i, [128, 5, 5, 128], BF16) for i in range(2)]
                    r_bias = [Res(), Res()]
                    sta = [psum(s1, "sta%d" % i, [128, 4, 128], F32) for i in range(2)]
                    stb = [psum(s1, "stb%d" % i, [128, 4, 128], F32) for i in range(2)]
                    r_sta = [Res(), Res()]; r_stb = [Res(), Res()]
                    po = [psum(s1, "po%d" % i, [64, 2, 128], F32) for i in range(2)]
                    r_po = [Res(), Res()]
                    PT = [sbuf(s1, "PT%d" % i, [128, 7, 128], BF16) for i in range(2)]
                    r_PT = [Res(), Res()]
                    rc = [sbuf(s1, "rc%d" % i, [64, 128], F32) for i in range(2)]
                    r_rc = [Res(), Res()]
                    it = 0
                    for h in range(8):
                        hp, hc = h % 2, h // 2
                        bsel = h % 2
                        S.dma("pool", lambda e, h=h, bsel=bsel: e.dma_start(out=bias[bsel][:], in_=biasT[h].rearrange("t c p q -> p t c q")),
                              writes=[r_bias[bsel]])
                        jobs = [("ctx", 0), ("ctx", 1)] + [("lat", j) for j in range(16)]
                        for kind, j in jobs:
                            k = it % 2
                            it += 1
                            if kind == "ctx":
                                qtile = j
                                ktiles = [0, 1]
                                ty = None
                            else:
                                qtile = 2 + j
                                ws = min(max(2 * j - 4, 0), 23)
                                a = ws // 2
                                ktiles = [0, 1] + [2 + a + cc for cc in range(5)]
                                ty = {0: 0, 1: 1, 14: 3, 15: 4}.get(j, 2)
                            nk = len(ktiles)
                            q_ap = qT[hp * 64:(hp + 1) * 64, hc, qtile * 128:(qtile + 1) * 128]
                            for kc, kt in enumerate(ktiles):
                                dstp, r_dst = (sta[k], r_sta[k]) if kc < 4 else (stb[k], r_stb[k])
                                has_b = (ty is not None and kc >= 2)
                                last = (kc == min(3, nk - 1)) or (kc == nk - 1)
                                S.op("pe", lambda e, dstp=dstp, kc=kc, kt=kt, q_ap=q_ap, has_b=has_b, hp=hp, hc=hc: e.matmul(
                                    dstp[:, kc % 4, :], lhsT=kT[hp * 64:(hp + 1) * 64, hc, kt * 128:(kt + 1) * 128], rhs=q_ap,
                                    start=True, stop=(not has_b)), reads=[r_kT, r_qT], writes=[r_dst], signal=(last and not has_b))
                                if has_b:
                                    S.op("pe", lambda e, dstp=dstp, kc=kc, ty=ty, bsel=bsel: e.matmul(
                                        dstp[:, kc % 4, :], lhsT=identb[:], rhs=bias[bsel][:, ty, kc - 2, :], start=False, stop=True),
                                        reads=[r_bias[bsel], R_c], writes=[r_dst], signal=last)
                            na = min(4, nk)
                            S.op("act", lambda e, k=k, na=na: e.activation(out=PT[k][:, 0:na, :], in_=sta[k][:, 0:na, :], func=AF.Exp),
                                 reads=[r_sta[k]], writes=[r_PT[k]])
                            if nk > 4:
                                S.op("act", lambda e, k=k, nk=nk: e.activation(out=PT[k][:, 4:nk, :], in_=stb[k][:, 0:nk - 4, :], func=AF.Exp),
                                     reads=[r_stb[k]], writes=[r_PT[k]])
                            for kc, kt in enumerate(ktiles):
                                S.op("pe", lambda e, k=k, kc=kc, kt=kt, nk=nk, h=h: e.matmul(
                                    po[k][:, 0, :], lhsT=V[:, kt, h * 64:(h + 1) * 64], rhs=PT[k][:, kc, :], start=(kc == 0), stop=(kc == nk - 1)),
                                    reads=[r_V, r_PT[k]], writes=[r_po[k]], signal=False)
                            for kc, kt in enumerate(ktiles):
                                S.op("pe", lambda e, k=k, kc=kc, nk=nk: e.matmul(
                                    po[k][:, 1, :], lhsT=onesb[:, 0:64], rhs=PT[k][:, kc, :], start=(kc == 0), stop=(kc == nk - 1)),
                                    reads=[R_c, r_PT[k]], writes=[r_po[k]], signal=(kc == nk - 1))
                            S.op("dve", lambda e, k=k: e.reciprocal(out=rc[k][:], in_=po[k][:, 1, :]), reads=[r_po[k]], writes=[r_rc[k]])
                            S.op("dve", lambda e, k=k, hp=hp, hc=hc, qtile=qtile: e.tensor_tensor(
                                out=OA[hp * 64:(hp + 1) * 64, hc, qtile * 128:(qtile + 1) * 128], in0=po[k][:, 0, :], in1=rc[k][:], op=ALU.mult),
                                reads=[r_po[k], r_rc[k]], writes=[r_OA])
                    S.barrier()
                    S.emit()
                srcs = [(OA, c, r_OA, 0) for c in range(4)] + [(OB, c, r_OB, 0) for c in range(4)]
                outproj_residual(st, 0, b, srcs, ev_w_out, xin, R_XIN, list(range(NTI)), VEC_OF_TILE(b))

        def layer1_mixer(b):
            with contextlib.ExitStack() as st:
                UC = sbuf(st, "UC", [128, 8, NT], BF16); r_UC = Res()
                GG = sbuf(st, "GG", [128, 8, T], BF16); r_GG = Res()
                with contextlib.ExitStack() as s1:
                    hT = sbuf(s1, "hT1", [128, 8, NT], BF16); r_hT = Res()
                    norm_to_hT(s1, 1, b, XR, R_XRT, hT, r_hT, VEC_OF_TILE(b))
                    wb, r_wb = stream_w(s1, "wi1")
                    pj = [psum(s1, "pj1%d" % i, [128, 512], F32) for i in range(3)]
                    r_pj = [Res() for _ in range(3)]
                    u_f = sbuf(s1, "u_f", [128, NT], F32); r_u = Res()
                    t_f = sbuf(s1, "t1_f", [128, NT], F32); r_t = Res()
                    cnt = [0]
                    for grp in range(4):
                        bi = grp % 2
                        load_w(wb[bi], r_wb[bi], od_w_in, grp * 512)
                        for wc in range(4):
                            f = (grp % 2) * 4 + wc
                            for tp in range(len(TOKP)):
                                t0, nt_ = TOKP[tp]
                                if grp < 2 and t0 + nt_ <= LC:
                                    continue
                                k = cnt[0] % 3
                                cnt[0] += 1
                                for c in range(8):
                                    S.op("pe", lambda e, c=c, k=k, bi=bi, wc=wc, t0=t0, nt_=nt_: e.matmul(
                                        pj[k][:, 0:nt_], lhsT=wb[bi][:, c, wc * 128:(wc + 1) * 128], rhs=hT[:, c, t0:t0 + nt_],
                                        start=(c == 0), stop=(c == 7)), reads=[r_wb[bi], r_hT], writes=[r_pj[k]], signal=(c == 7))
                                if grp < 2:
                                    lo = max(t0, LC)
                                    S.op("act", lambda e, k=k, f=f, lo=lo, t0=t0, nt_=nt_: e.activation(
                                        out=GG[:, f, lo - LC:t0 + nt_ - LC], in_=pj[k][:, lo - t0:nt_], func=AF.Gelu),
                                        reads=[r_pj[k]], writes=[r_GG])
                                else:
                                    S.op("act", lambda e, k=k, t0=t0, nt_=nt_: e.activation(out=u_f[:, t0:t0 + nt_], in_=pj[k][:, 0:nt_], func=AF.Copy),
                                         reads=[r_pj[k]], writes=[r_u])
                            if grp >= 2:
                                wj = [pcols[:, PC_OCW + j * 8 + f:PC_OCW + j * 8 + f + 1] for j in range(4)]
                                bb = pcols[:, PC_OCB + f:PC_OCB + f + 1]
                                S.op("dve", lambda e, wj=wj, bb=bb: e.tensor_scalar(out=t_f[:], in0=u_f[:], scalar1=wj[1], scalar2=bb, op0=ALU.mult, op1=ALU.add),
                                     reads=[r_u, R_c], writes=[r_t])
                                for (s0, sn) in ((0, LC), (LC, T)):
                                    S.op("dve", lambda e, s0=s0, sn=sn, wj=wj: e.scalar_tensor_tensor(
                                        out=t_f[:, s0 + 1:s0 + sn], in0=u_f[:, s0:s0 + sn - 1], scalar=wj[0], in1=t_f[:, s0 + 1:s0 + sn],
                                        op0=ALU.mult, op1=ALU.add), reads=[r_u, r_t, R_c], writes=[r_t])
                                    S.op("dve", lambda e, s0=s0, sn=sn, wj=wj: e.scalar_tensor_tensor(
                                        out=t_f[:, s0:s0 + sn - 1], in0=u_f[:, s0 + 1:s0 + sn], scalar=wj[2], in1=t_f[:, s0:s0 + sn - 1],
                                        op0=ALU.mult, op1=ALU.add), reads=[r_u, r_t, R_c], writes=[r_t])
                                    S.op("dve", lambda e, s0=s0, sn=sn, wj=wj: e.scalar_tensor_tensor(
                                        out=t_f[:, s0:s0 + sn - 2], in0=u_f[:, s0 + 2:s0 + sn], scalar=wj[3], in1=t_f[:, s0:s0 + sn - 2],
                                        op0=ALU.mult, op1=ALU.add), reads=[r_u, r_t, R_c], writes=[r_t])
                                S.op("act", lambda e, f=f: e.activation(out=UC[:, f, :], in_=t_f[:], func=AF.Copy), reads=[r_t], writes=[r_UC])
                    S.barrier()
                    S.emit()
                YI = sbuf(st, "YI", [128, 8, T], BF16); r_YI = Res()
                with contextlib.ExitStack() as s1:
                    gw = sbuf(s1, "gw", [128, 4, 4, 2, 256], BF16); r_gw = Res()
                    for m in range(4):
                        S.dma("pool", lambda e, m=m: e.dma_start(out=gw[:, m], in_=od_gw[m].rearrange("b (k p) n -> p b k n", p=128)),
                              writes=[r_gw])
                    cl = sbuf(s1, "cl", [128, 2, 8], F32); r_cl = Res()
                    for d_ in range(2):
                        lam = pcols[:, PC_G + d_ * 24 + 16:PC_G + d_ * 24 + 24]
                        S.op("act", lambda e, d_=d_, lam=lam: e.activation(out=cl[:, d_, :], in_=lam, func=AF.Exp, scale=-1.0), reads=[R_c], writes=[r_cl])
                        S.op("act", lambda e, d_=d_: e.activation(out=cl[:, d_, :], in_=cl[:, d_, :], func=AF.Ln, bias=1.0, scale=1.0), reads=[r_cl], writes=[r_cl])
                        S.op("dve", lambda e, d_=d_: e.tensor_scalar(out=cl[:, d_, :], in0=cl[:, d_, :], scalar1=-8.0, scalar2=None, op0=ALU.mult),
                             reads=[r_cl], writes=[r_cl])
                    pg = [psum(s1, "pg%d" % i, [128, 512], F32) for i in range(4)]
                    r_pg = [Res() for _ in range(4)]
                    Rt = sbuf(s1, "Rt", [128, NT], F32); r_R = Res()
                    It = sbuf(s1, "It", [128, NT], F32); r_I = Res()
                    At = sbuf(s1, "At", [128, NT], F32); r_A = Res()
                    Bt = sbuf(s1, "Bt", [128, NT], F32); r_B = Res()
                    Hf = sbuf(s1, "Hf", [128, NT], F32); r_Hf = Res()
                    Hb = sbuf(s1, "Hb", [128, NT], F32); r_Hb = Res()
                    cnt = [0]
                    for f in range(8):
                        blk = f // 2
                        for d_ in range(2):
                            for gi, (dstt, r_dst) in enumerate(((Rt, r_R), (It, r_I))):
                                m = d_ * 2 + gi
                                bcol = pcols[:, PC_G + d_ * 24 + gi * 8 + f:PC_G + d_ * 24 + gi * 8 + f + 1]
                                for tp in range(len(TOKP)):
                                    t0, nt_ = TOKP[tp]
                                    k = cnt[0] % 4
                                    cnt[0] += 1
                                    for kc in range(2):
                                        S.op("pe", lambda e, k=k, m=m, kc=kc, t0=t0, nt_=nt_, blk=blk, f=f: e.matmul(
                                            pg[k][:, 0:nt_], lhsT=gw[:, m, blk, kc, (f % 2) * 128:(f % 2 + 1) * 128],
                                            rhs=UC[:, 2 * blk + kc, t0:t0 + nt_], start=(kc == 0), stop=(kc == 1)),
                                            reads=[r_gw, r_UC], writes=[r_pg[k]], signal=(kc == 1))
                                    S.op("act", lambda e, k=k, dstt=dstt, t0=t0, nt_=nt_, bcol=bcol: e.activation(
                                        out=dstt[:, t0:t0 + nt_], in_=pg[k][:, 0:nt_], func=AF.Sigmoid, bias=bcol, scale=1.0),
                                        reads=[r_pg[k], R_c], writes=[r_dst])
                            S.op("act", lambda e, d_=d_, f=f: e.activation(out=At[:], in_=Rt[:], func=AF.Exp, scale=cl[:, d_, f:f + 1]),
                                 reads=[r_R, r_cl], writes=[r_A])
                            S.op("pool", lambda e: e.tensor_tensor(out=Bt[:], in0=At[:], in1=At[:], op=ALU.mult), reads=[r_A], writes=[r_B])
                            S.op("act", lambda e: e.activation(out=Bt[:], in_=Bt[:], func=AF.Sqrt, scale=-1.0, bias=1.0), reads=[r_B], writes=[r_B])
                            S.op("dve", lambda e: e.tensor_tensor(out=Bt[:], in0=Bt[:], in1=It[:], op=ALU.mult), reads=[r_B, r_I], writes=[r_B])
                            S.op("dve", lambda e, f=f: e.tensor_tensor(out=Bt[:], in0=Bt[:], in1=UC[:, f, :], op=ALU.mult), reads=[r_B, r_UC], writes=[r_B])
                            if d_ == 0:
                                S.op("dve", lambda e: e.tensor_tensor_scan(out=Hf[:], data0=At[:], data1=Bt[:], initial=0.0, op0=ALU.mult, op1=ALU.add),
                                     reads=[r_A, r_B], writes=[r_Hf])
                            else:
                                S.op("dve", lambda e: e.tensor_tensor_scan(out=Hb[:, 0:LC][:, ::-1],
                                                                           data0=At[:, 0:LC][:, ::-1], data1=Bt[:, 0:LC][:, ::-1],
                                                                           initial=0.0, op0=ALU.mult, op1=ALU.add),
                                     reads=[r_A, r_B], writes=[r_Hb])
                                S.op("dve", lambda e: e.tensor_tensor_scan(out=Hb[:, LC:NT][:, ::-1], data0=At[:, LC:NT][:, ::-1],
                                                                           data1=Bt[:, LC:NT][:, ::-1], initial=Hb[:, 0:1],
                                                                           op0=ALU.mult, op1=ALU.add),
                                     reads=[r_A, r_B, r_Hb], writes=[r_Hb])
                        S.op("pool", lambda e: e.tensor_tensor(out=Hf[:, LC:NT], in0=Hf[:, LC:NT], in1=Hb[:, LC:NT], op=ALU.add),
                             reads=[r_Hf, r_Hb], writes=[r_Hf])
                        S.op("dve", lambda e, f=f: e.tensor_tensor(out=YI[:, f, :], in0=Hf[:, LC:NT], in1=GG[:, f, :], op=ALU.mult),
                             reads=[r_Hf, r_GG], writes=[r_YI])
                    S.barrier()
                    S.emit()
                srcs = [(YI, c, r_YI, LC) for c in range(8)]
                outproj_residual(st, 1, b, srcs, od_w_out, XR, R_XRT, list(range(2, NTI)), VEC_OF_TILE(b))

        def moe_layer(l, tok_tiles, final):
            ntile = len(tok_tiles)
            ntok = ntile * 128
            nblk = ntok * 4 // BLK + n_exp
            with contextlib.ExitStack() as st:
                sH2 = contextlib.ExitStack()
                LG = sbuf(st, "LG", [128, ntile, NE], F32); r_LG = Res()
                D4 = sbuf(st, "D4", [128, ntile * 4], I32); r_D4 = Res()
                G4 = sbuf(st, "G4", [128, ntile * 4], F32); r_G4 = Res()
                IDXW = sbuf(st, "IDXW", [128, nblk], I32); r_IDXW = Res()
                BEI = sbuf(st, "BEI", [128, nblk], I32)
                H2 = sbuf(sH2, "H2", [128, ntile * D], BF16); r_H2 = Res()
                with contextlib.ExitStack() as s1:
                    vecs = sorted(set((NB if i < 2 else b) for b, i in tok_tiles))
                    tiles = norm_mod_tiles(s1, l, 1, vecs)
                    small, r_small = zip(*[new_small(s1, "n2small%d" % i) for i in range(2)])
                    junk = [sbuf(s1, "n2junk%d" % i, [128, D], F32) for i in range(2)]; r_junk = [Res(), Res()]
                    xt = [sbuf(s1, "n2x%d" % i, [128, D], F32) for i in range(2)]
                    r_xt = [Res(), Res()]
                    ht = [sbuf(s1, "n2h%d" % i, [128, D], F32) for i in range(2)]
                    r_ht = [Res(), Res()]
                    hT32 = [sbuf(s1, "n2t%d" % i, [128, 8, 128], F32) for i in range(2)]
                    r_hT32 = [Res(), Res()]
                    ptp = [psum(s1, "n2p%d" % i, [128, 4, 128], F32) for i in range(2)]
                    r_ptp = [Res(), Res()]
                    plg = [psum(s1, "n2l%d" % i, [128, NE], F32) for i in range(2)]
                    r_plg = [Res(), Res()]
                    wr = sbuf(s1, "wr", [128, 8, NE], F32); r_wr = Res()
                    S.dma("sp", lambda e: e.dma_start(out=wr[:], in_=router_w[l].rearrange("(c p) n -> p c n", p=128)), writes=[r_wr])
                    rb, r_rb = load_bcast(s1, "rb", router_b[l, :], [], n=NE)
                    for n_i, (b, i) in enumerate(tok_tiles):
                        k = n_i % 2
                        G, rG, SH, rSH = tiles[NB if i < 2 else b]
                        S.dma("sp", lambda e, b=b, i=i, k=k: e.dma_start(out=xt[k][:], in_=XR[b, i * 128:(i + 1) * 128, :]),
                              reads=[R_XRT[(b, i)]], writes=[r_xt[k]])
                        rms_mod(s1, xt[k], r_xt[k], G, rG, SH, rSH, ht[k], r_ht[k], small[k], r_small[k], junk[k], r_junk[k])
                        S.op("act", lambda e, k=k, n_i=n_i: e.activation(out=H2[:, n_i * D:(n_i + 1) * D], in_=ht[k][:], func=AF.Copy), reads=[r_ht[k]], writes=[r_H2])
                        for hf in range(2):
                            for c4 in range(4):
                                c = hf * 4 + c4
                                S.op("pe", lambda e, k=k, hf=hf, c4=c4, c=c: e.transpose(
                                    out=ptp[hf][:, c4, :], in_=ht[k][:, c * 128:(c + 1) * 128], identity=ident[:]),
                                    reads=[r_ht[k], R_c], writes=[r_ptp[hf]], signal=(c4 == 3))
                            if hf == 0:
                                S.op("act", lambda e, k=k, hf=hf: e.activation(out=hT32[k][:, 0:4, :], in_=ptp[0][:], func=AF.Copy),
                                     reads=[r_ptp[0]], writes=[r_hT32[k]])
                            else:
                                S.op("dve", lambda e, k=k: e.tensor_copy(out=hT32[k][:, 4:8, :], in_=ptp[1][:]),
                                     reads=[r_ptp[1]], writes=[r_hT32[k]])
                        for c in range(8):
                            S.op("pe", lambda e, k=k, c=c: e.matmul(plg[k][:], lhsT=hT32[k][:, c, :], rhs=wr[:, c, :], start=(c == 0), stop=(c == 7)),
                                 reads=[r_hT32[k], r_wr], writes=[r_plg[k]], signal=(c == 7))
                        S.op("dve", lambda e, k=k, n_i=n_i: e.tensor_tensor(out=LG[:, n_i, :], in0=plg[k][:], in1=rb[:], op=ALU.add),
                             reads=[r_plg[k], r_rb], writes=[r_LG])
                    S.barrier()
                    S.emit()
                with contextlib.ExitStack() as s1:
                    MX = sbuf(s1, "MX", [128, ntile, 8], F32)
                    MASK = sbuf(s1, "MASK", [128, ntile, NE], F32)
                    MASKb = sbuf(s1, "MASKb", [128, ntile, NE], BF16)
                    CUM = sbuf(s1, "CUM", [128, ntile, NE], BF16)
                    GAT = sbuf(s1, "GAT", [128, ntile, NE], F32)
                    TMP = sbuf(s1, "TMP", [128, ntile, NE], F32)
                    KEY = sbuf(s1, "KEY", [128, ntile, NE], F32)
                    K8 = sbuf(s1, "K8", [128, ntile, 8], F32)
                    DEN = sbuf(s1, "DEN", [128, ntile], F32)
                    TRI = sbuf(s1, "TRI", [128, 128], BF16)
                    CNT = sbuf(s1, "CNT", [128, NE], F32)
                    CNTI = sbuf(s1, "CNTI", [128, NE], I32)
                    PAD = sbuf(s1, "PAD", [128, NE], F32)
                    PEND = sbuf(s1, "PEND", [128, NE], F32)
                    BASE = sbuf(s1, "BASE", [128, NE], F32)
                    ONE32 = sbuf(s1, "ONE32", [128, NE], F32)
                    CMP = sbuf(s1, "CMP", [128, nblk, NE], F32)
                    BEF = sbuf(s1, "BEF", [128, nblk], F32)
                    JB = sbuf(s1, "JB", [128, 80], F32)
                    IOP = sbuf(s1, "IOP", [128, 1], F32)
                    pposb = [psum(s1, "ppos%d" % i, [128, 16, NE], F32) for i in range((ntile + 15) // 16)]
                    pcnt = psum(s1, "pcnt", [128, NE], F32)
                    R = Res("route")
                    V_ = lambda fn, eng="dve": S.op(eng, fn, reads=[R, r_LG], writes=[R, r_D4, r_G4, r_IDXW])
                    S.dma("sp", lambda e: e.dma_start(out=JB[:], in_=blk_start_d[:, :]), writes=[R])
                    S.dma("sp", lambda e: e.dma_start(out=IOP[:], in_=iota_p_d[:, :]), reads=[R], writes=[R])
                    V_(lambda e: e.memset(TRI[:], 1.0))
                    V_(lambda e: e.affine_select(out=TRI[:], in_=TRI[:], pattern=[[1, 128]], compare_op=ALU.is_gt, fill=0.0,
                                                 base=0, channel_multiplier=-1), "pool")
                    V_(lambda e: e.memset(ONE32[:], 1.0))
                    for i in range(ntile):
                        V_(lambda e, i=i: e.max(out=MX[:, i, :], in_=LG[:, i, :]))
                    V_(lambda e: e.tensor_tensor(out=MASK[:], in0=LG[:], in1=MX[:, :, 3:4].to_broadcast([128, ntile, NE]), op=ALU.is_ge))
                    V_(lambda e: e.tensor_tensor(out=TMP[:], in0=LG[:], in1=MX[:, :, 0:1].to_broadcast([128, ntile, NE]), op=ALU.subtract))
                    V_(lambda e: e.activation(out=TMP[:], in_=TMP[:], func=AF.Exp), "act")
                    V_(lambda e: e.tensor_tensor(out=TMP[:], in0=TMP[:], in1=MASK[:], op=ALU.mult))
                    V_(lambda e: e.tensor_reduce(out=DEN[:], in_=TMP[:], axis=AX.X, op=ALU.add))
                    V_(lambda e: e.reciprocal(out=DEN[:], in_=DEN[:]))
                    V_(lambda e: e.tensor_tensor(out=GAT[:], in0=TMP[:], in1=DEN[:].unsqueeze(2).to_broadcast([128, ntile, NE]), op=ALU.mult))
                    V_(lambda e: e.tensor_copy(out=MASKb[:], in_=MASK[:]))
                    V_(lambda e: e.memset(CUM[:, 0, :], 0.0))
                    for i in range(1, ntile):
                        V_(lambda e, i=i: e.tensor_tensor(out=CUM[:, i, :], in0=CUM[:, i - 1, :], in1=MASKb[:, i - 1, :], op=ALU.add))
                    V_(lambda e: e.tensor_tensor(out=TMP[:, 0, :], in0=CUM[:, ntile - 1, :], in1=MASKb[:, ntile - 1, :], op=ALU.add))
                    TOTb = sbuf(s1, "TOTb", [128, NE], BF16)
                    V_(lambda e: e.tensor_copy(out=TOTb[:], in_=TMP[:, 0, :]))
                    for i in range(ntile):
                        V_(lambda e, i=i: e.matmul(pposb[i // 16][:, i % 16, :], lhsT=TRI[:], rhs=MASKb[:, i, :], start=True, stop=False), "pe")
                        V_(lambda e, i=i: e.matmul(pposb[i // 16][:, i % 16, :], lhsT=onesb[:, 0:128], rhs=CUM[:, i, :], start=False, stop=True), "pe")
                    V_(lambda e: e.matmul(pcnt[:], lhsT=onesb[:, 0:128], rhs=TOTb[:], start=True, stop=True), "pe")
                    V_(lambda e: e.tensor_copy(out=CNT[:], in_=pcnt[:]))
                    NM = ntok // BLK + 1
                    CMP2 = sbuf(s1, "CMP2", [128, NE, NM], F32)
                    V_(lambda e: e.tensor_tensor(out=CMP2[:], in0=CNT[:].unsqueeze(2).to_broadcast([128, NE, NM]),
                                                 in1=JB[:, 0:NM].unsqueeze(1).to_broadcast([128, NE, NM]), op=ALU.is_gt))
                    V_(lambda e: e.tensor_reduce(out=PAD[:], in_=CMP2[:], axis=AX.X, op=ALU.add))
                    V_(lambda e: e.tensor_scalar(out=PAD[:], in0=PAD[:], scalar1=float(BLK), scalar2=None, op0=ALU.mult))
                    V_(lambda e: e.tensor_tensor_scan(out=PEND[:], data0=ONE32[:], data1=PAD[:], initial=0.0, op0=ALU.mult, op1=ALU.add))
                    V_(lambda e: e.tensor_tensor(out=BASE[:], in0=PEND[:], in1=PAD[:], op=ALU.subtract))
                    for bk in range((ntile + 15) // 16):
                        n_ = min(16, ntile - bk * 16)
                        V_(lambda e, bk=bk, n_=n_: e.tensor_tensor(out=KEY[:, bk * 16:bk * 16 + n_, :], in0=pposb[bk][:, 0:n_, :],
                                                                  in1=BASE[:].unsqueeze(1).to_broadcast([128, n_, NE]), op=ALU.add))
                    V_(lambda e: e.scalar_tensor_tensor(out=KEY[:], in0=KEY[:], scalar=1.0, in1=MASK[:], op0=ALU.add, op1=ALU.mult))
                    for i in range(ntile):
                        V_(lambda e, i=i: e.max(out=K8[:, i, :], in_=KEY[:, i, :]))
                    V_(lambda e: e.tensor_scalar(out=TMP[:, :, 0:4], in0=K8[:, :, 0:4], scalar1=-1.0, scalar2=0.0, op0=ALU.add, op1=ALU.max))
                    V_(lambda e: e.tensor_scalar(out=TMP[:, :, 0:4], in0=TMP[:, :, 0:4], scalar1=float(nblk * BLK - 1), scalar2=None, op0=ALU.min))
                    V_(lambda e: e.tensor_copy(out=D4[:].rearrange("p (n j) -> p n j", j=4), in_=TMP[:, :, 0:4]))
                    for j in range(4):
                        V_(lambda e, j=j: e.tensor_tensor(out=TMP[:], in0=KEY[:], in1=K8[:, :, j:j + 1].to_broadcast([128, ntile, NE]), op=ALU.is_equal))
                        V_(lambda e: e.tensor_tensor(out=TMP[:], in0=TMP[:], in1=GAT[:], op=ALU.mult))
                        V_(lambda e, j=j: e.tensor_reduce(out=G4[:].rearrange("p (n j) -> p n j", j=4)[:, :, j], in_=TMP[:], axis=AX.X, op=ALU.add))
                    V_(lambda e: e.tensor_tensor(out=CMP[:], in0=PEND[:].unsqueeze(1).to_broadcast([128, nblk, NE]),
                                                 in1=JB[:, 0:nblk].unsqueeze(2).to_broadcast([128, nblk, NE]), op=ALU.is_le))
                    V_(lambda e: e.tensor_reduce(out=BEF[:], in_=CMP[:], axis=AX.X, op=ALU.add))
                    V_(lambda e: e.tensor_scalar(out=BEF[:], in0=BEF[:], scalar1=float(n_exp - 1), scalar2=0.0, op0=ALU.min, op1=ALU.max))
                    V_(lambda e: e.tensor_copy(out=BEI[:], in_=BEF[:]))
                    V_(lambda e: e.tensor_scalar(out=BEF[:], in0=BEF[:], scalar1=128.0, scalar2=IOP[:, 0:1], op0=ALU.mult, op1=ALU.add))
                    V_(lambda e: e.tensor_copy(out=IDXW[:], in_=BEF[:]))
                    if dbg:
                        S.dma("sp", lambda e: e.dma_start(out=dbg_d4[:, 0:ntile * 4], in_=D4[:]), reads=[R, r_D4])
                        S.dma("sp", lambda e: e.dma_start(out=dbg_g4[:, 0:ntile * 4], in_=G4[:]), reads=[R, r_G4])
                        S.dma("sp", lambda e: e.dma_start(out=dbg_iw[:, 0:nblk], in_=IDXW[:]), reads=[R, r_IDXW])
                    for n_i in range(ntile if moe_stop >= 2 else 0):
                        for j in range(4):
                            S.dma("pool", lambda e, n_i=n_i, j=j: e.indirect_dma_start(
                                out=XS[:, :], out_offset=bass.IndirectOffsetOnAxis(ap=D4[:, n_i * 4 + j:n_i * 4 + j + 1], axis=0),
                                in_=H2[:, n_i * D:(n_i + 1) * D], in_offset=None),
                                reads=[R, r_D4, r_H2], writes=[R_XS])
                    S.barrier()
                    S.emit()
                sH2.close()
                if moe_stop < 3:
                    return
                w_aps = (w_gate[l], w_up[l], w_down[l])
                with contextlib.ExitStack() as s1:
                    WG = [sbuf(s1, "WG%d" % i, [128, 8192], BF16) for i in range(2)]
                    WU = [sbuf(s1, "WU%d" % i, [128, 8192], BF16) for i in range(2)]
                    WD = [sbuf(s1, "WD%d" % i, [128, 8192], BF16) for i in range(2)]
                    EB = [sbuf(s1, "EB%d" % i, [2, 3072], BF16) for i in range(2)]
                    EBC = [sbuf(s1, "EBC%d" % i, [128, 16], F32) for i in range(2)]
                    r_EBC = [Res(), Res()]
                    r_W = [[Res() for _ in range(4)] for _ in range(2)]
                    xs = [sbuf(s1, "xsb%d" % i, [128, 4, D], BF16) for i in range(2)]
                    r_xs = [Res(), Res()]
                    xeT = [sbuf(s1, "xeT%d" % i, [128, 8, BLK], BF16) for i in range(2)]
                    r_xeT = [Res(), Res()]
                    actT = sbuf(s1, "actT", [128, 8, BLK], BF16); r_actT = Res()
                    gs = [sbuf(s1, "gs%d" % i, [128, BLK], F32) for i in range(2)]
                    sg = [sbuf(s1, "sg%d" % i, [128, BLK], F32) for i in range(2)]
                    us = [sbuf(s1, "us%d" % i, [128, BLK], F32) for i in range(2)]
                    r_gs = [Res(), Res()]; r_sg = [Res(), Res()]; r_us = [Res(), Res()]
                    ysb = [sbuf(s1, "ysb0", [128, 4, D], F32)] * 2
                    r_ysb = [Res()] * 2
                    ptr = [psum(s1, "ptr%d" % i, [128, 8, 128], BF16) for i in range(2)]
                    r_ptr = [Res(), Res()]
                    pgp = [psum(s1, "pgp%d" % i, [128, BLK], F32) for i in range(2)]
                    pup = [psum(s1, "pup%d" % i, [128, BLK], F32) for i in range(2)]
                    r_pgp = [Res(), Res()]; r_pup = [Res(), Res()]
                    pyp = [psum(s1, "pyp%d" % i, [128, 512], F32) for i in range(2)]
                    r_pyp = [Res(), Res()]
                    def load_xs(jb_):
                        k_ = jb_ % 2
                        S.dma("sp", lambda e: e.dma_start(
                            out=xs[k_][:], in_=XS[jb_ * BLK:(jb_ + 1) * BLK, :].rearrange("(s p) d -> p s d", p=128)),
                            reads=[R_XS], writes=[r_xs[k_]])

                    for jb in range(nblk):
                        k = jb % 2
                        for mi, (wbuf, wap) in enumerate(((WG[k], w_aps[0]), (WU[k], w_aps[1]), (WD[k], w_aps[2]))):
                            S.dma("pool", lambda e, wbuf=wbuf, wap=wap, jb=jb: e.indirect_dma_start(
                                out=wbuf[:, :], out_offset=None, in_=wap[:, :],
                                in_offset=bass.IndirectOffsetOnAxis(ap=IDXW[:, jb:jb + 1], axis=0)), reads=[r_IDXW], writes=[r_W[k][mi]])
                        S.dma("pool", lambda e, k=k, jb=jb: e.indirect_dma_start(
                            out=EB[k][:, :], out_offset=None, in_=exp_b[l][:, :],
                            in_offset=bass.IndirectOffsetOnAxis(ap=BEI[0:2, jb:jb + 1], axis=0)), reads=[r_IDXW], writes=[r_W[k][3]])
                        S.dma("pool", lambda e, k=k, jb=jb: e.indirect_dma_start(
                            out=EBC[k][:, :], out_offset=None, in_=exp_bc[l][:, :],
                            in_offset=bass.IndirectOffsetOnAxis(ap=IDXW[:, jb:jb + 1], axis=0)), reads=[r_IDXW], writes=[r_EBC[k]])
                        S.op("dve", lambda e, k=k: e.tensor_scalar(out=EBC[k][:, 8:16], in0=EBC[k][:, 8:16], scalar1=1.0, scalar2=None, op0=ALU.add),
                             reads=[r_EBC[k]], writes=[r_EBC[k]])
                        if jb == 0:
                            load_xs(0)
                        for s in range(4):
                            kk = s % 2
                            for c in range(8):
                                S.op("pe", lambda e, k=k, kk=kk, s=s, c=c: e.transpose(out=ptr[kk][:, c, :], in_=xs[k][:, s, c * 128:(c + 1) * 128],
                                                                                 identity=identb[:]),
                                     reads=[r_xs[k], R_c], writes=[r_ptr[kk]], signal=(c == 7))
                            if s % 2 == 0:
                                S.op("act", lambda e, k=k, kk=kk, s=s: e.activation(out=xeT[k][:, :, s * 128:(s + 1) * 128], in_=ptr[kk][:], func=AF.Copy),
                                     reads=[r_ptr[kk]], writes=[r_xeT[k]])
                            else:
                                S.op("dve", lambda e, k=k, kk=kk, s=s: e.tensor_copy(out=xeT[k][:, :, s * 128:(s + 1) * 128], in_=ptr[kk][:]),
                                     reads=[r_ptr[kk]], writes=[r_xeT[k]])
                        if jb + 1 < nblk:
                            load_xs(jb + 1)
                        for f in range(8):
                            kf = f % 2
                            for (pp, r_pp, wbuf, mi) in ((pgp[kf], r_pgp[kf], WG[k], 0), (pup[kf], r_pup[kf], WU[k], 1)):
                                for c in range(8):
                                    S.op("pe", lambda e, pp=pp, wbuf=wbuf, c=c, f=f, k=k: e.matmul(
                                        pp[:], lhsT=wbuf[:, c * 1024 + f * 128:c * 1024 + (f + 1) * 128], rhs=xeT[k][:, c, :], start=(c == 0), stop=(c == 7)),
                                        reads=[r_W[k][mi], r_xeT[k]], writes=[r_pp], signal=(c == 7))
                            S.op("dve", lambda e, kf=kf, k=k, f=f: e.tensor_scalar(out=gs[kf][:], in0=pgp[kf][:], scalar1=EBC[k][:, f:f + 1], scalar2=7.0,
                                                                                 op0=ALU.add, op1=ALU.min),
                                 reads=[r_pgp[kf], r_EBC[k]], writes=[r_gs[kf]])
                            S.op("act", lambda e, kf=kf: e.activation(out=sg[kf][:], in_=gs[kf][:], func=AF.Sigmoid, scale=1.702),
                                 reads=[r_gs[kf]], writes=[r_sg[kf]])
                            S.op("dve", lambda e, kf=kf, k=k, f=f: e.tensor_scalar(out=us[kf][:], in0=pup[kf][:], scalar1=EBC[k][:, 8 + f:9 + f], scalar2=8.0,
                                                                                 op0=ALU.add, op1=ALU.min),
                                 reads=[r_pup[kf], r_EBC[k]], writes=[r_us[kf]])
                            S.op("dve", lambda e, kf=kf: e.scalar_tensor_tensor(out=us[kf][:], in0=us[kf][:], scalar=-6.0, in1=gs[kf][:],
                                                                                op0=ALU.max, op1=ALU.mult),
                                 reads=[r_us[kf], r_gs[kf]], writes=[r_us[kf]])
                            S.op("dve", lambda e, kf=kf, f=f: e.tensor_tensor(out=actT[:, f, :], in0=us[kf][:], in1=sg[kf][:], op=ALU.mult),
                                 reads=[r_us[kf], r_sg[kf]], writes=[r_actT])
                        for s in range(4):
                            for hh in range(2):
                                kp = (s * 2 + hh) % 2
                                for f in range(8):
                                    S.op("pe", lambda e, kp=kp, f=f, s=s, hh=hh, k=k: e.matmul(
                                        pyp[kp][:], lhsT=actT[:, f, s * 128:(s + 1) * 128],
                                        rhs=WD[k][:, f * 1024 + hh * 512:f * 1024 + (hh + 1) * 512], start=(f == 0), stop=False),
                                        reads=[r_W[k][2], r_actT], writes=[r_pyp[kp]], signal=False)
                                S.op("pe", lambda e, kp=kp, hh=hh, k=k: e.matmul(
                                    pyp[kp][:], lhsT=halfb[0:2, 0:128], rhs=EB[k][0:2, 2048 + hh * 512:2048 + (hh + 1) * 512], start=False, stop=True),
                                    reads=[r_W[k][3], R_c], writes=[r_pyp[kp]])
                                S.op("act", lambda e, kp=kp, k=k, s=s, hh=hh: e.activation(out=ysb[k][:, s, hh * 512:(hh + 1) * 512], in_=pyp[kp][:], func=AF.Copy),
                                     reads=[r_pyp[kp]], writes=[r_ysb[k]])
                        S.dma("sp", lambda e, k=k, jb=jb: e.dma_start(
                            out=YS[jb * BLK:(jb + 1) * BLK, :].rearrange("(s p) d -> p s d", p=128), in_=ysb[k][:]),
                            reads=[r_ysb[k]], writes=[R_YS])
                    S.barrier()
                    S.emit()
                if moe_stop < 4:
                    return
                with contextlib.ExitStack() as s1:
                    g2 = {}
                    for v in sorted(set((NB if i < 2 else b) for b, i in tok_tiles)):
                        g2[v] = load_bcast(s1, "g2_%d" % v, MOD[l, v, 5 * D:6 * D], [R_MOD])
                    yg = [[sbuf(s1, "yg%d_%d" % (i, j), [128, D], F32) for j in range(4)] for i in range(2)]
                    r_yg = [[Res() for _ in range(4)] for _ in range(2)]
                    xt = [sbuf(s1, "cbx%d" % i, [128, D], F32) for i in range(2)]
                    r_xt = [Res(), Res()]
                    for n_i, (b, i) in enumerate(tok_tiles):
                        k = n_i % 2
                        gt, rgt = g2[NB if i < 2 else b]
                        S.dma("sp", lambda e, b=b, i=i, k=k: e.dma_start(out=xt[k][:], in_=XR[b, i * 128:(i + 1) * 128, :]),
                              reads=[R_XRT[(b, i)]], writes=[r_xt[k]])
                        for j in range(4):
                            S.dma("pool", lambda e, k=k, j=j, n_i=n_i: e.indirect_dma_start(
                                out=yg[k][j][:, :], out_offset=None, in_=YS[:, :],
                                in_offset=bass.IndirectOffsetOnAxis(ap=D4[:, n_i * 4 + j:n_i * 4 + j + 1], axis=0)), reads=[R_YS, r_D4], writes=[r_yg[k][j]])
                        S.op("dve", lambda e, k=k, n_i=n_i: e.tensor_scalar(out=yg[k][0][:], in0=yg[k][0][:], scalar1=G4[:, n_i * 4:n_i * 4 + 1], scalar2=None, op0=ALU.mult),
                             reads=[r_yg[k][0], r_G4], writes=[r_yg[k][0]])
                        for j in range(1, 4):
                            S.op("dve", lambda e, k=k, j=j, n_i=n_i: e.scalar_tensor_tensor(
                                out=yg[k][0][:], in0=yg[k][j][:], scalar=G4[:, n_i * 4 + j:n_i * 4 + j + 1], in1=yg[k][0][:], op0=ALU.mult, op1=ALU.add),
                                reads=[r_yg[k][j], r_yg[k][0], r_G4], writes=[r_yg[k][0]])
                        S.op("dve", lambda e, k=k, gt=gt: e.tensor_tensor(out=yg[k][0][:], in0=yg[k][0][:], in1=gt[:], op=ALU.mult),
                             reads=[r_yg[k][0], rgt], writes=[r_yg[k][0]])
                        S.op("pool", lambda e, k=k: e.tensor_tensor(out=xt[k][:], in0=xt[k][:], in1=yg[k][0][:], op=ALU.add),
                             reads=[r_yg[k][0], r_xt[k]], writes=[r_xt[k]])
                        if final:
                            S.dma("sp", lambda e, b=b, i=i, k=k: e.dma_start(out=out[b, (i - 2) * 128:(i - 1) * 128, :], in_=xt[k][:]),
                                  reads=[r_xt[k]], writes=[R_OUT])
                        else:
                            S.dma("sp", lambda e, b=b, i=i, k=k: e.dma_start(out=XR[b, i * 128:(i + 1) * 128, :], in_=xt[k][:]),
                                  reads=[r_xt[k]], writes=[R_XRT[(b, i)]])
                    S.barrier()
                    S.emit()

        if 0 in layers:
            for b in range(NB):
                layer0_mixer(b)
            if do_moe:
                moe_layer(0, [(b, i) for b in range(NB) for i in range(NTI)], final=False)
        if 1 in layers:
            for b in range(NB):
                layer1_mixer(b)
            if do_moe:
                moe_layer(1, [(b, i) for b in range(NB) for i in range(2, NTI)], final=True)
        if dbg:
            with contextlib.ExitStack() as st:
                t = sbuf(st, "dbgt", [128, D], F32); r_t = Res()
                for b in range(NB):
                    for i in range(2, NTI):
                        S.dma("sp", lambda e, b=b, i=i: e.dma_start(out=t[:], in_=XR[b, i * 128:(i + 1) * 128, :]), reads=[R_XRT[(b, i)]], writes=[r_t])
                        S.dma("sp", lambda e, b=b, i=i: e.dma_start(out=out[b, (i - 2) * 128:(i - 1) * 128, :], in_=t[:]), reads=[r_t], writes=[R_OUT])
                S.barrier()
                S.emit()
        S.barrier()
        S.emit()
        print("program instructions (incl waits):", S.n_ins, "sem counts:", S.cnt, "max dma sem:", max(S.dma_cnt.values()))
    return nc


def _core_inputs(inp, sh, c, NB=2):
    b0 = c * NB
    d = dict(sh)
    d["xin"] = np.ascontiguousarray(np.concatenate([inp["ctx"][b0:b0 + NB], inp["x"][b0:b0 + NB]], axis=1), np.float32)
    d["cvec"] = np.ascontiguousarray(np.concatenate([inp["c"][b0:b0 + NB], inp["c_ctx"][None]], 0), np.float32)
    return d


def kernel(**inputs):
    inp = {k: np.asarray(v) for k, v in inputs.items()}
    sh = _prep_shared(inp)
    nc = build_program(NB=2)
    in_maps = [_core_inputs(inp, sh, c) for c in range(8)]
    res = run_bass_kernel_spmd(nc, in_maps, core_ids=list(range(8)))
    return np.concatenate([r["out"] for r in res.results], axis=0).astype(np.float32)
```

```python
import contextlib
import numpy as np
import concourse.bass as bass
import concourse.mybir as mybir
from concourse.bass_utils import run_bass_kernel_spmd

F32 = mybir.dt.float32
BF16 = mybir.dt.bfloat16
I32 = mybir.dt.int32
AF = mybir.ActivationFunctionType
ALU = mybir.AluOpType
AX = mybir.AxisListType

ENGS = ("pe", "dve", "act", "pool", "sp")

D = 1024
T = 2048
LC = 256
NT = T + LC
NTI = NT // 128
NE = 32
BLK = 512
EPS = 1e-6
NEG = -30000.0


class Res:
    __slots__ = ("name", "w", "r")

    def __init__(self, name=""):
        self.name = name
        self.w = None
        self.r = []


class Sched:
    def __init__(self, nc, stack, n_dma_sems=12):
        self.nc = nc
        self.streams = {e: [] for e in ENGS}
        self.cnt = {e: 0 for e in ENGS}
        self.sems = {}
        for e in ENGS:
            self.sems[e] = stack.enter_context(nc.semaphore("s_" + e))
        self.dma_sems = {}
        self.dma_cnt = {}
        self.dma_rr = {}
        for q in ("sp", "pool"):
            self.dma_sems[q] = []
            for i in range(n_dma_sems):
                k = "d_%s_%d" % (q, i)
                self.sems[k] = stack.enter_context(nc.semaphore(k))
                self.dma_sems[q].append(k)
                self.dma_cnt[k] = 0
            self.dma_rr[q] = 0
        self.seen = {e: {} for e in ENGS}
        self.n_ins = 0

    def _need(self, eng, deps):
        best = {}
        for d in deps:
            if d is None:
                continue
            k, v = d
            if k == eng and eng == "pe":
                continue
            if self.seen[eng].get(k, 0) >= v:
                continue
            if best.get(k, 0) < v:
                best[k] = v
        for k, v in best.items():
            self.seen[eng][k] = v
        return list(best.items())

    @staticmethod
    def _deps(reads, writes):
        deps = []
        for r in reads:
            deps.append(r.w)
        for w in writes:
            deps.append(w.w)
            deps.extend(w.r)
        return deps

    @staticmethod
    def _commit(reads, writes, tok):
        for r in reads:
            r.r.append(tok)
            if len(r.r) > 16:
                m = {}
                for k, v in r.r:
                    if m.get(k, 0) < v:
                        m[k] = v
                r.r = list(m.items())
        for w in writes:
            w.w = tok
            w.r = []

    def op(self, eng, fn, reads=(), writes=(), signal=True):
        waits = self._need(eng, self._deps(reads, writes))
        if signal:
            self.cnt[eng] += 1
            tok = (eng, self.cnt[eng])
        else:
            tok = (eng, self.cnt[eng] + 1)
        self.streams[eng].append((waits, fn, signal, None))
        self._commit(reads, writes, tok)
        self.n_ins += 1 + len(waits)
        return tok

    def dma(self, q, fn, reads=(), writes=()):
        lst = self.dma_sems[q]
        k = lst[self.dma_rr[q] % len(lst)]
        self.dma_rr[q] += 1
        deps = self._deps(reads, writes)
        deps.append((k, self.dma_cnt[k]))
        waits = self._need(q, deps)
        self.dma_cnt[k] += 16
        tok = (k, self.dma_cnt[k])
        self.streams[q].append((waits, fn, False, k))
        self._commit(reads, writes, tok)
        self.n_ins += 1 + len(waits)
        return tok

    def barrier(self):
        final = []
        for e in ENGS:
            if self.cnt[e]:
                final.append((e, self.cnt[e]))
        for k, v in self.dma_cnt.items():
            if v:
                final.append((k, v))
        for e in ENGS:
            waits = self._need(e, [f for f in final if not (f[0] == e and e == "pe")])
            if waits:
                self.streams[e].append((waits, None, False, None))

    def emit(self):
        nc = self.nc
        sems = self.sems
        streams = self.streams

        def run(engname, engobj):
            for waits, fn, signal, dsem in streams[engname]:
                fold = None
                if fn is not None and dsem is None and waits:
                    fold = waits[-1]
                    waits = waits[:-1]
                for k, v in waits:
                    engobj.wait_ge(sems[k], v)
                if fn is None:
                    continue
                ins = fn(engobj)
                if fold is not None:
                    ins._wait_ge(sems[fold[0]], fold[1])
                if dsem is not None:
                    ins.then_inc(sems[dsem], 16)
                elif signal:
                    ins.then_inc(sems[engname], 1)

        with nc.Block() as block:
            @block.tensor
            def _(e):
                run("pe", e)

            @block.vector
            def _(e):
                run("dve", e)

            @block.scalar
            def _(e):
                run("act", e)

            @block.gpsimd
            def _(e):
                run("pool", e)

            @block.sync
            def _(e):
                run("sp", e)
        self.streams = {e: [] for e in ENGS}


def _na_bias_T(rpb):
    H = rpb.shape[0]
    out = np.full((H, 5, 640, 128), NEG, np.float32)
    pair_of_type = [0, 1, 5, 14, 15]
    for ty, j in enumerate(pair_of_type):
        ws = int(np.clip(2 * j - 4, 0, 23))
        a = ws // 2
        krow0 = 2 * a
        for rr in range(2):
            r = 2 * j + rr
            r0 = int(np.clip(r - 4, 0, 24))
            for i in range(10):
                kr = krow0 + i
                if not (r0 <= kr < r0 + 8):
                    continue
                dr = kr - r + 7
                c = np.arange(64)
                c0 = np.clip(c - 8, 0, 48)
                for cq in range(64):
                    kc = np.arange(c0[cq], c0[cq] + 16)
                    dc = kc - cq + 15
                    out[:, ty, i * 64 + kc, rr * 64 + cq] = rpb[:, dr, dc]
    return np.ascontiguousarray(out.reshape(H, 5, 5, 128, 128))


def _pcol(v):
    v = np.asarray(v, np.float32)
    return np.ascontiguousarray(v.reshape(-1, 128).T)


_GATE_INPUTS = ("od_fwd_wa", "od_fwd_ba", "od_fwd_wx", "od_fwd_bx", "od_fwd_lam",
                "od_bwd_wa", "od_bwd_ba", "od_bwd_wx", "od_bwd_bx", "od_bwd_lam")


def _prep_shared(inp):
    sh = {}
    for nm in _GATE_INPUTS:
        assert nm in inp
    sh["ada_w"] = np.ascontiguousarray(inp["ada_w"], np.float32)
    sh["ada_b"] = np.ascontiguousarray(inp["ada_b"], np.float32)
    sh["norm_g"] = np.ascontiguousarray(np.stack([inp["norm1_g"], inp["norm2_g"]], 1), np.float32)
    sh["ev_w_in"] = np.ascontiguousarray(inp["ev_w_in"][0], np.float32)
    sh["ev_w_out"] = np.ascontiguousarray(inp["ev_w_out"][0], np.float32)
    sh["biasT"] = _na_bias_T(np.asarray(inp["ev_rpb"][0], np.float32))
    sh["od_w_in"] = np.ascontiguousarray(inp["od_w_in"][0], np.float32)
    sh["od_w_out"] = np.ascontiguousarray(inp["od_w_out"][0], np.float32)
    gates = []
    for dr in ("fwd", "bwd"):
        for nm in ("wa", "wx"):
            gates.append(np.asarray(inp["od_%s_%s" % (dr, nm)][0], np.float32))
    sh["od_gw"] = np.ascontiguousarray(np.stack(gates, 0))
    cols = []
    qg = np.tile(np.asarray(inp["ev_q_gain"][0], np.float32), 2)
    kg = np.tile(np.asarray(inp["ev_k_gain"][0], np.float32), 2)
    cols.append(qg[:, None]); cols.append(kg[:, None])
    for j in range(3):
        cols.append(_pcol(inp["ev_conv_w"][0, j]))
    cols.append(_pcol(inp["ev_conv_b"][0]))
    for j in range(4):
        cols.append(_pcol(inp["od_conv_w"][0, j]))
    cols.append(_pcol(inp["od_conv_b"][0]))
    for dr in ("fwd", "bwd"):
        for nm in ("ba", "bx", "lam"):
            cols.append(_pcol(inp["od_%s_%s" % (dr, nm)][0]))
    sh["pcols"] = np.ascontiguousarray(np.concatenate(cols, 1), np.float32)
    sh["router_w"] = np.ascontiguousarray(inp["router_w"], np.float32)
    sh["router_b"] = np.ascontiguousarray(inp["router_b"], np.float32)
    for nm in ("gate", "up", "down"):
        w = np.asarray(inp["exp_w_" + nm], np.float32)
        w = w.reshape(2, NE, 8, 128, 1024).transpose(0, 1, 3, 2, 4).reshape(2, NE * 128, 8 * 1024)
        for l in range(2):
            sh["w_%s%d" % (nm, l)] = np.ascontiguousarray(w[l])
    eb = np.concatenate([inp["exp_b_gate"], inp["exp_b_up"], inp["exp_b_down"]], -1).astype(np.float32)
    for l in range(2):
        sh["exp_b%d" % l] = np.ascontiguousarray(eb[l])
        bgc = np.asarray(inp["exp_b_gate"][l], np.float32).reshape(NE, 8, 128).transpose(0, 2, 1)
        buc = np.asarray(inp["exp_b_up"][l], np.float32).reshape(NE, 8, 128).transpose(0, 2, 1)
        sh["exp_bc%d" % l] = np.ascontiguousarray(np.concatenate([bgc, buc], -1).reshape(NE * 128, 16))
    sh["iota_p"] = np.arange(128, dtype=np.float32)[:, None].copy()
    sh["blk_start"] = np.tile((np.arange(80, dtype=np.float32) * BLK)[None], (128, 1)).copy()
    return sh


PC_QG, PC_KG, PC_ECW, PC_ECB, PC_OCW, PC_OCB, PC_G = 0, 1, 2, 14, 18, 50, 58


def build_program(NB=2, layers=(0, 1), do_moe=True, dbg=False, n_exp=NE, moe_stop=4):
    nc = bass.Bass("TRN2", target_bir_lowering=False)
    NTOK0 = NB * NT
    dt = nc.dram_tensor

    def din(name, shape, dtp=F32):
        return dt(name, list(shape), dtp, kind="ExternalInput").ap()

    xin = din("xin", [NB, NT, D])
    cvec = din("cvec", [NB + 1, D])
    ada_w = din("ada_w", [2, D, 6 * D])
    ada_b = din("ada_b", [2, 6 * D])
    norm_g = din("norm_g", [2, 2, D])
    ev_w_in = din("ev_w_in", [D, 3072])
    ev_w_out = din("ev_w_out", [D, D])
    biasT = din("biasT", [8, 5, 5, 128, 128])
    od_w_in = din("od_w_in", [D, 2048])
    od_w_out = din("od_w_out", [D, D])
    od_gw = din("od_gw", [4, 4, 256, 256])
    pcols_d = din("pcols", [128, 106])
    router_w = din("router_w", [2, D, NE])
    router_b = din("router_b", [2, NE])
    w_gate = [din("w_gate%d" % l, [n_exp * 128, 8192]) for l in range(2)]
    w_up = [din("w_up%d" % l, [n_exp * 128, 8192]) for l in range(2)]
    w_down = [din("w_down%d" % l, [n_exp * 128, 8192]) for l in range(2)]
    exp_b = [din("exp_b%d" % l, [n_exp, 3072]) for l in range(2)]
    exp_bc = [din("exp_bc%d" % l, [n_exp * 128, 16]) for l in range(2)]
    iota_p_d = din("iota_p", [128, 1])
    blk_start_d = din("blk_start", [128, 80])
    out = dt("out", [NB, T, D], F32, kind="ExternalOutput").ap()
    if dbg:
        dbg_d4 = dt("dbg_d4", [128, NB * NTI * 4], I32, kind="ExternalOutput").ap()
        dbg_g4 = dt("dbg_g4", [128, NB * NTI * 4], F32, kind="ExternalOutput").ap()
        dbg_iw = dt("dbg_iw", [128, 80], I32, kind="ExternalOutput").ap()

    NBLK0 = NTOK0 * 4 // BLK + n_exp
    XR = dt("XR", [NB, NT, D], F32).ap()
    MOD = dt("MODs", [2, NB + 1, 6 * D], F32).ap()
    XS = dt("XS", [NBLK0 * BLK, D], BF16).ap()
    YS = dt("YS", [NBLK0 * BLK, D], BF16).ap()
    R_MOD, R_XS, R_YS, R_OUT = Res("MOD"), Res("XS"), Res("YS"), Res("OUT")
    R_XRT = {(b_, i_): Res() for b_ in range(NB) for i_ in range(NTI)}
    R_XIN = {(b_, i_): Res() for b_ in range(NB) for i_ in range(NTI)}

    with contextlib.ExitStack() as g:
        S = Sched(nc, g)

        uid = [0]

        def sbuf(st, name, shape, dtp):
            uid[0] += 1
            return st.enter_context(nc.sbuf_tensor("%s_%d" % (name, uid[0]), list(shape), dtp))

        def psum(st, name, shape, dtp=F32):
            uid[0] += 1
            return st.enter_context(nc.psum_tensor("%s_%d" % (name, uid[0]), list(shape), dtp))

        ident = sbuf(g, "ident", [128, 128], F32)
        identb = sbuf(g, "identb", [128, 128], BF16)
        onesb = sbuf(g, "onesb", [128, 512], BF16)
        halfb = sbuf(g, "halfb", [2, 512], BF16)
        pcols = sbuf(g, "pcols_sb", [128, 106], F32)
        R_c = Res("const")
        S.op("dve", lambda e: e.memset(ident[:], 0.0), writes=[R_c])
        S.op("pool", lambda e: e.affine_select(out=ident[:], in_=ident[:], pattern=[[-1, 128]], compare_op=ALU.not_equal,
                                               fill=1.0, base=0, channel_multiplier=1), reads=[R_c], writes=[R_c])
        S.op("dve", lambda e: e.tensor_copy(out=identb[:], in_=ident[:]), reads=[R_c], writes=[R_c])
        S.op("dve", lambda e: e.memset(onesb[:], 1.0), writes=[R_c])
        S.op("dve", lambda e: e.memset(halfb[:], 0.5), writes=[R_c])
        S.dma("sp", lambda e: e.dma_start(out=pcols[:], in_=pcols_d[:, :]), writes=[R_c])
        S.barrier()
        S.emit()

        NV = NB + 1
        with contextlib.ExitStack() as st:
            cs = sbuf(st, "cs", [NV, D], F32)
            sT = sbuf(st, "sT", [128, 8, NV], F32)
            aw = [sbuf(st, "aw%d" % i, [128, 8, 512], F32) for i in range(2)]
            R_aw = [Res(), Res()]
            ab = sbuf(st, "ab", [NV, 6 * D], F32)
            msb = sbuf(st, "msb", [NV, 6 * D], F32)
            pT = psum(st, "pT", [128, 8, NV], F32)
            pm = [psum(st, "pm%d" % i, [NV, 512], F32) for i in range(2)]
            R_pm = [Res(), Res()]
            R_cs, R_sT, R_ab, R_msb, R_pT = Res(), Res(), Res(), Res(), Res()
            S.dma("sp", lambda e: e.dma_start(out=cs[:], in_=cvec[:, :]), writes=[R_cs])
            S.op("act", lambda e: e.activation(out=cs[:], in_=cs[:], func=AF.Silu), reads=[R_cs], writes=[R_cs])
            for c in range(8):
                S.op("pe", lambda e, c=c: e.transpose(out=pT[:, c, :], in_=cs[:, c * 128:(c + 1) * 128], identity=ident[0:NV, 0:NV]),
                     reads=[R_cs, R_c], writes=[R_pT], signal=(c == 7))
            S.op("dve", lambda e: e.tensor_copy(out=sT[:], in_=pT[:]), reads=[R_pT], writes=[R_sT])
            for l in range(2):
                for v in range(NV):
                    S.dma("sp", lambda e, l=l, v=v: e.dma_start(out=ab[v:v + 1, :], in_=ada_b[l:l + 1, :]), writes=[R_ab])
                for j in range(12):
                    k = j % 2
                    S.dma("sp", lambda e, l=l, j=j, k=k: e.dma_start(
                        out=aw[k][:], in_=ada_w[l, :, j * 512:(j + 1) * 512].rearrange("(c p) n -> p c n", p=128)),
                        writes=[R_aw[k]])
                    for c in range(8):
                        S.op("pe", lambda e, c=c, k=k: e.matmul(pm[k][:], lhsT=sT[:, c, :], rhs=aw[k][:, c, :],
                                                                start=(c == 0), stop=(c == 7)),
                             reads=[R_sT, R_aw[k]], writes=[R_pm[k]], signal=(c == 7))
                    S.op("dve", lambda e, j=j, k=k: e.tensor_tensor(out=msb[:, j * 512:(j + 1) * 512], in0=pm[k][:],
                                                                     in1=ab[:, j * 512:(j + 1) * 512], op=ALU.add),
                         reads=[R_pm[k], R_ab], writes=[R_msb])
                S.dma("sp", lambda e, l=l: e.dma_start(out=MOD[l, :, :], in_=msb[:]), reads=[R_msb], writes=[R_MOD])
            S.barrier()
            S.emit()

        def load_bcast(st, name, src_ap, reads, n=D):
            t = sbuf(st, name, [128, n], F32)
            r = Res(name)
            S.dma("sp", lambda e: e.dma_start(out=t[:], in_=src_ap.partition_broadcast(128)), reads=reads, writes=[r])
            return t, r

        def norm_mod_tiles(st, l, which, vecs):
            res = {}
            gt, rg = load_bcast(st, "ng%d%d" % (l, which), norm_g[l, which, :], [])
            for v in vecs:
                sc, rsc = load_bcast(st, "sc%d" % v, MOD[l, v, (3 * which + 1) * D:(3 * which + 2) * D], [R_MOD])
                shh, rsh = load_bcast(st, "sh%d" % v, MOD[l, v, (3 * which) * D:(3 * which + 1) * D], [R_MOD])
                S.op("dve", lambda e, sc=sc: e.scalar_tensor_tensor(out=sc[:], in0=sc[:], scalar=1.0, in1=gt[:],
                                                                      op0=ALU.add, op1=ALU.mult),
                     reads=[rsc, rg], writes=[rsc])
                res[v] = (sc, rsc, shh, rsh)
            return res

        def rms_mod(st_tmp, xt, r_xt, G, rG, SH, rSH, ht, r_ht, small, r_small, junk, r_junk):
            S.op("dve", lambda e: e.memset(small[:, 0:1], 0.0), reads=[r_small], writes=[r_small])
            S.op("act", lambda e: e.activation(out=junk[:], in_=xt[:], func=AF.Square, accum_out=small[:, 0:1]),
                 reads=[r_xt, r_small], writes=[r_junk, r_small])
            S.op("act", lambda e: e.activation(out=small[:, 1:2], in_=small[:, 0:1], func=AF.Sqrt, scale=1.0 / D, bias=small[:, 3:4]),
                 reads=[r_small], writes=[r_small])
            S.op("dve", lambda e: e.reciprocal(out=small[:, 2:3], in_=small[:, 1:2]), reads=[r_small], writes=[r_small])
            S.op("dve", lambda e: e.scalar_tensor_tensor(out=ht[:], in0=xt[:], scalar=small[:, 2:3], in1=G[:],
                                                         op0=ALU.mult, op1=ALU.mult),
                 reads=[r_xt, r_small, rG], writes=[r_ht])
            S.op("dve", lambda e: e.tensor_tensor(out=ht[:], in0=ht[:], in1=SH[:], op=ALU.add),
                 reads=[r_ht, rSH], writes=[r_ht])

        def new_small(st, name):
            small = sbuf(st, name, [128, 4], F32)
            r = Res(name)
            S.op("dve", lambda e: e.memset(small[:], 0.0), writes=[r])
            S.op("dve", lambda e: e.memset(small[:, 3:4], EPS), reads=[r], writes=[r])
            return small, r

        def norm_to_hT(st, l, b, src_ap, src_res, hT, r_hT, vec_of_tile):
            with contextlib.ExitStack() as s2:
                vecs = sorted(set(vec_of_tile))
                tiles = norm_mod_tiles(s2, l, 0, vecs)
                small, r_small = zip(*[new_small(s2, "n1small%d" % i) for i in range(2)])
                junk = [sbuf(s2, "n1junk%d" % i, [128, D], F32) for i in range(2)]; r_junk = [Res(), Res()]
                xt = [sbuf(s2, "n1x%d" % i, [128, D], F32) for i in range(2)]
                r_xt = [Res(), Res()]
                ht = [sbuf(s2, "n1h%d" % i, [128, D], F32) for i in range(2)]
                r_ht = [Res(), Res()]
                ptp = [psum(s2, "n1p%d" % i, [128, 4, 128], F32) for i in range(2)]
                r_ptp = [Res(), Res()]
                def stage_a(i):
                    k = i % 2
                    G, rG, SH, rSH = tiles[vec_of_tile[i]]
                    S.dma("sp", lambda e: e.dma_start(out=xt[k][:], in_=src_ap[b, i * 128:(i + 1) * 128, :]),
                          reads=[src_res[(b, i)]], writes=[r_xt[k]])
                    rms_mod(s2, xt[k], r_xt[k], G, rG, SH, rSH, ht[k], r_ht[k], small[k], r_small[k], junk[k], r_junk[k])

                def stage_b(i):
                    k = i % 2
                    for hf in range(2):
                        for c4 in range(4):
                            c = hf * 4 + c4
                            S.op("pe", lambda e, hf=hf, c4=c4, c=c: e.transpose(
                                out=ptp[hf][:, c4, :], in_=ht[k][:, c * 128:(c + 1) * 128], identity=ident[:]),
                                reads=[r_ht[k], R_c], writes=[r_ptp[hf]], signal=(c4 == 3))
                        if (hf + i) % 2 == 0:
                            S.op("act", lambda e, hf=hf: e.activation(
                                out=hT[:, hf * 4:(hf + 1) * 4, i * 128:(i + 1) * 128], in_=ptp[hf][:], func=AF.Copy),
                                reads=[r_ptp[hf]], writes=[r_hT])
                        else:
                            S.op("dve", lambda e, hf=hf: e.tensor_copy(
                                out=hT[:, hf * 4:(hf + 1) * 4, i * 128:(i + 1) * 128], in_=ptp[hf][:]),
                                reads=[r_ptp[hf]], writes=[r_hT])

                stage_a(0)
                for i in range(NTI):
                    if i + 1 < NTI:
                        stage_a(i + 1)
                    stage_b(i)
                S.barrier()
                S.emit()

        def stream_w(st, name, n=2):
            bufs = [sbuf(st, "%s%d" % (name, i), [128, 8, 512], BF16) for i in range(n)]
            return bufs, [Res() for _ in range(n)]

        def load_w(buf, r, w_ap, col0, ncol=512):
            S.dma("pool", lambda e: e.dma_start(out=buf[:, :, 0:ncol],
                                                 in_=w_ap[:, col0:col0 + ncol].rearrange("(c p) n -> p c n", p=128)),
                  writes=[r])

        TOKP = [(i * 512, min(512, NT - i * 512)) for i in range((NT + 511) // 512)]

        def outproj_residual(st, l, b, srcs, w_ap, x_src, x_res, tiles_range, vec_of_tile, x_tok_off=0):
            with contextlib.ExitStack() as s2:
                wo = sbuf(s2, "wo", [128, 8, D], BF16); r_wo = Res()
                for hh in range(2):
                    S.dma("pool", lambda e, hh=hh: e.dma_start(
                        out=wo[:, :, hh * 512:(hh + 1) * 512],
                        in_=w_ap[:, hh * 512:(hh + 1) * 512].rearrange("(c p) n -> p c n", p=128)), writes=[r_wo])
                g1 = {}
                for v in sorted(set(vec_of_tile[i] for i in tiles_range)):
                    g1[v] = load_bcast(s2, "g1_%d" % v, MOD[l, v, 2 * D:3 * D], [R_MOD])
                xt = [sbuf(s2, "opx%d" % i, [128, D], F32) for i in range(2)]
                r_xt = [Res(), Res()]
                py = [psum(s2, "opy%d" % i, [128, 512], F32) for i in range(4)]
                r_py = [Res() for _ in range(4)]
                for n_i, i in enumerate(tiles_range):
                    k = n_i % 2
                    gt, rgt = g1[vec_of_tile[i]]
                    S.dma("sp", lambda e, i=i, k=k: e.dma_start(out=xt[k][:], in_=x_src[b, i * 128:(i + 1) * 128, :]),
                          reads=[x_res[(b, i)]], writes=[r_xt[k]])
                    for hh in range(2):
                        pk = k * 2 + hh
                        for c in range(8):
                            tsr, ch, rs, toff = srcs[c]
                            S.op("pe", lambda e, tsr=tsr, ch=ch, toff=toff, i=i, c=c, hh=hh, pk=pk: e.matmul(
                                py[pk][:], lhsT=tsr[:, ch, i * 128 - toff:(i + 1) * 128 - toff],
                                rhs=wo[:, c, hh * 512:(hh + 1) * 512], start=(c == 0), stop=(c == 7)),
                                reads=[rs, r_wo], writes=[r_py[pk]], signal=(c == 7))
                        S.op("dve", lambda e, hh=hh, pk=pk, gt=gt, k=k: e.tensor_tensor(
                            out=xg[k][:, hh * 512:(hh + 1) * 512],
                            in0=py[pk][:], in1=gt[:, hh * 512:(hh + 1) * 512], op=ALU.mult),
                            reads=[r_py[pk], rgt], writes=[r_xg[k]])
                    S.op("pool", lambda e, k=k: e.tensor_tensor(out=xt[k][:], in0=xt[k][:], in1=xg[k][:], op=ALU.add),
                         reads=[r_xg[k], r_xt[k]], writes=[r_xt[k]])
                    S.dma("sp", lambda e, i=i, k=k: e.dma_start(out=XR[b, i * 128:(i + 1) * 128, :], in_=xt[k][:]),
                          reads=[r_xt[k]], writes=[R_XRT[(b, i)]])
                S.barrier()
                S.emit()

        xg = [sbuf(g, "xg%d" % i, [128, D], F32) for i in range(2)]
        r_xg = [Res(), Res()]

        VEC_OF_TILE = lambda b: [NB, NB] + [b] * 16

        def layer0_mixer(b):
            with contextlib.ExitStack() as st:
                qT = sbuf(st, "qT", [128, 4, NT], BF16); r_qT = Res()
                kT = sbuf(st, "kT", [128, 4, NT], BF16); r_kT = Res()
                V = sbuf(st, "V", [128, NTI, 512], BF16); r_V = Res()
                OB = sbuf(st, "OB", [128, 4, NT], BF16); r_OB = Res()
                with contextlib.ExitStack() as s1:
                    hT = sbuf(s1, "hT", [128, 8, NT], BF16); r_hT = Res()
                    norm_to_hT(s1, 0, b, xin, R_XIN, hT, r_hT, VEC_OF_TILE(b))
                    wb, r_wb = stream_w(s1, "wi")
                    blk1 = sbuf(s1, "blk1", [128, 128], BF16); r_blk = Res()
                    S.op("dve", lambda e: e.memset(blk1[:], 0.0), writes=[r_blk])
                    S.op("dve", lambda e: e.memset(blk1[0:64, 0:64], 1.0 / 64), reads=[r_blk], writes=[r_blk])
                    S.op("dve", lambda e: e.memset(blk1[64:128, 64:128], 1.0 / 64), reads=[r_blk], writes=[r_blk])
                    pj = [psum(s1, "pj%d" % i, [128, 512], F32) for i in range(3)]
                    r_pj = [Res() for _ in range(3)]
                    pq = [psum(s1, "pq%d" % i, [128, 512], F32) for i in range(2)]
                    r_pq = [Res() for _ in range(2)]
                    sq = [sbuf(s1, "sq%d" % i, [128, 512], BF16) for i in range(2)]
                    r_sq = [Res(), Res()]
                    rs_t = [sbuf(s1, "rst%d" % i, [128, 512], F32) for i in range(2)]
                    r_rs = [Res(), Res()]
                    eps64 = sbuf(s1, "eps64", [128, 2], F32); r_e64 = Res()
                    S.op("dve", lambda e: e.memset(eps64[:, 0:1], EPS), writes=[r_e64])
                    S.op("dve", lambda e: e.memset(eps64[:, 1:2], 64.0 * EPS), reads=[r_e64], writes=[r_e64])
                    cnt = [0]

                    def proj_piece(wbuf, r_w, wc, tp):
                        t0, nt_ = TOKP[tp]
                        k = cnt[0] % 3
                        cnt[0] += 1
                        for c in range(8):
                            S.op("pe", lambda e, c=c, k=k: e.matmul(pj[k][:, 0:nt_], lhsT=wbuf[:, c, wc * 128:(wc + 1) * 128],
                                                                   rhs=hT[:, c, t0:t0 + nt_], start=(c == 0), stop=(c == 7)),
                                 reads=[r_w, r_hT], writes=[r_pj[k]], signal=(c == 7))
                        return k, t0, nt_

                    for grp, (dst, r_dst, gcol, sc_, ecol) in enumerate(((qT, r_qT, PC_QG, 64.0, 1), (kT, r_kT, PC_KG, 1.0, 0))):
                        bi = grp % 2
                        load_w(wb[bi], r_wb[bi], ev_w_in, grp * 512)
                        for wc in range(4):
                            for tp in range(len(TOKP)):
                                k, t0, nt_ = proj_piece(wb[bi], r_wb[bi], wc, tp)
                                k2 = cnt[0] % 2
                                S.op("act", lambda e, k=k, k2=k2, nt_=nt_: e.activation(out=sq[k2][:, 0:nt_], in_=pj[k][:, 0:nt_], func=AF.Square),
                                     reads=[r_pj[k]], writes=[r_sq[k2]])
                                S.op("pe", lambda e, k2=k2, nt_=nt_: e.matmul(pq[k2][:, 0:nt_], lhsT=blk1[:], rhs=sq[k2][:, 0:nt_], start=True, stop=True),
                                     reads=[r_blk, r_sq[k2]], writes=[r_pq[k2]])
                                S.op("act", lambda e, k2=k2, nt_=nt_, sc_=sc_, ecol=ecol: e.activation(
                                    out=rs_t[k2][:, 0:nt_], in_=pq[k2][:, 0:nt_], func=AF.Sqrt, scale=sc_, bias=eps64[:, ecol:ecol + 1]),
                                    reads=[r_pq[k2], r_e64], writes=[r_rs[k2]])
                                S.op("dve", lambda e, k2=k2, nt_=nt_: e.reciprocal(out=rs_t[k2][:, 0:nt_], in_=rs_t[k2][:, 0:nt_]),
                                     reads=[r_rs[k2]], writes=[r_rs[k2]])
                                S.op("dve", lambda e, k=k, k2=k2, nt_=nt_, t0=t0, wc=wc, dst=dst, gcol=gcol: e.scalar_tensor_tensor(
                                    out=dst[:, wc, t0:t0 + nt_], in0=pj[k][:, 0:nt_], scalar=pcols[:, gcol:gcol + 1], in1=rs_t[k2][:, 0:nt_],
                                    op0=ALU.mult, op1=ALU.mult), reads=[r_pj[k], r_rs[k2], R_c], writes=[r_dst])
                    load_w(wb[0], r_wb[0], ev_w_in, 1024)
                    for i in range(NTI):
                        k = cnt[0] % 3
                        cnt[0] += 1
                        for c in range(8):
                            S.op("pe", lambda e, c=c, k=k, i=i: e.matmul(pj[k][:], lhsT=hT[:, c, i * 128:(i + 1) * 128], rhs=wb[0][:, c, :],
                                                                        start=(c == 0), stop=(c == 7)),
                                 reads=[r_wb[0], r_hT], writes=[r_pj[k]], signal=(c == 7))
                        S.op("act", lambda e, k=k, i=i: e.activation(out=V[:, i, :], in_=pj[k][:], func=AF.Copy),
                             reads=[r_pj[k]], writes=[r_V])
                    load_w(wb[1], r_wb[1], ev_w_in, 2560)
                    wcg = sbuf(s1, "wcg", [128, 8, 512], BF16); r_wcg = Res()
                    load_w(wcg, r_wcg, ev_w_in, 2048)
                    load_w(wb[0], r_wb[0], ev_w_in, 1536)
                    xs_f = sbuf(s1, "xs_f", [128, NT], F32); r_xs = Res()
                    cx_f = sbuf(s1, "cx_f", [128, NT], F32); r_cx = Res()
                    t_f = sbuf(s1, "t_f", [128, NT], F32); r_t = Res()
                    for f in range(4):
                        for tp in range(len(TOKP)):
                            k, t0, nt_ = proj_piece(wb[1], r_wb[1], f, tp)
                            S.op("act", lambda e, k=k, t0=t0, nt_=nt_: e.activation(out=xs_f[:, t0:t0 + nt_], in_=pj[k][:, 0:nt_], func=AF.Copy),
                                 reads=[r_pj[k]], writes=[r_xs])
                        for tp in range(len(TOKP)):
                            k, t0, nt_ = proj_piece(wcg, r_wcg, f, tp)
                            S.op("dve", lambda e, k=k, t0=t0, nt_=nt_: e.tensor_tensor(out=cx_f[:, t0:t0 + nt_], in0=pj[k][:, 0:nt_],
                                                                                      in1=xs_f[:, t0:t0 + nt_], op=ALU.mult),
                                 reads=[r_pj[k], r_xs], writes=[r_cx])
                        w0 = pcols[:, PC_ECW + 0 * 4 + f:PC_ECW + 0 * 4 + f + 1]
                        w1 = pcols[:, PC_ECW + 1 * 4 + f:PC_ECW + 1 * 4 + f + 1]
                        w2 = pcols[:, PC_ECW + 2 * 4 + f:PC_ECW + 2 * 4 + f + 1]
                        bb = pcols[:, PC_ECB + f:PC_ECB + f + 1]
                        S.op("dve", lambda e, w1=w1, bb=bb: e.tensor_scalar(out=t_f[:], in0=cx_f[:], scalar1=w1, scalar2=bb, op0=ALU.mult, op1=ALU.add),
                             reads=[r_cx, R_c], writes=[r_t])
                        for (s0, sn) in ((0, LC), (LC, T)):
                            S.op("dve", lambda e, s0=s0, sn=sn, w0=w0: e.scalar_tensor_tensor(
                                out=t_f[:, s0 + 1:s0 + sn], in0=cx_f[:, s0:s0 + sn - 1], scalar=w0, in1=t_f[:, s0 + 1:s0 + sn],
                                op0=ALU.mult, op1=ALU.add), reads=[r_cx, r_t, R_c], writes=[r_t])
                            S.op("dve", lambda e, s0=s0, sn=sn, w2=w2: e.scalar_tensor_tensor(
                                out=t_f[:, s0:s0 + sn - 1], in0=cx_f[:, s0 + 1:s0 + sn], scalar=w2, in1=t_f[:, s0:s0 + sn - 1],
                                op0=ALU.mult, op1=ALU.add), reads=[r_cx, r_t, R_c], writes=[r_t])
                        for tp in range(len(TOKP)):
                            k, t0, nt_ = proj_piece(wb[0], r_wb[0], f, tp)
                            S.op("dve", lambda e, k=k, t0=t0, nt_=nt_, f=f: e.tensor_tensor(out=OB[:, f, t0:t0 + nt_], in0=pj[k][:, 0:nt_],
                                                                                           in1=t_f[:, t0:t0 + nt_], op=ALU.mult),
                                 reads=[r_pj[k], r_t], writes=[r_OB])
                    S.barrier()
                    S.emit()
                OA = sbuf(st, "OA", [128, 4, NT], BF16); r_OA = Res()
                with contextlib.ExitStack() as s1:
                    bias = [sbuf(s1, "bias%d" % i, [128, 5, 5, 128], BF16) for i in range(2)]
                    r_bias = [Res(), Res()]
                    sta = [psum(s1, "sta%d" % i, [128, 4, 128], F32) for i in range(2)]
                    stb = [psum(s1, "stb%d" % i, [128, 4, 128], F32) for i in range(2)]
                    r_sta = [Res(), Res()]; r_stb = [Res(), Res()]
                    po = [psum(s1, "po%d" % i, [64, 2, 128], F32) for i in range(2)]
                    r_po = [Res(), Res()]
                    PT = [sbuf(s1, "PT%d" % i, [128, 7, 128], BF16) for i in range(2)]
                    r_PT = [Res(), Res()]
                    rc = [sbuf(s1, "rc%d" % i, [64, 128], F32) for i in range(2)]
                    r_rc = [Res(), Res()]
                    it = 0
                    for h in range(8):
                        hp, hc = h % 2, h // 2
                        bsel = h % 2
                        S.dma("pool", lambda e, h=h, bsel=bsel: e.dma_start(out=bias[bsel][:], in_=biasT[h].rearrange("t c p q -> p t c q")),
                              writes=[r_bias[bsel]])
                        jobs = [("ctx", 0), ("ctx", 1)] + [("lat", j) for j in range(16)]
                        for kind, j in jobs:
                            k = it % 2
                            it += 1
                            if kind == "ctx":
                                qtile = j
                                ktiles = [0, 1]
                                ty = None
                            else:
                                qtile = 2 + j
                                ws = min(max(2 * j - 4, 0), 23)
                                a = ws // 2
                                ktiles = [0, 1] + [2 + a + cc for cc in range(5)]
                                ty = {0: 0, 1: 1, 14: 3, 15: 4}.get(j, 2)
                            nk = len(ktiles)
                            q_ap = qT[hp * 64:(hp + 1) * 64, hc, qtile * 128:(qtile + 1) * 128]
                            for kc, kt in enumerate(ktiles):
                                dstp, r_dst = (sta[k], r_sta[k]) if kc < 4 else (stb[k], r_stb[k])
                                has_b = (ty is not None and kc >= 2)
                                last = (kc == min(3, nk - 1)) or (kc == nk - 1)
                                S.op("pe", lambda e, dstp=dstp, kc=kc, kt=kt, q_ap=q_ap, has_b=has_b, hp=hp, hc=hc: e.matmul(
                                    dstp[:, kc % 4, :], lhsT=kT[hp * 64:(hp + 1) * 64, hc, kt * 128:(kt + 1) * 128], rhs=q_ap,
                                    start=True, stop=(not has_b)), reads=[r_kT, r_qT], writes=[r_dst], signal=(last and not has_b))
                                if has_b:
                                    S.op("pe", lambda e, dstp=dstp, kc=kc, ty=ty, bsel=bsel: e.matmul(
                                        dstp[:, kc % 4, :], lhsT=identb[:], rhs=bias[bsel][:, ty, kc - 2, :], start=False, stop=True),
                                        reads=[r_bias[bsel], R_c], writes=[r_dst], signal=last)
                            na = min(4, nk)
                            S.op("act", lambda e, k=k, na=na: e.activation(out=PT[k][:, 0:na, :], in_=sta[k][:, 0:na, :], func=AF.Exp),
                                 reads=[r_sta[k]], writes=[r_PT[k]])
                            if nk > 4:
                                S.op("act", lambda e, k=k, nk=nk: e.activation(out=PT[k][:, 4:nk, :], in_=stb[k][:, 0:nk - 4, :], func=AF.Exp),
                                     reads=[r_stb[k]], writes=[r_PT[k]])
                            for kc, kt in enumerate(ktiles):
                                S.op("pe", lambda e, k=k, kc=kc, kt=kt, nk=nk, h=h: e.matmul(
                                    po[k][:, 0, :], lhsT=V[:, kt, h * 64:(h + 1) * 64], rhs=PT[k][:, kc, :], start=(kc == 0), stop=(kc == nk - 1)),
                                    reads=[r_V, r_PT[k]], writes=[r_po[k]], signal=False)
                            for kc, kt in enumerate(ktiles):
                                S.op("pe", lambda e, k=k, kc=kc, nk=nk: e.matmul(
                                    po[k][:, 1, :], lhsT=onesb[:, 0:64], rhs=PT[k][:, kc, :], start=(kc == 0), stop=(kc == nk - 1)),
                                    reads=[R_c, r_PT[k]], writes=[r_po[k]], signal=(kc == nk - 1))
                            S.op("dve", lambda e, k=k: e.reciprocal(out=rc[k][:], in_=po[k][:, 1, :]), reads=[r_po[k]], writes=[r_rc[k]])
                            S.op("dve", lambda e, k=k, hp=hp, hc=hc, qtile=qtile: e.tensor_tensor(
                                out=OA[hp * 64:(hp + 1) * 64, hc, qtile * 128:(qtile + 1) * 128], in0=po[k][:, 0, :], in1=rc[k][:], op=ALU.mult),
                                reads=[r_po[k], r_rc[k]], writes=[r_OA])
                    S.barrier()
                    S.emit()
                srcs = [(OA, c, r_OA, 0) for c in range(4)] + [(OB, c, r_OB, 0) for c in range(4)]
                outproj_residual(st, 0, b, srcs, ev_w_out, xin, R_XIN, list(range(NTI)), VEC_OF_TILE(b))

        def layer1_mixer(b):
            with contextlib.ExitStack() as st:
                UC = sbuf(st, "UC", [128, 8, NT], BF16); r_UC = Res()
                GG = sbuf(st, "GG", [128, 8, T], BF16); r_GG = Res()
                with contextlib.ExitStack() as s1:
                    hT = sbuf(s1, "hT1", [128, 8, NT], BF16); r_hT = Res()
                    norm_to_hT(s1, 1, b, XR, R_XRT, hT, r_hT, VEC_OF_TILE(b))
                    wb, r_wb = stream_w(s1, "wi1")
                    pj = [psum(s1, "pj1%d" % i, [128, 512], F32) for i in range(3)]
                    r_pj = [Res() for _ in range(3)]
                    u_f = sbuf(s1, "u_f", [128, NT], F32); r_u = Res()
                    t_f = sbuf(s1, "t1_f", [128, NT], F32); r_t = Res()
                    cnt = [0]
                    for grp in range(4):
                        bi = grp % 2
                        load_w(wb[bi], r_wb[bi], od_w_in, grp * 512)
                        for wc in range(4):
                            f = (grp % 2) * 4 + wc
                            for tp in range(len(TOKP)):
                                t0, nt_ = TOKP[tp]
                                if grp < 2 and t0 + nt_ <= LC:
                                    continue
                                k = cnt[0] % 3
                                cnt[0] += 1
                                for c in range(8):
                                    S.op("pe", lambda e, c=c, k=k, bi=bi, wc=wc, t0=t0, nt_=nt_: e.matmul(
                                        pj[k][:, 0:nt_], lhsT=wb[bi][:, c, wc * 128:(wc + 1) * 128], rhs=hT[:, c, t0:t0 + nt_],
                                        start=(c == 0), stop=(c == 7)), reads=[r_wb[bi], r_hT], writes=[r_pj[k]], signal=(c == 7))
                                if grp < 2:
                                    lo = max(t0, LC)
                                    S.op("act", lambda e, k=k, f=f, lo=lo, t0=t0, nt_=nt_: e.activation(
                                        out=GG[:, f, lo - LC:t0 + nt_ - LC], in_=pj[k][:, lo - t0:nt_], func=AF.Gelu),
                                        reads=[r_pj[k]], writes=[r_GG])
                                else:
                                    S.op("act", lambda e, k=k, t0=t0, nt_=nt_: e.activation(out=u_f[:, t0:t0 + nt_], in_=pj[k][:, 0:nt_], func=AF.Copy),
                                         reads=[r_pj[k]], writes=[r_u])
                            if grp >= 2:
                                wj = [pcols[:, PC_OCW + j * 8 + f:PC_OCW + j * 8 + f + 1] for j in range(4)]
                                bb = pcols[:, PC_OCB + f:PC_OCB + f + 1]
                                S.op("dve", lambda e, wj=wj, bb=bb: e.tensor_scalar(out=t_f[:], in0=u_f[:], scalar1=wj[1], scalar2=bb, op0=ALU.mult, op1=ALU.add),
                                     reads=[r_u, R_c], writes=[r_t])
                                for (s0, sn) in ((0, LC), (LC, T)):
                                    S.op("dve", lambda e, s0=s0, sn=sn, wj=wj: e.scalar_tensor_tensor(
                                        out=t_f[:, s0 + 1:s0 + sn], in0=u_f[:, s0:s0 + sn - 1], scalar=wj[0], in1=t_f[:, s0 + 1:s0 + sn],
                                        op0=ALU.mult, op1=ALU.add), reads=[r_u, r_t, R_c], writes=[r_t])
                                    S.op("dve", lambda e, s0=s0, sn=sn, wj=wj: e.scalar_tensor_tensor(
                                        out=t_f[:, s0:s0 + sn - 1], in0=u_f[:, s0 + 1:s0 + sn], scalar=wj[2], in1=t_f[:, s0:s0 + sn - 1],
                                        op0=ALU.mult, op1=ALU.add), reads=[r_u, r_t, R_c], writes=[r_t])
                                    S.op("dve", lambda e, s0=s0, sn=sn, wj=wj: e.scalar_tensor_tensor(
                                        out=t_f[:, s0:s0 + sn - 2], in0=u_f[:, s0 + 2:s0 + sn], scalar=wj[3], in1=t_f[:, s0:s0 + sn - 2],
                                        op0=ALU.mult, op1=ALU.add), reads=[r_u, r_t, R_c], writes=[r_t])
                                S.op("act", lambda e, f=f: e.activation(out=UC[:, f, :], in_=t_f[:], func=AF.Copy), reads=[r_t], writes=[r_UC])
                    S.barrier()
                    S.emit()
                YI, r_YI = GG, r_GG
                with contextlib.ExitStack() as s1:
                    gw = sbuf(s1, "gw", [128, 4, 4, 2, 256], BF16); r_gw = Res()
                    for m in range(4):
                        S.dma("pool", lambda e, m=m: e.dma_start(out=gw[:, m], in_=od_gw[m].rearrange("b (k p) n -> p b k n", p=128)),
                              writes=[r_gw])
                    cl = sbuf(s1, "cl", [128, 2, 8], F32); r_cl = Res()
                    for d_ in range(2):
                        lam = pcols[:, PC_G + d_ * 24 + 16:PC_G + d_ * 24 + 24]
                        S.op("act", lambda e, d_=d_, lam=lam: e.activation(out=cl[:, d_, :], in_=lam, func=AF.Exp, scale=-1.0), reads=[R_c], writes=[r_cl])
                        S.op("act", lambda e, d_=d_: e.activation(out=cl[:, d_, :], in_=cl[:, d_, :], func=AF.Ln, bias=1.0, scale=1.0), reads=[r_cl], writes=[r_cl])
                        S.op("dve", lambda e, d_=d_: e.tensor_scalar(out=cl[:, d_, :], in0=cl[:, d_, :], scalar1=-8.0, scalar2=None, op0=ALU.mult),
                             reads=[r_cl], writes=[r_cl])
                    pg = [psum(s1, "pg%d" % i, [128, 512], F32) for i in range(4)]
                    r_pg = [Res() for _ in range(4)]
                    Rts = [sbuf(s1, "Rt%d" % i, [128, NT], F32) for i in range(2)]; r_Rs = [Res(), Res()]
                    Its = [sbuf(s1, "It%d" % i, [128, NT], F32) for i in range(2)]; r_Is = [Res(), Res()]
                    Ats = [sbuf(s1, "At%d" % i, [128, NT], F32) for i in range(2)]; r_As = [Res(), Res()]
                    Bts = [sbuf(s1, "Bt%d" % i, [128, NT], F32) for i in range(2)]; r_Bs = [Res(), Res()]
                    Hf = sbuf(s1, "Hf", [128, NT], F32); r_Hf = Res()
                    Hb = sbuf(s1, "Hb", [128, NT], F32); r_Hb = Res()
                    cnt = [0]
                    for f in range(8):
                        blk = f // 2
                        for d_ in range(2):
                            Rt, It, At, Bt = Rts[d_], Its[d_], Ats[d_], Bts[d_]
                            r_R, r_I, r_A, r_B = r_Rs[d_], r_Is[d_], r_As[d_], r_Bs[d_]
                            for gi, (dstt, r_dst) in enumerate(((Rt, r_R), (It, r_I))):
                                m = d_ * 2 + gi
                                bcol = pcols[:, PC_G + d_ * 24 + gi * 8 + f:PC_G + d_ * 24 + gi * 8 + f + 1]
                                for tp in range(len(TOKP)):
                                    t0, nt_ = TOKP[tp]
                                    k = cnt[0] % 4
                                    cnt[0] += 1
                                    for kc in range(2):
                                        S.op("pe", lambda e, k=k, m=m, kc=kc, t0=t0, nt_=nt_, blk=blk, f=f: e.matmul(
                                            pg[k][:, 0:nt_], lhsT=gw[:, m, blk, kc, (f % 2) * 128:(f % 2 + 1) * 128],
                                            rhs=UC[:, 2 * blk + kc, t0:t0 + nt_], start=(kc == 0), stop=(kc == 1)),
                                            reads=[r_gw, r_UC], writes=[r_pg[k]], signal=(kc == 1))
                                    S.op("act", lambda e, k=k, dstt=dstt, t0=t0, nt_=nt_, bcol=bcol: e.activation(
                                        out=dstt[:, t0:t0 + nt_], in_=pg[k][:, 0:nt_], func=AF.Sigmoid, bias=bcol, scale=1.0),
                                        reads=[r_pg[k], R_c], writes=[r_dst])
                            S.op("act", lambda e, d_=d_, f=f, At=At, Bt=Bt, Rt=Rt, It=It: e.activation(out=At[:], in_=Rt[:], func=AF.Exp, scale=cl[:, d_, f:f + 1]),
                                 reads=[r_R, r_cl], writes=[r_A])
                            S.op("pool", lambda e, At=At, Bt=Bt, Rt=Rt, It=It: e.tensor_tensor(out=Bt[:], in0=At[:], in1=At[:], op=ALU.mult), reads=[r_A], writes=[r_B])
                            S.op("act", lambda e, At=At, Bt=Bt, Rt=Rt, It=It: e.activation(out=Bt[:], in_=Bt[:], func=AF.Sqrt, scale=-1.0, bias=1.0), reads=[r_B], writes=[r_B])
                            S.op("dve", lambda e, At=At, Bt=Bt, Rt=Rt, It=It: e.tensor_tensor(out=Bt[:], in0=Bt[:], in1=It[:], op=ALU.mult), reads=[r_B, r_I], writes=[r_B])
                            S.op("dve", lambda e, f=f, At=At, Bt=Bt, Rt=Rt, It=It: e.tensor_tensor(out=Bt[:], in0=Bt[:], in1=UC[:, f, :], op=ALU.mult), reads=[r_B, r_UC], writes=[r_B])
                            if d_ == 0:
                                S.op("dve", lambda e, At=At, Bt=Bt, Rt=Rt, It=It: e.tensor_tensor_scan(out=Hf[:], data0=At[:], data1=Bt[:], initial=0.0, op0=ALU.mult, op1=ALU.add),
                                     reads=[r_A, r_B], writes=[r_Hf])
                            else:
                                S.op("dve", lambda e, At=At, Bt=Bt, Rt=Rt, It=It: e.tensor_tensor_scan(out=Hb[:, 0:LC][:, ::-1],
                                                                           data0=At[:, 0:LC][:, ::-1], data1=Bt[:, 0:LC][:, ::-1],
                                                                           initial=0.0, op0=ALU.mult, op1=ALU.add),
                                     reads=[r_A, r_B], writes=[r_Hb])
                                S.op("dve", lambda e, At=At, Bt=Bt, Rt=Rt, It=It: e.tensor_tensor_scan(out=Hb[:, LC:NT][:, ::-1], data0=At[:, LC:NT][:, ::-1],
                                                                           data1=Bt[:, LC:NT][:, ::-1], initial=Hb[:, 0:1],
                                                                           op0=ALU.mult, op1=ALU.add),
                                     reads=[r_A, r_B, r_Hb], writes=[r_Hb])
                        S.op("pool", lambda e: e.tensor_tensor(out=Hf[:, LC:NT], in0=Hf[:, LC:NT], in1=Hb[:, LC:NT], op=ALU.add),
                             reads=[r_Hf, r_Hb], writes=[r_Hf])
                        S.op("dve", lambda e, f=f: e.tensor_tensor(out=YI[:, f, :], in0=Hf[:, LC:NT], in1=GG[:, f, :], op=ALU.mult),
                             reads=[r_Hf, r_GG], writes=[r_YI])
                    S.barrier()
                    S.emit()
                srcs = [(YI, c, r_YI, LC) for c in range(8)]
                outproj_residual(st, 1, b, srcs, od_w_out, XR, R_XRT, list(range(2, NTI)), VEC_OF_TILE(b))

        def moe_layer(l, tok_tiles, final):
            ntile = len(tok_tiles)
            ntok = ntile * 128
            nblk = ntok * 4 // BLK + n_exp
            with contextlib.ExitStack() as st:
                sH2 = contextlib.ExitStack()
                LG = sbuf(st, "LG", [128, ntile, NE], F32); r_LG = Res()
                D4 = sbuf(st, "D4", [128, ntile * 4], I32); r_D4 = Res()
                G4 = sbuf(st, "G4", [128, ntile * 4], F32); r_G4 = Res()
                IDXW = sbuf(st, "IDXW", [128, nblk], I32); r_IDXW = Res()
                BEI = sbuf(st, "BEI", [128, nblk], I32)
                H2 = sbuf(sH2, "H2", [128, ntile * D], BF16); r_H2 = Res()
                with contextlib.ExitStack() as s1:
                    vecs = sorted(set((NB if i < 2 else b) for b, i in tok_tiles))
                    tiles = norm_mod_tiles(s1, l, 1, vecs)
                    small, r_small = zip(*[new_small(s1, "n2small%d" % i) for i in range(2)])
                    junk = [sbuf(s1, "n2junk%d" % i, [128, D], F32) for i in range(2)]; r_junk = [Res(), Res()]
                    xt = [sbuf(s1, "n2x%d" % i, [128, D], F32) for i in range(2)]
                    r_xt = [Res(), Res()]
                    ht = [sbuf(s1, "n2h%d" % i, [128, D], F32) for i in range(2)]
                    r_ht = [Res(), Res()]
                    hT32 = [sbuf(s1, "n2t%d" % i, [128, 8, 128], F32) for i in range(2)]
                    r_hT32 = [Res(), Res()]
                    ptp = [psum(s1, "n2p%d" % i, [128, 4, 128], F32) for i in range(2)]
                    r_ptp = [Res(), Res()]
                    plg = [psum(s1, "n2l%d" % i, [128, NE], F32) for i in range(2)]
                    r_plg = [Res(), Res()]
                    wr = sbuf(s1, "wr", [128, 8, NE], F32); r_wr = Res()
                    S.dma("sp", lambda e: e.dma_start(out=wr[:], in_=router_w[l].rearrange("(c p) n -> p c n", p=128)), writes=[r_wr])
                    rb, r_rb = load_bcast(s1, "rb", router_b[l, :], [], n=NE)
                    def stage_a(n_i):
                        b, i = tok_tiles[n_i]
                        k = n_i % 2
                        G, rG, SH, rSH = tiles[NB if i < 2 else b]
                        S.dma("sp", lambda e: e.dma_start(out=xt[k][:], in_=XR[b, i * 128:(i + 1) * 128, :]),
                              reads=[R_XRT[(b, i)]], writes=[r_xt[k]])
                        rms_mod(s1, xt[k], r_xt[k], G, rG, SH, rSH, ht[k], r_ht[k], small[k], r_small[k], junk[k], r_junk[k])
                        S.op("act", lambda e: e.activation(out=H2[:, n_i * D:(n_i + 1) * D], in_=ht[k][:], func=AF.Copy), reads=[r_ht[k]], writes=[r_H2])

                    def stage_b(n_i):
                        k = n_i % 2
                        for hf in range(2):
                            for c4 in range(4):
                                c = hf * 4 + c4
                                S.op("pe", lambda e, hf=hf, c4=c4, c=c: e.transpose(
                                    out=ptp[hf][:, c4, :], in_=ht[k][:, c * 128:(c + 1) * 128], identity=ident[:]),
                                    reads=[r_ht[k], R_c], writes=[r_ptp[hf]], signal=(c4 == 3))
                            if hf == 0:
                                S.op("act", lambda e: e.activation(out=hT32[k][:, 0:4, :], in_=ptp[0][:], func=AF.Copy),
                                     reads=[r_ptp[0]], writes=[r_hT32[k]])
                            else:
                                S.op("dve", lambda e: e.tensor_copy(out=hT32[k][:, 4:8, :], in_=ptp[1][:]),
                                     reads=[r_ptp[1]], writes=[r_hT32[k]])
                        for c in range(8):
                            S.op("pe", lambda e, c=c: e.matmul(plg[k][:], lhsT=hT32[k][:, c, :], rhs=wr[:, c, :], start=(c == 0), stop=(c == 7)),
                                 reads=[r_hT32[k], r_wr], writes=[r_plg[k]], signal=(c == 7))
                        S.op("dve", lambda e: e.tensor_tensor(out=LG[:, n_i, :], in0=plg[k][:], in1=rb[:], op=ALU.add),
                             reads=[r_plg[k], r_rb], writes=[r_LG])

                    stage_a(0)
                    for n_i in range(ntile):
                        if n_i + 1 < ntile:
                            stage_a(n_i + 1)
                        stage_b(n_i)
                    S.barrier()
                    S.emit()
                with contextlib.ExitStack() as s1:
                    MX = sbuf(s1, "MX", [128, ntile, 8], F32)
                    MASK = sbuf(s1, "MASK", [128, ntile, NE], F32)
                    MASKb = sbuf(s1, "MASKb", [128, ntile, NE], BF16)
                    CUM = sbuf(s1, "CUM", [128, ntile, NE], BF16)
                    GAT = sbuf(s1, "GAT", [128, ntile, NE], F32)
                    TMP = sbuf(s1, "TMP", [128, ntile, NE], F32)
                    KEY = sbuf(s1, "KEY", [128, ntile, NE], F32)
                    K8 = sbuf(s1, "K8", [128, ntile, 8], F32)
                    DEN = sbuf(s1, "DEN", [128, ntile], F32)
                    TRI = sbuf(s1, "TRI", [128, 128], BF16)
                    CNT = sbuf(s1, "CNT", [128, NE], F32)
                    CNTI = sbuf(s1, "CNTI", [128, NE], I32)
                    PAD = sbuf(s1, "PAD", [128, NE], F32)
                    PEND = sbuf(s1, "PEND", [128, NE], F32)
                    BASE = sbuf(s1, "BASE", [128, NE], F32)
                    ONE32 = sbuf(s1, "ONE32", [128, NE], F32)
                    CMP = sbuf(s1, "CMP", [128, nblk, NE], F32)
                    BEF = sbuf(s1, "BEF", [128, nblk], F32)
                    JB = sbuf(s1, "JB", [128, 80], F32)
                    IOP = sbuf(s1, "IOP", [128, 1], F32)
                    pposb = [psum(s1, "ppos%d" % i, [128, 16, NE], F32) for i in range((ntile + 15) // 16)]
                    pcnt = psum(s1, "pcnt", [128, NE], F32)
                    R = Res("route")
                    V_ = lambda fn, eng="dve": S.op(eng, fn, reads=[R, r_LG], writes=[R, r_D4, r_G4, r_IDXW])
                    S.dma("sp", lambda e: e.dma_start(out=JB[:], in_=blk_start_d[:, :]), writes=[R])
                    S.dma("sp", lambda e: e.dma_start(out=IOP[:], in_=iota_p_d[:, :]), reads=[R], writes=[R])
                    V_(lambda e: e.memset(TRI[:], 1.0))
                    V_(lambda e: e.affine_select(out=TRI[:], in_=TRI[:], pattern=[[1, 128]], compare_op=ALU.is_gt, fill=0.0,
                                                 base=0, channel_multiplier=-1), "pool")
                    V_(lambda e: e.memset(ONE32[:], 1.0))
                    for i in range(ntile):
                        V_(lambda e, i=i: e.max(out=MX[:, i, :], in_=LG[:, i, :]))
                    V_(lambda e: e.tensor_tensor(out=MASK[:], in0=LG[:], in1=MX[:, :, 3:4].to_broadcast([128, ntile, NE]), op=ALU.is_ge))
                    V_(lambda e: e.tensor_tensor(out=TMP[:], in0=LG[:], in1=MX[:, :, 0:1].to_broadcast([128, ntile, NE]), op=ALU.subtract))
                    V_(lambda e: e.activation(out=TMP[:], in_=TMP[:], func=AF.Exp), "act")
                    V_(lambda e: e.tensor_tensor(out=TMP[:], in0=TMP[:], in1=MASK[:], op=ALU.mult))
                    V_(lambda e: e.tensor_reduce(out=DEN[:], in_=TMP[:], axis=AX.X, op=ALU.add))
                    V_(lambda e: e.reciprocal(out=DEN[:], in_=DEN[:]))
                    V_(lambda e: e.tensor_tensor(out=GAT[:], in0=TMP[:], in1=DEN[:].unsqueeze(2).to_broadcast([128, ntile, NE]), op=ALU.mult))
                    V_(lambda e: e.tensor_copy(out=MASKb[:], in_=MASK[:]))
                    V_(lambda e: e.memset(CUM[:, 0, :], 0.0))
                    for i in range(1, ntile):
                        V_(lambda e, i=i: e.tensor_tensor(out=CUM[:, i, :], in0=CUM[:, i - 1, :], in1=MASKb[:, i - 1, :], op=ALU.add))
                    V_(lambda e: e.tensor_tensor(out=TMP[:, 0, :], in0=CUM[:, ntile - 1, :], in1=MASKb[:, ntile - 1, :], op=ALU.add))
                    TOTb = sbuf(s1, "TOTb", [128, NE], BF16)
                    V_(lambda e: e.tensor_copy(out=TOTb[:], in_=TMP[:, 0, :]))
                    for i in range(ntile):
                        V_(lambda e, i=i: e.matmul(pposb[i // 16][:, i % 16, :], lhsT=TRI[:], rhs=MASKb[:, i, :], start=True, stop=False), "pe")
                        V_(lambda e, i=i: e.matmul(pposb[i // 16][:, i % 16, :], lhsT=onesb[:, 0:128], rhs=CUM[:, i, :], start=False, stop=True), "pe")
                    V_(lambda e: e.matmul(pcnt[:], lhsT=onesb[:, 0:128], rhs=TOTb[:], start=True, stop=True), "pe")
                    V_(lambda e: e.tensor_copy(out=CNT[:], in_=pcnt[:]))
                    NM = ntok // BLK + 1
                    CMP2 = sbuf(s1, "CMP2", [128, NE, NM], F32)
                    V_(lambda e: e.tensor_tensor(out=CMP2[:], in0=CNT[:].unsqueeze(2).to_broadcast([128, NE, NM]),
                                                 in1=JB[:, 0:NM].unsqueeze(1).to_broadcast([128, NE, NM]), op=ALU.is_gt))
                    V_(lambda e: e.tensor_reduce(out=PAD[:], in_=CMP2[:], axis=AX.X, op=ALU.add))
                    V_(lambda e: e.tensor_scalar(out=PAD[:], in0=PAD[:], scalar1=float(BLK), scalar2=None, op0=ALU.mult))
                    V_(lambda e: e.tensor_tensor_scan(out=PEND[:], data0=ONE32[:], data1=PAD[:], initial=0.0, op0=ALU.mult, op1=ALU.add))
                    V_(lambda e: e.tensor_tensor(out=BASE[:], in0=PEND[:], in1=PAD[:], op=ALU.subtract))
                    for bk in range((ntile + 15) // 16):
                        n_ = min(16, ntile - bk * 16)
                        V_(lambda e, bk=bk, n_=n_: e.tensor_tensor(out=KEY[:, bk * 16:bk * 16 + n_, :], in0=pposb[bk][:, 0:n_, :],
                                                                  in1=BASE[:].unsqueeze(1).to_broadcast([128, n_, NE]), op=ALU.add))
                    V_(lambda e: e.scalar_tensor_tensor(out=KEY[:], in0=KEY[:], scalar=1.0, in1=MASK[:], op0=ALU.add, op1=ALU.mult))
                    for i in range(ntile):
                        V_(lambda e, i=i: e.max(out=K8[:, i, :], in_=KEY[:, i, :]))
                    V_(lambda e: e.tensor_scalar(out=TMP[:, :, 0:4], in0=K8[:, :, 0:4], scalar1=-1.0, scalar2=0.0, op0=ALU.add, op1=ALU.max))
                    V_(lambda e: e.tensor_scalar(out=TMP[:, :, 0:4], in0=TMP[:, :, 0:4], scalar1=float(nblk * BLK - 1), scalar2=None, op0=ALU.min))
                    V_(lambda e: e.tensor_copy(out=D4[:].rearrange("p (n j) -> p n j", j=4), in_=TMP[:, :, 0:4]))
                    for j in range(4):
                        V_(lambda e, j=j: e.tensor_tensor(out=TMP[:], in0=KEY[:], in1=K8[:, :, j:j + 1].to_broadcast([128, ntile, NE]), op=ALU.is_equal))
                        V_(lambda e: e.tensor_tensor(out=TMP[:], in0=TMP[:], in1=GAT[:], op=ALU.mult))
                        V_(lambda e, j=j: e.tensor_reduce(out=G4[:].rearrange("p (n j) -> p n j", j=4)[:, :, j], in_=TMP[:], axis=AX.X, op=ALU.add))
                    V_(lambda e: e.tensor_tensor(out=CMP[:], in0=PEND[:].unsqueeze(1).to_broadcast([128, nblk, NE]),
                                                 in1=JB[:, 0:nblk].unsqueeze(2).to_broadcast([128, nblk, NE]), op=ALU.is_le))
                    V_(lambda e: e.tensor_reduce(out=BEF[:], in_=CMP[:], axis=AX.X, op=ALU.add))
                    V_(lambda e: e.tensor_scalar(out=BEF[:], in0=BEF[:], scalar1=float(n_exp - 1), scalar2=0.0, op0=ALU.min, op1=ALU.max))
                    V_(lambda e: e.tensor_copy(out=BEI[:], in_=BEF[:]))
                    V_(lambda e: e.tensor_scalar(out=BEF[:], in0=BEF[:], scalar1=128.0, scalar2=IOP[:, 0:1], op0=ALU.mult, op1=ALU.add))
                    V_(lambda e: e.tensor_copy(out=IDXW[:], in_=BEF[:]))
                    if dbg:
                        S.dma("sp", lambda e: e.dma_start(out=dbg_d4[:, 0:ntile * 4], in_=D4[:]), reads=[R, r_D4])
                        S.dma("sp", lambda e: e.dma_start(out=dbg_g4[:, 0:ntile * 4], in_=G4[:]), reads=[R, r_G4])
                        S.dma("sp", lambda e: e.dma_start(out=dbg_iw[:, 0:nblk], in_=IDXW[:]), reads=[R, r_IDXW])
                    for n_i in range(ntile if moe_stop >= 2 else 0):
                        for j in range(4):
                            S.dma("pool", lambda e, n_i=n_i, j=j: e.indirect_dma_start(
                                out=XS[:, :], out_offset=bass.IndirectOffsetOnAxis(ap=D4[:, n_i * 4 + j:n_i * 4 + j + 1], axis=0),
                                in_=H2[:, n_i * D:(n_i + 1) * D], in_offset=None),
                                reads=[R, r_D4, r_H2], writes=[R_XS])
                    S.barrier()
                    S.emit()
                sH2.close()
                if moe_stop < 3:
                    return
                w_aps = (w_gate[l], w_up[l], w_down[l])
                with contextlib.ExitStack() as s1:
                    WG = [sbuf(s1, "WG%d" % i, [128, 8192], BF16) for i in range(2)]
                    WU = [sbuf(s1, "WU%d" % i, [128, 8192], BF16) for i in range(2)]
                    WD = [sbuf(s1, "WD%d" % i, [128, 8192], BF16) for i in range(2)]
                    EB = [sbuf(s1, "EB%d" % i, [2, 3072], BF16) for i in range(2)]
                    EBC = [sbuf(s1, "EBC%d" % i, [128, 16], F32) for i in range(2)]
                    r_EBC = [Res(), Res()]
                    r_W = [[Res() for _ in range(4)] for _ in range(2)]
                    xs = [sbuf(s1, "xsb%d" % i, [128, 4, D], BF16) for i in range(2)]
                    r_xs = [Res(), Res()]
                    xeT = [sbuf(s1, "xeT%d" % i, [128, 8, BLK], BF16) for i in range(2)]
                    r_xeT = [Res(), Res()]
                    actT = sbuf(s1, "actT", [128, 8, BLK], BF16); r_actT = Res()
                    gs = [sbuf(s1, "gs%d" % i, [128, BLK], F32) for i in range(2)]
                    sg = [sbuf(s1, "sg%d" % i, [128, BLK], F32) for i in range(2)]
                    us = [sbuf(s1, "us%d" % i, [128, BLK], F32) for i in range(2)]
                    r_gs = [Res(), Res()]; r_sg = [Res(), Res()]; r_us = [Res(), Res()]
                    ysb = [sbuf(s1, "ysb%d" % i, [128, 4, D], BF16) for i in range(2)]
                    r_ysb = [Res(), Res()]
                    ptr = [psum(s1, "ptr%d" % i, [128, 8, 128], BF16) for i in range(2)]
                    r_ptr = [Res(), Res()]
                    pgp = [psum(s1, "pgp%d" % i, [128, BLK], F32) for i in range(2)]
                    pup = [psum(s1, "pup%d" % i, [128, BLK], F32) for i in range(2)]
                    r_pgp = [Res(), Res()]; r_pup = [Res(), Res()]
                    pyp = [psum(s1, "pyp%d" % i, [128, 512], F32) for i in range(2)]
                    r_pyp = [Res(), Res()]
                    def load_xs(jb_):
                        k_ = jb_ % 2
                        S.dma("sp", lambda e: e.dma_start(
                            out=xs[k_][:], in_=XS[jb_ * BLK:(jb_ + 1) * BLK, :].rearrange("(s p) d -> p s d", p=128)),
                            reads=[R_XS], writes=[r_xs[k_]])

                    for jb in range(nblk):
                        k = jb % 2
                        for mi, (wbuf, wap) in enumerate(((WG[k], w_aps[0]), (WU[k], w_aps[1]), (WD[k], w_aps[2]))):
                            S.dma("pool", lambda e, wbuf=wbuf, wap=wap, jb=jb: e.indirect_dma_start(
                                out=wbuf[:, :], out_offset=None, in_=wap[:, :],
                                in_offset=bass.IndirectOffsetOnAxis(ap=IDXW[:, jb:jb + 1], axis=0)), reads=[r_IDXW], writes=[r_W[k][mi]])
                        S.dma("pool", lambda e, k=k, jb=jb: e.indirect_dma_start(
                            out=EB[k][:, :], out_offset=None, in_=exp_b[l][:, :],
                            in_offset=bass.IndirectOffsetOnAxis(ap=BEI[0:2, jb:jb + 1], axis=0)), reads=[r_IDXW], writes=[r_W[k][3]])
                        S.dma("pool", lambda e, k=k, jb=jb: e.indirect_dma_start(
                            out=EBC[k][:, :], out_offset=None, in_=exp_bc[l][:, :],
                            in_offset=bass.IndirectOffsetOnAxis(ap=IDXW[:, jb:jb + 1], axis=0)), reads=[r_IDXW], writes=[r_EBC[k]])
                        S.op("dve", lambda e, k=k: e.tensor_scalar(out=EBC[k][:, 8:16], in0=EBC[k][:, 8:16], scalar1=1.0, scalar2=None, op0=ALU.add),
                             reads=[r_EBC[k]], writes=[r_EBC[k]])
                        if jb == 0:
                            load_xs(0)
                        for s in range(4):
                            kk = s % 2
                            for c in range(8):
                                S.op("pe", lambda e, k=k, kk=kk, s=s, c=c: e.transpose(out=ptr[kk][:, c, :], in_=xs[k][:, s, c * 128:(c + 1) * 128],
                                                                                 identity=identb[:]),
                                     reads=[r_xs[k], R_c], writes=[r_ptr[kk]], signal=(c == 7))
                            if s % 2 == 0:
                                S.op("act", lambda e, k=k, kk=kk, s=s: e.activation(out=xeT[k][:, :, s * 128:(s + 1) * 128], in_=ptr[kk][:], func=AF.Copy),
                                     reads=[r_ptr[kk]], writes=[r_xeT[k]])
                            else:
                                S.op("dve", lambda e, k=k, kk=kk, s=s: e.tensor_copy(out=xeT[k][:, :, s * 128:(s + 1) * 128], in_=ptr[kk][:]),
                                     reads=[r_ptr[kk]], writes=[r_xeT[k]])
                        if jb + 1 < nblk:
                            load_xs(jb + 1)
                        for f in range(8):
                            kf = f % 2
                            for (pp, r_pp, wbuf, mi) in ((pgp[kf], r_pgp[kf], WG[k], 0), (pup[kf], r_pup[kf], WU[k], 1)):
                                for c in range(8):
                                    S.op("pe", lambda e, pp=pp, wbuf=wbuf, c=c, f=f, k=k: e.matmul(
                                        pp[:], lhsT=wbuf[:, c * 1024 + f * 128:c * 1024 + (f + 1) * 128], rhs=xeT[k][:, c, :], start=(c == 0), stop=(c == 7)),
                                        reads=[r_W[k][mi], r_xeT[k]], writes=[r_pp], signal=(c == 7))
                            S.op("dve", lambda e, kf=kf, k=k, f=f: e.tensor_scalar(out=gs[kf][:], in0=pgp[kf][:], scalar1=EBC[k][:, f:f + 1], scalar2=7.0,
                                                                                 op0=ALU.add, op1=ALU.min),
                                 reads=[r_pgp[kf], r_EBC[k]], writes=[r_gs[kf]])
                            S.op("act", lambda e, kf=kf: e.activation(out=sg[kf][:], in_=gs[kf][:], func=AF.Sigmoid, scale=1.702),
                                 reads=[r_gs[kf]], writes=[r_sg[kf]])
                            S.op("dve", lambda e, kf=kf, k=k, f=f: e.tensor_scalar(out=us[kf][:], in0=pup[kf][:], scalar1=EBC[k][:, 8 + f:9 + f], scalar2=8.0,
                                                                                 op0=ALU.add, op1=ALU.min),
                                 reads=[r_pup[kf], r_EBC[k]], writes=[r_us[kf]])
                            S.op("dve", lambda e, kf=kf: e.scalar_tensor_tensor(out=us[kf][:], in0=us[kf][:], scalar=-6.0, in1=gs[kf][:],
                                                                                op0=ALU.max, op1=ALU.mult),
                                 reads=[r_us[kf], r_gs[kf]], writes=[r_us[kf]])
                            S.op("dve", lambda e, kf=kf, f=f: e.tensor_tensor(out=actT[:, f, :], in0=us[kf][:], in1=sg[kf][:], op=ALU.mult),
                                 reads=[r_us[kf], r_sg[kf]], writes=[r_actT])
                        for s in range(4):
                            for hh in range(2):
                                kp = (s * 2 + hh) % 2
                                for f in range(8):
                                    S.op("pe", lambda e, kp=kp, f=f, s=s, hh=hh, k=k: e.matmul(
                                        pyp[kp][:], lhsT=actT[:, f, s * 128:(s + 1) * 128],
                                        rhs=WD[k][:, f * 1024 + hh * 512:f * 1024 + (hh + 1) * 512], start=(f == 0), stop=False),
                                        reads=[r_W[k][2], r_actT], writes=[r_pyp[kp]], signal=False)
                                S.op("pe", lambda e, kp=kp, hh=hh, k=k: e.matmul(
                                    pyp[kp][:], lhsT=halfb[0:2, 0:128], rhs=EB[k][0:2, 2048 + hh * 512:2048 + (hh + 1) * 512], start=False, stop=True),
                                    reads=[r_W[k][3], R_c], writes=[r_pyp[kp]])
                                S.op("act", lambda e, kp=kp, k=k, s=s, hh=hh: e.activation(out=ysb[k][:, s, hh * 512:(hh + 1) * 512], in_=pyp[kp][:], func=AF.Copy),
                                     reads=[r_pyp[kp]], writes=[r_ysb[k]])
                        S.dma("sp", lambda e, k=k, jb=jb: e.dma_start(
                            out=YS[jb * BLK:(jb + 1) * BLK, :].rearrange("(s p) d -> p s d", p=128), in_=ysb[k][:]),
                            reads=[r_ysb[k]], writes=[R_YS])
                    S.barrier()
                    S.emit()
                if moe_stop < 4:
                    return
                with contextlib.ExitStack() as s1:
                    g2 = {}
                    for v in sorted(set((NB if i < 2 else b) for b, i in tok_tiles)):
                        g2[v] = load_bcast(s1, "g2_%d" % v, MOD[l, v, 5 * D:6 * D], [R_MOD])
                    yg = [[sbuf(s1, "yg%d_%d" % (i, j), [128, D], BF16) for j in range(4)] for i in range(2)]
                    r_yg = [[Res() for _ in range(4)] for _ in range(2)]
                    acc = [sbuf(s1, "cacc%d" % i, [128, D], F32) for i in range(2)]
                    r_acc = [Res(), Res()]
                    xt = [sbuf(s1, "cbx%d" % i, [128, D], F32) for i in range(2)]
                    r_xt = [Res(), Res()]
                    for n_i, (b, i) in enumerate(tok_tiles):
                        k = n_i % 2
                        gt, rgt = g2[NB if i < 2 else b]
                        S.dma("sp", lambda e, b=b, i=i, k=k: e.dma_start(out=xt[k][:], in_=XR[b, i * 128:(i + 1) * 128, :]),
                              reads=[R_XRT[(b, i)]], writes=[r_xt[k]])
                        for j in range(4):
                            S.dma("pool", lambda e, k=k, j=j, n_i=n_i: e.indirect_dma_start(
                                out=yg[k][j][:, :], out_offset=None, in_=YS[:, :],
                                in_offset=bass.IndirectOffsetOnAxis(ap=D4[:, n_i * 4 + j:n_i * 4 + j + 1], axis=0)), reads=[R_YS, r_D4], writes=[r_yg[k][j]])
                        S.op("dve", lambda e, k=k, n_i=n_i: e.tensor_scalar(out=acc[k][:], in0=yg[k][0][:], scalar1=G4[:, n_i * 4:n_i * 4 + 1], scalar2=None, op0=ALU.mult),
                             reads=[r_yg[k][0], r_G4], writes=[r_acc[k]])
                        for j in range(1, 4):
                            S.op("dve", lambda e, k=k, j=j, n_i=n_i: e.scalar_tensor_tensor(
                                out=acc[k][:], in0=yg[k][j][:], scalar=G4[:, n_i * 4 + j:n_i * 4 + j + 1], in1=acc[k][:], op0=ALU.mult, op1=ALU.add),
                                reads=[r_yg[k][j], r_acc[k], r_G4], writes=[r_acc[k]])
                        S.op("pool", lambda e, k=k, gt=gt: e.tensor_tensor(out=acc[k][:], in0=acc[k][:], in1=gt[:], op=ALU.mult),
                             reads=[r_acc[k], rgt], writes=[r_acc[k]])
                        S.op("pool", lambda e, k=k: e.tensor_tensor(out=xt[k][:], in0=xt[k][:], in1=acc[k][:], op=ALU.add),
                             reads=[r_acc[k], r_xt[k]], writes=[r_xt[k]])
                        if final:
                            S.dma("sp", lambda e, b=b, i=i, k=k: e.dma_start(out=out[b, (i - 2) * 128:(i - 1) * 128, :], in_=xt[k][:]),
                                  reads=[r_xt[k]], writes=[R_OUT])
                        else:
                            S.dma("sp", lambda e, b=b, i=i, k=k: e.dma_start(out=XR[b, i * 128:(i + 1) * 128, :], in_=xt[k][:]),
                                  reads=[r_xt[k]], writes=[R_XRT[(b, i)]])
                    S.barrier()
                    S.emit()

        if 0 in layers:
            for b in range(NB):
                layer0_mixer(b)
            if do_moe:
                moe_layer(0, [(b, i) for b in range(NB) for i in range(NTI)], final=False)
        if 1 in layers:
            for b in range(NB):
                layer1_mixer(b)
            if do_moe:
                moe_layer(1, [(b, i) for b in range(NB) for i in range(2, NTI)], final=True)
        if dbg:
            with contextlib.ExitStack() as st:
                t = sbuf(st, "dbgt", [128, D], F32); r_t = Res()
                for b in range(NB):
                    for i in range(2, NTI):
                        S.dma("sp", lambda e, b=b, i=i: e.dma_start(out=t[:], in_=XR[b, i * 128:(i + 1) * 128, :]), reads=[R_XRT[(b, i)]], writes=[r_t])
                        S.dma("sp", lambda e, b=b, i=i: e.dma_start(out=out[b, (i - 2) * 128:(i - 1) * 128, :], in_=t[:]), reads=[r_t], writes=[R_OUT])
                S.barrier()
                S.emit()
        S.barrier()
        S.emit()
        print("program instructions (incl waits):", S.n_ins, "sem counts:", S.cnt, "max dma sem:", max(S.dma_cnt.values()))
    return nc


def _core_inputs(inp, sh, c, NB=2):
    b0 = c * NB
    d = dict(sh)
    d["xin"] = np.ascontiguousarray(np.concatenate([inp["ctx"][b0:b0 + NB], inp["x"][b0:b0 + NB]], axis=1), np.float32)
    d["cvec"] = np.ascontiguousarray(np.concatenate([inp["c"][b0:b0 + NB], inp["c_ctx"][None]], 0), np.float32)
    return d


def kernel(**inputs):
    inp = {k: np.asarray(v) for k, v in inputs.items()}
    sh = _prep_shared(inp)
    nc = build_program(NB=2)
    in_maps = [_core_inputs(inp, sh, c) for c in range(8)]
    res = run_bass_kernel_spmd(nc, in_maps, core_ids=list(range(8)))
    return np.concatenate([r["out"] for r in res.results], axis=0).astype(np.float32)
```

```python
import contextlib
import numpy as np
import concourse.bass as bass
import concourse.mybir as mybir
from concourse.bass_utils import run_bass_kernel_spmd

F32 = mybir.dt.float32
BF16 = mybir.dt.bfloat16
I32 = mybir.dt.int32
AF = mybir.ActivationFunctionType
ALU = mybir.AluOpType
AX = mybir.AxisListType

ENGS = ("pe", "dve", "act", "pool", "sp")

D = 1024
T = 2048
LC = 256
NT = T + LC
NTI = NT // 128
NE = 32
BLK = 512
EPS = 1e-6
NEG = -30000.0


class Res:
    __slots__ = ("name", "w", "r")

    def __init__(self, name=""):
        self.name = name
        self.w = None
        self.r = []


class Sched:
    def __init__(self, nc, stack, n_dma_sems=12):
        self.nc = nc
        self.streams = {e: [] for e in ENGS}
        self.cnt = {e: 0 for e in ENGS}
        self.sems = {}
        for e in ENGS:
            self.sems[e] = stack.enter_context(nc.semaphore("s_" + e))
        self.dma_sems = {}
        self.dma_cnt = {}
        self.dma_rr = {}
        for q in ("sp", "pool"):
            self.dma_sems[q] = []
            for i in range(n_dma_sems):
                k = "d_%s_%d" % (q, i)
                self.sems[k] = stack.enter_context(nc.semaphore(k))
                self.dma_sems[q].append(k)
                self.dma_cnt[k] = 0
            self.dma_rr[q] = 0
        self.seen = {e: {} for e in ENGS}
        self.n_ins = 0

    def _need(self, eng, deps):
        best = {}
        for d in deps:
            if d is None:
                continue
            k, v = d
            if k == eng and eng == "pe":
                continue
            if self.seen[eng].get(k, 0) >= v:
                continue
            if best.get(k, 0) < v:
                best[k] = v
        for k, v in best.items():
            self.seen[eng][k] = v
        return list(best.items())

    @staticmethod
    def _deps(reads, writes):
        deps = []
        for r in reads:
            deps.append(r.w)
        for w in writes:
            deps.append(w.w)
            deps.extend(w.r)
        return deps

    @staticmethod
    def _commit(reads, writes, tok):
        for r in reads:
            r.r.append(tok)
            if len(r.r) > 16:
                m = {}
                for k, v in r.r:
                    if m.get(k, 0) < v:
                        m[k] = v
                r.r = list(m.items())
        for w in writes:
            w.w = tok
            w.r = []

    def op(self, eng, fn, reads=(), writes=(), signal=True):
        waits = self._need(eng, self._deps(reads, writes))
        if signal:
            self.cnt[eng] += 1
            tok = (eng, self.cnt[eng])
        else:
            tok = (eng, self.cnt[eng] + 1)
        self.streams[eng].append((waits, fn, signal, None))
        self._commit(reads, writes, tok)
        self.n_ins += 1 + len(waits)
        return tok

    def dma(self, q, fn, reads=(), writes=()):
        lst = self.dma_sems[q]
        k = lst[self.dma_rr[q] % len(lst)]
        self.dma_rr[q] += 1
        deps = self._deps(reads, writes)
        deps.append((k, self.dma_cnt[k]))
        waits = self._need(q, deps)
        self.dma_cnt[k] += 16
        tok = (k, self.dma_cnt[k])
        self.streams[q].append((waits, fn, False, k))
        self._commit(reads, writes, tok)
        self.n_ins += 1 + len(waits)
        return tok

    def barrier(self):
        final = []
        for e in ENGS:
            if self.cnt[e]:
                final.append((e, self.cnt[e]))
        for k, v in self.dma_cnt.items():
            if v:
                final.append((k, v))
        for e in ENGS:
            waits = self._need(e, [f for f in final if not (f[0] == e and e == "pe")])
            if waits:
                self.streams[e].append((waits, None, False, None))

    def emit(self):
        nc = self.nc
        sems = self.sems
        streams = self.streams

        def run(engname, engobj):
            for waits, fn, signal, dsem in streams[engname]:
                fold = None
                if fn is not None and dsem is None and waits:
                    fold = waits[-1]
                    waits = waits[:-1]
                for k, v in waits:
                    engobj.wait_ge(sems[k], v)
                if fn is None:
                    continue
                ins = fn(engobj)
                if fold is not None:
                    ins._wait_ge(sems[fold[0]], fold[1])
                if dsem is not None:
                    ins.then_inc(sems[dsem], 16)
                elif signal:
                    ins.then_inc(sems[engname], 1)

        with nc.Block() as block:
            @block.tensor
            def _(e):
                run("pe", e)

            @block.vector
            def _(e):
                run("dve", e)

            @block.scalar
            def _(e):
                run("act", e)

            @block.gpsimd
            def _(e):
                run("pool", e)

            @block.sync
            def _(e):
                run("sp", e)
        self.streams = {e: [] for e in ENGS}


def _na_bias_T(rpb):
    H = rpb.shape[0]
    out = np.full((H, 5, 640, 128), NEG, np.float32)
    pair_of_type = [0, 1, 5, 14, 15]
    for ty, j in enumerate(pair_of_type):
        ws = int(np.clip(2 * j - 4, 0, 23))
        a = ws // 2
        krow0 = 2 * a
        for rr in range(2):
            r = 2 * j + rr
            r0 = int(np.clip(r - 4, 0, 24))
            for i in range(10):
                kr = krow0 + i
                if not (r0 <= kr < r0 + 8):
                    continue
                dr = kr - r + 7
                c = np.arange(64)
                c0 = np.clip(c - 8, 0, 48)
                for cq in range(64):
                    kc = np.arange(c0[cq], c0[cq] + 16)
                    dc = kc - cq + 15
                    out[:, ty, i * 64 + kc, rr * 64 + cq] = rpb[:, dr, dc]
    return np.ascontiguousarray(out.reshape(H, 5, 5, 128, 128))


def _pcol(v):
    v = np.asarray(v, np.float32)
    return np.ascontiguousarray(v.reshape(-1, 128).T)


_GATE_INPUTS = ("od_fwd_wa", "od_fwd_ba", "od_fwd_wx", "od_fwd_bx", "od_fwd_lam",
                "od_bwd_wa", "od_bwd_ba", "od_bwd_wx", "od_bwd_bx", "od_bwd_lam")


def _prep_shared(inp):
    sh = {}
    for nm in _GATE_INPUTS:
        assert nm in inp
    sh["ada_w"] = np.ascontiguousarray(inp["ada_w"], np.float32)
    sh["ada_b"] = np.ascontiguousarray(inp["ada_b"], np.float32)
    sh["norm_g"] = np.ascontiguousarray(np.stack([inp["norm1_g"], inp["norm2_g"]], 1), np.float32)
    sh["ev_w_in"] = np.ascontiguousarray(inp["ev_w_in"][0], np.float32)
    sh["ev_w_out"] = np.ascontiguousarray(inp["ev_w_out"][0], np.float32)
    sh["biasT"] = _na_bias_T(np.asarray(inp["ev_rpb"][0], np.float32))
    sh["od_w_in"] = np.ascontiguousarray(inp["od_w_in"][0], np.float32)
    sh["od_w_out"] = np.ascontiguousarray(inp["od_w_out"][0], np.float32)
    gates = []
    for dr in ("fwd", "bwd"):
        for nm in ("wa", "wx"):
            gates.append(np.asarray(inp["od_%s_%s" % (dr, nm)][0], np.float32))
    sh["od_gw"] = np.ascontiguousarray(np.stack(gates, 0))
    cols = []
    qg = np.tile(np.asarray(inp["ev_q_gain"][0], np.float32), 2)
    kg = np.tile(np.asarray(inp["ev_k_gain"][0], np.float32), 2)
    cols.append(qg[:, None]); cols.append(kg[:, None])
    for j in range(3):
        cols.append(_pcol(inp["ev_conv_w"][0, j]))
    cols.append(_pcol(inp["ev_conv_b"][0]))
    for j in range(4):
        cols.append(_pcol(inp["od_conv_w"][0, j]))
    cols.append(_pcol(inp["od_conv_b"][0]))
    for dr in ("fwd", "bwd"):
        for nm in ("ba", "bx", "lam"):
            cols.append(_pcol(inp["od_%s_%s" % (dr, nm)][0]))
    sh["pcols"] = np.ascontiguousarray(np.concatenate(cols, 1), np.float32)
    sh["router_w"] = np.ascontiguousarray(inp["router_w"], np.float32)
    sh["router_b"] = np.ascontiguousarray(inp["router_b"], np.float32)
    for nm in ("gate", "up", "down"):
        w = np.asarray(inp["exp_w_" + nm], np.float32)
        w = w.reshape(2, NE, 8, 128, 1024).transpose(0, 1, 3, 2, 4).reshape(2, NE * 128, 8 * 1024)
        for l in range(2):
            sh["w_%s%d" % (nm, l)] = np.ascontiguousarray(w[l])
    eb = np.concatenate([inp["exp_b_gate"], inp["exp_b_up"], inp["exp_b_down"]], -1).astype(np.float32)
    for l in range(2):
        sh["exp_b%d" % l] = np.ascontiguousarray(eb[l])
        bgc = np.asarray(inp["exp_b_gate"][l], np.float32).reshape(NE, 8, 128).transpose(0, 2, 1)
        buc = np.asarray(inp["exp_b_up"][l], np.float32).reshape(NE, 8, 128).transpose(0, 2, 1)
        sh["exp_bc%d" % l] = np.ascontiguousarray(np.concatenate([bgc, buc], -1).reshape(NE * 128, 16))
    sh["iota_p"] = np.arange(128, dtype=np.float32)[:, None].copy()
    sh["blk_start"] = np.tile((np.arange(80, dtype=np.float32) * BLK)[None], (128, 1)).copy()
    return sh


PC_QG, PC_KG, PC_ECW, PC_ECB, PC_OCW, PC_OCB, PC_G = 0, 1, 2, 14, 18, 50, 58


def build_program(NB=2, layers=(0, 1), do_moe=True, dbg=False, n_exp=NE, moe_stop=4):
    nc = bass.Bass("TRN2", target_bir_lowering=False)
    NTOK0 = NB * NT
    dt = nc.dram_tensor

    def din(name, shape, dtp=F32):
        return dt(name, list(shape), dtp, kind="ExternalInput").ap()

    xin = din("xin", [NB, NT, D])
    cvec = din("cvec", [NB + 1, D])
    ada_w = din("ada_w", [2, D, 6 * D])
    ada_b = din("ada_b", [2, 6 * D])
    norm_g = din("norm_g", [2, 2, D])
    ev_w_in = din("ev_w_in", [D, 3072])
    ev_w_out = din("ev_w_out", [D, D])
    biasT = din("biasT", [8, 5, 5, 128, 128])
    od_w_in = din("od_w_in", [D, 2048])
    od_w_out = din("od_w_out", [D, D])
    od_gw = din("od_gw", [4, 4, 256, 256])
    pcols_d = din("pcols", [128, 106])
    router_w = din("router_w", [2, D, NE])
    router_b = din("router_b", [2, NE])
    w_gate = [din("w_gate%d" % l, [n_exp * 128, 8192]) for l in range(2)]
    w_up = [din("w_up%d" % l, [n_exp * 128, 8192]) for l in range(2)]
    w_down = [din("w_down%d" % l, [n_exp * 128, 8192]) for l in range(2)]
    exp_b = [din("exp_b%d" % l, [n_exp, 3072]) for l in range(2)]
    exp_bc = [din("exp_bc%d" % l, [n_exp * 128, 16]) for l in range(2)]
    iota_p_d = din("iota_p", [128, 1])
    blk_start_d = din("blk_start", [128, 80])
    out = dt("out", [NB, T, D], F32, kind="ExternalOutput").ap()
    if dbg:
        dbg_d4 = dt("dbg_d4", [128, NB * NTI * 4], I32, kind="ExternalOutput").ap()
        dbg_g4 = dt("dbg_g4", [128, NB * NTI * 4], F32, kind="ExternalOutput").ap()
        dbg_iw = dt("dbg_iw", [128, 80], I32, kind="ExternalOutput").ap()

    NBLK0 = NTOK0 * 4 // BLK + n_exp
    XR = dt("XR", [NB, NT, D], F32).ap()
    MOD = dt("MODs", [2, NB + 1, 6 * D], F32).ap()
    XS = dt("XS", [NBLK0 * BLK, D], BF16).ap()
    YS = dt("YS", [NBLK0 * BLK, D], BF16).ap()
    R_MOD, R_XS, R_YS, R_OUT = Res("MOD"), Res("XS"), Res("YS"), Res("OUT")
    R_XRT = {(b_, i_): Res() for b_ in range(NB) for i_ in range(NTI)}
    R_XIN = {(b_, i_): Res() for b_ in range(NB) for i_ in range(NTI)}

    with contextlib.ExitStack() as g:
        S = Sched(nc, g)

        uid = [0]

        def sbuf(st, name, shape, dtp):
            uid[0] += 1
            return st.enter_context(nc.sbuf_tensor("%s_%d" % (name, uid[0]), list(shape), dtp))

        def psum(st, name, shape, dtp=F32):
            uid[0] += 1
            return st.enter_context(nc.psum_tensor("%s_%d" % (name, uid[0]), list(shape), dtp))

        ident = sbuf(g, "ident", [128, 128], F32)
        identb = sbuf(g, "identb", [128, 128], BF16)
        onesb = sbuf(g, "onesb", [128, 512], BF16)
        halfb = sbuf(g, "halfb", [2, 512], BF16)
        pcols = sbuf(g, "pcols_sb", [128, 106], F32)
        R_c = Res("const")
        S.op("dve", lambda e: e.memset(ident[:], 0.0), writes=[R_c])
        S.op("pool", lambda e: e.affine_select(out=ident[:], in_=ident[:], pattern=[[-1, 128]], compare_op=ALU.not_equal,
                                               fill=1.0, base=0, channel_multiplier=1), reads=[R_c], writes=[R_c])
        S.op("dve", lambda e: e.tensor_copy(out=identb[:], in_=ident[:]), reads=[R_c], writes=[R_c])
        S.op("dve", lambda e: e.memset(onesb[:], 1.0), writes=[R_c])
        S.op("dve", lambda e: e.memset(halfb[:], 0.5), writes=[R_c])
        S.dma("sp", lambda e: e.dma_start(out=pcols[:], in_=pcols_d[:, :]), writes=[R_c])
        S.barrier()
        S.emit()

        NV = NB + 1
        with contextlib.ExitStack() as st:
            cs = sbuf(st, "cs", [NV, D], F32)
            sT = sbuf(st, "sT", [128, 8, NV], F32)
            aw = [sbuf(st, "aw%d" % i, [128, 8, 512], F32) for i in range(2)]
            R_aw = [Res(), Res()]
            ab = sbuf(st, "ab", [NV, 6 * D], F32)
            msb = sbuf(st, "msb", [NV, 6 * D], F32)
            pT = psum(st, "pT", [128, 8, NV], F32)
            pm = [psum(st, "pm%d" % i, [NV, 512], F32) for i in range(2)]
            R_pm = [Res(), Res()]
            R_cs, R_sT, R_ab, R_msb, R_pT = Res(), Res(), Res(), Res(), Res()
            S.dma("sp", lambda e: e.dma_start(out=cs[:], in_=cvec[:, :]), writes=[R_cs])
            S.op("act", lambda e: e.activation(out=cs[:], in_=cs[:], func=AF.Silu), reads=[R_cs], writes=[R_cs])
            for c in range(8):
                S.op("pe", lambda e, c=c: e.transpose(out=pT[:, c, :], in_=cs[:, c * 128:(c + 1) * 128], identity=ident[0:NV, 0:NV]),
                     reads=[R_cs, R_c], writes=[R_pT], signal=(c == 7))
            S.op("dve", lambda e: e.tensor_copy(out=sT[:], in_=pT[:]), reads=[R_pT], writes=[R_sT])
            for l in range(2):
                for v in range(NV):
                    S.dma("sp", lambda e, l=l, v=v: e.dma_start(out=ab[v:v + 1, :], in_=ada_b[l:l + 1, :]), writes=[R_ab])
                for j in range(12):
                    k = j % 2
                    S.dma("sp", lambda e, l=l, j=j, k=k: e.dma_start(
                        out=aw[k][:], in_=ada_w[l, :, j * 512:(j + 1) * 512].rearrange("(c p) n -> p c n", p=128)),
                        writes=[R_aw[k]])
                    for c in range(8):
                        S.op("pe", lambda e, c=c, k=k: e.matmul(pm[k][:], lhsT=sT[:, c, :], rhs=aw[k][:, c, :],
                                                                start=(c == 0), stop=(c == 7)),
                             reads=[R_sT, R_aw[k]], writes=[R_pm[k]], signal=(c == 7))
                    S.op("dve", lambda e, j=j, k=k: e.tensor_tensor(out=msb[:, j * 512:(j + 1) * 512], in0=pm[k][:],
                                                                     in1=ab[:, j * 512:(j + 1) * 512], op=ALU.add),
                         reads=[R_pm[k], R_ab], writes=[R_msb])
                S.dma("sp", lambda e, l=l: e.dma_start(out=MOD[l, :, :], in_=msb[:]), reads=[R_msb], writes=[R_MOD])
            S.barrier()
            S.emit()

        def load_bcast(st, name, src_ap, reads, n=D):
            t = sbuf(st, name, [128, n], F32)
            r = Res(name)
            S.dma("sp", lambda e: e.dma_start(out=t[:], in_=src_ap.partition_broadcast(128)), reads=reads, writes=[r])
            return t, r

        def norm_mod_tiles(st, l, which, vecs):
            res = {}
            gt, rg = load_bcast(st, "ng%d%d" % (l, which), norm_g[l, which, :], [])
            for v in vecs:
                sc, rsc = load_bcast(st, "sc%d" % v, MOD[l, v, (3 * which + 1) * D:(3 * which + 2) * D], [R_MOD])
                shh, rsh = load_bcast(st, "sh%d" % v, MOD[l, v, (3 * which) * D:(3 * which + 1) * D], [R_MOD])
                S.op("dve", lambda e, sc=sc: e.scalar_tensor_tensor(out=sc[:], in0=sc[:], scalar=1.0, in1=gt[:],
                                                                      op0=ALU.add, op1=ALU.mult),
                     reads=[rsc, rg], writes=[rsc])
                res[v] = (sc, rsc, shh, rsh)
            return res

        def rms_mod(st_tmp, xt, r_xt, G, rG, SH, rSH, ht, r_ht, small, r_small, junk, r_junk):
            S.op("dve", lambda e: e.memset(small[:, 0:1], 0.0), reads=[r_small], writes=[r_small])
            S.op("act", lambda e: e.activation(out=junk[:], in_=xt[:], func=AF.Square, accum_out=small[:, 0:1]),
                 reads=[r_xt, r_small], writes=[r_junk, r_small])
            S.op("act", lambda e: e.activation(out=small[:, 1:2], in_=small[:, 0:1], func=AF.Sqrt, scale=1.0 / D, bias=small[:, 3:4]),
                 reads=[r_small], writes=[r_small])
            S.op("dve", lambda e: e.reciprocal(out=small[:, 2:3], in_=small[:, 1:2]), reads=[r_small], writes=[r_small])
            S.op("dve", lambda e: e.scalar_tensor_tensor(out=ht[:], in0=xt[:], scalar=small[:, 2:3], in1=G[:],
                                                         op0=ALU.mult, op1=ALU.mult),
                 reads=[r_xt, r_small, rG], writes=[r_ht])
            S.op("dve", lambda e: e.tensor_tensor(out=ht[:], in0=ht[:], in1=SH[:], op=ALU.add),
                 reads=[r_ht, rSH], writes=[r_ht])

        def new_small(st, name):
            small = sbuf(st, name, [128, 4], F32)
            r = Res(name)
            S.op("dve", lambda e: e.memset(small[:], 0.0), writes=[r])
            S.op("dve", lambda e: e.memset(small[:, 3:4], EPS), reads=[r], writes=[r])
            return small, r

        def norm_to_hT(st, l, b, src_ap, src_res, hT, r_hT, vec_of_tile):
            with contextlib.ExitStack() as s2:
                vecs = sorted(set(vec_of_tile))
                tiles = norm_mod_tiles(s2, l, 0, vecs)
                small, r_small = zip(*[new_small(s2, "n1small%d" % i) for i in range(2)])
                junk = [sbuf(s2, "n1junk%d" % i, [128, D], F32) for i in range(2)]; r_junk = [Res(), Res()]
                xt = [sbuf(s2, "n1x%d" % i, [128, D], F32) for i in range(2)]
                r_xt = [Res(), Res()]
                ht = [sbuf(s2, "n1h%d" % i, [128, D], F32) for i in range(2)]
                r_ht = [Res(), Res()]
                ptp = [psum(s2, "n1p%d" % i, [128, 4, 128], F32) for i in range(2)]
                r_ptp = [Res(), Res()]
                def stage_a(i):
                    k = i % 2
                    G, rG, SH, rSH = tiles[vec_of_tile[i]]
                    S.dma("sp", lambda e: e.dma_start(out=xt[k][:], in_=src_ap[b, i * 128:(i + 1) * 128, :]),
                          reads=[src_res[(b, i)]], writes=[r_xt[k]])
                    rms_mod(s2, xt[k], r_xt[k], G, rG, SH, rSH, ht[k], r_ht[k], small[k], r_small[k], junk[k], r_junk[k])

                def stage_b(i):
                    k = i % 2
                    for hf in range(2):
                        for c4 in range(4):
                            c = hf * 4 + c4
                            S.op("pe", lambda e, hf=hf, c4=c4, c=c: e.transpose(
                                out=ptp[hf][:, c4, :], in_=ht[k][:, c * 128:(c + 1) * 128], identity=ident[:]),
                                reads=[r_ht[k], R_c], writes=[r_ptp[hf]], signal=(c4 == 3))
                        if (hf + i) % 2 == 0:
                            S.op("act", lambda e, hf=hf: e.activation(
                                out=hT[:, hf * 4:(hf + 1) * 4, i * 128:(i + 1) * 128], in_=ptp[hf][:], func=AF.Copy),
                                reads=[r_ptp[hf]], writes=[r_hT])
                        else:
                            S.op("dve", lambda e, hf=hf: e.tensor_copy(
                                out=hT[:, hf * 4:(hf + 1) * 4, i * 128:(i + 1) * 128], in_=ptp[hf][:]),
                                reads=[r_ptp[hf]], writes=[r_hT])

                stage_a(0)
                for i in range(NTI):
                    if i + 1 < NTI:
                        stage_a(i + 1)
                    stage_b(i)
                S.barrier()
                S.emit()

        def stream_w(st, name, n=2):
            bufs = [sbuf(st, "%s%d" % (name, i), [128, 8, 512], BF16) for i in range(n)]
            return bufs, [Res() for _ in range(n)]

        def load_w(buf, r, w_ap, col0, ncol=512):
            S.dma("pool", lambda e: e.dma_start(out=buf[:, :, 0:ncol],
                                                 in_=w_ap[:, col0:col0 + ncol].rearrange("(c p) n -> p c n", p=128)),
                  writes=[r])

        TOKP = [(i * 512, min(512, NT - i * 512)) for i in range((NT + 511) // 512)]

        def outproj_residual(st, l, b, srcs, w_ap, x_src, x_res, tiles_range, vec_of_tile, x_tok_off=0):
            with contextlib.ExitStack() as s2:
                wo = sbuf(s2, "wo", [128, 8, D], BF16); r_wo = Res()
                for hh in range(2):
                    S.dma("pool", lambda e, hh=hh: e.dma_start(
                        out=wo[:, :, hh * 512:(hh + 1) * 512],
                        in_=w_ap[:, hh * 512:(hh + 1) * 512].rearrange("(c p) n -> p c n", p=128)), writes=[r_wo])
                g1 = {}
                for v in sorted(set(vec_of_tile[i] for i in tiles_range)):
                    g1[v] = load_bcast(s2, "g1_%d" % v, MOD[l, v, 2 * D:3 * D], [R_MOD])
                xt = [sbuf(s2, "opx%d" % i, [128, D], F32) for i in range(2)]
                r_xt = [Res(), Res()]
                py = [psum(s2, "opy%d" % i, [128, 512], F32) for i in range(4)]
                r_py = [Res() for _ in range(4)]
                for n_i, i in enumerate(tiles_range):
                    k = n_i % 2
                    gt, rgt = g1[vec_of_tile[i]]
                    S.dma("sp", lambda e, i=i, k=k: e.dma_start(out=xt[k][:], in_=x_src[b, i * 128:(i + 1) * 128, :]),
                          reads=[x_res[(b, i)]], writes=[r_xt[k]])
                    for hh in range(2):
                        pk = k * 2 + hh
                        for c in range(8):
                            tsr, ch, rs, toff = srcs[c]
                            S.op("pe", lambda e, tsr=tsr, ch=ch, toff=toff, i=i, c=c, hh=hh, pk=pk: e.matmul(
                                py[pk][:], lhsT=tsr[:, ch, i * 128 - toff:(i + 1) * 128 - toff],
                                rhs=wo[:, c, hh * 512:(hh + 1) * 512], start=(c == 0), stop=(c == 7)),
                                reads=[rs, r_wo], writes=[r_py[pk]], signal=(c == 7))
                        S.op("dve", lambda e, hh=hh, pk=pk, gt=gt, k=k: e.tensor_tensor(
                            out=xg[k][:, hh * 512:(hh + 1) * 512],
                            in0=py[pk][:], in1=gt[:, hh * 512:(hh + 1) * 512], op=ALU.mult),
                            reads=[r_py[pk], rgt], writes=[r_xg[k]])
                    S.op("pool", lambda e, k=k: e.tensor_tensor(out=xt[k][:], in0=xt[k][:], in1=xg[k][:], op=ALU.add),
                         reads=[r_xg[k], r_xt[k]], writes=[r_xt[k]])
                    S.dma("sp", lambda e, i=i, k=k: e.dma_start(out=XR[b, i * 128:(i + 1) * 128, :], in_=xt[k][:]),
                          reads=[r_xt[k]], writes=[R_XRT[(b, i)]])
                S.barrier()
                S.emit()

        xg = [sbuf(g, "xg%d" % i, [128, D], F32) for i in range(2)]
        r_xg = [Res(), Res()]

        VEC_OF_TILE = lambda b: [NB, NB] + [b] * 16

        def layer0_mixer(b):
            with contextlib.ExitStack() as st:
                qT = sbuf(st, "qT", [128, 4, NT], BF16); r_qT = Res()
                kT = sbuf(st, "kT", [128, 4, NT], BF16); r_kT = Res()
                V = sbuf(st, "V", [128, NTI, 512], BF16); r_V = Res()
                OB = sbuf(st, "OB", [128, 4, NT], BF16); r_OB = Res()
                with contextlib.ExitStack() as s1:
                    hT = sbuf(s1, "hT", [128, 8, NT], BF16); r_hT = Res()
                    norm_to_hT(s1, 0, b, xin, R_XIN, hT, r_hT, VEC_OF_TILE(b))
                    wb, r_wb = stream_w(s1, "wi")
                    blk1 = sbuf(s1, "blk1", [128, 128], BF16); r_blk = Res()
                    S.op("dve", lambda e: e.memset(blk1[:], 0.0), writes=[r_blk])
                    S.op("dve", lambda e: e.memset(blk1[0:64, 0:64], 1.0 / 64), reads=[r_blk], writes=[r_blk])
                    S.op("dve", lambda e: e.memset(blk1[64:128, 64:128], 1.0 / 64), reads=[r_blk], writes=[r_blk])
                    pj = [psum(s1, "pj%d" % i, [128, 512], F32) for i in range(3)]
                    r_pj = [Res() for _ in range(3)]
                    pq = [psum(s1, "pq%d" % i, [128, 512], F32) for i in range(2)]
                    r_pq = [Res() for _ in range(2)]
                    sq = [sbuf(s1, "sq%d" % i, [128, 512], BF16) for i in range(2)]
                    r_sq = [Res(), Res()]
                    rs_t = [sbuf(s1, "rst%d" % i, [128, 512], F32) for i in range(2)]
                    r_rs = [Res(), Res()]
                    eps64 = sbuf(s1, "eps64", [128, 2], F32); r_e64 = Res()
                    S.op("dve", lambda e: e.memset(eps64[:, 0:1], EPS), writes=[r_e64])
                    S.op("dve", lambda e: e.memset(eps64[:, 1:2], 64.0 * EPS), reads=[r_e64], writes=[r_e64])
                    cnt = [0]

                    def proj_piece(wbuf, r_w, wc, tp):
                        t0, nt_ = TOKP[tp]
                        k = cnt[0] % 3
                        cnt[0] += 1
                        for c in range(8):
                            S.op("pe", lambda e, c=c, k=k: e.matmul(pj[k][:, 0:nt_], lhsT=wbuf[:, c, wc * 128:(wc + 1) * 128],
                                                                   rhs=hT[:, c, t0:t0 + nt_], start=(c == 0), stop=(c == 7)),
                                 reads=[r_w, r_hT], writes=[r_pj[k]], signal=(c == 7))
                        return k, t0, nt_

                    for grp, (dst, r_dst, gcol, sc_, ecol) in enumerate(((qT, r_qT, PC_QG, 64.0, 1), (kT, r_kT, PC_KG, 1.0, 0))):
                        bi = grp % 2
                        load_w(wb[bi], r_wb[bi], ev_w_in, grp * 512)
                        for wc in range(4):
                            for tp in range(len(TOKP)):
                                k, t0, nt_ = proj_piece(wb[bi], r_wb[bi], wc, tp)
                                k2 = cnt[0] % 2
                                S.op("act", lambda e, k=k, k2=k2, nt_=nt_: e.activation(out=sq[k2][:, 0:nt_], in_=pj[k][:, 0:nt_], func=AF.Square),
                                     reads=[r_pj[k]], writes=[r_sq[k2]])
                                S.op("pe", lambda e, k2=k2, nt_=nt_: e.matmul(pq[k2][:, 0:nt_], lhsT=blk1[:], rhs=sq[k2][:, 0:nt_], start=True, stop=True),
                                     reads=[r_blk, r_sq[k2]], writes=[r_pq[k2]])
                                S.op("act", lambda e, k2=k2, nt_=nt_, sc_=sc_, ecol=ecol: e.activation(
                                    out=rs_t[k2][:, 0:nt_], in_=pq[k2][:, 0:nt_], func=AF.Sqrt, scale=sc_, bias=eps64[:, ecol:ecol + 1]),
                                    reads=[r_pq[k2], r_e64], writes=[r_rs[k2]])
                                S.op("dve", lambda e, k2=k2, nt_=nt_: e.reciprocal(out=rs_t[k2][:, 0:nt_], in_=rs_t[k2][:, 0:nt_]),
                                     reads=[r_rs[k2]], writes=[r_rs[k2]])
                                S.op("dve", lambda e, k=k, k2=k2, nt_=nt_, t0=t0, wc=wc, dst=dst, gcol=gcol: e.scalar_tensor_tensor(
                                    out=dst[:, wc, t0:t0 + nt_], in0=pj[k][:, 0:nt_], scalar=pcols[:, gcol:gcol + 1], in1=rs_t[k2][:, 0:nt_],
                                    op0=ALU.mult, op1=ALU.mult), reads=[r_pj[k], r_rs[k2], R_c], writes=[r_dst])
                    load_w(wb[0], r_wb[0], ev_w_in, 1024)
                    for i in range(NTI):
                        k = cnt[0] % 3
                        cnt[0] += 1
                        for c in range(8):
                            S.op("pe", lambda e, c=c, k=k, i=i: e.matmul(pj[k][:], lhsT=hT[:, c, i * 128:(i + 1) * 128], rhs=wb[0][:, c, :],
                                                                        start=(c == 0), stop=(c == 7)),
                                 reads=[r_wb[0], r_hT], writes=[r_pj[k]], signal=(c == 7))
                        S.op("act", lambda e, k=k, i=i: e.activation(out=V[:, i, :], in_=pj[k][:], func=AF.Copy),
                             reads=[r_pj[k]], writes=[r_V])
                    load_w(wb[1], r_wb[1], ev_w_in, 2560)
                    wcg = sbuf(s1, "wcg", [128, 8, 512], BF16); r_wcg = Res()
                    load_w(wcg, r_wcg, ev_w_in, 2048)
                    load_w(wb[0], r_wb[0], ev_w_in, 1536)
                    xs_f = sbuf(s1, "xs_f", [128, NT], F32); r_xs = Res()
                    cx_f = sbuf(s1, "cx_f", [128, NT], F32); r_cx = Res()
                    t_f = sbuf(s1, "t_f", [128, NT], F32); r_t = Res()
                    for f in range(4):
                        for tp in range(len(TOKP)):
                            k, t0, nt_ = proj_piece(wb[1], r_wb[1], f, tp)
                            S.op("act", lambda e, k=k, t0=t0, nt_=nt_: e.activation(out=xs_f[:, t0:t0 + nt_], in_=pj[k][:, 0:nt_], func=AF.Copy),
                                 reads=[r_pj[k]], writes=[r_xs])
                        for tp in range(len(TOKP)):
                            k, t0, nt_ = proj_piece(wcg, r_wcg, f, tp)
                            S.op("dve", lambda e, k=k, t0=t0, nt_=nt_: e.tensor_tensor(out=cx_f[:, t0:t0 + nt_], in0=pj[k][:, 0:nt_],
                                                                                      in1=xs_f[:, t0:t0 + nt_], op=ALU.mult),
                                 reads=[r_pj[k], r_xs], writes=[r_cx])
                        w0 = pcols[:, PC_ECW + 0 * 4 + f:PC_ECW + 0 * 4 + f + 1]
                        w1 = pcols[:, PC_ECW + 1 * 4 + f:PC_ECW + 1 * 4 + f + 1]
                        w2 = pcols[:, PC_ECW + 2 * 4 + f:PC_ECW + 2 * 4 + f + 1]
                        bb = pcols[:, PC_ECB + f:PC_ECB + f + 1]
                        S.op("dve", lambda e, w1=w1, bb=bb: e.tensor_scalar(out=t_f[:], in0=cx_f[:], scalar1=w1, scalar2=bb, op0=ALU.mult, op1=ALU.add),
                             reads=[r_cx, R_c], writes=[r_t])
                        for (s0, sn) in ((0, LC), (LC, T)):
                            S.op("dve", lambda e, s0=s0, sn=sn, w0=w0: e.scalar_tensor_tensor(
                                out=t_f[:, s0 + 1:s0 + sn], in0=cx_f[:, s0:s0 + sn - 1], scalar=w0, in1=t_f[:, s0 + 1:s0 + sn],
                                op0=ALU.mult, op1=ALU.add), reads=[r_cx, r_t, R_c], writes=[r_t])
                            S.op("dve", lambda e, s0=s0, sn=sn, w2=w2: e.scalar_tensor_tensor(
                                out=t_f[:, s0:s0 + sn - 1], in0=cx_f[:, s0 + 1:s0 + sn], scalar=w2, in1=t_f[:, s0:s0 + sn - 1],
                                op0=ALU.mult, op1=ALU.add), reads=[r_cx, r_t, R_c], writes=[r_t])
                        for tp in range(len(TOKP)):
                            k, t0, nt_ = proj_piece(wb[0], r_wb[0], f, tp)
                            S.op("dve", lambda e, k=k, t0=t0, nt_=nt_, f=f: e.tensor_tensor(out=OB[:, f, t0:t0 + nt_], in0=pj[k][:, 0:nt_],
                                                                                           in1=t_f[:, t0:t0 + nt_], op=ALU.mult),
                                 reads=[r_pj[k], r_t], writes=[r_OB])
                    S.barrier()
                    S.emit()
                OA = sbuf(st, "OA", [128, 4, NT], BF16); r_OA = Res()
                with contextlib.ExitStack() as s1:
                    bias = [sbuf(s1, "bias%d" % i, [128, 5, 5, 128], BF16) for i in range(2)]
                    r_bias = [Res(), Res()]
                    sta = [psum(s1, "sta%d" % i, [128, 4, 128], F32) for i in range(2)]
                    stb = [psum(s1, "stb%d" % i, [128, 4, 128], F32) for i in range(2)]
                    r_sta = [Res(), Res()]; r_stb = [Res(), Res()]
                    po = [psum(s1, "po%d" % i, [64, 2, 128], F32) for i in range(2)]
                    r_po = [Res(), Res()]
                    PT = [sbuf(s1, "PT%d" % i, [128, 7, 128], BF16) for i in range(2)]
                    r_PT = [Res(), Res()]
                    rc = [sbuf(s1, "rc%d" % i, [64, 128], F32) for i in range(2)]
                    r_rc = [Res(), Res()]
                    it = 0
                    for h in range(8):
                        hp, hc = h % 2, h // 2
                        bsel = h % 2
                        S.dma("pool", lambda e, h=h, bsel=bsel: e.dma_start(out=bias[bsel][:], in_=biasT[h].rearrange("t c p q -> p t c q")),
                              writes=[r_bias[bsel]])
                        jobs = [("ctx", 0), ("ctx", 1)] + [("lat", j) for j in range(16)]
                        for kind, j in jobs:
                            k = it % 2
                            it += 1
                            if kind == "ctx":
                                qtile = j
                                ktiles = [0, 1]
                                ty = None
                            else:
                                qtile = 2 + j
                                ws = min(max(2 * j - 4, 0), 23)
                                a = ws // 2
                                ktiles = [0, 1] + [2 + a + cc for cc in range(5)]
                                ty = {0: 0, 1: 1, 14: 3, 15: 4}.get(j, 2)
                            nk = len(ktiles)
                            q_ap = qT[hp * 64:(hp + 1) * 64, hc, qtile * 128:(qtile + 1) * 128]
                            for kc, kt in enumerate(ktiles):
                                dstp, r_dst = (sta[k], r_sta[k]) if kc < 4 else (stb[k], r_stb[k])
                                last = (ty is None) and ((kc == min(3, nk - 1)) or (kc == nk - 1))
                                S.op("pe", lambda e, dstp=dstp, kc=kc, kt=kt, q_ap=q_ap, hp=hp, hc=hc: e.matmul(
                                    dstp[:, kc % 4, :], lhsT=kT[hp * 64:(hp + 1) * 64, hc, kt * 128:(kt + 1) * 128], rhs=q_ap,
                                    start=(kc % 4 == 0), stop=True, skip_group_check=True), reads=[r_kT, r_qT], writes=[r_dst], signal=last)
                            if ty is not None:
                                for kc in range(2, nk):
                                    dstp, r_dst = (sta[k], r_sta[k]) if kc < 4 else (stb[k], r_stb[k])
                                    last = (kc == 3) or (kc == nk - 1)
                                    S.op("pe", lambda e, dstp=dstp, kc=kc, ty=ty, bsel=bsel: e.matmul(
                                        dstp[:, kc % 4, :], lhsT=identb[:], rhs=bias[bsel][:, ty, kc - 2, :], start=False, stop=True,
                                        skip_group_check=True), reads=[r_bias[bsel], R_c], writes=[r_dst], signal=last)
                            na = min(4, nk)
                            S.op("act", lambda e, k=k, na=na: e.activation(out=PT[k][:, 0:na, :], in_=sta[k][:, 0:na, :], func=AF.Exp),
                                 reads=[r_sta[k]], writes=[r_PT[k]])
                            if nk > 4:
                                S.op("act", lambda e, k=k, nk=nk: e.activation(out=PT[k][:, 4:nk, :], in_=stb[k][:, 0:nk - 4, :], func=AF.Exp),
                                     reads=[r_stb[k]], writes=[r_PT[k]])
                            for kc, kt in enumerate(ktiles):
                                S.op("pe", lambda e, k=k, kc=kc, kt=kt, nk=nk, h=h: e.matmul(
                                    po[k][:, 0, :], lhsT=V[:, kt, h * 64:(h + 1) * 64], rhs=PT[k][:, kc, :], start=(kc == 0), stop=(kc == nk - 1)),
                                    reads=[r_V, r_PT[k]], writes=[r_po[k]], signal=False)
                            for kc, kt in enumerate(ktiles):
                                S.op("pe", lambda e, k=k, kc=kc, nk=nk: e.matmul(
                                    po[k][:, 1, :], lhsT=onesb[:, 0:64], rhs=PT[k][:, kc, :], start=(kc == 0), stop=(kc == nk - 1)),
                                    reads=[R_c, r_PT[k]], writes=[r_po[k]], signal=(kc == nk - 1))
                            S.op("dve", lambda e, k=k: e.reciprocal(out=rc[k][:], in_=po[k][:, 1, :]), reads=[r_po[k]], writes=[r_rc[k]])
                            S.op("dve", lambda e, k=k, hp=hp, hc=hc, qtile=qtile: e.tensor_tensor(
                                out=OA[hp * 64:(hp + 1) * 64, hc, qtile * 128:(qtile + 1) * 128], in0=po[k][:, 0, :], in1=rc[k][:], op=ALU.mult),
                                reads=[r_po[k], r_rc[k]], writes=[r_OA])
                    S.barrier()
                    S.emit()
                srcs = [(OA, c, r_OA, 0) for c in range(4)] + [(OB, c, r_OB, 0) for c in range(4)]
                outproj_residual(st, 0, b, srcs, ev_w_out, xin, R_XIN, list(range(NTI)), VEC_OF_TILE(b))

        def layer1_mixer(b):
            with contextlib.ExitStack() as st:
                UC = sbuf(st, "UC", [128, 8, NT], BF16); r_UC = Res()
                GG = sbuf(st, "GG", [128, 8, T], BF16); r_GG = Res()
                with contextlib.ExitStack() as s1:
                    hT = sbuf(s1, "hT1", [128, 8, NT], BF16); r_hT = Res()
                    norm_to_hT(s1, 1, b, XR, R_XRT, hT, r_hT, VEC_OF_TILE(b))
                    wb, r_wb = stream_w(s1, "wi1")
                    pj = [psum(s1, "pj1%d" % i, [128, 512], F32) for i in range(3)]
                    r_pj = [Res() for _ in range(3)]
                    u_f = sbuf(s1, "u_f", [128, NT], F32); r_u = Res()
                    t_f = sbuf(s1, "t1_f", [128, NT], F32); r_t = Res()
                    cnt = [0]
                    for grp in range(4):
                        bi = grp % 2
                        load_w(wb[bi], r_wb[bi], od_w_in, grp * 512)
                        for wc in range(4):
                            f = (grp % 2) * 4 + wc
                            for tp in range(len(TOKP)):
                                t0, nt_ = TOKP[tp]
                                if grp < 2 and t0 + nt_ <= LC:
                                    continue
                                k = cnt[0] % 3
                                cnt[0] += 1
                                for c in range(8):
                                    S.op("pe", lambda e, c=c, k=k, bi=bi, wc=wc, t0=t0, nt_=nt_: e.matmul(
                                        pj[k][:, 0:nt_], lhsT=wb[bi][:, c, wc * 128:(wc + 1) * 128], rhs=hT[:, c, t0:t0 + nt_],
                                        start=(c == 0), stop=(c == 7)), reads=[r_wb[bi], r_hT], writes=[r_pj[k]], signal=(c == 7))
                                if grp < 2:
                                    lo = max(t0, LC)
                                    S.op("act", lambda e, k=k, f=f, lo=lo, t0=t0, nt_=nt_: e.activation(
                                        out=GG[:, f, lo - LC:t0 + nt_ - LC], in_=pj[k][:, lo - t0:nt_], func=AF.Gelu),
                                        reads=[r_pj[k]], writes=[r_GG])
                                else:
                                    S.op("act", lambda e, k=k, t0=t0, nt_=nt_: e.activation(out=u_f[:, t0:t0 + nt_], in_=pj[k][:, 0:nt_], func=AF.Copy),
                                         reads=[r_pj[k]], writes=[r_u])
                            if grp >= 2:
                                wj = [pcols[:, PC_OCW + j * 8 + f:PC_OCW + j * 8 + f + 1] for j in range(4)]
                                bb = pcols[:, PC_OCB + f:PC_OCB + f + 1]
                                S.op("dve", lambda e, wj=wj, bb=bb: e.tensor_scalar(out=t_f[:], in0=u_f[:], scalar1=wj[1], scalar2=bb, op0=ALU.mult, op1=ALU.add),
                                     reads=[r_u, R_c], writes=[r_t])
                                for (s0, sn) in ((0, LC), (LC, T)):
                                    S.op("dve", lambda e, s0=s0, sn=sn, wj=wj: e.scalar_tensor_tensor(
                                        out=t_f[:, s0 + 1:s0 + sn], in0=u_f[:, s0:s0 + sn - 1], scalar=wj[0], in1=t_f[:, s0 + 1:s0 + sn],
                                        op0=ALU.mult, op1=ALU.add), reads=[r_u, r_t, R_c], writes=[r_t])
                                    S.op("dve", lambda e, s0=s0, sn=sn, wj=wj: e.scalar_tensor_tensor(
                                        out=t_f[:, s0:s0 + sn - 1], in0=u_f[:, s0 + 1:s0 + sn], scalar=wj[2], in1=t_f[:, s0:s0 + sn - 1],
                                        op0=ALU.mult, op1=ALU.add), reads=[r_u, r_t, R_c], writes=[r_t])
                                    S.op("dve", lambda e, s0=s0, sn=sn, wj=wj: e.scalar_tensor_tensor(
                                        out=t_f[:, s0:s0 + sn - 2], in0=u_f[:, s0 + 2:s0 + sn], scalar=wj[3], in1=t_f[:, s0:s0 + sn - 2],
                                        op0=ALU.mult, op1=ALU.add), reads=[r_u, r_t, R_c], writes=[r_t])
                                S.op("act", lambda e, f=f: e.activation(out=UC[:, f, :], in_=t_f[:], func=AF.Copy), reads=[r_t], writes=[r_UC])
                    S.barrier()
                    S.emit()
                YI, r_YI = GG, r_GG
                with contextlib.ExitStack() as s1:
                    gw = sbuf(s1, "gw", [128, 4, 4, 2, 256], BF16); r_gw = Res()
                    for m in range(4):
                        S.dma("pool", lambda e, m=m: e.dma_start(out=gw[:, m], in_=od_gw[m].rearrange("b (k p) n -> p b k n", p=128)),
                              writes=[r_gw])
                    cl = sbuf(s1, "cl", [128, 2, 8], F32); r_cl = Res()
                    for d_ in range(2):
                        lam = pcols[:, PC_G + d_ * 24 + 16:PC_G + d_ * 24 + 24]
                        S.op("act", lambda e, d_=d_, lam=lam: e.activation(out=cl[:, d_, :], in_=lam, func=AF.Exp, scale=-1.0), reads=[R_c], writes=[r_cl])
                        S.op("act", lambda e, d_=d_: e.activation(out=cl[:, d_, :], in_=cl[:, d_, :], func=AF.Ln, bias=1.0, scale=1.0), reads=[r_cl], writes=[r_cl])
                        S.op("dve", lambda e, d_=d_: e.tensor_scalar(out=cl[:, d_, :], in0=cl[:, d_, :], scalar1=-8.0, scalar2=None, op0=ALU.mult),
                             reads=[r_cl], writes=[r_cl])
                    pg = [psum(s1, "pg%d" % i, [128, 512], F32) for i in range(4)]
                    r_pg = [Res() for _ in range(4)]
                    Rts = [sbuf(s1, "Rt%d" % i, [128, NT], F32) for i in range(2)]; r_Rs = [Res(), Res()]
                    Its = [sbuf(s1, "It%d" % i, [128, NT], F32) for i in range(2)]; r_Is = [Res(), Res()]
                    Ats = [sbuf(s1, "At%d" % i, [128, NT], F32) for i in range(2)]; r_As = [Res(), Res()]
                    Bts = [sbuf(s1, "Bt%d" % i, [128, NT], F32) for i in range(2)]; r_Bs = [Res(), Res()]
                    Hf = sbuf(s1, "Hf", [128, NT], F32); r_Hf = Res()
                    Hb = sbuf(s1, "Hb", [128, NT], F32); r_Hb = Res()
                    cnt = [0]
                    for f in range(8):
                        blk = f // 2
                        for d_ in range(2):
                            Rt, It, At, Bt = Rts[d_], Its[d_], Ats[d_], Bts[d_]
                            r_R, r_I, r_A, r_B = r_Rs[d_], r_Is[d_], r_As[d_], r_Bs[d_]
                            for gi, (dstt, r_dst) in enumerate(((Rt, r_R), (It, r_I))):
                                m = d_ * 2 + gi
                                bcol = pcols[:, PC_G + d_ * 24 + gi * 8 + f:PC_G + d_ * 24 + gi * 8 + f + 1]
                                for tp in range(len(TOKP)):
                                    t0, nt_ = TOKP[tp]
                                    k = cnt[0] % 4
                                    cnt[0] += 1
                                    for kc in range(2):
                                        S.op("pe", lambda e, k=k, m=m, kc=kc, t0=t0, nt_=nt_, blk=blk, f=f: e.matmul(
                                            pg[k][:, 0:nt_], lhsT=gw[:, m, blk, kc, (f % 2) * 128:(f % 2 + 1) * 128],
                                            rhs=UC[:, 2 * blk + kc, t0:t0 + nt_], start=(kc == 0), stop=(kc == 1)),
                                            reads=[r_gw, r_UC], writes=[r_pg[k]], signal=(kc == 1))
                                    S.op("act", lambda e, k=k, dstt=dstt, t0=t0, nt_=nt_, bcol=bcol: e.activation(
                                        out=dstt[:, t0:t0 + nt_], in_=pg[k][:, 0:nt_], func=AF.Sigmoid, bias=bcol, scale=1.0),
                                        reads=[r_pg[k], R_c], writes=[r_dst])
                            S.op("act", lambda e, d_=d_, f=f, At=At, Bt=Bt, Rt=Rt, It=It: e.activation(out=At[:], in_=Rt[:], func=AF.Exp, scale=cl[:, d_, f:f + 1]),
                                 reads=[r_R, r_cl], writes=[r_A])
                            S.op("pool", lambda e, At=At, Bt=Bt, Rt=Rt, It=It: e.tensor_tensor(out=Bt[:], in0=At[:], in1=At[:], op=ALU.mult), reads=[r_A], writes=[r_B])
                            S.op("act", lambda e, At=At, Bt=Bt, Rt=Rt, It=It: e.activation(out=Bt[:], in_=Bt[:], func=AF.Sqrt, scale=-1.0, bias=1.0), reads=[r_B], writes=[r_B])
                            S.op("dve", lambda e, At=At, Bt=Bt, Rt=Rt, It=It: e.tensor_tensor(out=Bt[:], in0=Bt[:], in1=It[:], op=ALU.mult), reads=[r_B, r_I], writes=[r_B])
                            S.op("dve", lambda e, f=f, At=At, Bt=Bt, Rt=Rt, It=It: e.tensor_tensor(out=Bt[:], in0=Bt[:], in1=UC[:, f, :], op=ALU.mult), reads=[r_B, r_UC], writes=[r_B])
                            if d_ == 0:
                                S.op("dve", lambda e, At=At, Bt=Bt, Rt=Rt, It=It: e.tensor_tensor_scan(out=Hf[:], data0=At[:], data1=Bt[:], initial=0.0, op0=ALU.mult, op1=ALU.add),
                                     reads=[r_A, r_B], writes=[r_Hf])
                            else:
                                S.op("dve", lambda e, At=At, Bt=Bt, Rt=Rt, It=It: e.tensor_tensor_scan(out=Hb[:, 0:LC][:, ::-1],
                                                                           data0=At[:, 0:LC][:, ::-1], data1=Bt[:, 0:LC][:, ::-1],
                                                                           initial=0.0, op0=ALU.mult, op1=ALU.add),
                                     reads=[r_A, r_B], writes=[r_Hb])
                                S.op("dve", lambda e, At=At, Bt=Bt, Rt=Rt, It=It: e.tensor_tensor_scan(out=Hb[:, LC:NT][:, ::-1], data0=At[:, LC:NT][:, ::-1],
                                                                           data1=Bt[:, LC:NT][:, ::-1], initial=Hb[:, 0:1],
                                                                           op0=ALU.mult, op1=ALU.add),
                                     reads=[r_A, r_B, r_Hb], writes=[r_Hb])
                        S.op("pool", lambda e: e.tensor_tensor(out=Hf[:, LC:NT], in0=Hf[:, LC:NT], in1=Hb[:, LC:NT], op=ALU.add),
                             reads=[r_Hf, r_Hb], writes=[r_Hf])
                        S.op("dve", lambda e, f=f: e.tensor_tensor(out=YI[:, f, :], in0=Hf[:, LC:NT], in1=GG[:, f, :], op=ALU.mult),
                             reads=[r_Hf, r_GG], writes=[r_YI])
                    S.barrier()
                    S.emit()
                srcs = [(YI, c, r_YI, LC) for c in range(8)]
                outproj_residual(st, 1, b, srcs, od_w_out, XR, R_XRT, list(range(2, NTI)), VEC_OF_TILE(b))

        def moe_layer(l, tok_tiles, final):
            ntile = len(tok_tiles)
            ntok = ntile * 128
            nblk = ntok * 4 // BLK + n_exp
            with contextlib.ExitStack() as st:
                sH2 = contextlib.ExitStack()
                LG = sbuf(st, "LG", [128, ntile, NE], F32); r_LG = Res()
                D4 = sbuf(st, "D4", [128, ntile * 4], I32); r_D4 = Res()
                G4 = sbuf(st, "G4", [128, ntile * 4], F32); r_G4 = Res()
                IDXW = sbuf(st, "IDXW", [128, nblk], I32); r_IDXW = Res()
                BEI = sbuf(st, "BEI", [128, nblk], I32)
                H2 = sbuf(sH2, "H2", [128, ntile * D], BF16); r_H2 = Res()
                with contextlib.ExitStack() as s1:
                    vecs = sorted(set((NB if i < 2 else b) for b, i in tok_tiles))
                    tiles = norm_mod_tiles(s1, l, 1, vecs)
                    small, r_small = zip(*[new_small(s1, "n2small%d" % i) for i in range(2)])
                    junk = [sbuf(s1, "n2junk%d" % i, [128, D], F32) for i in range(2)]; r_junk = [Res(), Res()]
                    xt = [sbuf(s1, "n2x%d" % i, [128, D], F32) for i in range(2)]
                    r_xt = [Res(), Res()]
                    ht = [sbuf(s1, "n2h%d" % i, [128, D], F32) for i in range(2)]
                    r_ht = [Res(), Res()]
                    hT32 = [sbuf(s1, "n2t%d" % i, [128, 8, 128], F32) for i in range(2)]
                    r_hT32 = [Res(), Res()]
                    ptp = [psum(s1, "n2p%d" % i, [128, 4, 128], F32) for i in range(2)]
                    r_ptp = [Res(), Res()]
                    plg = [psum(s1, "n2l%d" % i, [128, NE], F32) for i in range(2)]
                    r_plg = [Res(), Res()]
                    wr = sbuf(s1, "wr", [128, 8, NE], F32); r_wr = Res()
                    S.dma("sp", lambda e: e.dma_start(out=wr[:], in_=router_w[l].rearrange("(c p) n -> p c n", p=128)), writes=[r_wr])
                    rb, r_rb = load_bcast(s1, "rb", router_b[l, :], [], n=NE)
                    def stage_a(n_i):
                        b, i = tok_tiles[n_i]
                        k = n_i % 2
                        G, rG, SH, rSH = tiles[NB if i < 2 else b]
                        S.dma("sp", lambda e: e.dma_start(out=xt[k][:], in_=XR[b, i * 128:(i + 1) * 128, :]),
                              reads=[R_XRT[(b, i)]], writes=[r_xt[k]])
                        rms_mod(s1, xt[k], r_xt[k], G, rG, SH, rSH, ht[k], r_ht[k], small[k], r_small[k], junk[k], r_junk[k])
                        S.op("act", lambda e: e.activation(out=H2[:, n_i * D:(n_i + 1) * D], in_=ht[k][:], func=AF.Copy), reads=[r_ht[k]], writes=[r_H2])

                    def stage_b(n_i):
                        k = n_i % 2
                        for hf in range(2):
                            for c4 in range(4):
                                c = hf * 4 + c4
                                S.op("pe", lambda e, hf=hf, c4=c4, c=c: e.transpose(
                                    out=ptp[hf][:, c4, :], in_=ht[k][:, c * 128:(c + 1) * 128], identity=ident[:]),
                                    reads=[r_ht[k], R_c], writes=[r_ptp[hf]], signal=(c4 == 3))
                            if hf == 0:
                                S.op("act", lambda e: e.activation(out=hT32[k][:, 0:4, :], in_=ptp[0][:], func=AF.Copy),
                                     reads=[r_ptp[0]], writes=[r_hT32[k]])
                            else:
                                S.op("dve", lambda e: e.tensor_copy(out=hT32[k][:, 4:8, :], in_=ptp[1][:]),
                                     reads=[r_ptp[1]], writes=[r_hT32[k]])
                        for c in range(8):
                            S.op("pe", lambda e, c=c: e.matmul(plg[k][:], lhsT=hT32[k][:, c, :], rhs=wr[:, c, :], start=(c == 0), stop=(c == 7)),
                                 reads=[r_hT32[k], r_wr], writes=[r_plg[k]], signal=(c == 7))
                        S.op("dve", lambda e: e.tensor_tensor(out=LG[:, n_i, :], in0=plg[k][:], in1=rb[:], op=ALU.add),
                             reads=[r_plg[k], r_rb], writes=[r_LG])

                    stage_a(0)
                    for n_i in range(ntile):
                        if n_i + 1 < ntile:
                            stage_a(n_i + 1)
                        stage_b(n_i)
                    S.barrier()
                    S.emit()
                with contextlib.ExitStack() as s1:
                    MX = sbuf(s1, "MX", [128, ntile, 8], F32)
                    MASK = sbuf(s1, "MASK", [128, ntile, NE], F32)
                    MASKb = sbuf(s1, "MASKb", [128, ntile, NE], BF16)
                    CUM = sbuf(s1, "CUM", [128, ntile, NE], BF16)
                    GAT = sbuf(s1, "GAT", [128, ntile, NE], F32)
                    TMP = sbuf(s1, "TMP", [128, ntile, NE], F32)
                    KEY = sbuf(s1, "KEY", [128, ntile, NE], F32)
                    K8 = sbuf(s1, "K8", [128, ntile, 8], F32)
                    DEN = sbuf(s1, "DEN", [128, ntile], F32)
                    TRI = sbuf(s1, "TRI", [128, 128], BF16)
                    CNT = sbuf(s1, "CNT", [128, NE], F32)
                    CNTI = sbuf(s1, "CNTI", [128, NE], I32)
                    PAD = sbuf(s1, "PAD", [128, NE], F32)
                    PEND = sbuf(s1, "PEND", [128, NE], F32)
                    BASE = sbuf(s1, "BASE", [128, NE], F32)
                    ONE32 = sbuf(s1, "ONE32", [128, NE], F32)
                    CMP = sbuf(s1, "CMP", [128, nblk, NE], F32)
                    BEF = sbuf(s1, "BEF", [128, nblk], F32)
                    JB = sbuf(s1, "JB", [128, 80], F32)
                    IOP = sbuf(s1, "IOP", [128, 1], F32)
                    pposb = [psum(s1, "ppos%d" % i, [128, 16, NE], F32) for i in range((ntile + 15) // 16)]
                    pcnt = psum(s1, "pcnt", [128, NE], F32)
                    R = Res("route")
                    V_ = lambda fn, eng="dve": S.op(eng, fn, reads=[R, r_LG], writes=[R, r_D4, r_G4, r_IDXW])
                    S.dma("sp", lambda e: e.dma_start(out=JB[:], in_=blk_start_d[:, :]), writes=[R])
                    S.dma("sp", lambda e: e.dma_start(out=IOP[:], in_=iota_p_d[:, :]), reads=[R], writes=[R])
                    V_(lambda e: e.memset(TRI[:], 1.0))
                    V_(lambda e: e.affine_select(out=TRI[:], in_=TRI[:], pattern=[[1, 128]], compare_op=ALU.is_gt, fill=0.0,
                                                 base=0, channel_multiplier=-1), "pool")
                    V_(lambda e: e.memset(ONE32[:], 1.0))
                    for i in range(ntile):
                        V_(lambda e, i=i: e.max(out=MX[:, i, :], in_=LG[:, i, :]))
                    V_(lambda e: e.tensor_tensor(out=MASK[:], in0=LG[:], in1=MX[:, :, 3:4].to_broadcast([128, ntile, NE]), op=ALU.is_ge))
                    V_(lambda e: e.tensor_tensor(out=TMP[:], in0=LG[:], in1=MX[:, :, 0:1].to_broadcast([128, ntile, NE]), op=ALU.subtract))
                    V_(lambda e: e.activation(out=TMP[:], in_=TMP[:], func=AF.Exp), "act")
                    V_(lambda e: e.tensor_tensor(out=TMP[:], in0=TMP[:], in1=MASK[:], op=ALU.mult))
                    V_(lambda e: e.tensor_reduce(out=DEN[:], in_=TMP[:], axis=AX.X, op=ALU.add))
                    V_(lambda e: e.reciprocal(out=DEN[:], in_=DEN[:]))
                    V_(lambda e: e.tensor_tensor(out=GAT[:], in0=TMP[:], in1=DEN[:].unsqueeze(2).to_broadcast([128, ntile, NE]), op=ALU.mult))
                    V_(lambda e: e.tensor_copy(out=MASKb[:], in_=MASK[:]))
                    V_(lambda e: e.memset(CUM[:, 0, :], 0.0))
                    for i in range(1, ntile):
                        V_(lambda e, i=i: e.tensor_tensor(out=CUM[:, i, :], in0=CUM[:, i - 1, :], in1=MASKb[:, i - 1, :], op=ALU.add))
                    V_(lambda e: e.tensor_tensor(out=TMP[:, 0, :], in0=CUM[:, ntile - 1, :], in1=MASKb[:, ntile - 1, :], op=ALU.add))
                    TOTb = sbuf(s1, "TOTb", [128, NE], BF16)
                    V_(lambda e: e.tensor_copy(out=TOTb[:], in_=TMP[:, 0, :]))
                    for i in range(ntile):
                        V_(lambda e, i=i: e.matmul(pposb[i // 16][:, i % 16, :], lhsT=TRI[:], rhs=MASKb[:, i, :], start=True, stop=False), "pe")
                        V_(lambda e, i=i: e.matmul(pposb[i // 16][:, i % 16, :], lhsT=onesb[:, 0:128], rhs=CUM[:, i, :], start=False, stop=True), "pe")
                    V_(lambda e: e.matmul(pcnt[:], lhsT=onesb[:, 0:128], rhs=TOTb[:], start=True, stop=True), "pe")
                    V_(lambda e: e.tensor_copy(out=CNT[:], in_=pcnt[:]))
                    NM = ntok // BLK + 1
                    CMP2 = sbuf(s1, "CMP2", [128, NE, NM], F32)
                    V_(lambda e: e.tensor_tensor(out=CMP2[:], in0=CNT[:].unsqueeze(2).to_broadcast([128, NE, NM]),
                                                 in1=JB[:, 0:NM].unsqueeze(1).to_broadcast([128, NE, NM]), op=ALU.is_gt))
                    V_(lambda e: e.tensor_reduce(out=PAD[:], in_=CMP2[:], axis=AX.X, op=ALU.add))
                    V_(lambda e: e.tensor_scalar(out=PAD[:], in0=PAD[:], scalar1=float(BLK), scalar2=None, op0=ALU.mult))
                    V_(lambda e: e.tensor_tensor_scan(out=PEND[:], data0=ONE32[:], data1=PAD[:], initial=0.0, op0=ALU.mult, op1=ALU.add))
                    V_(lambda e: e.tensor_tensor(out=BASE[:], in0=PEND[:], in1=PAD[:], op=ALU.subtract))
                    for bk in range((ntile + 15) // 16):
                        n_ = min(16, ntile - bk * 16)
                        V_(lambda e, bk=bk, n_=n_: e.tensor_tensor(out=KEY[:, bk * 16:bk * 16 + n_, :], in0=pposb[bk][:, 0:n_, :],
                                                                  in1=BASE[:].unsqueeze(1).to_broadcast([128, n_, NE]), op=ALU.add))
                    V_(lambda e: e.scalar_tensor_tensor(out=KEY[:], in0=KEY[:], scalar=1.0, in1=MASK[:], op0=ALU.add, op1=ALU.mult))
                    for i in range(ntile):
                        V_(lambda e, i=i: e.max(out=K8[:, i, :], in_=KEY[:, i, :]))
                    V_(lambda e: e.tensor_scalar(out=TMP[:, :, 0:4], in0=K8[:, :, 0:4], scalar1=-1.0, scalar2=0.0, op0=ALU.add, op1=ALU.max))
                    V_(lambda e: e.tensor_scalar(out=TMP[:, :, 0:4], in0=TMP[:, :, 0:4], scalar1=float(nblk * BLK - 1), scalar2=None, op0=ALU.min))
                    V_(lambda e: e.tensor_copy(out=D4[:].rearrange("p (n j) -> p n j", j=4), in_=TMP[:, :, 0:4]))
                    for j in range(4):
                        V_(lambda e, j=j: e.tensor_tensor(out=TMP[:], in0=KEY[:], in1=K8[:, :, j:j + 1].to_broadcast([128, ntile, NE]), op=ALU.is_equal))
                        V_(lambda e: e.tensor_tensor(out=TMP[:], in0=TMP[:], in1=GAT[:], op=ALU.mult))
                        V_(lambda e, j=j: e.tensor_reduce(out=G4[:].rearrange("p (n j) -> p n j", j=4)[:, :, j], in_=TMP[:], axis=AX.X, op=ALU.add))
                    V_(lambda e: e.tensor_tensor(out=CMP[:], in0=PEND[:].unsqueeze(1).to_broadcast([128, nblk, NE]),
                                                 in1=JB[:, 0:nblk].unsqueeze(2).to_broadcast([128, nblk, NE]), op=ALU.is_le))
                    V_(lambda e: e.tensor_reduce(out=BEF[:], in_=CMP[:], axis=AX.X, op=ALU.add))
                    V_(lambda e: e.tensor_scalar(out=BEF[:], in0=BEF[:], scalar1=float(n_exp - 1), scalar2=0.0, op0=ALU.min, op1=ALU.max))
                    V_(lambda e: e.tensor_copy(out=BEI[:], in_=BEF[:]))
                    V_(lambda e: e.tensor_scalar(out=BEF[:], in0=BEF[:], scalar1=128.0, scalar2=IOP[:, 0:1], op0=ALU.mult, op1=ALU.add))
                    V_(lambda e: e.tensor_copy(out=IDXW[:], in_=BEF[:]))
                    if dbg:
                        S.dma("sp", lambda e: e.dma_start(out=dbg_d4[:, 0:ntile * 4], in_=D4[:]), reads=[R, r_D4])
                        S.dma("sp", lambda e: e.dma_start(out=dbg_g4[:, 0:ntile * 4], in_=G4[:]), reads=[R, r_G4])
                        S.dma("sp", lambda e: e.dma_start(out=dbg_iw[:, 0:nblk], in_=IDXW[:]), reads=[R, r_IDXW])
                    for n_i in range(ntile if moe_stop >= 2 else 0):
                        for j in range(4):
                            S.dma("pool", lambda e, n_i=n_i, j=j: e.indirect_dma_start(
                                out=XS[:, :], out_offset=bass.IndirectOffsetOnAxis(ap=D4[:, n_i * 4 + j:n_i * 4 + j + 1], axis=0),
                                in_=H2[:, n_i * D:(n_i + 1) * D], in_offset=None),
                                reads=[R, r_D4, r_H2], writes=[R_XS])
                    S.barrier()
                    S.emit()
                sH2.close()
                if moe_stop < 3:
                    return
                w_aps = (w_gate[l], w_up[l], w_down[l])
                with contextlib.ExitStack() as s1:
                    WG = [sbuf(s1, "WG%d" % i, [128, 8192], BF16) for i in range(2)]
                    WU = [sbuf(s1, "WU%d" % i, [128, 8192], BF16) for i in range(2)]
                    WD = [sbuf(s1, "WD%d" % i, [128, 8192], BF16) for i in range(2)]
                    EB = [sbuf(s1, "EB%d" % i, [2, 3072], BF16) for i in range(2)]
                    EBC = [sbuf(s1, "EBC%d" % i, [128, 16], F32) for i in range(2)]
                    r_EBC = [Res(), Res()]
                    r_W = [[Res() for _ in range(4)] for _ in range(2)]
                    xs = [sbuf(s1, "xsb%d" % i, [128, 4, D], BF16) for i in range(2)]
                    r_xs = [Res(), Res()]
                    xeT = [sbuf(s1, "xeT%d" % i, [128, 8, BLK], BF16) for i in range(2)]
                    r_xeT = [Res(), Res()]
                    actT = sbuf(s1, "actT", [128, 8, BLK], BF16); r_actT = Res()
                    gs = [sbuf(s1, "gs%d" % i, [128, BLK], F32) for i in range(2)]
                    sg = [sbuf(s1, "sg%d" % i, [128, BLK], F32) for i in range(2)]
                    us = [sbuf(s1, "us%d" % i, [128, BLK], F32) for i in range(2)]
                    r_gs = [Res(), Res()]; r_sg = [Res(), Res()]; r_us = [Res(), Res()]
                    ysb = [sbuf(s1, "ysb%d" % i, [128, 4, D], BF16) for i in range(2)]
                    r_ysb = [Res(), Res()]
                    ptr = [psum(s1, "ptr%d" % i, [128, 8, 128], BF16) for i in range(2)]
                    r_ptr = [Res(), Res()]
                    pgp = [psum(s1, "pgp%d" % i, [128, BLK], F32) for i in range(2)]
                    pup = [psum(s1, "pup%d" % i, [128, BLK], F32) for i in range(2)]
                    r_pgp = [Res(), Res()]; r_pup = [Res(), Res()]
                    pyp = [psum(s1, "pyp%d" % i, [128, 512], F32) for i in range(2)]
                    r_pyp = [Res(), Res()]
                    def load_xs(jb_):
                        k_ = jb_ % 2
                        S.dma("sp", lambda e: e.dma_start(
                            out=xs[k_][:], in_=XS[jb_ * BLK:(jb_ + 1) * BLK, :].rearrange("(s p) d -> p s d", p=128)),
                            reads=[R_XS], writes=[r_xs[k_]])

                    for jb in range(nblk):
                        k = jb % 2
                        for mi, (wbuf, wap) in enumerate(((WG[k], w_aps[0]), (WU[k], w_aps[1]), (WD[k], w_aps[2]))):
                            S.dma("pool", lambda e, wbuf=wbuf, wap=wap, jb=jb: e.indirect_dma_start(
                                out=wbuf[:, :], out_offset=None, in_=wap[:, :],
                                in_offset=bass.IndirectOffsetOnAxis(ap=IDXW[:, jb:jb + 1], axis=0)), reads=[r_IDXW], writes=[r_W[k][mi]])
                        S.dma("pool", lambda e, k=k, jb=jb: e.indirect_dma_start(
                            out=EB[k][:, :], out_offset=None, in_=exp_b[l][:, :],
                            in_offset=bass.IndirectOffsetOnAxis(ap=BEI[0:2, jb:jb + 1], axis=0)), reads=[r_IDXW], writes=[r_W[k][3]])
                        S.dma("pool", lambda e, k=k, jb=jb: e.indirect_dma_start(
                            out=EBC[k][:, :], out_offset=None, in_=exp_bc[l][:, :],
                            in_offset=bass.IndirectOffsetOnAxis(ap=IDXW[:, jb:jb + 1], axis=0)), reads=[r_IDXW], writes=[r_EBC[k]])
                        S.op("dve", lambda e, k=k: e.tensor_scalar(out=EBC[k][:, 8:16], in0=EBC[k][:, 8:16], scalar1=1.0, scalar2=None, op0=ALU.add),
                             reads=[r_EBC[k]], writes=[r_EBC[k]])
                        if jb == 0:
                            load_xs(0)
                        for s in range(4):
                            kk = s % 2
                            for c in range(8):
                                S.op("pe", lambda e, k=k, kk=kk, s=s, c=c: e.transpose(out=ptr[kk][:, c, :], in_=xs[k][:, s, c * 128:(c + 1) * 128],
                                                                                 identity=identb[:]),
                                     reads=[r_xs[k], R_c], writes=[r_ptr[kk]], signal=(c == 7))
                            if s % 2 == 0:
                                S.op("act", lambda e, k=k, kk=kk, s=s: e.activation(out=xeT[k][:, :, s * 128:(s + 1) * 128], in_=ptr[kk][:], func=AF.Copy),
                                     reads=[r_ptr[kk]], writes=[r_xeT[k]])
                            else:
                                S.op("dve", lambda e, k=k, kk=kk, s=s: e.tensor_copy(out=xeT[k][:, :, s * 128:(s + 1) * 128], in_=ptr[kk][:]),
                                     reads=[r_ptr[kk]], writes=[r_xeT[k]])
                        if jb + 1 < nblk:
                            load_xs(jb + 1)
                        for f in range(8):
                            kf = f % 2
                            for (pp, r_pp, wbuf, mi) in ((pgp[kf], r_pgp[kf], WG[k], 0), (pup[kf], r_pup[kf], WU[k], 1)):
                                for c in range(8):
                                    S.op("pe", lambda e, pp=pp, wbuf=wbuf, c=c, f=f, k=k: e.matmul(
                                        pp[:], lhsT=wbuf[:, c * 1024 + f * 128:c * 1024 + (f + 1) * 128], rhs=xeT[k][:, c, :], start=(c == 0), stop=(c == 7)),
                                        reads=[r_W[k][mi], r_xeT[k]], writes=[r_pp], signal=(c == 7))
                            S.op("dve", lambda e, kf=kf, k=k, f=f: e.tensor_scalar(out=gs[kf][:], in0=pgp[kf][:], scalar1=EBC[k][:, f:f + 1], scalar2=7.0,
                                                                                 op0=ALU.add, op1=ALU.min),
                                 reads=[r_pgp[kf], r_EBC[k]], writes=[r_gs[kf]])
                            S.op("act", lambda e, kf=kf: e.activation(out=sg[kf][:], in_=gs[kf][:], func=AF.Sigmoid, scale=1.702),
                                 reads=[r_gs[kf]], writes=[r_sg[kf]])
                            S.op("dve", lambda e, kf=kf, k=k, f=f: e.tensor_scalar(out=us[kf][:], in0=pup[kf][:], scalar1=EBC[k][:, 8 + f:9 + f], scalar2=8.0,
                                                                                 op0=ALU.add, op1=ALU.min),
                                 reads=[r_pup[kf], r_EBC[k]], writes=[r_us[kf]])
                            S.op("dve", lambda e, kf=kf: e.scalar_tensor_tensor(out=us[kf][:], in0=us[kf][:], scalar=-6.0, in1=gs[kf][:],
                                                                                op0=ALU.max, op1=ALU.mult),
                                 reads=[r_us[kf], r_gs[kf]], writes=[r_us[kf]])
                            S.op("dve", lambda e, kf=kf, f=f: e.tensor_tensor(out=actT[:, f, :], in0=us[kf][:], in1=sg[kf][:], op=ALU.mult),
                                 reads=[r_us[kf], r_sg[kf]], writes=[r_actT])
                        for s in range(4):
                            for hh in range(2):
                                kp = (s * 2 + hh) % 2
                                for f in range(8):
                                    S.op("pe", lambda e, kp=kp, f=f, s=s, hh=hh, k=k: e.matmul(
                                        pyp[kp][:], lhsT=actT[:, f, s * 128:(s + 1) * 128],
                                        rhs=WD[k][:, f * 1024 + hh * 512:f * 1024 + (hh + 1) * 512], start=(f == 0), stop=False),
                                        reads=[r_W[k][2], r_actT], writes=[r_pyp[kp]], signal=False)
                                S.op("pe", lambda e, kp=kp, hh=hh, k=k: e.matmul(
                                    pyp[kp][:], lhsT=halfb[0:2, 0:128], rhs=EB[k][0:2, 2048 + hh * 512:2048 + (hh + 1) * 512], start=False, stop=True),
                                    reads=[r_W[k][3], R_c], writes=[r_pyp[kp]])
                                S.op("act", lambda e, kp=kp, k=k, s=s, hh=hh: e.activation(out=ysb[k][:, s, hh * 512:(hh + 1) * 512], in_=pyp[kp][:], func=AF.Copy),
                                     reads=[r_pyp[kp]], writes=[r_ysb[k]])
                        S.dma("sp", lambda e, k=k, jb=jb: e.dma_start(
                            out=YS[jb * BLK:(jb + 1) * BLK, :].rearrange("(s p) d -> p s d", p=128), in_=ysb[k][:]),
                            reads=[r_ysb[k]], writes=[R_YS])
                    S.barrier()
                    S.emit()
                if moe_stop < 4:
                    return
                with contextlib.ExitStack() as s1:
                    g2 = {}
                    for v in sorted(set((NB if i < 2 else b) for b, i in tok_tiles)):
                        g2[v] = load_bcast(s1, "g2_%d" % v, MOD[l, v, 5 * D:6 * D], [R_MOD])
                    yg = [[sbuf(s1, "yg%d_%d" % (i, j), [128, D], BF16) for j in range(4)] for i in range(2)]
                    r_yg = [[Res() for _ in range(4)] for _ in range(2)]
                    acc = [sbuf(s1, "cacc%d" % i, [128, D], F32) for i in range(2)]
                    r_acc = [Res(), Res()]
                    xt = [sbuf(s1, "cbx%d" % i, [128, D], F32) for i in range(2)]
                    r_xt = [Res(), Res()]
                    for n_i, (b, i) in enumerate(tok_tiles):
                        k = n_i % 2
                        gt, rgt = g2[NB if i < 2 else b]
                        S.dma("sp", lambda e, b=b, i=i, k=k: e.dma_start(out=xt[k][:], in_=XR[b, i * 128:(i + 1) * 128, :]),
                              reads=[R_XRT[(b, i)]], writes=[r_xt[k]])
                        for j in range(4):
                            S.dma("pool", lambda e, k=k, j=j, n_i=n_i: e.indirect_dma_start(
                                out=yg[k][j][:, :], out_offset=None, in_=YS[:, :],
                                in_offset=bass.IndirectOffsetOnAxis(ap=D4[:, n_i * 4 + j:n_i * 4 + j + 1], axis=0)), reads=[R_YS, r_D4], writes=[r_yg[k][j]])
                        S.op("dve", lambda e, k=k, n_i=n_i: e.tensor_scalar(out=acc[k][:], in0=yg[k][0][:], scalar1=G4[:, n_i * 4:n_i * 4 + 1], scalar2=None, op0=ALU.mult),
                             reads=[r_yg[k][0], r_G4], writes=[r_acc[k]])
                        for j in range(1, 4):
                            S.op("dve", lambda e, k=k, j=j, n_i=n_i: e.scalar_tensor_tensor(
                                out=acc[k][:], in0=yg[k][j][:], scalar=G4[:, n_i * 4 + j:n_i * 4 + j + 1], in1=acc[k][:], op0=ALU.mult, op1=ALU.add),
                                reads=[r_yg[k][j], r_acc[k], r_G4], writes=[r_acc[k]])
                        S.op("dve", lambda e, k=k, gt=gt: e.tensor_tensor(out=acc[k][:], in0=acc[k][:], in1=gt[:], op=ALU.mult),
                             reads=[r_acc[k], rgt], writes=[r_acc[k]])
                        S.op("dve", lambda e, k=k: e.tensor_tensor(out=xt[k][:], in0=xt[k][:], in1=acc[k][:], op=ALU.add),
                             reads=[r_acc[k], r_xt[k]], writes=[r_xt[k]])
                        if final:
                            S.dma("sp", lambda e, b=b, i=i, k=k: e.dma_start(out=out[b, (i - 2) * 128:(i - 1) * 128, :], in_=xt[k][:]),
                                  reads=[r_xt[k]], writes=[R_OUT])
                        else:
                            S.dma("sp", lambda e, b=b, i=i, k=k: e.dma_start(out=XR[b, i * 128:(i + 1) * 128, :], in_=xt[k][:]),
                                  reads=[r_xt[k]], writes=[R_XRT[(b, i)]])
                    S.barrier()
                    S.emit()

        if 0 in layers:
            for b in range(NB):
                layer0_mixer(b)
            if do_moe:
                moe_layer(0, [(b, i) for b in range(NB) for i in range(NTI)], final=False)
        if 1 in layers:
            for b in range(NB):
                layer1_mixer(b)
            if do_moe:
                moe_layer(1, [(b, i) for b in range(NB) for i in range(2, NTI)], final=True)
        if dbg:
            with contextlib.ExitStack() as st:
                t = sbuf(st, "dbgt", [128, D], F32); r_t = Res()
                for b in range(NB):
                    for i in range(2, NTI):
                        S.dma("sp", lambda e, b=b, i=i: e.dma_start(out=t[:], in_=XR[b, i * 128:(i + 1) * 128, :]), reads=[R_XRT[(b, i)]], writes=[r_t])
                        S.dma("sp", lambda e, b=b, i=i: e.dma_start(out=out[b, (i - 2) * 128:(i - 1) * 128, :], in_=t[:]), reads=[r_t], writes=[R_OUT])
                S.barrier()
                S.emit()
        S.barrier()
        S.emit()
        print("program instructions (incl waits):", S.n_ins, "sem counts:", S.cnt, "max dma sem:", max(S.dma_cnt.values()))
    return nc


def _core_inputs(inp, sh, c, NB=2):
    b0 = c * NB
    d = dict(sh)
    d["xin"] = np.ascontiguousarray(np.concatenate([inp["ctx"][b0:b0 + NB], inp["x"][b0:b0 + NB]], axis=1), np.float32)
    d["cvec"] = np.ascontiguousarray(np.concatenate([inp["c"][b0:b0 + NB], inp["c_ctx"][None]], 0), np.float32)
    return d


def kernel(**inputs):
    inp = {k: np.asarray(v) for k, v in inputs.items()}
    sh = _prep_shared(inp)
    nc = build_program(NB=2)
    in_maps = [_core_inputs(inp, sh, c) for c in range(8)]
    res = run_bass_kernel_spmd(nc, in_maps, core_ids=list(range(8)))
    return np.concatenate([r["out"] for r in res.results], axis=0).astype(np.float32)
```

```python
import contextlib
import numpy as np
import concourse.bass as bass
import concourse.mybir as mybir
from concourse.bass_utils import run_bass_kernel_spmd

F32 = mybir.dt.float32
BF16 = mybir.dt.bfloat16
I32 = mybir.dt.int32
AF = mybir.ActivationFunctionType
ALU = mybir.AluOpType
AX = mybir.AxisListType

ENGS = ("pe", "dve", "act", "pool", "sp")

D = 1024
T = 2048
LC = 256
NT = T + LC
NTI = NT // 128
NE = 32
BLK = 512
EPS = 1e-6
NEG = -30000.0


class Res:
    __slots__ = ("name", "w", "r")

    def __init__(self, name=""):
        self.name = name
        self.w = None
        self.r = []


class Sched:
    def __init__(self, nc, stack, n_dma_sems=12):
        self.nc = nc
        self.streams = {e: [] for e in ENGS}
        self.cnt = {e: 0 for e in ENGS}
        self.sems = {}
        for e in ENGS:
            self.sems[e] = stack.enter_context(nc.semaphore("s_" + e))
        self.dma_sems = {}
        self.dma_cnt = {}
        self.dma_rr = {}
        for q in ("sp", "pool"):
            self.dma_sems[q] = []
            for i in range(n_dma_sems):
                k = "d_%s_%d" % (q, i)
                self.sems[k] = stack.enter_context(nc.semaphore(k))
                self.dma_sems[q].append(k)
                self.dma_cnt[k] = 0
            self.dma_rr[q] = 0
        self.seen = {e: {} for e in ENGS}
        self.n_ins = 0

    def _need(self, eng, deps):
        best = {}
        for d in deps:
            if d is None:
                continue
            k, v = d
            if k == eng and eng == "pe":
                continue
            if self.seen[eng].get(k, 0) >= v:
                continue
            if best.get(k, 0) < v:
                best[k] = v
        for k, v in best.items():
            self.seen[eng][k] = v
        return list(best.items())

    @staticmethod
    def _deps(reads, writes):
        deps = []
        for r in reads:
            deps.append(r.w)
        for w in writes:
            deps.append(w.w)
            deps.extend(w.r)
        return deps

    @staticmethod
    def _commit(reads, writes, tok):
        for r in reads:
            r.r.append(tok)
            if len(r.r) > 16:
                m = {}
                for k, v in r.r:
                    if m.get(k, 0) < v:
                        m[k] = v
                r.r = list(m.items())
        for w in writes:
            w.w = tok
            w.r = []

    def op(self, eng, fn, reads=(), writes=(), signal=True):
        waits = self._need(eng, self._deps(reads, writes))
        if signal:
            self.cnt[eng] += 1
            tok = (eng, self.cnt[eng])
        else:
            tok = (eng, self.cnt[eng] + 1)
        self.streams[eng].append((waits, fn, signal, None))
        self._commit(reads, writes, tok)
        self.n_ins += 1 + len(waits)
        return tok

    def dma(self, q, fn, reads=(), writes=()):
        lst = self.dma_sems[q]
        k = lst[self.dma_rr[q] % len(lst)]
        self.dma_rr[q] += 1
        deps = self._deps(reads, writes)
        deps.append((k, self.dma_cnt[k]))
        waits = self._need(q, deps)
        self.dma_cnt[k] += 16
        tok = (k, self.dma_cnt[k])
        self.streams[q].append((waits, fn, False, k))
        self._commit(reads, writes, tok)
        self.n_ins += 1 + len(waits)
        return tok

    def barrier(self):
        final = []
        for e in ENGS:
            if self.cnt[e]:
                final.append((e, self.cnt[e]))
        for k, v in self.dma_cnt.items():
            if v:
                final.append((k, v))
        for e in ENGS:
            waits = self._need(e, [f for f in final if not (f[0] == e and e == "pe")])
            if waits:
                self.streams[e].append((waits, None, False, None))

    def emit(self):
        nc = self.nc
        sems = self.sems
        streams = self.streams

        def run(engname, engobj):
            for waits, fn, signal, dsem in streams[engname]:
                fold = None
                if fn is not None and dsem is None and waits:
                    fold = waits[-1]
                    waits = waits[:-1]
                for k, v in waits:
                    engobj.wait_ge(sems[k], v)
                if fn is None:
                    continue
                ins = fn(engobj)
                if fold is not None:
                    ins._wait_ge(sems[fold[0]], fold[1])
                if dsem is not None:
                    ins.then_inc(sems[dsem], 16)
                elif signal:
                    ins.then_inc(sems[engname], 1)

        with nc.Block() as block:
            @block.tensor
            def _(e):
                run("pe", e)

            @block.vector
            def _(e):
                run("dve", e)

            @block.scalar
            def _(e):
                run("act", e)

            @block.gpsimd
            def _(e):
                run("pool", e)

            @block.sync
            def _(e):
                run("sp", e)
        self.streams = {e: [] for e in ENGS}


def _na_bias_T(rpb):
    H = rpb.shape[0]
    out = np.full((H, 5, 640, 128), NEG, np.float32)
    pair_of_type = [0, 1, 5, 14, 15]
    for ty, j in enumerate(pair_of_type):
        ws = int(np.clip(2 * j - 4, 0, 23))
        a = ws // 2
        krow0 = 2 * a
        for rr in range(2):
            r = 2 * j + rr
            r0 = int(np.clip(r - 4, 0, 24))
            for i in range(10):
                kr = krow0 + i
                if not (r0 <= kr < r0 + 8):
                    continue
                dr = kr - r + 7
                c = np.arange(64)
                c0 = np.clip(c - 8, 0, 48)
                for cq in range(64):
                    kc = np.arange(c0[cq], c0[cq] + 16)
                    dc = kc - cq + 15
                    out[:, ty, i * 64 + kc, rr * 64 + cq] = rpb[:, dr, dc]
    return np.ascontiguousarray(out.reshape(H, 5, 5, 128, 128))


def _pcol(v):
    v = np.asarray(v, np.float32)
    return np.ascontiguousarray(v.reshape(-1, 128).T)


_GATE_INPUTS = ("od_fwd_wa", "od_fwd_ba", "od_fwd_wx", "od_fwd_bx", "od_fwd_lam",
                "od_bwd_wa", "od_bwd_ba", "od_bwd_wx", "od_bwd_bx", "od_bwd_lam")


def _prep_shared(inp):
    sh = {}
    for nm in _GATE_INPUTS:
        assert nm in inp
    sh["ada_w"] = np.ascontiguousarray(inp["ada_w"], np.float32)
    sh["ada_b"] = np.ascontiguousarray(inp["ada_b"], np.float32)
    sh["norm_g"] = np.ascontiguousarray(np.stack([inp["norm1_g"], inp["norm2_g"]], 1), np.float32)
    sh["ev_w_in"] = np.ascontiguousarray(inp["ev_w_in"][0], np.float32)
    sh["ev_w_out"] = np.ascontiguousarray(inp["ev_w_out"][0], np.float32)
    sh["biasT"] = _na_bias_T(np.asarray(inp["ev_rpb"][0], np.float32))
    sh["od_w_in"] = np.ascontiguousarray(inp["od_w_in"][0], np.float32)
    sh["od_w_out"] = np.ascontiguousarray(inp["od_w_out"][0], np.float32)
    gates = []
    for dr in ("fwd", "bwd"):
        for nm in ("wa", "wx"):
            gates.append(np.asarray(inp["od_%s_%s" % (dr, nm)][0], np.float32))
    sh["od_gw"] = np.ascontiguousarray(np.stack(gates, 0))
    cols = []
    qg = np.tile(np.asarray(inp["ev_q_gain"][0], np.float32), 2)
    kg = np.tile(np.asarray(inp["ev_k_gain"][0], np.float32), 2)
    cols.append(qg[:, None]); cols.append(kg[:, None])
    for j in range(3):
        cols.append(_pcol(inp["ev_conv_w"][0, j]))
    cols.append(_pcol(inp["ev_conv_b"][0]))
    for j in range(4):
        cols.append(_pcol(inp["od_conv_w"][0, j]))
    cols.append(_pcol(inp["od_conv_b"][0]))
    for dr in ("fwd", "bwd"):
        for nm in ("ba", "bx", "lam"):
            cols.append(_pcol(inp["od_%s_%s" % (dr, nm)][0]))
    sh["pcols"] = np.ascontiguousarray(np.concatenate(cols, 1), np.float32)
    sh["router_w"] = np.ascontiguousarray(inp["router_w"], np.float32)
    sh["router_b"] = np.ascontiguousarray(inp["router_b"], np.float32)
    for nm in ("gate", "up", "down"):
        w = np.asarray(inp["exp_w_" + nm], np.float32)
        w = w.reshape(2, NE, 8, 128, 1024).transpose(0, 1, 3, 2, 4).reshape(2, NE * 128, 8 * 1024)
        for l in range(2):
            sh["w_%s%d" % (nm, l)] = np.ascontiguousarray(w[l])
    eb = np.concatenate([inp["exp_b_gate"], inp["exp_b_up"], inp["exp_b_down"]], -1).astype(np.float32)
    for l in range(2):
        sh["exp_b%d" % l] = np.ascontiguousarray(eb[l])
        bgc = np.asarray(inp["exp_b_gate"][l], np.float32).reshape(NE, 8, 128).transpose(0, 2, 1)
        buc = np.asarray(inp["exp_b_up"][l], np.float32).reshape(NE, 8, 128).transpose(0, 2, 1)
        sh["exp_bc%d" % l] = np.ascontiguousarray(np.concatenate([bgc, buc], -1).reshape(NE * 128, 16))
    sh["iota_p"] = np.arange(128, dtype=np.float32)[:, None].copy()
    sh["blk_start"] = np.tile((np.arange(80, dtype=np.float32) * BLK)[None], (128, 1)).copy()
    return sh


PC_QG, PC_KG, PC_ECW, PC_ECB, PC_OCW, PC_OCB, PC_G = 0, 1, 2, 14, 18, 50, 58


def build_program(NB=2, layers=(0, 1), do_moe=True, dbg=False, n_exp=NE, moe_stop=4):
    nc = bass.Bass("TRN2", target_bir_lowering=False)
    NTOK0 = NB * NT
    dt = nc.dram_tensor

    def din(name, shape, dtp=F32):
        return dt(name, list(shape), dtp, kind="ExternalInput").ap()

    xin = din("xin", [NB, NT, D])
    cvec = din("cvec", [NB + 1, D])
    ada_w = din("ada_w", [2, D, 6 * D])
    ada_b = din("ada_b", [2, 6 * D])
    norm_g = din("norm_g", [2, 2, D])
    ev_w_in = din("ev_w_in", [D, 3072])
    ev_w_out = din("ev_w_out", [D, D])
    biasT = din("biasT", [8, 5, 5, 128, 128])
    od_w_in = din("od_w_in", [D, 2048])
    od_w_out = din("od_w_out", [D, D])
    od_gw = din("od_gw", [4, 4, 256, 256])
    pcols_d = din("pcols", [128, 106])
    router_w = din("router_w", [2, D, NE])
    router_b = din("router_b", [2, NE])
    w_gate = [din("w_gate%d" % l, [n_exp * 128, 8192]) for l in range(2)]
    w_up = [din("w_up%d" % l, [n_exp * 128, 8192]) for l in range(2)]
    w_down = [din("w_down%d" % l, [n_exp * 128, 8192]) for l in range(2)]
    exp_b = [din("exp_b%d" % l, [n_exp, 3072]) for l in range(2)]
    exp_bc = [din("exp_bc%d" % l, [n_exp * 128, 16]) for l in range(2)]
    iota_p_d = din("iota_p", [128, 1])
    blk_start_d = din("blk_start", [128, 80])
    out = dt("out", [NB, T, D], F32, kind="ExternalOutput").ap()
    if dbg:
        dbg_d4 = dt("dbg_d4", [128, NB * NTI * 4], I32, kind="ExternalOutput").ap()
        dbg_g4 = dt("dbg_g4", [128, NB * NTI * 4], F32, kind="ExternalOutput").ap()
        dbg_iw = dt("dbg_iw", [128, 80], I32, kind="ExternalOutput").ap()

    NBLK0 = NTOK0 * 4 // BLK + n_exp
    XR = dt("XR", [NB, NT, D], F32).ap()
    MOD = dt("MODs", [2, NB + 1, 6 * D], F32).ap()
    XS = dt("XS", [NBLK0 * BLK, D], BF16).ap()
    YS = dt("YS", [NBLK0 * BLK, D], BF16).ap()
    R_MOD, R_XS, R_YS, R_OUT = Res("MOD"), Res("XS"), Res("YS"), Res("OUT")
    R_XRT = {(b_, i_): Res() for b_ in range(NB) for i_ in range(NTI)}
    R_XIN = {(b_, i_): Res() for b_ in range(NB) for i_ in range(NTI)}

    with contextlib.ExitStack() as g:
        S = Sched(nc, g)

        uid = [0]

        def sbuf(st, name, shape, dtp):
            uid[0] += 1
            return st.enter_context(nc.sbuf_tensor("%s_%d" % (name, uid[0]), list(shape), dtp))

        def psum(st, name, shape, dtp=F32):
            uid[0] += 1
            return st.enter_context(nc.psum_tensor("%s_%d" % (name, uid[0]), list(shape), dtp))

        ident = sbuf(g, "ident", [128, 128], F32)
        identb = sbuf(g, "identb", [128, 128], BF16)
        onesb = sbuf(g, "onesb", [128, 512], BF16)
        halfb = sbuf(g, "halfb", [2, 512], BF16)
        pcols = sbuf(g, "pcols_sb", [128, 106], F32)
        R_c = Res("const")
        S.op("dve", lambda e: e.memset(ident[:], 0.0), writes=[R_c])
        S.op("pool", lambda e: e.affine_select(out=ident[:], in_=ident[:], pattern=[[-1, 128]], compare_op=ALU.not_equal,
                                               fill=1.0, base=0, channel_multiplier=1), reads=[R_c], writes=[R_c])
        S.op("dve", lambda e: e.tensor_copy(out=identb[:], in_=ident[:]), reads=[R_c], writes=[R_c])
        S.op("dve", lambda e: e.memset(onesb[:], 1.0), writes=[R_c])
        S.op("dve", lambda e: e.memset(halfb[:], 0.5), writes=[R_c])
        S.dma("sp", lambda e: e.dma_start(out=pcols[:], in_=pcols_d[:, :]), writes=[R_c])
        S.barrier()
        S.emit()

        NV = NB + 1
        with contextlib.ExitStack() as st:
            cs = sbuf(st, "cs", [NV, D], F32)
            sT = sbuf(st, "sT", [128, 8, NV], F32)
            aw = [sbuf(st, "aw%d" % i, [128, 8, 512], F32) for i in range(2)]
            R_aw = [Res(), Res()]
            ab = sbuf(st, "ab", [NV, 6 * D], F32)
            msb = sbuf(st, "msb", [NV, 6 * D], F32)
            pT = psum(st, "pT", [128, 8, NV], F32)
            pm = [psum(st, "pm%d" % i, [NV, 512], F32) for i in range(2)]
            R_pm = [Res(), Res()]
            R_cs, R_sT, R_ab, R_msb, R_pT = Res(), Res(), Res(), Res(), Res()
            S.dma("sp", lambda e: e.dma_start(out=cs[:], in_=cvec[:, :]), writes=[R_cs])
            S.op("act", lambda e: e.activation(out=cs[:], in_=cs[:], func=AF.Silu), reads=[R_cs], writes=[R_cs])
            for c in range(8):
                S.op("pe", lambda e, c=c: e.transpose(out=pT[:, c, :], in_=cs[:, c * 128:(c + 1) * 128], identity=ident[0:NV, 0:NV]),
                     reads=[R_cs, R_c], writes=[R_pT], signal=(c == 7))
            S.op("dve", lambda e: e.tensor_copy(out=sT[:], in_=pT[:]), reads=[R_pT], writes=[R_sT])
            for l in range(2):
                for v in range(NV):
                    S.dma("sp", lambda e, l=l, v=v: e.dma_start(out=ab[v:v + 1, :], in_=ada_b[l:l + 1, :]), writes=[R_ab])
                for j in range(12):
                    k = j % 2
                    S.dma("sp", lambda e, l=l, j=j, k=k: e.dma_start(
                        out=aw[k][:], in_=ada_w[l, :, j * 512:(j + 1) * 512].rearrange("(c p) n -> p c n", p=128)),
                        writes=[R_aw[k]])
                    for c in range(8):
                        S.op("pe", lambda e, c=c, k=k: e.matmul(pm[k][:], lhsT=sT[:, c, :], rhs=aw[k][:, c, :],
                                                                start=(c == 0), stop=(c == 7)),
                             reads=[R_sT, R_aw[k]], writes=[R_pm[k]], signal=(c == 7))
                    S.op("dve", lambda e, j=j, k=k: e.tensor_tensor(out=msb[:, j * 512:(j + 1) * 512], in0=pm[k][:],
                                                                     in1=ab[:, j * 512:(j + 1) * 512], op=ALU.add),
                         reads=[R_pm[k], R_ab], writes=[R_msb])
                S.dma("sp", lambda e, l=l: e.dma_start(out=MOD[l, :, :], in_=msb[:]), reads=[R_msb], writes=[R_MOD])
            S.barrier()
            S.emit()

        def load_bcast(st, name, src_ap, reads, n=D):
            t = sbuf(st, name, [128, n], F32)
            r = Res(name)
            S.dma("sp", lambda e: e.dma_start(out=t[:], in_=src_ap.partition_broadcast(128)), reads=reads, writes=[r])
            return t, r

        def norm_mod_tiles(st, l, which, vecs):
            res = {}
            gt, rg = load_bcast(st, "ng%d%d" % (l, which), norm_g[l, which, :], [])
            for v in vecs:
                sc, rsc = load_bcast(st, "sc%d" % v, MOD[l, v, (3 * which + 1) * D:(3 * which + 2) * D], [R_MOD])
                shh, rsh = load_bcast(st, "sh%d" % v, MOD[l, v, (3 * which) * D:(3 * which + 1) * D], [R_MOD])
                S.op("dve", lambda e, sc=sc: e.scalar_tensor_tensor(out=sc[:], in0=sc[:], scalar=1.0, in1=gt[:],
                                                                      op0=ALU.add, op1=ALU.mult),
                     reads=[rsc, rg], writes=[rsc])
                res[v] = (sc, rsc, shh, rsh)
            return res

        def rms_mod(st_tmp, xt, r_xt, G, rG, SH, rSH, ht, r_ht, small, r_small, junk, r_junk):
            S.op("dve", lambda e: e.memset(small[:, 0:1], 0.0), reads=[r_small], writes=[r_small])
            S.op("act", lambda e: e.activation(out=junk[:], in_=xt[:], func=AF.Square, accum_out=small[:, 0:1]),
                 reads=[r_xt, r_small], writes=[r_junk, r_small])
            S.op("act", lambda e: e.activation(out=small[:, 1:2], in_=small[:, 0:1], func=AF.Sqrt, scale=1.0 / D, bias=small[:, 3:4]),
                 reads=[r_small], writes=[r_small])
            S.op("dve", lambda e: e.reciprocal(out=small[:, 2:3], in_=small[:, 1:2]), reads=[r_small], writes=[r_small])
            S.op("dve", lambda e: e.scalar_tensor_tensor(out=ht[:], in0=xt[:], scalar=small[:, 2:3], in1=G[:],
                                                         op0=ALU.mult, op1=ALU.mult),
                 reads=[r_xt, r_small, rG], writes=[r_ht])
            S.op("dve", lambda e: e.tensor_tensor(out=ht[:], in0=ht[:], in1=SH[:], op=ALU.add),
                 reads=[r_ht, rSH], writes=[r_ht])

        def new_small(st, name):
            small = sbuf(st, name, [128, 4], F32)
            r = Res(name)
            S.op("dve", lambda e: e.memset(small[:], 0.0), writes=[r])
            S.op("dve", lambda e: e.memset(small[:, 3:4], EPS), reads=[r], writes=[r])
            return small, r

        def norm_to_hT(st, l, b, src_ap, src_res, hT, r_hT, vec_of_tile):
            with contextlib.ExitStack() as s2:
                vecs = sorted(set(vec_of_tile))
                tiles = norm_mod_tiles(s2, l, 0, vecs)
                small, r_small = zip(*[new_small(s2, "n1small%d" % i) for i in range(2)])
                junk = [sbuf(s2, "n1junk%d" % i, [128, D], F32) for i in range(2)]; r_junk = [Res(), Res()]
                xt = [sbuf(s2, "n1x%d" % i, [128, D], F32) for i in range(2)]
                r_xt = [Res(), Res()]
                ht = [sbuf(s2, "n1h%d" % i, [128, D], F32) for i in range(2)]
                r_ht = [Res(), Res()]
                ptp = [psum(s2, "n1p%d" % i, [128, 4, 128], F32) for i in range(2)]
                r_ptp = [Res(), Res()]
                def stage_a(i):
                    k = i % 2
                    G, rG, SH, rSH = tiles[vec_of_tile[i]]
                    S.dma("sp", lambda e: e.dma_start(out=xt[k][:], in_=src_ap[b, i * 128:(i + 1) * 128, :]),
                          reads=[src_res[(b, i)]], writes=[r_xt[k]])
                    rms_mod(s2, xt[k], r_xt[k], G, rG, SH, rSH, ht[k], r_ht[k], small[k], r_small[k], junk[k], r_junk[k])

                def stage_b(i):
                    k = i % 2
                    for hf in range(2):
                        for c4 in range(4):
                            c = hf * 4 + c4
                            S.op("pe", lambda e, hf=hf, c4=c4, c=c: e.transpose(
                                out=ptp[hf][:, c4, :], in_=ht[k][:, c * 128:(c + 1) * 128], identity=ident[:]),
                                reads=[r_ht[k], R_c], writes=[r_ptp[hf]], signal=(c4 == 3))
                        if (hf + i) % 2 == 0:
                            S.op("act", lambda e, hf=hf: e.activation(
                                out=hT[:, hf * 4:(hf + 1) * 4, i * 128:(i + 1) * 128], in_=ptp[hf][:], func=AF.Copy),
                                reads=[r_ptp[hf]], writes=[r_hT])
                        else:
                            S.op("dve", lambda e, hf=hf: e.tensor_copy(
                                out=hT[:, hf * 4:(hf + 1) * 4, i * 128:(i + 1) * 128], in_=ptp[hf][:]),
                                reads=[r_ptp[hf]], writes=[r_hT])

                stage_a(0)
                for i in range(NTI):
                    if i + 1 < NTI:
                        stage_a(i + 1)
                    stage_b(i)
                S.barrier()
                S.emit()

        def stream_w(st, name, n=2):
            bufs = [sbuf(st, "%s%d" % (name, i), [128, 8, 512], BF16) for i in range(n)]
            return bufs, [Res() for _ in range(n)]

        def load_w(buf, r, w_ap, col0, ncol=512):
            S.dma("pool", lambda e: e.dma_start(out=buf[:, :, 0:ncol],
                                                 in_=w_ap[:, col0:col0 + ncol].rearrange("(c p) n -> p c n", p=128)),
                  writes=[r])

        TOKP = [(i * 512, min(512, NT - i * 512)) for i in range((NT + 511) // 512)]

        def outproj_residual(st, l, b, srcs, w_ap, x_src, x_res, tiles_range, vec_of_tile, x_tok_off=0):
            with contextlib.ExitStack() as s2:
                wo = sbuf(s2, "wo", [128, 8, D], BF16); r_wo = Res()
                for hh in range(2):
                    S.dma("pool", lambda e, hh=hh: e.dma_start(
                        out=wo[:, :, hh * 512:(hh + 1) * 512],
                        in_=w_ap[:, hh * 512:(hh + 1) * 512].rearrange("(c p) n -> p c n", p=128)), writes=[r_wo])
                g1 = {}
                for v in sorted(set(vec_of_tile[i] for i in tiles_range)):
                    g1[v] = load_bcast(s2, "g1_%d" % v, MOD[l, v, 2 * D:3 * D], [R_MOD])
                xt = [sbuf(s2, "opx%d" % i, [128, D], F32) for i in range(2)]
                r_xt = [Res(), Res()]
                py = [psum(s2, "opy%d" % i, [128, 512], F32) for i in range(4)]
                r_py = [Res() for _ in range(4)]
                for n_i, i in enumerate(tiles_range):
                    k = n_i % 2
                    gt, rgt = g1[vec_of_tile[i]]
                    S.dma("sp", lambda e, i=i, k=k: e.dma_start(out=xt[k][:], in_=x_src[b, i * 128:(i + 1) * 128, :]),
                          reads=[x_res[(b, i)]], writes=[r_xt[k]])
                    for hh in range(2):
                        pk = k * 2 + hh
                        for c in range(8):
                            tsr, ch, rs, toff = srcs[c]
                            S.op("pe", lambda e, tsr=tsr, ch=ch, toff=toff, i=i, c=c, hh=hh, pk=pk: e.matmul(
                                py[pk][:], lhsT=tsr[:, ch, i * 128 - toff:(i + 1) * 128 - toff],
                                rhs=wo[:, c, hh * 512:(hh + 1) * 512], start=(c == 0), stop=(c == 7)),
                                reads=[rs, r_wo], writes=[r_py[pk]], signal=(c == 7))
                        S.op("dve", lambda e, hh=hh, pk=pk, gt=gt, k=k: e.tensor_tensor(
                            out=xg[k][:, hh * 512:(hh + 1) * 512],
                            in0=py[pk][:], in1=gt[:, hh * 512:(hh + 1) * 512], op=ALU.mult),
                            reads=[r_py[pk], rgt], writes=[r_xg[k]])
                    S.op("pool", lambda e, k=k: e.tensor_tensor(out=xt[k][:], in0=xt[k][:], in1=xg[k][:], op=ALU.add),
                         reads=[r_xg[k], r_xt[k]], writes=[r_xt[k]])
                    S.dma("sp", lambda e, i=i, k=k: e.dma_start(out=XR[b, i * 128:(i + 1) * 128, :], in_=xt[k][:]),
                          reads=[r_xt[k]], writes=[R_XRT[(b, i)]])
                S.barrier()
                S.emit()

        xg = [sbuf(g, "xg%d" % i, [128, D], F32) for i in range(2)]
        r_xg = [Res(), Res()]

        VEC_OF_TILE = lambda b: [NB, NB] + [b] * 16

        def layer0_mixer(b):
            with contextlib.ExitStack() as st:
                qT = sbuf(st, "qT", [128, 4, NT], BF16); r_qT = Res()
                kT = sbuf(st, "kT", [128, 4, NT], BF16); r_kT = Res()
                V = sbuf(st, "V", [128, NTI, 512], BF16); r_V = Res()
                OB = sbuf(st, "OB", [128, 4, NT], BF16); r_OB = Res()
                with contextlib.ExitStack() as s1:
                    hT = sbuf(s1, "hT", [128, 8, NT], BF16); r_hT = Res()
                    norm_to_hT(s1, 0, b, xin, R_XIN, hT, r_hT, VEC_OF_TILE(b))
                    wb, r_wb = stream_w(s1, "wi")
                    blk1 = sbuf(s1, "blk1", [128, 128], BF16); r_blk = Res()
                    S.op("dve", lambda e: e.memset(blk1[:], 0.0), writes=[r_blk])
                    S.op("dve", lambda e: e.memset(blk1[0:64, 0:64], 1.0 / 64), reads=[r_blk], writes=[r_blk])
                    S.op("dve", lambda e: e.memset(blk1[64:128, 64:128], 1.0 / 64), reads=[r_blk], writes=[r_blk])
                    pj = [psum(s1, "pj%d" % i, [128, 512], F32) for i in range(3)]
                    r_pj = [Res() for _ in range(3)]
                    pq = [psum(s1, "pq%d" % i, [128, 512], F32) for i in range(2)]
                    r_pq = [Res() for _ in range(2)]
                    sq = [sbuf(s1, "sq%d" % i, [128, 512], BF16) for i in range(2)]
                    r_sq = [Res(), Res()]
                    rs_t = [sbuf(s1, "rst%d" % i, [128, 512], F32) for i in range(2)]
                    r_rs = [Res(), Res()]
                    eps64 = sbuf(s1, "eps64", [128, 2], F32); r_e64 = Res()
                    S.op("dve", lambda e: e.memset(eps64[:, 0:1], EPS), writes=[r_e64])
                    S.op("dve", lambda e: e.memset(eps64[:, 1:2], 64.0 * EPS), reads=[r_e64], writes=[r_e64])
                    cnt = [0]

                    def proj_piece(wbuf, r_w, wc, tp):
                        t0, nt_ = TOKP[tp]
                        k = cnt[0] % 3
                        cnt[0] += 1
                        for c in range(8):
                            S.op("pe", lambda e, c=c, k=k: e.matmul(pj[k][:, 0:nt_], lhsT=wbuf[:, c, wc * 128:(wc + 1) * 128],
                                                                   rhs=hT[:, c, t0:t0 + nt_], start=(c == 0), stop=(c == 7)),
                                 reads=[r_w, r_hT], writes=[r_pj[k]], signal=(c == 7))
                        return k, t0, nt_

                    for grp, (dst, r_dst, gcol, sc_, ecol) in enumerate(((qT, r_qT, PC_QG, 64.0, 1), (kT, r_kT, PC_KG, 1.0, 0))):
                        bi = grp % 2
                        load_w(wb[bi], r_wb[bi], ev_w_in, grp * 512)
                        for wc in range(4):
                            for tp in range(len(TOKP)):
                                k, t0, nt_ = proj_piece(wb[bi], r_wb[bi], wc, tp)
                                k2 = cnt[0] % 2
                                S.op("act", lambda e, k=k, k2=k2, nt_=nt_: e.activation(out=sq[k2][:, 0:nt_], in_=pj[k][:, 0:nt_], func=AF.Square),
                                     reads=[r_pj[k]], writes=[r_sq[k2]])
                                S.op("pe", lambda e, k2=k2, nt_=nt_: e.matmul(pq[k2][:, 0:nt_], lhsT=blk1[:], rhs=sq[k2][:, 0:nt_], start=True, stop=True),
                                     reads=[r_blk, r_sq[k2]], writes=[r_pq[k2]])
                                S.op("act", lambda e, k2=k2, nt_=nt_, sc_=sc_, ecol=ecol: e.activation(
                                    out=rs_t[k2][:, 0:nt_], in_=pq[k2][:, 0:nt_], func=AF.Sqrt, scale=sc_, bias=eps64[:, ecol:ecol + 1]),
                                    reads=[r_pq[k2], r_e64], writes=[r_rs[k2]])
                                S.op("dve", lambda e, k2=k2, nt_=nt_: e.reciprocal(out=rs_t[k2][:, 0:nt_], in_=rs_t[k2][:, 0:nt_]),
                                     reads=[r_rs[k2]], writes=[r_rs[k2]])
                                S.op("dve", lambda e, k=k, k2=k2, nt_=nt_, t0=t0, wc=wc, dst=dst, gcol=gcol: e.scalar_tensor_tensor(
                                    out=dst[:, wc, t0:t0 + nt_], in0=pj[k][:, 0:nt_], scalar=pcols[:, gcol:gcol + 1], in1=rs_t[k2][:, 0:nt_],
                                    op0=ALU.mult, op1=ALU.mult), reads=[r_pj[k], r_rs[k2], R_c], writes=[r_dst])
                    load_w(wb[0], r_wb[0], ev_w_in, 1024)
                    for i in range(NTI):
                        k = cnt[0] % 3
                        cnt[0] += 1
                        for c in range(8):
                            S.op("pe", lambda e, c=c, k=k, i=i: e.matmul(pj[k][:], lhsT=hT[:, c, i * 128:(i + 1) * 128], rhs=wb[0][:, c, :],
                                                                        start=(c == 0), stop=(c == 7)),
                                 reads=[r_wb[0], r_hT], writes=[r_pj[k]], signal=(c == 7))
                        S.op("act", lambda e, k=k, i=i: e.activation(out=V[:, i, :], in_=pj[k][:], func=AF.Copy),
                             reads=[r_pj[k]], writes=[r_V])
                    load_w(wb[1], r_wb[1], ev_w_in, 2560)
                    wcg = sbuf(s1, "wcg", [128, 8, 512], BF16); r_wcg = Res()
                    load_w(wcg, r_wcg, ev_w_in, 2048)
                    load_w(wb[0], r_wb[0], ev_w_in, 1536)
                    xs_f = sbuf(s1, "xs_f", [128, NT], F32); r_xs = Res()
                    cx_f = sbuf(s1, "cx_f", [128, NT], F32); r_cx = Res()
                    t_f = sbuf(s1, "t_f", [128, NT], F32); r_t = Res()
                    for f in range(4):
                        for tp in range(len(TOKP)):
                            k, t0, nt_ = proj_piece(wb[1], r_wb[1], f, tp)
                            S.op("act", lambda e, k=k, t0=t0, nt_=nt_: e.activation(out=xs_f[:, t0:t0 + nt_], in_=pj[k][:, 0:nt_], func=AF.Copy),
                                 reads=[r_pj[k]], writes=[r_xs])
                        for tp in range(len(TOKP)):
                            k, t0, nt_ = proj_piece(wcg, r_wcg, f, tp)
                            S.op("dve", lambda e, k=k, t0=t0, nt_=nt_: e.tensor_tensor(out=cx_f[:, t0:t0 + nt_], in0=pj[k][:, 0:nt_],
                                                                                      in1=xs_f[:, t0:t0 + nt_], op=ALU.mult),
                                 reads=[r_pj[k], r_xs], writes=[r_cx])
                        w0 = pcols[:, PC_ECW + 0 * 4 + f:PC_ECW + 0 * 4 + f + 1]
                        w1 = pcols[:, PC_ECW + 1 * 4 + f:PC_ECW + 1 * 4 + f + 1]
                        w2 = pcols[:, PC_ECW + 2 * 4 + f:PC_ECW + 2 * 4 + f + 1]
                        bb = pcols[:, PC_ECB + f:PC_ECB + f + 1]
                        S.op("dve", lambda e, w1=w1, bb=bb: e.tensor_scalar(out=t_f[:], in0=cx_f[:], scalar1=w1, scalar2=bb, op0=ALU.mult, op1=ALU.add),
                             reads=[r_cx, R_c], writes=[r_t])
                        for (s0, sn) in ((0, LC), (LC, T)):
                            S.op("dve", lambda e, s0=s0, sn=sn, w0=w0: e.scalar_tensor_tensor(
                                out=t_f[:, s0 + 1:s0 + sn], in0=cx_f[:, s0:s0 + sn - 1], scalar=w0, in1=t_f[:, s0 + 1:s0 + sn],
                                op0=ALU.mult, op1=ALU.add), reads=[r_cx, r_t, R_c], writes=[r_t])
                            S.op("dve", lambda e, s0=s0, sn=sn, w2=w2: e.scalar_tensor_tensor(
                                out=t_f[:, s0:s0 + sn - 1], in0=cx_f[:, s0 + 1:s0 + sn], scalar=w2, in1=t_f[:, s0:s0 + sn - 1],
                                op0=ALU.mult, op1=ALU.add), reads=[r_cx, r_t, R_c], writes=[r_t])
                        for tp in range(len(TOKP)):
                            k, t0, nt_ = proj_piece(wb[0], r_wb[0], f, tp)
                            S.op("dve", lambda e, k=k, t0=t0, nt_=nt_, f=f: e.tensor_tensor(out=OB[:, f, t0:t0 + nt_], in0=pj[k][:, 0:nt_],
                                                                                           in1=t_f[:, t0:t0 + nt_], op=ALU.mult),
                                 reads=[r_pj[k], r_t], writes=[r_OB])
                    S.barrier()
                    S.emit()
                OA = sbuf(st, "OA", [128, 4, NT], BF16); r_OA = Res()
                with contextlib.ExitStack() as s1:
                    bias = [sbuf(s1, "bias%d" % i, [128, 5, 5, 128], BF16) for i in range(2)]
                    r_bias = [Res(), Res()]
                    sta = [psum(s1, "sta%d" % i, [128, 4, 128], F32) for i in range(2)]
                    stb = [psum(s1, "stb%d" % i, [128, 4, 128], F32) for i in range(2)]
                    r_sta = [Res(), Res()]; r_stb = [Res(), Res()]
                    po = [psum(s1, "po%d" % i, [64, 2, 128], F32) for i in range(2)]
                    r_po = [Res(), Res()]
                    PT = [sbuf(s1, "PT%d" % i, [128, 7, 128], BF16) for i in range(2)]
                    r_PT = [Res(), Res()]
                    rc = [sbuf(s1, "rc%d" % i, [64, 128], F32) for i in range(2)]
                    r_rc = [Res(), Res()]
                    it = 0
                    for h in range(8):
                        hp, hc = h % 2, h // 2
                        bsel = h % 2
                        S.dma("pool", lambda e, h=h, bsel=bsel: e.dma_start(out=bias[bsel][:], in_=biasT[h].rearrange("t c p q -> p t c q")),
                              writes=[r_bias[bsel]])
                        jobs = [("ctx", 0), ("ctx", 1)] + [("lat", j) for j in range(16)]
                        for kind, j in jobs:
                            k = it % 2
                            it += 1
                            if kind == "ctx":
                                qtile = j
                                ktiles = [0, 1]
                                ty = None
                            else:
                                qtile = 2 + j
                                ws = min(max(2 * j - 4, 0), 23)
                                a = ws // 2
                                ktiles = [0, 1] + [2 + a + cc for cc in range(5)]
                                ty = {0: 0, 1: 1, 14: 3, 15: 4}.get(j, 2)
                            nk = len(ktiles)
                            q_ap = qT[hp * 64:(hp + 1) * 64, hc, qtile * 128:(qtile + 1) * 128]
                            for kc, kt in enumerate(ktiles):
                                dstp, r_dst = (sta[k], r_sta[k]) if kc < 4 else (stb[k], r_stb[k])
                                last = (ty is None) and ((kc == min(3, nk - 1)) or (kc == nk - 1))
                                S.op("pe", lambda e, dstp=dstp, kc=kc, kt=kt, q_ap=q_ap, hp=hp, hc=hc: e.matmul(
                                    dstp[:, kc % 4, :], lhsT=kT[hp * 64:(hp + 1) * 64, hc, kt * 128:(kt + 1) * 128], rhs=q_ap,
                                    start=(kc % 4 == 0), stop=True, skip_group_check=True), reads=[r_kT, r_qT], writes=[r_dst], signal=last)
                            if ty is not None:
                                for kc in range(2, nk):
                                    dstp, r_dst = (sta[k], r_sta[k]) if kc < 4 else (stb[k], r_stb[k])
                                    last = (kc == 3) or (kc == nk - 1)
                                    S.op("pe", lambda e, dstp=dstp, kc=kc, ty=ty, bsel=bsel: e.matmul(
                                        dstp[:, kc % 4, :], lhsT=identb[:], rhs=bias[bsel][:, ty, kc - 2, :], start=False, stop=True,
                                        skip_group_check=True), reads=[r_bias[bsel], R_c], writes=[r_dst], signal=last)
                            na = min(4, nk)
                            S.op("act", lambda e, k=k, na=na: e.activation(out=PT[k][:, 0:na, :], in_=sta[k][:, 0:na, :], func=AF.Exp),
                                 reads=[r_sta[k]], writes=[r_PT[k]])
                            if nk > 4:
                                S.op("act", lambda e, k=k, nk=nk: e.activation(out=PT[k][:, 4:nk, :], in_=stb[k][:, 0:nk - 4, :], func=AF.Exp),
                                     reads=[r_stb[k]], writes=[r_PT[k]])
                            for kc, kt in enumerate(ktiles):
                                S.op("pe", lambda e, k=k, kc=kc, kt=kt, nk=nk, h=h: e.matmul(
                                    po[k][:, 0, :], lhsT=V[:, kt, h * 64:(h + 1) * 64], rhs=PT[k][:, kc, :], start=(kc == 0), stop=(kc == nk - 1)),
                                    reads=[r_V, r_PT[k]], writes=[r_po[k]], signal=False)
                            for kc, kt in enumerate(ktiles):
                                S.op("pe", lambda e, k=k, kc=kc, nk=nk: e.matmul(
                                    po[k][:, 1, :], lhsT=onesb[:, 0:64], rhs=PT[k][:, kc, :], start=(kc == 0), stop=(kc == nk - 1)),
                                    reads=[R_c, r_PT[k]], writes=[r_po[k]], signal=(kc == nk - 1))
                            S.op("dve", lambda e, k=k: e.reciprocal(out=rc[k][:], in_=po[k][:, 1, :]), reads=[r_po[k]], writes=[r_rc[k]])
                            S.op("dve", lambda e, k=k, hp=hp, hc=hc, qtile=qtile: e.tensor_tensor(
                                out=OA[hp * 64:(hp + 1) * 64, hc, qtile * 128:(qtile + 1) * 128], in0=po[k][:, 0, :], in1=rc[k][:], op=ALU.mult),
                                reads=[r_po[k], r_rc[k]], writes=[r_OA])
                    S.barrier()
                    S.emit()
                srcs = [(OA, c, r_OA, 0) for c in range(4)] + [(OB, c, r_OB, 0) for c in range(4)]
                outproj_residual(st, 0, b, srcs, ev_w_out, xin, R_XIN, list(range(NTI)), VEC_OF_TILE(b))

        def layer1_mixer(b):
            with contextlib.ExitStack() as st:
                UC = sbuf(st, "UC", [128, 8, NT], BF16); r_UC = Res()
                GG = sbuf(st, "GG", [128, 8, T], BF16); r_GG = Res()
                with contextlib.ExitStack() as s1:
                    hT = sbuf(s1, "hT1", [128, 8, NT], BF16); r_hT = Res()
                    norm_to_hT(s1, 1, b, XR, R_XRT, hT, r_hT, VEC_OF_TILE(b))
                    wb, r_wb = stream_w(s1, "wi1")
                    pj = [psum(s1, "pj1%d" % i, [128, 512], F32) for i in range(3)]
                    r_pj = [Res() for _ in range(3)]
                    u_f = sbuf(s1, "u_f", [128, NT], F32); r_u = Res()
                    t_f = sbuf(s1, "t1_f", [128, NT], F32); r_t = Res()
                    cnt = [0]
                    for grp in range(4):
                        bi = grp % 2
                        load_w(wb[bi], r_wb[bi], od_w_in, grp * 512)
                        for wc in range(4):
                            f = (grp % 2) * 4 + wc
                            for tp in range(len(TOKP)):
                                t0, nt_ = TOKP[tp]
                                if grp < 2 and t0 + nt_ <= LC:
                                    continue
                                k = cnt[0] % 3
                                cnt[0] += 1
                                for c in range(8):
                                    S.op("pe", lambda e, c=c, k=k, bi=bi, wc=wc, t0=t0, nt_=nt_: e.matmul(
                                        pj[k][:, 0:nt_], lhsT=wb[bi][:, c, wc * 128:(wc + 1) * 128], rhs=hT[:, c, t0:t0 + nt_],
                                        start=(c == 0), stop=(c == 7)), reads=[r_wb[bi], r_hT], writes=[r_pj[k]], signal=(c == 7))
                                if grp < 2:
                                    lo = max(t0, LC)
                                    S.op("act", lambda e, k=k, f=f, lo=lo, t0=t0, nt_=nt_: e.activation(
                                        out=GG[:, f, lo - LC:t0 + nt_ - LC], in_=pj[k][:, lo - t0:nt_], func=AF.Gelu),
                                        reads=[r_pj[k]], writes=[r_GG])
                                else:
                                    S.op("act", lambda e, k=k, t0=t0, nt_=nt_: e.activation(out=u_f[:, t0:t0 + nt_], in_=pj[k][:, 0:nt_], func=AF.Copy),
                                         reads=[r_pj[k]], writes=[r_u])
                            if grp >= 2:
                                wj = [pcols[:, PC_OCW + j * 8 + f:PC_OCW + j * 8 + f + 1] for j in range(4)]
                                bb = pcols[:, PC_OCB + f:PC_OCB + f + 1]
                                S.op("dve", lambda e, wj=wj, bb=bb: e.tensor_scalar(out=t_f[:], in0=u_f[:], scalar1=wj[1], scalar2=bb, op0=ALU.mult, op1=ALU.add),
                                     reads=[r_u, R_c], writes=[r_t])
                                for (s0, sn) in ((0, LC), (LC, T)):
                                    S.op("dve", lambda e, s0=s0, sn=sn, wj=wj: e.scalar_tensor_tensor(
                                        out=t_f[:, s0 + 1:s0 + sn], in0=u_f[:, s0:s0 + sn - 1], scalar=wj[0], in1=t_f[:, s0 + 1:s0 + sn],
                                        op0=ALU.mult, op1=ALU.add), reads=[r_u, r_t, R_c], writes=[r_t])
                                    S.op("dve", lambda e, s0=s0, sn=sn, wj=wj: e.scalar_tensor_tensor(
                                        out=t_f[:, s0:s0 + sn - 1], in0=u_f[:, s0 + 1:s0 + sn], scalar=wj[2], in1=t_f[:, s0:s0 + sn - 1],
                                        op0=ALU.mult, op1=ALU.add), reads=[r_u, r_t, R_c], writes=[r_t])
                                    S.op("dve", lambda e, s0=s0, sn=sn, wj=wj: e.scalar_tensor_tensor(
                                        out=t_f[:, s0:s0 + sn - 2], in0=u_f[:, s0 + 2:s0 + sn], scalar=wj[3], in1=t_f[:, s0:s0 + sn - 2],
                                        op0=ALU.mult, op1=ALU.add), reads=[r_u, r_t, R_c], writes=[r_t])
                                S.op("act", lambda e, f=f: e.activation(out=UC[:, f, :], in_=t_f[:], func=AF.Copy), reads=[r_t], writes=[r_UC])
                    S.barrier()
                    S.emit()
                YI, r_YI = GG, r_GG
                with contextlib.ExitStack() as s1:
                    gw = sbuf(s1, "gw", [128, 4, 4, 2, 256], BF16); r_gw = Res()
                    for m in range(4):
                        S.dma("pool", lambda e, m=m: e.dma_start(out=gw[:, m], in_=od_gw[m].rearrange("b (k p) n -> p b k n", p=128)),
                              writes=[r_gw])
                    cl = sbuf(s1, "cl", [128, 2, 8], F32); r_cl = Res()
                    for d_ in range(2):
                        lam = pcols[:, PC_G + d_ * 24 + 16:PC_G + d_ * 24 + 24]
                        S.op("act", lambda e, d_=d_, lam=lam: e.activation(out=cl[:, d_, :], in_=lam, func=AF.Exp, scale=-1.0), reads=[R_c], writes=[r_cl])
                        S.op("act", lambda e, d_=d_: e.activation(out=cl[:, d_, :], in_=cl[:, d_, :], func=AF.Ln, bias=1.0, scale=1.0), reads=[r_cl], writes=[r_cl])
                        S.op("dve", lambda e, d_=d_: e.tensor_scalar(out=cl[:, d_, :], in0=cl[:, d_, :], scalar1=-8.0, scalar2=None, op0=ALU.mult),
                             reads=[r_cl], writes=[r_cl])
                    pg = [psum(s1, "pg%d" % i, [128, 512], F32) for i in range(4)]
                    r_pg = [Res() for _ in range(4)]
                    Rts = [sbuf(s1, "Rt%d" % i, [128, NT], F32) for i in range(2)]; r_Rs = [Res(), Res()]
                    Its = [sbuf(s1, "It%d" % i, [128, NT], F32) for i in range(2)]; r_Is = [Res(), Res()]
                    Ats = [sbuf(s1, "At%d" % i, [128, NT], F32) for i in range(2)]; r_As = [Res(), Res()]
                    Bts = [sbuf(s1, "Bt%d" % i, [128, NT], F32) for i in range(2)]; r_Bs = [Res(), Res()]
                    Hf = sbuf(s1, "Hf", [128, NT], F32); r_Hf = Res()
                    Hb = sbuf(s1, "Hb", [128, NT], F32); r_Hb = Res()
                    cnt = [0]
                    for f in range(8):
                        blk = f // 2
                        for d_ in range(2):
                            Rt, It, At, Bt = Rts[d_], Its[d_], Ats[d_], Bts[d_]
                            r_R, r_I, r_A, r_B = r_Rs[d_], r_Is[d_], r_As[d_], r_Bs[d_]
                            for gi, (dstt, r_dst) in enumerate(((Rt, r_R), (It, r_I))):
                                m = d_ * 2 + gi
                                bcol = pcols[:, PC_G + d_ * 24 + gi * 8 + f:PC_G + d_ * 24 + gi * 8 + f + 1]
                                for tp in range(len(TOKP)):
                                    t0, nt_ = TOKP[tp]
                                    k = cnt[0] % 4
                                    cnt[0] += 1
                                    for kc in range(2):
                                        S.op("pe", lambda e, k=k, m=m, kc=kc, t0=t0, nt_=nt_, blk=blk, f=f: e.matmul(
                                            pg[k][:, 0:nt_], lhsT=gw[:, m, blk, kc, (f % 2) * 128:(f % 2 + 1) * 128],
                                            rhs=UC[:, 2 * blk + kc, t0:t0 + nt_], start=(kc == 0), stop=(kc == 1)),
                                            reads=[r_gw, r_UC], writes=[r_pg[k]], signal=(kc == 1))
                                    S.op("act", lambda e, k=k, dstt=dstt, t0=t0, nt_=nt_, bcol=bcol: e.activation(
                                        out=dstt[:, t0:t0 + nt_], in_=pg[k][:, 0:nt_], func=AF.Sigmoid, bias=bcol, scale=1.0),
                                        reads=[r_pg[k], R_c], writes=[r_dst])
                            S.op("act", lambda e, d_=d_, f=f, At=At, Bt=Bt, Rt=Rt, It=It: e.activation(out=At[:], in_=Rt[:], func=AF.Exp, scale=cl[:, d_, f:f + 1]),
                                 reads=[r_R, r_cl], writes=[r_A])
                            S.op("pool", lambda e, At=At, Bt=Bt, Rt=Rt, It=It: e.tensor_tensor(out=Bt[:], in0=At[:], in1=At[:], op=ALU.mult), reads=[r_A], writes=[r_B])
                            S.op("act", lambda e, At=At, Bt=Bt, Rt=Rt, It=It: e.activation(out=Bt[:], in_=Bt[:], func=AF.Sqrt, scale=-1.0, bias=1.0), reads=[r_B], writes=[r_B])
                            S.op("dve", lambda e, At=At, Bt=Bt, Rt=Rt, It=It: e.tensor_tensor(out=Bt[:], in0=Bt[:], in1=It[:], op=ALU.mult), reads=[r_B, r_I], writes=[r_B])
                            S.op("dve", lambda e, f=f, At=At, Bt=Bt, Rt=Rt, It=It: e.tensor_tensor(out=Bt[:], in0=Bt[:], in1=UC[:, f, :], op=ALU.mult), reads=[r_B, r_UC], writes=[r_B])
                            if d_ == 0:
                                S.op("dve", lambda e, At=At, Bt=Bt, Rt=Rt, It=It: e.tensor_tensor_scan(out=Hf[:], data0=At[:], data1=Bt[:], initial=0.0, op0=ALU.mult, op1=ALU.add),
                                     reads=[r_A, r_B], writes=[r_Hf])
                            else:
                                S.op("dve", lambda e, At=At, Bt=Bt, Rt=Rt, It=It: e.tensor_tensor_scan(out=Hb[:, 0:LC][:, ::-1],
                                                                           data0=At[:, 0:LC][:, ::-1], data1=Bt[:, 0:LC][:, ::-1],
                                                                           initial=0.0, op0=ALU.mult, op1=ALU.add),
                                     reads=[r_A, r_B], writes=[r_Hb])
                                S.op("dve", lambda e, At=At, Bt=Bt, Rt=Rt, It=It: e.tensor_tensor_scan(out=Hb[:, LC:NT][:, ::-1], data0=At[:, LC:NT][:, ::-1],
                                                                           data1=Bt[:, LC:NT][:, ::-1], initial=Hb[:, 0:1],
                                                                           op0=ALU.mult, op1=ALU.add),
                                     reads=[r_A, r_B, r_Hb], writes=[r_Hb])
                        S.op("pool", lambda e: e.tensor_tensor(out=Hf[:, LC:NT], in0=Hf[:, LC:NT], in1=Hb[:, LC:NT], op=ALU.add),
                             reads=[r_Hf, r_Hb], writes=[r_Hf])
                        S.op("dve", lambda e, f=f: e.tensor_tensor(out=YI[:, f, :], in0=Hf[:, LC:NT], in1=GG[:, f, :], op=ALU.mult),
                             reads=[r_Hf, r_GG], writes=[r_YI])
                    S.barrier()
                    S.emit()
                srcs = [(YI, c, r_YI, LC) for c in range(8)]
                outproj_residual(st, 1, b, srcs, od_w_out, XR, R_XRT, list(range(2, NTI)), VEC_OF_TILE(b))

        def moe_layer(l, tok_tiles, final):
            ntile = len(tok_tiles)
            ntok = ntile * 128
            nblk = ntok * 4 // BLK + n_exp
            with contextlib.ExitStack() as st:
                sH2 = contextlib.ExitStack()
                LG = sbuf(st, "LG", [128, ntile, NE], F32); r_LG = Res()
                D4 = sbuf(st, "D4", [128, ntile * 4], I32); r_D4 = Res()
                G4 = sbuf(st, "G4", [128, ntile * 4], F32); r_G4 = Res()
                IDXW = sbuf(st, "IDXW", [128, nblk], I32); r_IDXW = Res()
                BEI = sbuf(st, "BEI", [128, nblk], I32)
                H2 = sbuf(sH2, "H2", [128, ntile * D], BF16); r_H2 = Res()
                with contextlib.ExitStack() as s1:
                    vecs = sorted(set((NB if i < 2 else b) for b, i in tok_tiles))
                    tiles = norm_mod_tiles(s1, l, 1, vecs)
                    small, r_small = zip(*[new_small(s1, "n2small%d" % i) for i in range(2)])
                    junk = [sbuf(s1, "n2junk%d" % i, [128, D], F32) for i in range(2)]; r_junk = [Res(), Res()]
                    xt = [sbuf(s1, "n2x%d" % i, [128, D], F32) for i in range(2)]
                    r_xt = [Res(), Res()]
                    ht = [sbuf(s1, "n2h%d" % i, [128, D], F32) for i in range(2)]
                    r_ht = [Res(), Res()]
                    hT32 = [sbuf(s1, "n2t%d" % i, [128, 8, 128], F32) for i in range(2)]
                    r_hT32 = [Res(), Res()]
                    ptp = [psum(s1, "n2p%d" % i, [128, 4, 128], F32) for i in range(2)]
                    r_ptp = [Res(), Res()]
                    plg = [psum(s1, "n2l%d" % i, [128, NE], F32) for i in range(2)]
                    r_plg = [Res(), Res()]
                    wr = sbuf(s1, "wr", [128, 8, NE], F32); r_wr = Res()
                    S.dma("sp", lambda e: e.dma_start(out=wr[:], in_=router_w[l].rearrange("(c p) n -> p c n", p=128)), writes=[r_wr])
                    rb, r_rb = load_bcast(s1, "rb", router_b[l, :], [], n=NE)
                    def stage_a(n_i):
                        b, i = tok_tiles[n_i]
                        k = n_i % 2
                        G, rG, SH, rSH = tiles[NB if i < 2 else b]
                        S.dma("sp", lambda e: e.dma_start(out=xt[k][:], in_=XR[b, i * 128:(i + 1) * 128, :]),
                              reads=[R_XRT[(b, i)]], writes=[r_xt[k]])
                        rms_mod(s1, xt[k], r_xt[k], G, rG, SH, rSH, ht[k], r_ht[k], small[k], r_small[k], junk[k], r_junk[k])
                        S.op("act", lambda e: e.activation(out=H2[:, n_i * D:(n_i + 1) * D], in_=ht[k][:], func=AF.Copy), reads=[r_ht[k]], writes=[r_H2])

                    def stage_b(n_i):
                        k = n_i % 2
                        for hf in range(2):
                            for c4 in range(4):
                                c = hf * 4 + c4
                                S.op("pe", lambda e, hf=hf, c4=c4, c=c: e.transpose(
                                    out=ptp[hf][:, c4, :], in_=ht[k][:, c * 128:(c + 1) * 128], identity=ident[:]),
                                    reads=[r_ht[k], R_c], writes=[r_ptp[hf]], signal=(c4 == 3))
                            if hf == 0:
                                S.op("act", lambda e: e.activation(out=hT32[k][:, 0:4, :], in_=ptp[0][:], func=AF.Copy),
                                     reads=[r_ptp[0]], writes=[r_hT32[k]])
                            else:
                                S.op("dve", lambda e: e.tensor_copy(out=hT32[k][:, 4:8, :], in_=ptp[1][:]),
                                     reads=[r_ptp[1]], writes=[r_hT32[k]])
                        for c in range(8):
                            S.op("pe", lambda e, c=c: e.matmul(plg[k][:], lhsT=hT32[k][:, c, :], rhs=wr[:, c, :], start=(c == 0), stop=(c == 7)),
                                 reads=[r_hT32[k], r_wr], writes=[r_plg[k]], signal=(c == 7))
                        S.op("dve", lambda e: e.tensor_tensor(out=LG[:, n_i, :], in0=plg[k][:], in1=rb[:], op=ALU.add),
                             reads=[r_plg[k], r_rb], writes=[r_LG])

                    stage_a(0)
                    for n_i in range(ntile):
                        if n_i + 1 < ntile:
                            stage_a(n_i + 1)
                        stage_b(n_i)
                    S.barrier()
                    S.emit()
                with contextlib.ExitStack() as s1:
                    MX = sbuf(s1, "MX", [128, ntile, 8], F32)
                    MASK = sbuf(s1, "MASK", [128, ntile, NE], F32)
                    MASKb = sbuf(s1, "MASKb", [128, ntile, NE], BF16)
                    CUM = sbuf(s1, "CUM", [128, ntile, NE], BF16)
                    GAT = sbuf(s1, "GAT", [128, ntile, NE], F32)
                    TMP = sbuf(s1, "TMP", [128, ntile, NE], F32)
                    KEY = sbuf(s1, "KEY", [128, ntile, NE], F32)
                    K8 = sbuf(s1, "K8", [128, ntile, 8], F32)
                    DEN = sbuf(s1, "DEN", [128, ntile], F32)
                    TRI = sbuf(s1, "TRI", [128, 128], BF16)
                    CNT = sbuf(s1, "CNT", [128, NE], F32)
                    CNTI = sbuf(s1, "CNTI", [128, NE], I32)
                    PAD = sbuf(s1, "PAD", [128, NE], F32)
                    PEND = sbuf(s1, "PEND", [128, NE], F32)
                    BASE = sbuf(s1, "BASE", [128, NE], F32)
                    ONE32 = sbuf(s1, "ONE32", [128, NE], F32)
                    CMP = sbuf(s1, "CMP", [128, nblk, NE], F32)
                    BEF = sbuf(s1, "BEF", [128, nblk], F32)
                    JB = sbuf(s1, "JB", [128, 80], F32)
                    IOP = sbuf(s1, "IOP", [128, 1], F32)
                    pposb = [psum(s1, "ppos%d" % i, [128, 16, NE], F32) for i in range((ntile + 15) // 16)]
                    pcnt = psum(s1, "pcnt", [128, NE], F32)
                    R = Res("route")
                    V_ = lambda fn, eng="dve": S.op(eng, fn, reads=[R, r_LG], writes=[R, r_D4, r_G4, r_IDXW])
                    S.dma("sp", lambda e: e.dma_start(out=JB[:], in_=blk_start_d[:, :]), writes=[R])
                    S.dma("sp", lambda e: e.dma_start(out=IOP[:], in_=iota_p_d[:, :]), reads=[R], writes=[R])
                    V_(lambda e: e.memset(TRI[:], 1.0))
                    V_(lambda e: e.affine_select(out=TRI[:], in_=TRI[:], pattern=[[1, 128]], compare_op=ALU.is_gt, fill=0.0,
                                                 base=0, channel_multiplier=-1), "pool")
                    V_(lambda e: e.memset(ONE32[:], 1.0))
                    for i in range(ntile):
                        V_(lambda e, i=i: e.max(out=MX[:, i, :], in_=LG[:, i, :]))
                    V_(lambda e: e.tensor_tensor(out=MASK[:], in0=LG[:], in1=MX[:, :, 3:4].to_broadcast([128, ntile, NE]), op=ALU.is_ge))
                    V_(lambda e: e.tensor_tensor(out=TMP[:], in0=LG[:], in1=MX[:, :, 0:1].to_broadcast([128, ntile, NE]), op=ALU.subtract))
                    V_(lambda e: e.activation(out=TMP[:], in_=TMP[:], func=AF.Exp), "act")
                    V_(lambda e: e.tensor_tensor(out=TMP[:], in0=TMP[:], in1=MASK[:], op=ALU.mult))
                    V_(lambda e: e.tensor_reduce(out=DEN[:], in_=TMP[:], axis=AX.X, op=ALU.add))
                    V_(lambda e: e.reciprocal(out=DEN[:], in_=DEN[:]))
                    V_(lambda e: e.tensor_tensor(out=GAT[:], in0=TMP[:], in1=DEN[:].unsqueeze(2).to_broadcast([128, ntile, NE]), op=ALU.mult))
                    V_(lambda e: e.tensor_copy(out=MASKb[:], in_=MASK[:]))
                    V_(lambda e: e.memset(CUM[:, 0, :], 0.0))
                    for i in range(1, ntile):
                        V_(lambda e, i=i: e.tensor_tensor(out=CUM[:, i, :], in0=CUM[:, i - 1, :], in1=MASKb[:, i - 1, :], op=ALU.add))
                    V_(lambda e: e.tensor_tensor(out=TMP[:, 0, :], in0=CUM[:, ntile - 1, :], in1=MASKb[:, ntile - 1, :], op=ALU.add))
                    TOTb = sbuf(s1, "TOTb", [128, NE], BF16)
                    V_(lambda e: e.tensor_copy(out=TOTb[:], in_=TMP[:, 0, :]))
                    for i in range(ntile):
                        V_(lambda e, i=i: e.matmul(pposb[i // 16][:, i % 16, :], lhsT=TRI[:], rhs=MASKb[:, i, :], start=True, stop=False), "pe")
                        V_(lambda e, i=i: e.matmul(pposb[i // 16][:, i % 16, :], lhsT=onesb[:, 0:128], rhs=CUM[:, i, :], start=False, stop=True), "pe")
                    V_(lambda e: e.matmul(pcnt[:], lhsT=onesb[:, 0:128], rhs=TOTb[:], start=True, stop=True), "pe")
                    V_(lambda e: e.tensor_copy(out=CNT[:], in_=pcnt[:]))
                    NM = ntok // BLK + 1
                    CMP2 = sbuf(s1, "CMP2", [128, NE, NM], F32)
                    V_(lambda e: e.tensor_tensor(out=CMP2[:], in0=CNT[:].unsqueeze(2).to_broadcast([128, NE, NM]),
                                                 in1=JB[:, 0:NM].unsqueeze(1).to_broadcast([128, NE, NM]), op=ALU.is_gt))
                    V_(lambda e: e.tensor_reduce(out=PAD[:], in_=CMP2[:], axis=AX.X, op=ALU.add))
                    V_(lambda e: e.tensor_scalar(out=PAD[:], in0=PAD[:], scalar1=float(BLK), scalar2=None, op0=ALU.mult))
                    V_(lambda e: e.tensor_tensor_scan(out=PEND[:], data0=ONE32[:], data1=PAD[:], initial=0.0, op0=ALU.mult, op1=ALU.add))
                    V_(lambda e: e.tensor_tensor(out=BASE[:], in0=PEND[:], in1=PAD[:], op=ALU.subtract))
                    for bk in range((ntile + 15) // 16):
                        n_ = min(16, ntile - bk * 16)
                        V_(lambda e, bk=bk, n_=n_: e.tensor_tensor(out=KEY[:, bk * 16:bk * 16 + n_, :], in0=pposb[bk][:, 0:n_, :],
                                                                  in1=BASE[:].unsqueeze(1).to_broadcast([128, n_, NE]), op=ALU.add))
                    V_(lambda e: e.scalar_tensor_tensor(out=KEY[:], in0=KEY[:], scalar=1.0, in1=MASK[:], op0=ALU.add, op1=ALU.mult))
                    for i in range(ntile):
                        V_(lambda e, i=i: e.max(out=K8[:, i, :], in_=KEY[:, i, :]))
                    V_(lambda e: e.tensor_scalar(out=TMP[:, :, 0:4], in0=K8[:, :, 0:4], scalar1=-1.0, scalar2=0.0, op0=ALU.add, op1=ALU.max))
                    V_(lambda e: e.tensor_scalar(out=TMP[:, :, 0:4], in0=TMP[:, :, 0:4], scalar1=float(nblk * BLK - 1), scalar2=None, op0=ALU.min))
                    V_(lambda e: e.tensor_copy(out=D4[:].rearrange("p (n j) -> p n j", j=4), in_=TMP[:, :, 0:4]))
                    for j in range(4):
                        V_(lambda e, j=j: e.tensor_tensor(out=TMP[:], in0=KEY[:], in1=K8[:, :, j:j + 1].to_broadcast([128, ntile, NE]), op=ALU.is_equal))
                        V_(lambda e: e.tensor_tensor(out=TMP[:], in0=TMP[:], in1=GAT[:], op=ALU.mult))
                        V_(lambda e, j=j: e.tensor_reduce(out=G4[:].rearrange("p (n j) -> p n j", j=4)[:, :, j], in_=TMP[:], axis=AX.X, op=ALU.add))
                    V_(lambda e: e.tensor_tensor(out=CMP[:], in0=PEND[:].unsqueeze(1).to_broadcast([128, nblk, NE]),
                                                 in1=JB[:, 0:nblk].unsqueeze(2).to_broadcast([128, nblk, NE]), op=ALU.is_le))
                    V_(lambda e: e.tensor_reduce(out=BEF[:], in_=CMP[:], axis=AX.X, op=ALU.add))
                    V_(lambda e: e.tensor_scalar(out=BEF[:], in0=BEF[:], scalar1=float(n_exp - 1), scalar2=0.0, op0=ALU.min, op1=ALU.max))
                    V_(lambda e: e.tensor_copy(out=BEI[:], in_=BEF[:]))
                    V_(lambda e: e.tensor_scalar(out=BEF[:], in0=BEF[:], scalar1=128.0, scalar2=IOP[:, 0:1], op0=ALU.mult, op1=ALU.add))
                    V_(lambda e: e.tensor_copy(out=IDXW[:], in_=BEF[:]))
                    if dbg:
                        S.dma("sp", lambda e: e.dma_start(out=dbg_d4[:, 0:ntile * 4], in_=D4[:]), reads=[R, r_D4])
                        S.dma("sp", lambda e: e.dma_start(out=dbg_g4[:, 0:ntile * 4], in_=G4[:]), reads=[R, r_G4])
                        S.dma("sp", lambda e: e.dma_start(out=dbg_iw[:, 0:nblk], in_=IDXW[:]), reads=[R, r_IDXW])
                    for n_i in range(ntile if moe_stop >= 2 else 0):
                        for j in range(4):
                            S.dma("pool", lambda e, n_i=n_i, j=j: e.indirect_dma_start(
                                out=XS[:, :], out_offset=bass.IndirectOffsetOnAxis(ap=D4[:, n_i * 4 + j:n_i * 4 + j + 1], axis=0),
                                in_=H2[:, n_i * D:(n_i + 1) * D], in_offset=None),
                                reads=[R, r_D4, r_H2], writes=[])
                    S.barrier()
                    S.emit()
                sH2.close()
                if moe_stop < 3:
                    return
                w_aps = (w_gate[l], w_up[l], w_down[l])
                with contextlib.ExitStack() as s1:
                    WG = [sbuf(s1, "WG%d" % i, [128, 8192], BF16) for i in range(2)]
                    WU = [sbuf(s1, "WU%d" % i, [128, 8192], BF16) for i in range(2)]
                    WD = [sbuf(s1, "WD%d" % i, [128, 8192], BF16) for i in range(2)]
                    EB = [sbuf(s1, "EB%d" % i, [2, 3072], BF16) for i in range(2)]
                    EBC = [sbuf(s1, "EBC%d" % i, [128, 16], F32) for i in range(2)]
                    r_EBC = [Res(), Res()]
                    r_W = [[Res() for _ in range(4)] for _ in range(2)]
                    xs = [sbuf(s1, "xsb%d" % i, [128, 4, D], BF16) for i in range(2)]
                    r_xs = [Res(), Res()]
                    xeT = [sbuf(s1, "xeT%d" % i, [128, 8, BLK], BF16) for i in range(2)]
                    r_xeT = [Res(), Res()]
                    actT = sbuf(s1, "actT", [128, 8, BLK], BF16); r_actT = Res()
                    gs = [sbuf(s1, "gs%d" % i, [128, BLK], F32) for i in range(2)]
                    sg = [sbuf(s1, "sg%d" % i, [128, BLK], F32) for i in range(2)]
                    us = [sbuf(s1, "us%d" % i, [128, BLK], F32) for i in range(2)]
                    r_gs = [Res(), Res()]; r_sg = [Res(), Res()]; r_us = [Res(), Res()]
                    ysb = [sbuf(s1, "ysb%d" % i, [128, 4, D], BF16) for i in range(2)]
                    r_ysb = [Res(), Res()]
                    ptr = [psum(s1, "ptr%d" % i, [128, 8, 128], BF16) for i in range(2)]
                    r_ptr = [Res(), Res()]
                    pgp = [psum(s1, "pgp%d" % i, [128, BLK], F32) for i in range(2)]
                    pup = [psum(s1, "pup%d" % i, [128, BLK], F32) for i in range(2)]
                    r_pgp = [Res(), Res()]; r_pup = [Res(), Res()]
                    pyp = [psum(s1, "pyp%d" % i, [128, 512], F32) for i in range(2)]
                    r_pyp = [Res(), Res()]
                    def load_xs(jb_):
                        k_ = jb_ % 2
                        S.dma("sp", lambda e: e.dma_start(
                            out=xs[k_][:], in_=XS[jb_ * BLK:(jb_ + 1) * BLK, :].rearrange("(s p) d -> p s d", p=128)),
                            reads=[R_XS], writes=[r_xs[k_]])

                    for jb in range(nblk):
                        k = jb % 2
                        for mi, (wbuf, wap) in enumerate(((WG[k], w_aps[0]), (WU[k], w_aps[1]), (WD[k], w_aps[2]))):
                            S.dma("pool", lambda e, wbuf=wbuf, wap=wap, jb=jb: e.indirect_dma_start(
                                out=wbuf[:, :], out_offset=None, in_=wap[:, :],
                                in_offset=bass.IndirectOffsetOnAxis(ap=IDXW[:, jb:jb + 1], axis=0)), reads=[r_IDXW], writes=[r_W[k][mi]])
                        S.dma("pool", lambda e, k=k, jb=jb: e.indirect_dma_start(
                            out=EB[k][:, :], out_offset=None, in_=exp_b[l][:, :],
                            in_offset=bass.IndirectOffsetOnAxis(ap=BEI[0:2, jb:jb + 1], axis=0)), reads=[r_IDXW], writes=[r_W[k][3]])
                        S.dma("pool", lambda e, k=k, jb=jb: e.indirect_dma_start(
                            out=EBC[k][:, :], out_offset=None, in_=exp_bc[l][:, :],
                            in_offset=bass.IndirectOffsetOnAxis(ap=IDXW[:, jb:jb + 1], axis=0)), reads=[r_IDXW], writes=[r_EBC[k]])
                        S.op("dve", lambda e, k=k: e.tensor_scalar(out=EBC[k][:, 8:16], in0=EBC[k][:, 8:16], scalar1=1.0, scalar2=None, op0=ALU.add),
                             reads=[r_EBC[k]], writes=[r_EBC[k]])
                        if jb == 0:
                            load_xs(0)
                        for s in range(4):
                            kk = s % 2
                            for c in range(8):
                                S.op("pe", lambda e, k=k, kk=kk, s=s, c=c: e.transpose(out=ptr[kk][:, c, :], in_=xs[k][:, s, c * 128:(c + 1) * 128],
                                                                                 identity=identb[:]),
                                     reads=[r_xs[k], R_c], writes=[r_ptr[kk]], signal=(c == 7))
                            if s % 2 == 0:
                                S.op("act", lambda e, k=k, kk=kk, s=s: e.activation(out=xeT[k][:, :, s * 128:(s + 1) * 128], in_=ptr[kk][:], func=AF.Copy),
                                     reads=[r_ptr[kk]], writes=[r_xeT[k]])
                            else:
                                S.op("dve", lambda e, k=k, kk=kk, s=s: e.tensor_copy(out=xeT[k][:, :, s * 128:(s + 1) * 128], in_=ptr[kk][:]),
                                     reads=[r_ptr[kk]], writes=[r_xeT[k]])
                        if jb + 1 < nblk:
                            load_xs(jb + 1)
                        for f in range(8):
                            kf = f % 2
                            for (pp, r_pp, wbuf, mi) in ((pgp[kf], r_pgp[kf], WG[k], 0), (pup[kf], r_pup[kf], WU[k], 1)):
                                for c in range(8):
                                    S.op("pe", lambda e, pp=pp, wbuf=wbuf, c=c, f=f, k=k: e.matmul(
                                        pp[:], lhsT=wbuf[:, c * 1024 + f * 128:c * 1024 + (f + 1) * 128], rhs=xeT[k][:, c, :], start=(c == 0), stop=(c == 7)),
                                        reads=[r_W[k][mi], r_xeT[k]], writes=[r_pp], signal=(c == 7))
                            S.op("dve", lambda e, kf=kf, k=k, f=f: e.tensor_scalar(out=gs[kf][:], in0=pgp[kf][:], scalar1=EBC[k][:, f:f + 1], scalar2=7.0,
                                                                                 op0=ALU.add, op1=ALU.min),
                                 reads=[r_pgp[kf], r_EBC[k]], writes=[r_gs[kf]])
                            S.op("act", lambda e, kf=kf: e.activation(out=sg[kf][:], in_=gs[kf][:], func=AF.Sigmoid, scale=1.702),
                                 reads=[r_gs[kf]], writes=[r_sg[kf]])
                            S.op("dve", lambda e, kf=kf, k=k, f=f: e.tensor_scalar(out=us[kf][:], in0=pup[kf][:], scalar1=EBC[k][:, 8 + f:9 + f], scalar2=8.0,
                                                                                 op0=ALU.add, op1=ALU.min),
                                 reads=[r_pup[kf], r_EBC[k]], writes=[r_us[kf]])
                            S.op("dve", lambda e, kf=kf: e.scalar_tensor_tensor(out=us[kf][:], in0=us[kf][:], scalar=-6.0, in1=gs[kf][:],
                                                                                op0=ALU.max, op1=ALU.mult),
                                 reads=[r_us[kf], r_gs[kf]], writes=[r_us[kf]])
                            S.op("dve", lambda e, kf=kf, f=f: e.tensor_tensor(out=actT[:, f, :], in0=us[kf][:], in1=sg[kf][:], op=ALU.mult),
                                 reads=[r_us[kf], r_sg[kf]], writes=[r_actT])
                        for s in range(4):
                            for hh in range(2):
                                kp = (s * 2 + hh) % 2
                                for f in range(8):
                                    S.op("pe", lambda e, kp=kp, f=f, s=s, hh=hh, k=k: e.matmul(
                                        pyp[kp][:], lhsT=actT[:, f, s * 128:(s + 1) * 128],
                                        rhs=WD[k][:, f * 1024 + hh * 512:f * 1024 + (hh + 1) * 512], start=(f == 0), stop=False),
                                        reads=[r_W[k][2], r_actT], writes=[r_pyp[kp]], signal=False)
                                S.op("pe", lambda e, kp=kp, hh=hh, k=k: e.matmul(
                                    pyp[kp][:], lhsT=halfb[0:2, 0:128], rhs=EB[k][0:2, 2048 + hh * 512:2048 + (hh + 1) * 512], start=False, stop=True),
                                    reads=[r_W[k][3], R_c], writes=[r_pyp[kp]])
                                S.op("act", lambda e, kp=kp, k=k, s=s, hh=hh: e.activation(out=ysb[k][:, s, hh * 512:(hh + 1) * 512], in_=pyp[kp][:], func=AF.Copy),
                                     reads=[r_pyp[kp]], writes=[r_ysb[k]])
                        S.dma("sp", lambda e, k=k, jb=jb: e.dma_start(
                            out=YS[jb * BLK:(jb + 1) * BLK, :].rearrange("(s p) d -> p s d", p=128), in_=ysb[k][:]),
                            reads=[r_ysb[k]], writes=[R_YS])
                    S.barrier()
                    S.emit()
                if moe_stop < 4:
                    return
                with contextlib.ExitStack() as s1:
                    g2 = {}
                    for v in sorted(set((NB if i < 2 else b) for b, i in tok_tiles)):
                        g2[v] = load_bcast(s1, "g2_%d" % v, MOD[l, v, 5 * D:6 * D], [R_MOD])
                    yg = [[sbuf(s1, "yg%d_%d" % (i, j), [128, D], BF16) for j in range(4)] for i in range(2)]
                    r_yg = [[Res() for _ in range(4)] for _ in range(2)]
                    acc = [sbuf(s1, "cacc%d" % i, [128, D], F32) for i in range(2)]
                    r_acc = [Res(), Res()]
                    xt = [sbuf(s1, "cbx%d" % i, [128, D], F32) for i in range(2)]
                    r_xt = [Res(), Res()]
                    for n_i, (b, i) in enumerate(tok_tiles):
                        k = n_i % 2
                        gt, rgt = g2[NB if i < 2 else b]
                        S.dma("sp", lambda e, b=b, i=i, k=k: e.dma_start(out=xt[k][:], in_=XR[b, i * 128:(i + 1) * 128, :]),
                              reads=[R_XRT[(b, i)]], writes=[r_xt[k]])
                        for j in range(4):
                            S.dma("pool", lambda e, k=k, j=j, n_i=n_i: e.indirect_dma_start(
                                out=yg[k][j][:, :], out_offset=None, in_=YS[:, :],
                                in_offset=bass.IndirectOffsetOnAxis(ap=D4[:, n_i * 4 + j:n_i * 4 + j + 1], axis=0)), reads=[R_YS, r_D4], writes=[r_yg[k][j]])
                        S.op("dve", lambda e, k=k, n_i=n_i: e.tensor_scalar(out=acc[k][:], in0=yg[k][0][:], scalar1=G4[:, n_i * 4:n_i * 4 + 1], scalar2=None, op0=ALU.mult),
                             reads=[r_yg[k][0], r_G4], writes=[r_acc[k]])
                        for j in range(1, 4):
                            S.op("dve", lambda e, k=k, j=j, n_i=n_i: e.scalar_tensor_tensor(
                                out=acc[k][:], in0=yg[k][j][:], scalar=G4[:, n_i * 4 + j:n_i * 4 + j + 1], in1=acc[k][:], op0=ALU.mult, op1=ALU.add),
                                reads=[r_yg[k][j], r_acc[k], r_G4], writes=[r_acc[k]])
                        S.op("dve", lambda e, k=k, gt=gt: e.tensor_tensor(out=acc[k][:], in0=acc[k][:], in1=gt[:], op=ALU.mult),
                             reads=[r_acc[k], rgt], writes=[r_acc[k]])
                        S.op("dve", lambda e, k=k: e.tensor_tensor(out=xt[k][:], in0=xt[k][:], in1=acc[k][:], op=ALU.add),
                             reads=[r_acc[k], r_xt[k]], writes=[r_xt[k]])
                        if final:
                            S.dma("sp", lambda e, b=b, i=i, k=k: e.dma_start(out=out[b, (i - 2) * 128:(i - 1) * 128, :], in_=xt[k][:]),
                                  reads=[r_xt[k]], writes=[Res()])
                        else:
                            S.dma("sp", lambda e, b=b, i=i, k=k: e.dma_start(out=XR[b, i * 128:(i + 1) * 128, :], in_=xt[k][:]),
                                  reads=[r_xt[k]], writes=[R_XRT[(b, i)]])
                    S.barrier()
                    S.emit()

        if 0 in layers:
            for b in range(NB):
                layer0_mixer(b)
            if do_moe:
                moe_layer(0, [(b, i) for b in range(NB) for i in range(NTI)], final=False)
        if 1 in layers:
            for b in range(NB):
                layer1_mixer(b)
            if do_moe:
                moe_layer(1, [(b, i) for b in range(NB) for i in range(2, NTI)], final=True)
        if dbg:
            with contextlib.ExitStack() as st:
                t = sbuf(st, "dbgt", [128, D], F32); r_t = Res()
                for b in range(NB):
                    for i in range(2, NTI):
                        S.dma("sp", lambda e, b=b, i=i: e.dma_start(out=t[:], in_=XR[b, i * 128:(i + 1) * 128, :]), reads=[R_XRT[(b, i)]], writes=[r_t])
                        S.dma("sp", lambda e, b=b, i=i: e.dma_start(out=out[b, (i - 2) * 128:(i - 1) * 128, :], in_=t[:]), reads=[r_t], writes=[R_OUT])
                S.barrier()
                S.emit()
        S.barrier()
        S.emit()
        print("program instructions (incl waits):", S.n_ins, "sem counts:", S.cnt, "max dma sem:", max(S.dma_cnt.values()))
    return nc


def _core_inputs(inp, sh, c, NB=2):
    b0 = c * NB
    d = dict(sh)
    d["xin"] = np.ascontiguousarray(np.concatenate([inp["ctx"][b0:b0 + NB], inp["x"][b0:b0 + NB]], axis=1), np.float32)
    d["cvec"] = np.ascontiguousarray(np.concatenate([inp["c"][b0:b0 + NB], inp["c_ctx"][None]], 0), np.float32)
    return d


def kernel(**inputs):
    inp = {k: np.asarray(v) for k, v in inputs.items()}
    sh = _prep_shared(inp)
    nc = build_program(NB=2)
    in_maps = [_core_inputs(inp, sh, c) for c in range(8)]
    res = run_bass_kernel_spmd(nc, in_maps, core_ids=list(range(8)))
    return np.concatenate([r["out"] for r in res.results], axis=0).astype(np.float32)
```

```python
import contextlib
import numpy as np
import concourse.bass as bass
import concourse.mybir as mybir
from concourse.bass_utils import run_bass_kernel_spmd

F32 = mybir.dt.float32
BF16 = mybir.dt.bfloat16
I32 = mybir.dt.int32
AF = mybir.ActivationFunctionType
ALU = mybir.AluOpType
AX = mybir.AxisListType

ENGS = ("pe", "dve", "act", "pool", "sp")

D = 1024
T = 2048
LC = 256
NT = T + LC
NTI = NT // 128
NE = 32
BLK = 512
EPS = 1e-6
NEG = -30000.0


class Res:
    __slots__ = ("name", "w", "r")

    def __init__(self, name=""):
        self.name = name
        self.w = None
        self.r = []


class Sched:
    def __init__(self, nc, stack, n_dma_sems=12):
        self.nc = nc
        self.streams = {e: [] for e in ENGS}
        self.cnt = {e: 0 for e in ENGS}
        self.sems = {}
        for e in ENGS:
            self.sems[e] = stack.enter_context(nc.semaphore("s_" + e))
        self.dma_sems = {}
        self.dma_cnt = {}
        self.dma_rr = {}
        for q in ("sp", "pool"):
            self.dma_sems[q] = []
            for i in range(n_dma_sems):
                k = "d_%s_%d" % (q, i)
                self.sems[k] = stack.enter_context(nc.semaphore(k))
                self.dma_sems[q].append(k)
                self.dma_cnt[k] = 0
            self.dma_rr[q] = 0
        self.seen = {e: {} for e in ENGS}
        self.n_ins = 0

    def _need(self, eng, deps):
        best = {}
        for d in deps:
            if d is None:
                continue
            k, v = d
            if k == eng and eng == "pe":
                continue
            if self.seen[eng].get(k, 0) >= v:
                continue
            if best.get(k, 0) < v:
                best[k] = v
        for k, v in best.items():
            self.seen[eng][k] = v
        return list(best.items())

    @staticmethod
    def _deps(reads, writes):
        deps = []
        for r in reads:
            deps.append(r.w)
        for w in writes:
            deps.append(w.w)
            deps.extend(w.r)
        return deps

    @staticmethod
    def _commit(reads, writes, tok):
        for r in reads:
            r.r.append(tok)
            if len(r.r) > 16:
                m = {}
                for k, v in r.r:
                    if m.get(k, 0) < v:
                        m[k] = v
                r.r = list(m.items())
        for w in writes:
            w.w = tok
            w.r = []

    def op(self, eng, fn, reads=(), writes=(), signal=True):
        waits = self._need(eng, self._deps(reads, writes))
        if signal:
            self.cnt[eng] += 1
            tok = (eng, self.cnt[eng])
        else:
            tok = (eng, self.cnt[eng] + 1)
        self.streams[eng].append((waits, fn, signal, None))
        self._commit(reads, writes, tok)
        self.n_ins += 1 + len(waits)
        return tok

    def dma(self, q, fn, reads=(), writes=()):
        lst = self.dma_sems[q]
        k = lst[self.dma_rr[q] % len(lst)]
        self.dma_rr[q] += 1
        deps = self._deps(reads, writes)
        deps.append((k, self.dma_cnt[k]))
        waits = self._need(q, deps)
        self.dma_cnt[k] += 16
        tok = (k, self.dma_cnt[k])
        self.streams[q].append((waits, fn, False, k))
        self._commit(reads, writes, tok)
        self.n_ins += 1 + len(waits)
        return tok

    def barrier(self):
        final = []
        for e in ENGS:
            if self.cnt[e]:
                final.append((e, self.cnt[e]))
        for k, v in self.dma_cnt.items():
            if v:
                final.append((k, v))
        for e in ENGS:
            waits = self._need(e, [f for f in final if not (f[0] == e and e == "pe")])
            if waits:
                self.streams[e].append((waits, None, False, None))

    def emit(self):
        nc = self.nc
        sems = self.sems
        streams = self.streams

        def run(engname, engobj):
            for waits, fn, signal, dsem in streams[engname]:
                fold = None
                if fn is not None and dsem is None and waits:
                    fold = waits[-1]
                    waits = waits[:-1]
                for k, v in waits:
                    engobj.wait_ge(sems[k], v)
                if fn is None:
                    continue
                ins = fn(engobj)
                if fold is not None:
                    ins._wait_ge(sems[fold[0]], fold[1])
                if dsem is not None:
                    ins.then_inc(sems[dsem], 16)
                elif signal:
                    ins.then_inc(sems[engname], 1)

        with nc.Block() as block:
            @block.tensor
            def _(e):
                run("pe", e)

            @block.vector
            def _(e):
                run("dve", e)

            @block.scalar
            def _(e):
                run("act", e)

            @block.gpsimd
            def _(e):
                run("pool", e)

            @block.sync
            def _(e):
                run("sp", e)
        self.streams = {e: [] for e in ENGS}


def _na_bias_T(rpb):
    H = rpb.shape[0]
    out = np.full((H, 5, 640, 128), NEG, np.float32)
    pair_of_type = [0, 1, 5, 14, 15]
    for ty, j in enumerate(pair_of_type):
        ws = int(np.clip(2 * j - 4, 0, 23))
        a = ws // 2
        krow0 = 2 * a
        for rr in range(2):
            r = 2 * j + rr
            r0 = int(np.clip(r - 4, 0, 24))
            for i in range(10):
                kr = krow0 + i
                if not (r0 <= kr < r0 + 8):
                    continue
                dr = kr - r + 7
                c = np.arange(64)
                c0 = np.clip(c - 8, 0, 48)
                for cq in range(64):
                    kc = np.arange(c0[cq], c0[cq] + 16)
                    dc = kc - cq + 15
                    out[:, ty, i * 64 + kc, rr * 64 + cq] = rpb[:, dr, dc]
    return np.ascontiguousarray(out.reshape(H, 5, 5, 128, 128))


def _pcol(v):
    v = np.asarray(v, np.float32)
    return np.ascontiguousarray(v.reshape(-1, 128).T)


_GATE_INPUTS = ("od_fwd_wa", "od_fwd_ba", "od_fwd_wx", "od_fwd_bx", "od_fwd_lam",
                "od_bwd_wa", "od_bwd_ba", "od_bwd_wx", "od_bwd_bx", "od_bwd_lam")


def _prep_shared(inp):
    sh = {}
    for nm in _GATE_INPUTS:
        assert nm in inp
    sh["ada_w"] = np.ascontiguousarray(inp["ada_w"], np.float32)
    sh["ada_b"] = np.ascontiguousarray(inp["ada_b"], np.float32)
    sh["norm_g"] = np.ascontiguousarray(np.stack([inp["norm1_g"], inp["norm2_g"]], 1), np.float32)
    sh["ev_w_in"] = np.ascontiguousarray(inp["ev_w_in"][0], np.float32)
    sh["ev_w_out"] = np.ascontiguousarray(inp["ev_w_out"][0], np.float32)
    sh["biasT"] = _na_bias_T(np.asarray(inp["ev_rpb"][0], np.float32))
    sh["od_w_in"] = np.ascontiguousarray(inp["od_w_in"][0], np.float32)
    sh["od_w_out"] = np.ascontiguousarray(inp["od_w_out"][0], np.float32)
    gates = []
    for dr in ("fwd", "bwd"):
        for nm in ("wa", "wx"):
            gates.append(np.asarray(inp["od_%s_%s" % (dr, nm)][0], np.float32))
    sh["od_gw"] = np.ascontiguousarray(np.stack(gates, 0))
    cols = []
    qg = np.tile(np.asarray(inp["ev_q_gain"][0], np.float32), 2)
    kg = np.tile(np.asarray(inp["ev_k_gain"][0], np.float32), 2)
    cols.append(qg[:, None]); cols.append(kg[:, None])
    for j in range(3):
        cols.append(_pcol(inp["ev_conv_w"][0, j]))
    cols.append(_pcol(inp["ev_conv_b"][0]))
    for j in range(4):
        cols.append(_pcol(inp["od_conv_w"][0, j]))
    cols.append(_pcol(inp["od_conv_b"][0]))
    for dr in ("fwd", "bwd"):
        for nm in ("ba", "bx", "lam"):
            cols.append(_pcol(inp["od_%s_%s" % (dr, nm)][0]))
    sh["pcols"] = np.ascontiguousarray(np.concatenate(cols, 1), np.float32)
    sh["router_w"] = np.ascontiguousarray(inp["router_w"], np.float32)
    sh["router_b"] = np.ascontiguousarray(inp["router_b"], np.float32)
    for nm in ("gate", "up", "down"):
        w = np.asarray(inp["exp_w_" + nm], np.float32)
        w = w.reshape(2, NE, 8, 128, 1024).transpose(0, 1, 3, 2, 4).reshape(2, NE * 128, 8 * 1024)
        for l in range(2):
            sh["w_%s%d" % (nm, l)] = np.ascontiguousarray(w[l])
    eb = np.concatenate([inp["exp_b_gate"], inp["exp_b_up"], inp["exp_b_down"]], -1).astype(np.float32)
    for l in range(2):
        sh["exp_b%d" % l] = np.ascontiguousarray(eb[l])
        bgc = np.asarray(inp["exp_b_gate"][l], np.float32).reshape(NE, 8, 128).transpose(0, 2, 1)
        buc = np.asarray(inp["exp_b_up"][l], np.float32).reshape(NE, 8, 128).transpose(0, 2, 1)
        sh["exp_bc%d" % l] = np.ascontiguousarray(np.concatenate([bgc, buc], -1).reshape(NE * 128, 16))
    sh["iota_p"] = np.arange(128, dtype=np.float32)[:, None].copy()
    sh["blk_start"] = np.tile((np.arange(80, dtype=np.float32) * BLK)[None], (128, 1)).copy()
    return sh


PC_QG, PC_KG, PC_ECW, PC_ECB, PC_OCW, PC_OCB, PC_G = 0, 1, 2, 14, 18, 50, 58


def build_program(NB=2, layers=(0, 1), do_moe=True, dbg=False, n_exp=NE, moe_stop=4):
    nc = bass.Bass("TRN2", target_bir_lowering=False)
    NTOK0 = NB * NT
    dt = nc.dram_tensor

    def din(name, shape, dtp=F32):
        return dt(name, list(shape), dtp, kind="ExternalInput").ap()

    xin = din("xin", [NB, NT, D])
    cvec = din("cvec", [NB + 1, D])
    ada_w = din("ada_w", [2, D, 6 * D])
    ada_b = din("ada_b", [2, 6 * D])
    norm_g = din("norm_g", [2, 2, D])
    ev_w_in = din("ev_w_in", [D, 3072])
    ev_w_out = din("ev_w_out", [D, D])
    biasT = din("biasT", [8, 5, 5, 128, 128])
    od_w_in = din("od_w_in", [D, 2048])
    od_w_out = din("od_w_out", [D, D])
    od_gw = din("od_gw", [4, 4, 256, 256])
    pcols_d = din("pcols", [128, 106])
    router_w = din("router_w", [2, D, NE])
    router_b = din("router_b", [2, NE])
    w_gate = [din("w_gate%d" % l, [n_exp * 128, 8192]) for l in range(2)]
    w_up = [din("w_up%d" % l, [n_exp * 128, 8192]) for l in range(2)]
    w_down = [din("w_down%d" % l, [n_exp * 128, 8192]) for l in range(2)]
    exp_b = [din("exp_b%d" % l, [n_exp, 3072]) for l in range(2)]
    exp_bc = [din("exp_bc%d" % l, [n_exp * 128, 16]) for l in range(2)]
    iota_p_d = din("iota_p", [128, 1])
    blk_start_d = din("blk_start", [128, 80])
    out = dt("out", [NB, T, D], F32, kind="ExternalOutput").ap()
    if dbg:
        dbg_d4 = dt("dbg_d4", [128, NB * NTI * 4], I32, kind="ExternalOutput").ap()
        dbg_g4 = dt("dbg_g4", [128, NB * NTI * 4], F32, kind="ExternalOutput").ap()
        dbg_iw = dt("dbg_iw", [128, 80], I32, kind="ExternalOutput").ap()

    NBLK0 = NTOK0 * 4 // BLK + n_exp
    XR = dt("XR", [NB, NT, D], F32).ap()
    MOD = dt("MODs", [2, NB + 1, 6 * D], F32).ap()
    XS = dt("XS", [NBLK0 * BLK, D], BF16).ap()
    YS = dt("YS", [NBLK0 * BLK, D], BF16).ap()
    R_MOD, R_XS, R_YS, R_OUT = Res("MOD"), Res("XS"), Res("YS"), Res("OUT")
    R_XRT = {(b_, i_): Res() for b_ in range(NB) for i_ in range(NTI)}
    R_XIN = {(b_, i_): Res() for b_ in range(NB) for i_ in range(NTI)}

    with contextlib.ExitStack() as g:
        S = Sched(nc, g)

        uid = [0]

        def sbuf(st, name, shape, dtp):
            uid[0] += 1
            return st.enter_context(nc.sbuf_tensor("%s_%d" % (name, uid[0]), list(shape), dtp))

        def psum(st, name, shape, dtp=F32):
            uid[0] += 1
            return st.enter_context(nc.psum_tensor("%s_%d" % (name, uid[0]), list(shape), dtp))

        ident = sbuf(g, "ident", [128, 128], F32)
        identb = sbuf(g, "identb", [128, 128], BF16)
        onesb = sbuf(g, "onesb", [128, 512], BF16)
        halfb = sbuf(g, "halfb", [2, 512], BF16)
        pcols = sbuf(g, "pcols_sb", [128, 106], F32)
        R_c = Res("const")
        S.op("dve", lambda e: e.memset(ident[:], 0.0), writes=[R_c])
        S.op("pool", lambda e: e.affine_select(out=ident[:], in_=ident[:], pattern=[[-1, 128]], compare_op=ALU.not_equal,
                                               fill=1.0, base=0, channel_multiplier=1), reads=[R_c], writes=[R_c])
        S.op("dve", lambda e: e.tensor_copy(out=identb[:], in_=ident[:]), reads=[R_c], writes=[R_c])
        S.op("dve", lambda e: e.memset(onesb[:], 1.0), writes=[R_c])
        S.op("dve", lambda e: e.memset(halfb[:], 0.5), writes=[R_c])
        S.dma("sp", lambda e: e.dma_start(out=pcols[:], in_=pcols_d[:, :]), writes=[R_c])
        S.barrier()
        S.emit()

        NV = NB + 1
        with contextlib.ExitStack() as st:
            cs = sbuf(st, "cs", [NV, D], F32)
            sT = sbuf(st, "sT", [128, 8, NV], F32)
            aw = [sbuf(st, "aw%d" % i, [128, 8, 512], F32) for i in range(2)]
            R_aw = [Res(), Res()]
            ab = sbuf(st, "ab", [NV, 6 * D], F32)
            msb = sbuf(st, "msb", [NV, 6 * D], F32)
            pT = psum(st, "pT", [128, 8, NV], F32)
            pm = [psum(st, "pm%d" % i, [NV, 512], F32) for i in range(2)]
            R_pm = [Res(), Res()]
            R_cs, R_sT, R_ab, R_msb, R_pT = Res(), Res(), Res(), Res(), Res()
            S.dma("sp", lambda e: e.dma_start(out=cs[:], in_=cvec[:, :]), writes=[R_cs])
            S.op("act", lambda e: e.activation(out=cs[:], in_=cs[:], func=AF.Silu), reads=[R_cs], writes=[R_cs])
            for c in range(8):
                S.op("pe", lambda e, c=c: e.transpose(out=pT[:, c, :], in_=cs[:, c * 128:(c + 1) * 128], identity=ident[0:NV, 0:NV]),
                     reads=[R_cs, R_c], writes=[R_pT], signal=(c == 7))
            S.op("dve", lambda e: e.tensor_copy(out=sT[:], in_=pT[:]), reads=[R_pT], writes=[R_sT])
            for l in range(2):
                for v in range(NV):
                    S.dma("sp", lambda e, l=l, v=v: e.dma_start(out=ab[v:v + 1, :], in_=ada_b[l:l + 1, :]), writes=[R_ab])
                for j in range(12):
                    k = j % 2
                    S.dma("sp", lambda e, l=l, j=j, k=k: e.dma_start(
                        out=aw[k][:], in_=ada_w[l, :, j * 512:(j + 1) * 512].rearrange("(c p) n -> p c n", p=128)),
                        writes=[R_aw[k]])
                    for c in range(8):
                        S.op("pe", lambda e, c=c, k=k: e.matmul(pm[k][:], lhsT=sT[:, c, :], rhs=aw[k][:, c, :],
                                                                start=(c == 0), stop=(c == 7)),
                             reads=[R_sT, R_aw[k]], writes=[R_pm[k]], signal=(c == 7))
                    S.op("dve", lambda e, j=j, k=k: e.tensor_tensor(out=msb[:, j * 512:(j + 1) * 512], in0=pm[k][:],
                                                                     in1=ab[:, j * 512:(j + 1) * 512], op=ALU.add),
                         reads=[R_pm[k], R_ab], writes=[R_msb])
                S.dma("sp", lambda e, l=l: e.dma_start(out=MOD[l, :, :], in_=msb[:]), reads=[R_msb], writes=[R_MOD])
            S.barrier()
            S.emit()

        def load_bcast(st, name, src_ap, reads, n=D):
            t = sbuf(st, name, [128, n], F32)
            r = Res(name)
            S.dma("sp", lambda e: e.dma_start(out=t[:], in_=src_ap.partition_broadcast(128)), reads=reads, writes=[r])
            return t, r

        def norm_mod_tiles(st, l, which, vecs):
            res = {}
            gt, rg = load_bcast(st, "ng%d%d" % (l, which), norm_g[l, which, :], [])
            for v in vecs:
                sc, rsc = load_bcast(st, "sc%d" % v, MOD[l, v, (3 * which + 1) * D:(3 * which + 2) * D], [R_MOD])
                shh, rsh = load_bcast(st, "sh%d" % v, MOD[l, v, (3 * which) * D:(3 * which + 1) * D], [R_MOD])
                S.op("dve", lambda e, sc=sc: e.scalar_tensor_tensor(out=sc[:], in0=sc[:], scalar=1.0, in1=gt[:],
                                                                      op0=ALU.add, op1=ALU.mult),
                     reads=[rsc, rg], writes=[rsc])
                res[v] = (sc, rsc, shh, rsh)
            return res

        def rms_mod(st_tmp, xt, r_xt, G, rG, SH, rSH, ht, r_ht, small, r_small, junk, r_junk):
            S.op("dve", lambda e: e.memset(small[:, 0:1], 0.0), reads=[r_small], writes=[r_small])
            S.op("act", lambda e: e.activation(out=junk[:], in_=xt[:], func=AF.Square, accum_out=small[:, 0:1]),
                 reads=[r_xt, r_small], writes=[r_junk, r_small])
            S.op("act", lambda e: e.activation(out=small[:, 1:2], in_=small[:, 0:1], func=AF.Sqrt, scale=1.0 / D, bias=small[:, 3:4]),
                 reads=[r_small], writes=[r_small])
            S.op("dve", lambda e: e.reciprocal(out=small[:, 2:3], in_=small[:, 1:2]), reads=[r_small], writes=[r_small])
            S.op("dve", lambda e: e.scalar_tensor_tensor(out=ht[:], in0=xt[:], scalar=small[:, 2:3], in1=G[:],
                                                         op0=ALU.mult, op1=ALU.mult),
                 reads=[r_xt, r_small, rG], writes=[r_ht])
            S.op("dve", lambda e: e.tensor_tensor(out=ht[:], in0=ht[:], in1=SH[:], op=ALU.add),
                 reads=[r_ht, rSH], writes=[r_ht])

        def new_small(st, name):
            small = sbuf(st, name, [128, 4], F32)
            r = Res(name)
            S.op("dve", lambda e: e.memset(small[:], 0.0), writes=[r])
            S.op("dve", lambda e: e.memset(small[:, 3:4], EPS), reads=[r], writes=[r])
            return small, r

        def norm_to_hT(st, l, b, src_ap, src_res, hT, r_hT, vec_of_tile):
            with contextlib.ExitStack() as s2:
                vecs = sorted(set(vec_of_tile))
                tiles = norm_mod_tiles(s2, l, 0, vecs)
                NBF = 4
                small, r_small = zip(*[new_small(s2, "n1small%d" % i) for i in range(NBF)])
                junk1 = sbuf(s2, "n1junk", [128, D], F32)
                junk = [junk1] * NBF
                xt = [sbuf(s2, "n1x%d" % i, [128, D], F32) for i in range(NBF)]
                r_xt = [Res() for _ in range(NBF)]
                ht = [sbuf(s2, "n1h%d" % i, [128, D], F32) for i in range(NBF)]
                r_ht = [Res() for _ in range(NBF)]
                ptp = [psum(s2, "n1p%d" % i, [128, 4, 128], F32) for i in range(2)]
                r_ptp = [Res(), Res()]
                def stage_a(i):
                    k = i % NBF
                    G, rG, SH, rSH = tiles[vec_of_tile[i]]
                    S.dma("sp", lambda e: e.dma_start(out=xt[k][:], in_=src_ap[b, i * 128:(i + 1) * 128, :]),
                          reads=[src_res[(b, i)]], writes=[r_xt[k]])
                    rms_mod(s2, xt[k], r_xt[k], G, rG, SH, rSH, ht[k], r_ht[k], small[k], r_small[k], junk[k], Res())

                def stage_b(i):
                    k = i % NBF
                    for hf in range(2):
                        for c4 in range(4):
                            c = hf * 4 + c4
                            S.op("pe", lambda e, hf=hf, c4=c4, c=c: e.transpose(
                                out=ptp[hf][:, c4, :], in_=ht[k][:, c * 128:(c + 1) * 128], identity=ident[:]),
                                reads=[r_ht[k], R_c], writes=[r_ptp[hf]], signal=(c4 == 3))
                        if (hf + i) % 2 == 0:
                            S.op("act", lambda e, hf=hf: e.activation(
                                out=hT[:, hf * 4:(hf + 1) * 4, i * 128:(i + 1) * 128], in_=ptp[hf][:], func=AF.Copy),
                                reads=[r_ptp[hf]], writes=[r_hT])
                        else:
                            S.op("dve", lambda e, hf=hf: e.tensor_copy(
                                out=hT[:, hf * 4:(hf + 1) * 4, i * 128:(i + 1) * 128], in_=ptp[hf][:]),
                                reads=[r_ptp[hf]], writes=[r_hT])

                stage_a(0)
                stage_a(1)
                for i in range(NTI):
                    if i + 2 < NTI:
                        stage_a(i + 2)
                    stage_b(i)
                S.barrier()
                S.emit()

        def stream_w(st, name, n=2):
            bufs = [sbuf(st, "%s%d" % (name, i), [128, 8, 512], BF16) for i in range(n)]
            return bufs, [Res() for _ in range(n)]

        def load_w(buf, r, w_ap, col0, ncol=512):
            S.dma("pool", lambda e: e.dma_start(out=buf[:, :, 0:ncol],
                                                 in_=w_ap[:, col0:col0 + ncol].rearrange("(c p) n -> p c n", p=128)),
                  writes=[r])

        TOKP = [(i * 512, min(512, NT - i * 512)) for i in range((NT + 511) // 512)]

        def outproj_residual(st, l, b, srcs, w_ap, x_src, x_res, tiles_range, vec_of_tile, x_tok_off=0):
            with contextlib.ExitStack() as s2:
                wo = sbuf(s2, "wo", [128, 8, D], BF16); r_wo = Res()
                for hh in range(2):
                    S.dma("pool", lambda e, hh=hh: e.dma_start(
                        out=wo[:, :, hh * 512:(hh + 1) * 512],
                        in_=w_ap[:, hh * 512:(hh + 1) * 512].rearrange("(c p) n -> p c n", p=128)), writes=[r_wo])
                g1 = {}
                for v in sorted(set(vec_of_tile[i] for i in tiles_range)):
                    g1[v] = load_bcast(s2, "g1_%d" % v, MOD[l, v, 2 * D:3 * D], [R_MOD])
                xt = [sbuf(s2, "opx%d" % i, [128, D], F32) for i in range(2)]
                r_xt = [Res(), Res()]
                py = [psum(s2, "opy%d" % i, [128, 512], F32) for i in range(4)]
                r_py = [Res() for _ in range(4)]
                for n_i, i in enumerate(tiles_range):
                    k = n_i % 2
                    gt, rgt = g1[vec_of_tile[i]]
                    S.dma("sp", lambda e, i=i, k=k: e.dma_start(out=xt[k][:], in_=x_src[b, i * 128:(i + 1) * 128, :]),
                          reads=[x_res[(b, i)]], writes=[r_xt[k]])
                    for hh in range(2):
                        pk = k * 2 + hh
                        for c in range(8):
                            tsr, ch, rs, toff = srcs[c]
                            S.op("pe", lambda e, tsr=tsr, ch=ch, toff=toff, i=i, c=c, hh=hh, pk=pk: e.matmul(
                                py[pk][:], lhsT=tsr[:, ch, i * 128 - toff:(i + 1) * 128 - toff],
                                rhs=wo[:, c, hh * 512:(hh + 1) * 512], start=(c == 0), stop=(c == 7)),
                                reads=[rs, r_wo], writes=[r_py[pk]], signal=(c == 7))
                        S.op("dve", lambda e, hh=hh, pk=pk, gt=gt, k=k: e.tensor_tensor(
                            out=xg[k][:, hh * 512:(hh + 1) * 512],
                            in0=py[pk][:], in1=gt[:, hh * 512:(hh + 1) * 512], op=ALU.mult),
                            reads=[r_py[pk], rgt], writes=[r_xg[k]])
                    S.op("pool", lambda e, k=k: e.tensor_tensor(out=xt[k][:], in0=xt[k][:], in1=xg[k][:], op=ALU.add),
                         reads=[r_xg[k], r_xt[k]], writes=[r_xt[k]])
                    S.dma("sp", lambda e, i=i, k=k: e.dma_start(out=XR[b, i * 128:(i + 1) * 128, :], in_=xt[k][:]),
                          reads=[r_xt[k]], writes=[R_XRT[(b, i)]])
                S.barrier()
                S.emit()

        xg = [sbuf(g, "xg%d" % i, [128, D], F32) for i in range(2)]
        r_xg = [Res(), Res()]

        VEC_OF_TILE = lambda b: [NB, NB] + [b] * 16

        def layer0_mixer(b):
            with contextlib.ExitStack() as st:
                qT = sbuf(st, "qT", [128, 4, NT], BF16); r_qT = Res()
                kT = sbuf(st, "kT", [128, 4, NT], BF16); r_kT = Res()
                V = sbuf(st, "V", [128, NTI, 512], BF16); r_V = Res()
                OB = sbuf(st, "OB", [128, 4, NT], BF16); r_OB = Res()
                with contextlib.ExitStack() as s1:
                    hT = sbuf(s1, "hT", [128, 8, NT], BF16); r_hT = Res()
                    norm_to_hT(s1, 0, b, xin, R_XIN, hT, r_hT, VEC_OF_TILE(b))
                    wb, r_wb = stream_w(s1, "wi")
                    blk1 = sbuf(s1, "blk1", [128, 128], BF16); r_blk = Res()
                    S.op("dve", lambda e: e.memset(blk1[:], 0.0), writes=[r_blk])
                    S.op("dve", lambda e: e.memset(blk1[0:64, 0:64], 1.0 / 64), reads=[r_blk], writes=[r_blk])
                    S.op("dve", lambda e: e.memset(blk1[64:128, 64:128], 1.0 / 64), reads=[r_blk], writes=[r_blk])
                    pj = [psum(s1, "pj%d" % i, [128, 512], F32) for i in range(3)]
                    r_pj = [Res() for _ in range(3)]
                    pq = [psum(s1, "pq%d" % i, [128, 512], F32) for i in range(2)]
                    r_pq = [Res() for _ in range(2)]
                    sq = [sbuf(s1, "sq%d" % i, [128, 512], BF16) for i in range(2)]
                    r_sq = [Res(), Res()]
                    rs_t = [sbuf(s1, "rst%d" % i, [128, 512], F32) for i in range(2)]
                    r_rs = [Res(), Res()]
                    eps64 = sbuf(s1, "eps64", [128, 2], F32); r_e64 = Res()
                    S.op("dve", lambda e: e.memset(eps64[:, 0:1], EPS), writes=[r_e64])
                    S.op("dve", lambda e: e.memset(eps64[:, 1:2], 64.0 * EPS), reads=[r_e64], writes=[r_e64])
                    cnt = [0]

                    def proj_piece(wbuf, r_w, wc, tp):
                        t0, nt_ = TOKP[tp]
                        k = cnt[0] % 3
                        cnt[0] += 1
                        for c in range(8):
                            S.op("pe", lambda e, c=c, k=k: e.matmul(pj[k][:, 0:nt_], lhsT=wbuf[:, c, wc * 128:(wc + 1) * 128],
                                                                   rhs=hT[:, c, t0:t0 + nt_], start=(c == 0), stop=(c == 7)),
                                 reads=[r_w, r_hT], writes=[r_pj[k]], signal=(c == 7))
                        return k, t0, nt_

                    for grp, (dst, r_dst, gcol, sc_, ecol) in enumerate(((qT, r_qT, PC_QG, 64.0, 1), (kT, r_kT, PC_KG, 1.0, 0))):
                        bi = grp % 2
                        load_w(wb[bi], r_wb[bi], ev_w_in, grp * 512)
                        for wc in range(4):
                            for tp in range(len(TOKP)):
                                k, t0, nt_ = proj_piece(wb[bi], r_wb[bi], wc, tp)
                                k2 = cnt[0] % 2
                                S.op("act", lambda e, k=k, k2=k2, nt_=nt_: e.activation(out=sq[k2][:, 0:nt_], in_=pj[k][:, 0:nt_], func=AF.Square),
                                     reads=[r_pj[k]], writes=[r_sq[k2]])
                                S.op("pe", lambda e, k2=k2, nt_=nt_: e.matmul(pq[k2][:, 0:nt_], lhsT=blk1[:], rhs=sq[k2][:, 0:nt_], start=True, stop=True),
                                     reads=[r_blk, r_sq[k2]], writes=[r_pq[k2]])
                                S.op("act", lambda e, k2=k2, nt_=nt_, sc_=sc_, ecol=ecol: e.activation(
                                    out=rs_t[k2][:, 0:nt_], in_=pq[k2][:, 0:nt_], func=AF.Sqrt, scale=sc_, bias=eps64[:, ecol:ecol + 1]),
                                    reads=[r_pq[k2], r_e64], writes=[r_rs[k2]])
                                S.op("dve", lambda e, k2=k2, nt_=nt_: e.reciprocal(out=rs_t[k2][:, 0:nt_], in_=rs_t[k2][:, 0:nt_]),
                                     reads=[r_rs[k2]], writes=[r_rs[k2]])
                                S.op("dve", lambda e, k=k, k2=k2, nt_=nt_, t0=t0, wc=wc, dst=dst, gcol=gcol: e.scalar_tensor_tensor(
                                    out=dst[:, wc, t0:t0 + nt_], in0=pj[k][:, 0:nt_], scalar=pcols[:, gcol:gcol + 1], in1=rs_t[k2][:, 0:nt_],
                                    op0=ALU.mult, op1=ALU.mult), reads=[r_pj[k], r_rs[k2], R_c], writes=[r_dst])
                    load_w(wb[0], r_wb[0], ev_w_in, 1024)
                    for i in range(NTI):
                        k = cnt[0] % 3
                        cnt[0] += 1
                        for c in range(8):
                            S.op("pe", lambda e, c=c, k=k, i=i: e.matmul(pj[k][:], lhsT=hT[:, c, i * 128:(i + 1) * 128], rhs=wb[0][:, c, :],
                                                                        start=(c == 0), stop=(c == 7)),
                                 reads=[r_wb[0], r_hT], writes=[r_pj[k]], signal=(c == 7))
                        S.op("act", lambda e, k=k, i=i: e.activation(out=V[:, i, :], in_=pj[k][:], func=AF.Copy),
                             reads=[r_pj[k]], writes=[r_V])
                    load_w(wb[1], r_wb[1], ev_w_in, 2560)
                    wcg = sbuf(s1, "wcg", [128, 8, 512], BF16); r_wcg = Res()
                    load_w(wcg, r_wcg, ev_w_in, 2048)
                    load_w(wb[0], r_wb[0], ev_w_in, 1536)
                    xs_f = sbuf(s1, "xs_f", [128, NT], F32); r_xs = Res()
                    cx_f = sbuf(s1, "cx_f", [128, NT], F32); r_cx = Res()
                    t_f = sbuf(s1, "t_f", [128, NT], F32); r_t = Res()
                    for f in range(4):
                        for tp in range(len(TOKP)):
                            k, t0, nt_ = proj_piece(wb[1], r_wb[1], f, tp)
                            S.op("act", lambda e, k=k, t0=t0, nt_=nt_: e.activation(out=xs_f[:, t0:t0 + nt_], in_=pj[k][:, 0:nt_], func=AF.Copy),
                                 reads=[r_pj[k]], writes=[r_xs])
                        for tp in range(len(TOKP)):
                            k, t0, nt_ = proj_piece(wcg, r_wcg, f, tp)
                            S.op("dve", lambda e, k=k, t0=t0, nt_=nt_: e.tensor_tensor(out=cx_f[:, t0:t0 + nt_], in0=pj[k][:, 0:nt_],
                                                                                      in1=xs_f[:, t0:t0 + nt_], op=ALU.mult),
                                 reads=[r_pj[k], r_xs], writes=[r_cx])
                        w0 = pcols[:, PC_ECW + 0 * 4 + f:PC_ECW + 0 * 4 + f + 1]
                        w1 = pcols[:, PC_ECW + 1 * 4 + f:PC_ECW + 1 * 4 + f + 1]
                        w2 = pcols[:, PC_ECW + 2 * 4 + f:PC_ECW + 2 * 4 + f + 1]
                        bb = pcols[:, PC_ECB + f:PC_ECB + f + 1]
                        S.op("dve", lambda e, w1=w1, bb=bb: e.tensor_scalar(out=t_f[:], in0=cx_f[:], scalar1=w1, scalar2=bb, op0=ALU.mult, op1=ALU.add),
                             reads=[r_cx, R_c], writes=[r_t])
                        for (s0, sn) in ((0, LC), (LC, T)):
                            S.op("dve", lambda e, s0=s0, sn=sn, w0=w0: e.scalar_tensor_tensor(
                                out=t_f[:, s0 + 1:s0 + sn], in0=cx_f[:, s0:s0 + sn - 1], scalar=w0, in1=t_f[:, s0 + 1:s0 + sn],
                                op0=ALU.mult, op1=ALU.add), reads=[r_cx, r_t, R_c], writes=[r_t])
                            S.op("dve", lambda e, s0=s0, sn=sn, w2=w2: e.scalar_tensor_tensor(
                                out=t_f[:, s0:s0 + sn - 1], in0=cx_f[:, s0 + 1:s0 + sn], scalar=w2, in1=t_f[:, s0:s0 + sn - 1],
                                op0=ALU.mult, op1=ALU.add), reads=[r_cx, r_t, R_c], writes=[r_t])
                        for tp in range(len(TOKP)):
                            k, t0, nt_ = proj_piece(wb[0], r_wb[0], f, tp)
                            S.op("dve", lambda e, k=k, t0=t0, nt_=nt_, f=f: e.tensor_tensor(out=OB[:, f, t0:t0 + nt_], in0=pj[k][:, 0:nt_],
                                                                                           in1=t_f[:, t0:t0 + nt_], op=ALU.mult),
                                 reads=[r_pj[k], r_t], writes=[r_OB])
                    S.barrier()
                    S.emit()
                OA = sbuf(st, "OA", [128, 4, NT], BF16); r_OA = Res()
                with contextlib.ExitStack() as s1:
                    bias = [sbuf(s1, "bias%d" % i, [128, 5, 5, 128], BF16) for i in range(2)]
                    r_bias = [Res(), Res()]
                    sta = [psum(s1, "sta%d" % i, [128, 4, 128], F32) for i in range(2)]
                    stb = [psum(s1, "stb%d" % i, [128, 4, 128], F32) for i in range(2)]
                    r_sta = [Res(), Res()]; r_stb = [Res(), Res()]
                    po = [psum(s1, "po%d" % i, [64, 2, 128], F32) for i in range(2)]
                    r_po = [Res(), Res()]
                    PT = [sbuf(s1, "PT%d" % i, [128, 7, 128], BF16) for i in range(2)]
                    r_PT = [Res(), Res()]
                    rc = [sbuf(s1, "rc%d" % i, [64, 128], F32) for i in range(2)]
                    r_rc = [Res(), Res()]
                    it = 0
                    for h in range(8):
                        hp, hc = h % 2, h // 2
                        bsel = h % 2
                        S.dma("pool", lambda e, h=h, bsel=bsel: e.dma_start(out=bias[bsel][:], in_=biasT[h].rearrange("t c p q -> p t c q")),
                              writes=[r_bias[bsel]])
                        jobs = [("ctx", 0), ("ctx", 1)] + [("lat", j) for j in range(16)]
                        for kind, j in jobs:
                            k = it % 2
                            it += 1
                            if kind == "ctx":
                                qtile = j
                                ktiles = [0, 1]
                                ty = None
                            else:
                                qtile = 2 + j
                                ws = min(max(2 * j - 4, 0), 23)
                                a = ws // 2
                                ktiles = [0, 1] + [2 + a + cc for cc in range(5)]
                                ty = {0: 0, 1: 1, 14: 3, 15: 4}.get(j, 2)
                            nk = len(ktiles)
                            q_ap = qT[hp * 64:(hp + 1) * 64, hc, qtile * 128:(qtile + 1) * 128]
                            for kc, kt in enumerate(ktiles):
                                dstp, r_dst = (sta[k], r_sta[k]) if kc < 4 else (stb[k], r_stb[k])
                                last = (ty is None) and ((kc == min(3, nk - 1)) or (kc == nk - 1))
                                S.op("pe", lambda e, dstp=dstp, kc=kc, kt=kt, q_ap=q_ap, hp=hp, hc=hc: e.matmul(
                                    dstp[:, kc % 4, :], lhsT=kT[hp * 64:(hp + 1) * 64, hc, kt * 128:(kt + 1) * 128], rhs=q_ap,
                                    start=(kc % 4 == 0), stop=True, skip_group_check=True), reads=[r_kT, r_qT], writes=[r_dst], signal=last)
                            if ty is not None:
                                for kc in range(2, nk):
                                    dstp, r_dst = (sta[k], r_sta[k]) if kc < 4 else (stb[k], r_stb[k])
                                    last = (kc == 3) or (kc == nk - 1)
                                    S.op("pe", lambda e, dstp=dstp, kc=kc, ty=ty, bsel=bsel: e.matmul(
                                        dstp[:, kc % 4, :], lhsT=identb[:], rhs=bias[bsel][:, ty, kc - 2, :], start=False, stop=True,
                                        skip_group_check=True), reads=[r_bias[bsel], R_c], writes=[r_dst], signal=last)
                            na = min(4, nk)
                            S.op("act", lambda e, k=k, na=na: e.activation(out=PT[k][:, 0:na, :], in_=sta[k][:, 0:na, :], func=AF.Exp),
                                 reads=[r_sta[k]], writes=[r_PT[k]])
                            if nk > 4:
                                S.op("act", lambda e, k=k, nk=nk: e.activation(out=PT[k][:, 4:nk, :], in_=stb[k][:, 0:nk - 4, :], func=AF.Exp),
                                     reads=[r_stb[k]], writes=[r_PT[k]])
                            for kc, kt in enumerate(ktiles):
                                S.op("pe", lambda e, k=k, kc=kc, kt=kt, nk=nk, h=h: e.matmul(
                                    po[k][:, 0, :], lhsT=V[:, kt, h * 64:(h + 1) * 64], rhs=PT[k][:, kc, :], start=(kc == 0), stop=(kc == nk - 1)),
                                    reads=[r_V, r_PT[k]], writes=[r_po[k]], signal=False)
                            for kc, kt in enumerate(ktiles):
                                S.op("pe", lambda e, k=k, kc=kc, nk=nk: e.matmul(
                                    po[k][:, 1, :], lhsT=onesb[:, 0:64], rhs=PT[k][:, kc, :], start=(kc == 0), stop=(kc == nk - 1)),
                                    reads=[R_c, r_PT[k]], writes=[r_po[k]], signal=(kc == nk - 1))
                            S.op("dve", lambda e, k=k: e.reciprocal(out=rc[k][:], in_=po[k][:, 1, :]), reads=[r_po[k]], writes=[r_rc[k]])
                            S.op("dve", lambda e, k=k, hp=hp, hc=hc, qtile=qtile: e.tensor_tensor(
                                out=OA[hp * 64:(hp + 1) * 64, hc, qtile * 128:(qtile + 1) * 128], in0=po[k][:, 0, :], in1=rc[k][:], op=ALU.mult),
                                reads=[r_po[k], r_rc[k]], writes=[r_OA])
                    S.barrier()
                    S.emit()
                srcs = [(OA, c, r_OA, 0) for c in range(4)] + [(OB, c, r_OB, 0) for c in range(4)]
                outproj_residual(st, 0, b, srcs, ev_w_out, xin, R_XIN, list(range(NTI)), VEC_OF_TILE(b))

        def layer1_mixer(b):
            with contextlib.ExitStack() as st:
                UC = sbuf(st, "UC", [128, 8, NT], BF16); r_UC = Res()
                GG = sbuf(st, "GG", [128, 8, T], BF16); r_GG = Res()
                with contextlib.ExitStack() as s1:
                    hT = sbuf(s1, "hT1", [128, 8, NT], BF16); r_hT = Res()
                    norm_to_hT(s1, 1, b, XR, R_XRT, hT, r_hT, VEC_OF_TILE(b))
                    wb, r_wb = stream_w(s1, "wi1")
                    pj = [psum(s1, "pj1%d" % i, [128, 512], F32) for i in range(3)]
                    r_pj = [Res() for _ in range(3)]
                    u_f = sbuf(s1, "u_f", [128, NT], F32); r_u = Res()
                    t_f = sbuf(s1, "t1_f", [128, NT], F32); r_t = Res()
                    cnt = [0]
                    for grp in range(4):
                        bi = grp % 2
                        load_w(wb[bi], r_wb[bi], od_w_in, grp * 512)
                        for wc in range(4):
                            f = (grp % 2) * 4 + wc
                            for tp in range(len(TOKP)):
                                t0, nt_ = TOKP[tp]
                                if grp < 2 and t0 + nt_ <= LC:
                                    continue
                                k = cnt[0] % 3
                                cnt[0] += 1
                                for c in range(8):
                                    S.op("pe", lambda e, c=c, k=k, bi=bi, wc=wc, t0=t0, nt_=nt_: e.matmul(
                                        pj[k][:, 0:nt_], lhsT=wb[bi][:, c, wc * 128:(wc + 1) * 128], rhs=hT[:, c, t0:t0 + nt_],
                                        start=(c == 0), stop=(c == 7)), reads=[r_wb[bi], r_hT], writes=[r_pj[k]], signal=(c == 7))
                                if grp < 2:
                                    lo = max(t0, LC)
                                    S.op("act", lambda e, k=k, f=f, lo=lo, t0=t0, nt_=nt_: e.activation(
                                        out=GG[:, f, lo - LC:t0 + nt_ - LC], in_=pj[k][:, lo - t0:nt_], func=AF.Gelu),
                                        reads=[r_pj[k]], writes=[r_GG])
                                else:
                                    S.op("act", lambda e, k=k, t0=t0, nt_=nt_: e.activation(out=u_f[:, t0:t0 + nt_], in_=pj[k][:, 0:nt_], func=AF.Copy),
                                         reads=[r_pj[k]], writes=[r_u])
                            if grp >= 2:
                                wj = [pcols[:, PC_OCW + j * 8 + f:PC_OCW + j * 8 + f + 1] for j in range(4)]
                                bb = pcols[:, PC_OCB + f:PC_OCB + f + 1]
                                S.op("dve", lambda e, wj=wj, bb=bb: e.tensor_scalar(out=t_f[:], in0=u_f[:], scalar1=wj[1], scalar2=bb, op0=ALU.mult, op1=ALU.add),
                                     reads=[r_u, R_c], writes=[r_t])
                                for (s0, sn) in ((0, LC), (LC, T)):
                                    S.op("dve", lambda e, s0=s0, sn=sn, wj=wj: e.scalar_tensor_tensor(
                                        out=t_f[:, s0 + 1:s0 + sn], in0=u_f[:, s0:s0 + sn - 1], scalar=wj[0], in1=t_f[:, s0 + 1:s0 + sn],
                                        op0=ALU.mult, op1=ALU.add), reads=[r_u, r_t, R_c], writes=[r_t])
                                    S.op("dve", lambda e, s0=s0, sn=sn, wj=wj: e.scalar_tensor_tensor(
                                        out=t_f[:, s0:s0 + sn - 1], in0=u_f[:, s0 + 1:s0 + sn], scalar=wj[2], in1=t_f[:, s0:s0 + sn - 1],
                                        op0=ALU.mult, op1=ALU.add), reads=[r_u, r_t, R_c], writes=[r_t])
                                    S.op("dve", lambda e, s0=s0, sn=sn, wj=wj: e.scalar_tensor_tensor(
                                        out=t_f[:, s0:s0 + sn - 2], in0=u_f[:, s0 + 2:s0 + sn], scalar=wj[3], in1=t_f[:, s0:s0 + sn - 2],
                                        op0=ALU.mult, op1=ALU.add), reads=[r_u, r_t, R_c], writes=[r_t])
                                S.op("act", lambda e, f=f: e.activation(out=UC[:, f, :], in_=t_f[:], func=AF.Copy), reads=[r_t], writes=[r_UC])
                    S.barrier()
                    S.emit()
                YI, r_YI = GG, r_GG
                with contextlib.ExitStack() as s1:
                    gw = sbuf(s1, "gw", [128, 4, 4, 2, 256], BF16); r_gw = Res()
                    for m in range(4):
                        S.dma("pool", lambda e, m=m: e.dma_start(out=gw[:, m], in_=od_gw[m].rearrange("b (k p) n -> p b k n", p=128)),
                              writes=[r_gw])
                    cl = sbuf(s1, "cl", [128, 2, 8], F32); r_cl = Res()
                    for d_ in range(2):
                        lam = pcols[:, PC_G + d_ * 24 + 16:PC_G + d_ * 24 + 24]
                        S.op("act", lambda e, d_=d_, lam=lam: e.activation(out=cl[:, d_, :], in_=lam, func=AF.Exp, scale=-1.0), reads=[R_c], writes=[r_cl])
                        S.op("act", lambda e, d_=d_: e.activation(out=cl[:, d_, :], in_=cl[:, d_, :], func=AF.Ln, bias=1.0, scale=1.0), reads=[r_cl], writes=[r_cl])
                        S.op("dve", lambda e, d_=d_: e.tensor_scalar(out=cl[:, d_, :], in0=cl[:, d_, :], scalar1=-8.0, scalar2=None, op0=ALU.mult),
                             reads=[r_cl], writes=[r_cl])
                    pg = [psum(s1, "pg%d" % i, [128, 512], F32) for i in range(4)]
                    r_pg = [Res() for _ in range(4)]
                    Rts = [sbuf(s1, "Rt%d" % i, [128, NT], F32) for i in range(2)]; r_Rs = [Res(), Res()]
                    Its = [sbuf(s1, "It%d" % i, [128, NT], F32) for i in range(2)]; r_Is = [Res(), Res()]
                    Ats = [sbuf(s1, "At%d" % i, [128, NT], F32) for i in range(2)]; r_As = [Res(), Res()]
                    Bts = [sbuf(s1, "Bt%d" % i, [128, NT], F32) for i in range(2)]; r_Bs = [Res(), Res()]
                    Hf = sbuf(s1, "Hf", [128, NT], F32); r_Hf = Res()
                    Hb = sbuf(s1, "Hb", [128, NT], F32); r_Hb = Res()
                    cnt = [0]
                    for f in range(8):
                        blk = f // 2
                        for d_ in range(2):
                            Rt, It, At, Bt = Rts[d_], Its[d_], Ats[d_], Bts[d_]
                            r_R, r_I, r_A, r_B = r_Rs[d_], r_Is[d_], r_As[d_], r_Bs[d_]
                            for gi, (dstt, r_dst) in enumerate(((Rt, r_R), (It, r_I))):
                                m = d_ * 2 + gi
                                bcol = pcols[:, PC_G + d_ * 24 + gi * 8 + f:PC_G + d_ * 24 + gi * 8 + f + 1]
                                for tp in range(len(TOKP)):
                                    t0, nt_ = TOKP[tp]
                                    k = cnt[0] % 4
                                    cnt[0] += 1
                                    for kc in range(2):
                                        S.op("pe", lambda e, k=k, m=m, kc=kc, t0=t0, nt_=nt_, blk=blk, f=f: e.matmul(
                                            pg[k][:, 0:nt_], lhsT=gw[:, m, blk, kc, (f % 2) * 128:(f % 2 + 1) * 128],
                                            rhs=UC[:, 2 * blk + kc, t0:t0 + nt_], start=(kc == 0), stop=(kc == 1)),
                                            reads=[r_gw, r_UC], writes=[r_pg[k]], signal=(kc == 1))
                                    S.op("act", lambda e, k=k, dstt=dstt, t0=t0, nt_=nt_, bcol=bcol: e.activation(
                                        out=dstt[:, t0:t0 + nt_], in_=pg[k][:, 0:nt_], func=AF.Sigmoid, bias=bcol, scale=1.0),
                                        reads=[r_pg[k], R_c], writes=[r_dst])
                            S.op("act", lambda e, d_=d_, f=f, At=At, Bt=Bt, Rt=Rt, It=It: e.activation(out=At[:], in_=Rt[:], func=AF.Exp, scale=cl[:, d_, f:f + 1]),
                                 reads=[r_R, r_cl], writes=[r_A])
                            S.op("pool", lambda e, At=At, Bt=Bt, Rt=Rt, It=It: e.tensor_tensor(out=Bt[:], in0=At[:], in1=At[:], op=ALU.mult), reads=[r_A], writes=[r_B])
                            S.op("act", lambda e, At=At, Bt=Bt, Rt=Rt, It=It: e.activation(out=Bt[:], in_=Bt[:], func=AF.Sqrt, scale=-1.0, bias=1.0), reads=[r_B], writes=[r_B])
                            S.op("dve", lambda e, At=At, Bt=Bt, Rt=Rt, It=It: e.tensor_tensor(out=Bt[:], in0=Bt[:], in1=It[:], op=ALU.mult), reads=[r_B, r_I], writes=[r_B])
                            S.op("dve", lambda e, f=f, At=At, Bt=Bt, Rt=Rt, It=It: e.tensor_tensor(out=Bt[:], in0=Bt[:], in1=UC[:, f, :], op=ALU.mult), reads=[r_B, r_UC], writes=[r_B])
                            if d_ == 0:
                                S.op("dve", lambda e, At=At, Bt=Bt, Rt=Rt, It=It: e.tensor_tensor_scan(out=Hf[:], data0=At[:], data1=Bt[:], initial=0.0, op0=ALU.mult, op1=ALU.add),
                                     reads=[r_A, r_B], writes=[r_Hf])
                            else:
                                S.op("dve", lambda e, At=At, Bt=Bt, Rt=Rt, It=It: e.tensor_tensor_scan(out=Hb[:, 0:LC][:, ::-1],
                                                                           data0=At[:, 0:LC][:, ::-1], data1=Bt[:, 0:LC][:, ::-1],
                                                                           initial=0.0, op0=ALU.mult, op1=ALU.add),
                                     reads=[r_A, r_B], writes=[r_Hb])
                                S.op("dve", lambda e, At=At, Bt=Bt, Rt=Rt, It=It: e.tensor_tensor_scan(out=Hb[:, LC:NT][:, ::-1], data0=At[:, LC:NT][:, ::-1],
                                                                           data1=Bt[:, LC:NT][:, ::-1], initial=Hb[:, 0:1],
                                                                           op0=ALU.mult, op1=ALU.add),
                                     reads=[r_A, r_B, r_Hb], writes=[r_Hb])
                        S.op("pool", lambda e: e.tensor_tensor(out=Hf[:, LC:NT], in0=Hf[:, LC:NT], in1=Hb[:, LC:NT], op=ALU.add),
                             reads=[r_Hf, r_Hb], writes=[r_Hf])
                        S.op("dve", lambda e, f=f: e.tensor_tensor(out=YI[:, f, :], in0=Hf[:, LC:NT], in1=GG[:, f, :], op=ALU.mult),
                             reads=[r_Hf, r_GG], writes=[r_YI])
                    S.barrier()
                    S.emit()
                srcs = [(YI, c, r_YI, LC) for c in range(8)]
                outproj_residual(st, 1, b, srcs, od_w_out, XR, R_XRT, list(range(2, NTI)), VEC_OF_TILE(b))

        def moe_layer(l, tok_tiles, final):
            ntile = len(tok_tiles)
            ntok = ntile * 128
            nblk = ntok * 4 // BLK + n_exp
            with contextlib.ExitStack() as st:
                sH2 = contextlib.ExitStack()
                LG = sbuf(st, "LG", [128, ntile, NE], F32); r_LG = Res()
                D4 = sbuf(st, "D4", [128, ntile * 4], I32); r_D4 = Res()
                G4 = sbuf(st, "G4", [128, ntile * 4], F32); r_G4 = Res()
                IDXW = sbuf(st, "IDXW", [128, nblk], I32); r_IDXW = Res()
                BEI = sbuf(st, "BEI", [128, nblk], I32)
                H2 = sbuf(sH2, "H2", [128, ntile * D], BF16); r_H2 = Res()
                with contextlib.ExitStack() as s1:
                    vecs = sorted(set((NB if i < 2 else b) for b, i in tok_tiles))
                    tiles = norm_mod_tiles(s1, l, 1, vecs)
                    NBF = 4
                    small, r_small = zip(*[new_small(s1, "n2small%d" % i) for i in range(NBF)])
                    junk1 = sbuf(s1, "n2junk", [128, D], F32)
                    junk = [junk1] * NBF
                    xt = [sbuf(s1, "n2x%d" % i, [128, D], F32) for i in range(NBF)]
                    r_xt = [Res() for _ in range(NBF)]
                    ht = [sbuf(s1, "n2h%d" % i, [128, D], F32) for i in range(NBF)]
                    r_ht = [Res() for _ in range(NBF)]
                    hT32 = [sbuf(s1, "n2t%d" % i, [128, 8, 128], F32) for i in range(2)]
                    r_hT32 = [Res(), Res()]
                    ptp = [psum(s1, "n2p%d" % i, [128, 4, 128], F32) for i in range(2)]
                    r_ptp = [Res(), Res()]
                    plg = [psum(s1, "n2l%d" % i, [128, NE], F32) for i in range(2)]
                    r_plg = [Res(), Res()]
                    wr = sbuf(s1, "wr", [128, 8, NE], F32); r_wr = Res()
                    S.dma("sp", lambda e: e.dma_start(out=wr[:], in_=router_w[l].rearrange("(c p) n -> p c n", p=128)), writes=[r_wr])
                    rb, r_rb = load_bcast(s1, "rb", router_b[l, :], [], n=NE)
                    def stage_a(n_i):
                        b, i = tok_tiles[n_i]
                        k = n_i % NBF
                        G, rG, SH, rSH = tiles[NB if i < 2 else b]
                        S.dma("sp", lambda e: e.dma_start(out=xt[k][:], in_=XR[b, i * 128:(i + 1) * 128, :]),
                              reads=[R_XRT[(b, i)]], writes=[r_xt[k]])
                        rms_mod(s1, xt[k], r_xt[k], G, rG, SH, rSH, ht[k], r_ht[k], small[k], r_small[k], junk[k], Res())
                        S.op("act", lambda e: e.activation(out=H2[:, n_i * D:(n_i + 1) * D], in_=ht[k][:], func=AF.Copy), reads=[r_ht[k]], writes=[r_H2])

                    def stage_b(n_i):
                        kb = n_i % NBF
                        k = n_i % 2
                        for hf in range(2):
                            for c4 in range(4):
                                c = hf * 4 + c4
                                S.op("pe", lambda e, hf=hf, c4=c4, c=c: e.transpose(
                                    out=ptp[hf][:, c4, :], in_=ht[kb][:, c * 128:(c + 1) * 128], identity=ident[:]),
                                    reads=[r_ht[kb], R_c], writes=[r_ptp[hf]], signal=(c4 == 3))
                            if hf == 0:
                                S.op("act", lambda e: e.activation(out=hT32[k][:, 0:4, :], in_=ptp[0][:], func=AF.Copy),
                                     reads=[r_ptp[0]], writes=[r_hT32[k]])
                            else:
                                S.op("dve", lambda e: e.tensor_copy(out=hT32[k][:, 4:8, :], in_=ptp[1][:]),
                                     reads=[r_ptp[1]], writes=[r_hT32[k]])
                        for c in range(8):
                            S.op("pe", lambda e, c=c: e.matmul(plg[k][:], lhsT=hT32[k][:, c, :], rhs=wr[:, c, :], start=(c == 0), stop=(c == 7)),
                                 reads=[r_hT32[k], r_wr], writes=[r_plg[k]], signal=(c == 7))
                        S.op("dve", lambda e: e.tensor_tensor(out=LG[:, n_i, :], in0=plg[k][:], in1=rb[:], op=ALU.add),
                             reads=[r_plg[k], r_rb], writes=[r_LG])

                    stage_a(0)
                    stage_a(1)
                    for n_i in range(ntile):
                        if n_i + 2 < ntile:
                            stage_a(n_i + 2)
                        stage_b(n_i)
                    S.barrier()
                    S.emit()
                with contextlib.ExitStack() as s1:
                    MX = sbuf(s1, "MX", [128, ntile, 8], F32)
                    MASK = sbuf(s1, "MASK", [128, ntile, NE], F32)
                    MASKb = sbuf(s1, "MASKb", [128, ntile, NE], BF16)
                    CUM = sbuf(s1, "CUM", [128, ntile, NE], BF16)
                    GAT = sbuf(s1, "GAT", [128, ntile, NE], F32)
                    TMP = sbuf(s1, "TMP", [128, ntile, NE], F32)
                    KEY = sbuf(s1, "KEY", [128, ntile, NE], F32)
                    K8 = sbuf(s1, "K8", [128, ntile, 8], F32)
                    DEN = sbuf(s1, "DEN", [128, ntile], F32)
                    TRI = sbuf(s1, "TRI", [128, 128], BF16)
                    CNT = sbuf(s1, "CNT", [128, NE], F32)
                    CNTI = sbuf(s1, "CNTI", [128, NE], I32)
                    PAD = sbuf(s1, "PAD", [128, NE], F32)
                    PEND = sbuf(s1, "PEND", [128, NE], F32)
                    BASE = sbuf(s1, "BASE", [128, NE], F32)
                    ONE32 = sbuf(s1, "ONE32", [128, NE], F32)
                    CMP = sbuf(s1, "CMP", [128, nblk, NE], F32)
                    BEF = sbuf(s1, "BEF", [128, nblk], F32)
                    JB = sbuf(s1, "JB", [128, 80], F32)
                    IOP = sbuf(s1, "IOP", [128, 1], F32)
                    pposb = [psum(s1, "ppos%d" % i, [128, 16, NE], F32) for i in range((ntile + 15) // 16)]
                    pcnt = psum(s1, "pcnt", [128, NE], F32)
                    R = Res("route")
                    V_ = lambda fn, eng="dve": S.op(eng, fn, reads=[R, r_LG], writes=[R, r_D4, r_G4, r_IDXW])
                    S.dma("sp", lambda e: e.dma_start(out=JB[:], in_=blk_start_d[:, :]), writes=[R])
                    S.dma("sp", lambda e: e.dma_start(out=IOP[:], in_=iota_p_d[:, :]), reads=[R], writes=[R])
                    V_(lambda e: e.memset(TRI[:], 1.0))
                    V_(lambda e: e.affine_select(out=TRI[:], in_=TRI[:], pattern=[[1, 128]], compare_op=ALU.is_gt, fill=0.0,
                                                 base=0, channel_multiplier=-1), "pool")
                    V_(lambda e: e.memset(ONE32[:], 1.0))
                    for i in range(ntile):
                        V_(lambda e, i=i: e.max(out=MX[:, i, :], in_=LG[:, i, :]))
                    V_(lambda e: e.tensor_tensor(out=MASK[:], in0=LG[:], in1=MX[:, :, 3:4].to_broadcast([128, ntile, NE]), op=ALU.is_ge))
                    V_(lambda e: e.tensor_tensor(out=TMP[:], in0=LG[:], in1=MX[:, :, 0:1].to_broadcast([128, ntile, NE]), op=ALU.subtract))
                    V_(lambda e: e.activation(out=TMP[:], in_=TMP[:], func=AF.Exp), "act")
                    V_(lambda e: e.tensor_tensor(out=TMP[:], in0=TMP[:], in1=MASK[:], op=ALU.mult))
                    V_(lambda e: e.tensor_reduce(out=DEN[:], in_=TMP[:], axis=AX.X, op=ALU.add))
                    V_(lambda e: e.reciprocal(out=DEN[:], in_=DEN[:]))
                    V_(lambda e: e.tensor_tensor(out=GAT[:], in0=TMP[:], in1=DEN[:].unsqueeze(2).to_broadcast([128, ntile, NE]), op=ALU.mult))
                    V_(lambda e: e.tensor_copy(out=MASKb[:], in_=MASK[:]))
                    V_(lambda e: e.memset(CUM[:, 0, :], 0.0))
                    for i in range(1, ntile):
                        V_(lambda e, i=i: e.tensor_tensor(out=CUM[:, i, :], in0=CUM[:, i - 1, :], in1=MASKb[:, i - 1, :], op=ALU.add))
                    V_(lambda e: e.tensor_tensor(out=TMP[:, 0, :], in0=CUM[:, ntile - 1, :], in1=MASKb[:, ntile - 1, :], op=ALU.add))
                    TOTb = sbuf(s1, "TOTb", [128, NE], BF16)
                    V_(lambda e: e.tensor_copy(out=TOTb[:], in_=TMP[:, 0, :]))
                    for i in range(ntile):
                        V_(lambda e, i=i: e.matmul(pposb[i // 16][:, i % 16, :], lhsT=TRI[:], rhs=MASKb[:, i, :], start=True, stop=False), "pe")
                        V_(lambda e, i=i: e.matmul(pposb[i // 16][:, i % 16, :], lhsT=onesb[:, 0:128], rhs=CUM[:, i, :], start=False, stop=True), "pe")
                    V_(lambda e: e.matmul(pcnt[:], lhsT=onesb[:, 0:128], rhs=TOTb[:], start=True, stop=True), "pe")
                    V_(lambda e: e.tensor_copy(out=CNT[:], in_=pcnt[:]))
                    NM = ntok // BLK + 1
                    CMP2 = sbuf(s1, "CMP2", [128, NE, NM], F32)
                    V_(lambda e: e.tensor_tensor(out=CMP2[:], in0=CNT[:].unsqueeze(2).to_broadcast([128, NE, NM]),
                                                 in1=JB[:, 0:NM].unsqueeze(1).to_broadcast([128, NE, NM]), op=ALU.is_gt))
                    V_(lambda e: e.tensor_reduce(out=PAD[:], in_=CMP2[:], axis=AX.X, op=ALU.add))
                    V_(lambda e: e.tensor_scalar(out=PAD[:], in0=PAD[:], scalar1=float(BLK), scalar2=None, op0=ALU.mult))
                    V_(lambda e: e.tensor_tensor_scan(out=PEND[:], data0=ONE32[:], data1=PAD[:], initial=0.0, op0=ALU.mult, op1=ALU.add))
                    V_(lambda e: e.tensor_tensor(out=BASE[:], in0=PEND[:], in1=PAD[:], op=ALU.subtract))
                    for bk in range((ntile + 15) // 16):
                        n_ = min(16, ntile - bk * 16)
                        V_(lambda e, bk=bk, n_=n_: e.tensor_tensor(out=KEY[:, bk * 16:bk * 16 + n_, :], in0=pposb[bk][:, 0:n_, :],
                                                                  in1=BASE[:].unsqueeze(1).to_broadcast([128, n_, NE]), op=ALU.add))
                    V_(lambda e: e.scalar_tensor_tensor(out=KEY[:], in0=KEY[:], scalar=1.0, in1=MASK[:], op0=ALU.add, op1=ALU.mult))
                    for i in range(ntile):
                        V_(lambda e, i=i: e.max(out=K8[:, i, :], in_=KEY[:, i, :]))
                    V_(lambda e: e.tensor_scalar(out=TMP[:, :, 0:4], in0=K8[:, :, 0:4], scalar1=-1.0, scalar2=0.0, op0=ALU.add, op1=ALU.max))
                    V_(lambda e: e.tensor_scalar(out=TMP[:, :, 0:4], in0=TMP[:, :, 0:4], scalar1=float(nblk * BLK - 1), scalar2=None, op0=ALU.min))
                    V_(lambda e: e.tensor_copy(out=D4[:].rearrange("p (n j) -> p n j", j=4), in_=TMP[:, :, 0:4]))
                    for j in range(4):
                        V_(lambda e, j=j: e.tensor_tensor(out=TMP[:], in0=KEY[:], in1=K8[:, :, j:j + 1].to_broadcast([128, ntile, NE]), op=ALU.is_equal))
                        V_(lambda e: e.tensor_tensor(out=TMP[:], in0=TMP[:], in1=GAT[:], op=ALU.mult))
                        V_(lambda e, j=j: e.tensor_reduce(out=G4[:].rearrange("p (n j) -> p n j", j=4)[:, :, j], in_=TMP[:], axis=AX.X, op=ALU.add))
                    V_(lambda e: e.tensor_tensor(out=CMP[:], in0=PEND[:].unsqueeze(1).to_broadcast([128, nblk, NE]),
                                                 in1=JB[:, 0:nblk].unsqueeze(2).to_broadcast([128, nblk, NE]), op=ALU.is_le))
                    V_(lambda e: e.tensor_reduce(out=BEF[:], in_=CMP[:], axis=AX.X, op=ALU.add))
                    V_(lambda e: e.tensor_scalar(out=BEF[:], in0=BEF[:], scalar1=float(n_exp - 1), scalar2=0.0, op0=ALU.min, op1=ALU.max))
                    V_(lambda e: e.tensor_copy(out=BEI[:], in_=BEF[:]))
                    V_(lambda e: e.tensor_scalar(out=BEF[:], in0=BEF[:], scalar1=128.0, scalar2=IOP[:, 0:1], op0=ALU.mult, op1=ALU.add))
                    V_(lambda e: e.tensor_copy(out=IDXW[:], in_=BEF[:]))
                    if dbg:
                        S.dma("sp", lambda e: e.dma_start(out=dbg_d4[:, 0:ntile * 4], in_=D4[:]), reads=[R, r_D4])
                        S.dma("sp", lambda e: e.dma_start(out=dbg_g4[:, 0:ntile * 4], in_=G4[:]), reads=[R, r_G4])
                        S.dma("sp", lambda e: e.dma_start(out=dbg_iw[:, 0:nblk], in_=IDXW[:]), reads=[R, r_IDXW])
                    for n_i in range(ntile if moe_stop >= 2 else 0):
                        for j in range(4):
                            S.dma("pool", lambda e, n_i=n_i, j=j: e.indirect_dma_start(
                                out=XS[:, :], out_offset=bass.IndirectOffsetOnAxis(ap=D4[:, n_i * 4 + j:n_i * 4 + j + 1], axis=0),
                                in_=H2[:, n_i * D:(n_i + 1) * D], in_offset=None),
                                reads=[R, r_D4, r_H2], writes=[])
                    S.barrier()
                    S.emit()
                sH2.close()
                if moe_stop < 3:
                    return
                w_aps = (w_gate[l], w_up[l], w_down[l])
                with contextlib.ExitStack() as s1:
                    WG = [sbuf(s1, "WG%d" % i, [128, 8192], BF16) for i in range(2)]
                    WU = [sbuf(s1, "WU%d" % i, [128, 8192], BF16) for i in range(2)]
                    WD = [sbuf(s1, "WD%d" % i, [128, 8192], BF16) for i in range(2)]
                    EB = [sbuf(s1, "EB%d" % i, [2, 3072], BF16) for i in range(2)]
                    EBC = [sbuf(s1, "EBC%d" % i, [128, 16], F32) for i in range(2)]
                    r_EBC = [Res(), Res()]
                    r_W = [[Res() for _ in range(4)] for _ in range(2)]
                    xs = [sbuf(s1, "xsb%d" % i, [128, 4, D], BF16) for i in range(2)]
                    r_xs = [Res(), Res()]
                    xeT = [sbuf(s1, "xeT%d" % i, [128, 8, BLK], BF16) for i in range(2)]
                    r_xeT = [Res(), Res()]
                    actT = sbuf(s1, "actT", [128, 8, BLK], BF16); r_actT = Res()
                    gs = [sbuf(s1, "gs%d" % i, [128, BLK], F32) for i in range(2)]
                    sg = [sbuf(s1, "sg%d" % i, [128, BLK], F32) for i in range(2)]
                    us = [sbuf(s1, "us%d" % i, [128, BLK], F32) for i in range(2)]
                    r_gs = [Res(), Res()]; r_sg = [Res(), Res()]; r_us = [Res(), Res()]
                    ysb = [sbuf(s1, "ysb%d" % i, [128, 4, D], BF16) for i in range(2)]
                    r_ysb = [Res(), Res()]
                    ptr = [psum(s1, "ptr%d" % i, [128, 8, 128], BF16) for i in range(2)]
                    r_ptr = [Res(), Res()]
                    pgp = [psum(s1, "pgp%d" % i, [128, BLK], F32) for i in range(2)]
                    pup = [psum(s1, "pup%d" % i, [128, BLK], F32) for i in range(2)]
                    r_pgp = [Res(), Res()]; r_pup = [Res(), Res()]
                    pyp = [psum(s1, "pyp%d" % i, [128, 512], F32) for i in range(2)]
                    r_pyp = [Res(), Res()]
                    def load_xs(jb_):
                        k_ = jb_ % 2
                        S.dma("sp", lambda e: e.dma_start(
                            out=xs[k_][:], in_=XS[jb_ * BLK:(jb_ + 1) * BLK, :].rearrange("(s p) d -> p s d", p=128)),
                            reads=[R_XS], writes=[r_xs[k_]])

                    for jb in range(nblk):
                        k = jb % 2
                        for mi, (wbuf, wap) in enumerate(((WG[k], w_aps[0]), (WU[k], w_aps[1]), (WD[k], w_aps[2]))):
                            S.dma("pool", lambda e, wbuf=wbuf, wap=wap, jb=jb: e.indirect_dma_start(
                                out=wbuf[:, :], out_offset=None, in_=wap[:, :],
                                in_offset=bass.IndirectOffsetOnAxis(ap=IDXW[:, jb:jb + 1], axis=0)), reads=[r_IDXW], writes=[r_W[k][mi]])
                        S.dma("pool", lambda e, k=k, jb=jb: e.indirect_dma_start(
                            out=EB[k][:, :], out_offset=None, in_=exp_b[l][:, :],
                            in_offset=bass.IndirectOffsetOnAxis(ap=BEI[0:2, jb:jb + 1], axis=0)), reads=[r_IDXW], writes=[r_W[k][3]])
                        S.dma("pool", lambda e, k=k, jb=jb: e.indirect_dma_start(
                            out=EBC[k][:, :], out_offset=None, in_=exp_bc[l][:, :],
                            in_offset=bass.IndirectOffsetOnAxis(ap=IDXW[:, jb:jb + 1], axis=0)), reads=[r_IDXW], writes=[r_EBC[k]])
                        S.op("dve", lambda e, k=k: e.tensor_scalar(out=EBC[k][:, 8:16], in0=EBC[k][:, 8:16], scalar1=1.0, scalar2=None, op0=ALU.add),
                             reads=[r_EBC[k]], writes=[r_EBC[k]])
                        if jb == 0:
                            load_xs(0)
                        for s in range(4):
                            kk = s % 2
                            for c in range(8):
                                S.op("pe", lambda e, k=k, kk=kk, s=s, c=c: e.transpose(out=ptr[kk][:, c, :], in_=xs[k][:, s, c * 128:(c + 1) * 128],
                                                                                 identity=identb[:]),
                                     reads=[r_xs[k], R_c], writes=[r_ptr[kk]], signal=(c == 7))
                            if s % 2 == 0:
                                S.op("act", lambda e, k=k, kk=kk, s=s: e.activation(out=xeT[k][:, :, s * 128:(s + 1) * 128], in_=ptr[kk][:], func=AF.Copy),
                                     reads=[r_ptr[kk]], writes=[r_xeT[k]])
                            else:
                                S.op("dve", lambda e, k=k, kk=kk, s=s: e.tensor_copy(out=xeT[k][:, :, s * 128:(s + 1) * 128], in_=ptr[kk][:]),
                                     reads=[r_ptr[kk]], writes=[r_xeT[k]])
                        if jb + 1 < nblk:
                            load_xs(jb + 1)
                        for f in range(8):
                            kf = f % 2
                            for (pp, r_pp, wbuf, mi) in ((pgp[kf], r_pgp[kf], WG[k], 0), (pup[kf], r_pup[kf], WU[k], 1)):
                                for c in range(8):
                                    S.op("pe", lambda e, pp=pp, wbuf=wbuf, c=c, f=f, k=k: e.matmul(
                                        pp[:], lhsT=wbuf[:, c * 1024 + f * 128:c * 1024 + (f + 1) * 128], rhs=xeT[k][:, c, :], start=(c == 0), stop=(c == 7)),
                                        reads=[r_W[k][mi], r_xeT[k]], writes=[r_pp], signal=(c == 7))
                            S.op("dve", lambda e, kf=kf, k=k, f=f: e.tensor_scalar(out=gs[kf][:], in0=pgp[kf][:], scalar1=EBC[k][:, f:f + 1], scalar2=7.0,
                                                                                 op0=ALU.add, op1=ALU.min),
                                 reads=[r_pgp[kf], r_EBC[k]], writes=[r_gs[kf]])
                            S.op("act", lambda e, kf=kf: e.activation(out=sg[kf][:], in_=gs[kf][:], func=AF.Sigmoid, scale=1.702),
                                 reads=[r_gs[kf]], writes=[r_sg[kf]])
                            S.op("dve", lambda e, kf=kf, k=k, f=f: e.tensor_scalar(out=us[kf][:], in0=pup[kf][:], scalar1=EBC[k][:, 8 + f:9 + f], scalar2=8.0,
                                                                                 op0=ALU.add, op1=ALU.min),
                                 reads=[r_pup[kf], r_EBC[k]], writes=[r_us[kf]])
                            S.op("dve", lambda e, kf=kf: e.scalar_tensor_tensor(out=us[kf][:], in0=us[kf][:], scalar=-6.0, in1=gs[kf][:],
                                                                                op0=ALU.max, op1=ALU.mult),
                                 reads=[r_us[kf], r_gs[kf]], writes=[r_us[kf]])
                            S.op("dve", lambda e, kf=kf, f=f: e.tensor_tensor(out=actT[:, f, :], in0=us[kf][:], in1=sg[kf][:], op=ALU.mult),
                                 reads=[r_us[kf], r_sg[kf]], writes=[r_actT])
                        for s in range(4):
                            for hh in range(2):
                                kp = (s * 2 + hh) % 2
                                for f in range(8):
                                    S.op("pe", lambda e, kp=kp, f=f, s=s, hh=hh, k=k: e.matmul(
                                        pyp[kp][:], lhsT=actT[:, f, s * 128:(s + 1) * 128],
                                        rhs=WD[k][:, f * 1024 + hh * 512:f * 1024 + (hh + 1) * 512], start=(f == 0), stop=False),
                                        reads=[r_W[k][2], r_actT], writes=[r_pyp[kp]], signal=False)
                                S.op("pe", lambda e, kp=kp, hh=hh, k=k: e.matmul(
                                    pyp[kp][:], lhsT=halfb[0:2, 0:128], rhs=EB[k][0:2, 2048 + hh * 512:2048 + (hh + 1) * 512], start=False, stop=True),
                                    reads=[r_W[k][3], R_c], writes=[r_pyp[kp]])
                                S.op("act", lambda e, kp=kp, k=k, s=s, hh=hh: e.activation(out=ysb[k][:, s, hh * 512:(hh + 1) * 512], in_=pyp[kp][:], func=AF.Copy),
                                     reads=[r_pyp[kp]], writes=[r_ysb[k]])
                        S.dma("sp", lambda e, k=k, jb=jb: e.dma_start(
                            out=YS[jb * BLK:(jb + 1) * BLK, :].rearrange("(s p) d -> p s d", p=128), in_=ysb[k][:]),
                            reads=[r_ysb[k]], writes=[R_YS])
                    S.barrier()
                    S.emit()
                if moe_stop < 4:
                    return
                with contextlib.ExitStack() as s1:
                    g2 = {}
                    for v in sorted(set((NB if i < 2 else b) for b, i in tok_tiles)):
                        g2[v] = load_bcast(s1, "g2_%d" % v, MOD[l, v, 5 * D:6 * D], [R_MOD])
                    yg = [[sbuf(s1, "yg%d_%d" % (i, j), [128, D], BF16) for j in range(4)] for i in range(2)]
                    r_yg = [[Res() for _ in range(4)] for _ in range(2)]
                    acc = [sbuf(s1, "cacc%d" % i, [128, D], F32) for i in range(2)]
                    r_acc = [Res(), Res()]
                    xt = [sbuf(s1, "cbx%d" % i, [128, D], F32) for i in range(2)]
                    r_xt = [Res(), Res()]
                    for n_i, (b, i) in enumerate(tok_tiles):
                        k = n_i % 2
                        gt, rgt = g2[NB if i < 2 else b]
                        S.dma("sp", lambda e, b=b, i=i, k=k: e.dma_start(out=xt[k][:], in_=XR[b, i * 128:(i + 1) * 128, :]),
                              reads=[R_XRT[(b, i)]], writes=[r_xt[k]])
                        for j in range(4):
                            S.dma("pool", lambda e, k=k, j=j, n_i=n_i: e.indirect_dma_start(
                                out=yg[k][j][:, :], out_offset=None, in_=YS[:, :],
                                in_offset=bass.IndirectOffsetOnAxis(ap=D4[:, n_i * 4 + j:n_i * 4 + j + 1], axis=0)), reads=[R_YS, r_D4], writes=[r_yg[k][j]])
                        S.op("dve", lambda e, k=k, n_i=n_i: e.tensor_scalar(out=acc[k][:], in0=yg[k][0][:], scalar1=G4[:, n_i * 4:n_i * 4 + 1], scalar2=None, op0=ALU.mult),
                             reads=[r_yg[k][0], r_G4], writes=[r_acc[k]])
                        for j in range(1, 4):
                            S.op("dve", lambda e, k=k, j=j, n_i=n_i: e.scalar_tensor_tensor(
                                out=acc[k][:], in0=yg[k][j][:], scalar=G4[:, n_i * 4 + j:n_i * 4 + j + 1], in1=acc[k][:], op0=ALU.mult, op1=ALU.add),
                                reads=[r_yg[k][j], r_acc[k], r_G4], writes=[r_acc[k]])
                        S.op("dve", lambda e, k=k, gt=gt: e.tensor_tensor(out=acc[k][:], in0=acc[k][:], in1=gt[:], op=ALU.mult),
                             reads=[r_acc[k], rgt], writes=[r_acc[k]])
                        S.op("dve", lambda e, k=k: e.tensor_tensor(out=xt[k][:], in0=xt[k][:], in1=acc[k][:], op=ALU.add),
                             reads=[r_acc[k], r_xt[k]], writes=[r_xt[k]])
                        if final:
                            S.dma("sp", lambda e, b=b, i=i, k=k: e.dma_start(out=out[b, (i - 2) * 128:(i - 1) * 128, :], in_=xt[k][:]),
                                  reads=[r_xt[k]], writes=[Res()])
                        else:
                            S.dma("sp", lambda e, b=b, i=i, k=k: e.dma_start(out=XR[b, i * 128:(i + 1) * 128, :], in_=xt[k][:]),
                                  reads=[r_xt[k]], writes=[R_XRT[(b, i)]])
                    S.barrier()
                    S.emit()

        if 0 in layers:
            for b in range(NB):
                layer0_mixer(b)
            if do_moe:
                moe_layer(0, [(b, i) for b in range(NB) for i in range(NTI)], final=False)
        if 1 in layers:
            for b in range(NB):
                layer1_mixer(b)
            if do_moe:
                moe_layer(1, [(b, i) for b in range(NB) for i in range(2, NTI)], final=True)
        if dbg:
            with contextlib.ExitStack() as st:
                t = sbuf(st, "dbgt", [128, D], F32); r_t = Res()
                for b in range(NB):
                    for i in range(2, NTI):
                        S.dma("sp", lambda e, b=b, i=i: e.dma_start(out=t[:], in_=XR[b, i * 128:(i + 1) * 128, :]), reads=[R_XRT[(b, i)]], writes=[r_t])
                        S.dma("sp", lambda e, b=b, i=i: e.dma_start(out=out[b, (i - 2) * 128:(i - 1) * 128, :], in_=t[:]), reads=[r_t], writes=[R_OUT])
                S.barrier()
                S.emit()
        S.barrier()
        S.emit()
        print("program instructions (incl waits):", S.n_ins, "sem counts:", S.cnt, "max dma sem:", max(S.dma_cnt.values()))
    return nc


def _core_inputs(inp, sh, c, NB=2):
    b0 = c * NB
    d = dict(sh)
    d["xin"] = np.ascontiguousarray(np.concatenate([inp["ctx"][b0:b0 + NB], inp["x"][b0:b0 + NB]], axis=1), np.float32)
    d["cvec"] = np.ascontiguousarray(np.concatenate([inp["c"][b0:b0 + NB], inp["c_ctx"][None]], 0), np.float32)
    return d


def kernel(**inputs):
    inp = {k: np.asarray(v) for k, v in inputs.items()}
    sh = _prep_shared(inp)
    nc = build_program(NB=2)
    in_maps = [_core_inputs(inp, sh, c) for c in range(8)]
    res = run_bass_kernel_spmd(nc, in_maps, core_ids=list(range(8)))
    return np.concatenate([r["out"] for r in res.results], axis=0).astype(np.float32)
```
